# Optimizing a Trainium2 kernel written in Bass

```python
import jax, jax.numpy as jnp
from jax import lax
import numpy as np

D_MODEL = 1024
BATCH = 8
SEQ = 4096
DEPTH = 1
DEC_BATCH = 32
DEC_SEQ = 1
PAST_LEN = 16384
PAGE_SIZE = 128

HEAD_DIM = 64
ATTN_GROUPS = ((128, 1), (512, 4), (2048, 16))
N_GROUPS = len(ATTN_GROUPS)
HEADS_PER_GROUP = (D_MODEL // 2) // HEAD_DIM
N_ATTN_HEADS = N_GROUPS * HEADS_PER_GROUP
ATTN_WIDTH = HEADS_PER_GROUP * HEAD_DIM
QKV_WIDTH = N_ATTN_HEADS * HEAD_DIM
ATTN_BLOCK = 128
POOL_WIDTH = D_MODEL // 2
POOL_WINDOWS = (2, 4, 8, 16)
POOL_GROUP = POOL_WIDTH // len(POOL_WINDOWS)
POOL_STATE = max(POOL_WINDOWS) - 1
IN_COLS = 3 * QKV_WIDTH + POOL_WIDTH + 2 * D_MODEL
N_BUCKETS = 32
REL_MAX_DIST = 2048
N_EXPERTS = 256
TOP_K = 8
N_EXPERT_GROUPS = 8
TOPK_GROUPS = 4
EXPERT_HIDDEN = D_MODEL // 4
SHARED_HIDDEN = D_MODEL // 4
ROUTED_SCALE = 2.5
MOE_BLOCK = 128
MOE_MIN_BLOCK = 8
EPS = 1e-6

kernel_name = 'hybrid_dilated_attn_pool_moe_adaln_step'


def t5_bucket(dist):
    exact = N_BUCKETS // 2
    d = np.asarray(dist)
    large = exact + (np.log(np.maximum(d, 1) / exact) / np.log(REL_MAX_DIST / exact) * (N_BUCKETS - exact)).astype(np.int32)
    large = np.minimum(large, N_BUCKETS - 1)
    return np.where(d < exact, d, large).astype(np.int32)


def group_bias(rel_bias, gi):
    window, dil = ATTN_GROUPS[gi]
    bkt = t5_bucket(np.arange(window // dil + 1) * dil)
    return rel_bias[bkt, gi * HEADS_PER_GROUP:(gi + 1) * HEADS_PER_GROUP].T.astype(jnp.float32)


def rmsnorm(x, g):
    xf = x.astype(jnp.float32)
    y = xf * lax.rsqrt(jnp.mean(xf * xf, axis=-1, keepdims=True) + EPS) * g.astype(jnp.float32)
    return y.astype(x.dtype)


def qk_norm(t, gain):
    tf = t.astype(jnp.float32)
    y = tf * lax.rsqrt(jnp.mean(tf * tf, axis=-1, keepdims=True) + EPS) * gain.astype(jnp.float32)[:, None, :]
    return y.astype(t.dtype)


def adaln(c, w, b):
    mod = jax.nn.silu(c) @ w + b
    return tuple(m[:, None, :] for m in jnp.split(mod, 6, axis=-1))


def modulate(h, shift, scale):
    return h * (1 + scale) + shift


def attn_prompt(q, k, v, bias_j, dil):
    B, S, H, E = q.shape
    n = bias_j.shape[1] - 1
    blk = ATTN_BLOCK
    span = dil * blk
    s_pad = -(-S // span) * span
    L = s_pad // dil
    nb = L // blk

    def to_classes(t):
        t = jnp.pad(t.astype(jnp.float32), ((0, 0), (0, s_pad - S), (0, 0), (0, 0)))
        return t.reshape(B, L, dil, H, E).transpose(0, 2, 1, 3, 4).reshape(B, dil, nb, blk, H, E)

    def with_prev(t):
        prev = jnp.pad(t, ((0, 0), (0, 0), (1, 0), (0, 0), (0, 0), (0, 0)))[:, :, :-1]
        return jnp.concatenate([prev, t], axis=3)

    qc = to_classes(q)
    kk = with_prev(to_classes(k))
    vv = with_prev(to_classes(v))
    s = jnp.einsum('brnqhe,brnkhe->brnhqk', qc, kk) * (E ** -0.5)
    a = np.arange(blk)[:, None]
    c = np.arange(2 * blk)[None, :]
    j = blk + a - c
    band = (j >= 0) & (j <= n)
    starts = np.arange(nb)[:, None, None] * blk
    valid = band[None] & (starts + a[None] - j[None] >= 0)
    bias = bias_j[:, np.clip(j, 0, n)]
    s = jnp.where(valid[None, None, :, None], s + bias, -jnp.inf)
    m = jnp.max(s, axis=-1, keepdims=True)
    p = jnp.exp(s - m)
    l = jnp.sum(p, axis=-1)
    o = jnp.einsum('brnhqk,brnkhe->brnqhe', p, vv) / jnp.swapaxes(l, -1, -2)[..., None]
    lse = jnp.swapaxes(m[..., 0] + jnp.log(l), -1, -2)
    o = o.reshape(B, dil, L, H, E).transpose(0, 2, 1, 3, 4).reshape(B, s_pad, H, E)[:, :S]
    lse = lse.reshape(B, dil, L, H).transpose(0, 2, 1, 3).reshape(B, s_pad, H)[:, :S]
    return o, lse


def attn_sample(q, k_new, v_new, k_buf, v_buf, bias_j, window, dil):
    R = k_buf.shape[1]
    T = q.shape[1]
    n = bias_j.shape[1] - 1
    kext = jnp.concatenate([k_buf, k_new.astype(k_buf.dtype)], axis=1)
    vext = jnp.concatenate([v_buf, v_new.astype(v_buf.dtype)], axis=1)
    idx = R + np.arange(T)[:, None] - dil * np.arange(n + 1)[None, :]
    valid = idx >= 0
    idx_c = np.maximum(idx, 0)
    kg = kext[:, idx_c].astype(jnp.float32)
    vg = vext[:, idx_c].astype(jnp.float32)
    s = jnp.einsum('bthe,btjhe->bthj', q.astype(jnp.float32), kg) * (q.shape[-1] ** -0.5) + bias_j
    s = jnp.where(valid[None, :, None, :], s, -jnp.inf)
    m = jnp.max(s, axis=-1, keepdims=True)
    p = jnp.exp(s - m)
    l = jnp.sum(p, axis=-1)
    o = jnp.einsum('bthj,btjhe->bthe', p, vg) / l[..., None]
    lse = m[..., 0] + jnp.log(l)
    keep = min(window, R + T)
    return o, lse, kext[:, -keep:], vext[:, -keep:]


def combine_groups(outs, lses):
    w = jax.nn.softmax(jnp.stack(lses), axis=0)
    return jnp.einsum('gbthe,gbth->bthe', jnp.stack(outs), w)


def pool_branch(u_prev, u, pos0, pool_w, pool_scale):
    B, T, C = u.shape
    P = u_prev.shape[1]
    ext = jnp.concatenate([u_prev, u], axis=1)
    csz = jnp.pad(jnp.cumsum(ext.astype(jnp.float32), axis=1), ((0, 0), (1, 0), (0, 0)))
    pos = pos0 + jnp.arange(T)
    means = []
    for gi, w in enumerate(POOL_WINDOWS):
        sl = slice(gi * POOL_GROUP, (gi + 1) * POOL_GROUP)
        tot = csz[:, P + 1:P + 1 + T, sl] - csz[:, P + 1 - w:P + 1 - w + T, sl]
        cnt = jnp.minimum(pos + 1, w).astype(jnp.float32)
        means.append(tot / cnt[None, :, None])
    pooled = jnp.concatenate(means, axis=-1) - u.astype(jnp.float32)
    mixed = jnp.einsum('btgc,gcd->btgd', pooled.reshape(B, T, len(POOL_WINDOWS), POOL_GROUP),
                       pool_w.astype(jnp.float32)).reshape(B, T, C) * pool_scale.astype(jnp.float32)
    return mixed.astype(u.dtype), ext[:, -P:]


def token_mixer(h, pos0, k_bufs, v_bufs, pool_prev, w_in, q_gain, k_gain, bias_js, pool_w, pool_scale, w_br_a, w_br_b, w_out):
    B, T, _ = h.shape
    z = h @ w_in
    shp = (B, T, N_GROUPS, HEADS_PER_GROUP, HEAD_DIM)
    q = qk_norm(z[..., :QKV_WIDTH].reshape(shp), q_gain)
    k = qk_norm(z[..., QKV_WIDTH:2 * QKV_WIDTH].reshape(shp), k_gain)
    v = z[..., 2 * QKV_WIDTH:3 * QKV_WIDTH].reshape(shp)
    u = z[..., 3 * QKV_WIDTH:3 * QKV_WIDTH + POOL_WIDTH]
    gates = z[..., 3 * QKV_WIDTH + POOL_WIDTH:]
    outs, lses, new_k, new_v = [], [], [], []
    for gi, (window, dil) in enumerate(ATTN_GROUPS):
        if k_bufs is None:
            o, lse = attn_prompt(q[:, :, gi], k[:, :, gi], v[:, :, gi], bias_js[gi], dil)
            keep = min(window, T)
            nk, nv = k[:, T - keep:, gi], v[:, T - keep:, gi]
        else:
            o, lse, nk, nv = attn_sample(q[:, :, gi], k[:, :, gi], v[:, :, gi], k_bufs[gi], v_bufs[gi], bias_js[gi], window, dil)
        outs.append(o)
        lses.append(lse)
        new_k.append(nk)
        new_v.append(nv)
    o_a = combine_groups(outs, lses).reshape(B, T, ATTN_WIDTH).astype(h.dtype)
    if pool_prev is None:
        pool_prev = jnp.zeros((B, POOL_STATE, POOL_WIDTH), h.dtype)
    y_b, new_pool = pool_branch(pool_prev, u, pos0, pool_w, pool_scale)
    g_a, g_b = jnp.split(gates, 2, axis=-1)
    merged = jax.nn.sigmoid(g_a) * (o_a @ w_br_a) + jax.nn.sigmoid(g_b) * (y_b @ w_br_b)
    return merged @ w_out, new_k, new_v, new_pool


def swiglu(h, wg, wu, wd):
    return (jax.nn.silu(h @ wg) * (h @ wu)) @ wd


def routed_experts(h, idx, wts, w_gate, w_up, w_down):
    n_tok, d = h.shape
    n_exp = w_gate.shape[0]
    n_assign = n_tok * TOP_K
    blk = MOE_BLOCK if n_assign >= MOE_BLOCK * n_exp else MOE_MIN_BLOCK
    n_blocks = -(-n_assign // blk) + n_exp
    flat_e = idx.reshape(-1)
    order = jnp.argsort(flat_e)
    se = flat_e[order]
    stok = (order // TOP_K).astype(jnp.int32)
    sw = wts.reshape(-1)[order]
    counts = jnp.bincount(flat_e, length=n_exp)
    offs = jnp.cumsum(counts) - counts
    pcounts = (counts + blk - 1) // blk * blk
    pends = jnp.cumsum(pcounts)
    dest = pends[se] - pcounts[se] + jnp.arange(n_assign) - offs[se]
    slot_tok = jnp.full((n_blocks * blk,), n_tok, jnp.int32).at[dest].set(stok)
    slot_w = jnp.zeros((n_blocks * blk,), jnp.float32).at[dest].set(sw)
    blk_exp = jnp.minimum(jnp.searchsorted(pends, jnp.arange(n_blocks) * blk, side='right'), n_exp - 1)
    h_pad = jnp.concatenate([h, jnp.zeros((1, d), h.dtype)], axis=0)

    def expert_block(args):
        tok, wb, e = args
        xb = h_pad[tok]
        y = swiglu(xb, w_gate[e], w_up[e], w_down[e])
        return y.astype(jnp.float32) * wb[:, None]

    yb = lax.map(expert_block, (slot_tok.reshape(n_blocks, blk), slot_w.reshape(n_blocks, blk), blk_exp))
    return jax.ops.segment_sum(yb.reshape(-1, d), slot_tok, num_segments=n_tok + 1)[:n_tok]


def moe(h, router_w, router_bias, w_gate, w_up, w_down, s_gate, s_up, s_down):
    n_tok = h.shape[0]
    scores = jax.nn.sigmoid(h.astype(jnp.float32) @ router_w.astype(jnp.float32))
    sel = scores + router_bias.astype(jnp.float32)
    grp_score = jnp.sum(lax.top_k(sel.reshape(n_tok, N_EXPERT_GROUPS, -1), 2)[0], axis=-1)
    top_g = lax.top_k(grp_score, TOPK_GROUPS)[1]
    gmask = jnp.any(top_g[..., None] == jnp.arange(N_EXPERT_GROUPS), axis=-2)
    masked = jnp.where(jnp.repeat(gmask, N_EXPERTS // N_EXPERT_GROUPS, axis=1), sel, -jnp.inf)
    idx = lax.top_k(masked, TOP_K)[1]
    wts = jnp.take_along_axis(scores, idx, axis=1)
    wts = wts / jnp.sum(wts, axis=-1, keepdims=True) * ROUTED_SCALE
    routed = routed_experts(h, idx, wts, w_gate, w_up, w_down)
    shared = swiglu(h, s_gate, s_up, s_down).astype(jnp.float32)
    return (routed + shared).astype(h.dtype)


def setup_inputs(seed: int = 0) -> dict:
    key = jax.random.key(seed)
    ks = iter(jax.random.split(key, 40))

    def nrm(shape, scale=1.0):
        return jax.random.normal(next(ks), shape, jnp.float32) * scale

    rows = [min(w, PAST_LEN) for w, _ in ATTN_GROUPS]

    def kv(r):
        return (DEPTH, DEC_BATCH, r, HEADS_PER_GROUP, HEAD_DIM)

    return {
        'x_prompt': nrm((BATCH, SEQ, D_MODEL)),
        'x_sample': nrm((DEC_BATCH, DEC_SEQ, D_MODEL)),
        'cache_k_w128': nrm(kv(rows[0])),
        'cache_v_w128': nrm(kv(rows[0])),
        'cache_k_w512': nrm(kv(rows[1])),
        'cache_v_w512': nrm(kv(rows[1])),
        'cache_k_w2048': nrm(kv(rows[2])),
        'cache_v_w2048': nrm(kv(rows[2])),
        'state_pool': nrm((DEPTH, DEC_BATCH, POOL_STATE, POOL_WIDTH)),
        'c_prompt': nrm((BATCH, D_MODEL)),
        'c_sample': nrm((DEC_BATCH, D_MODEL)),
        'ada_w': nrm((DEPTH, D_MODEL, 6 * D_MODEL), 0.5 * D_MODEL ** -0.5),
        'ada_b': nrm((DEPTH, 6 * D_MODEL), 0.01),
        'norm1': 1.0 + nrm((DEPTH, D_MODEL), 0.1),
        'norm2': 1.0 + nrm((DEPTH, D_MODEL), 0.1),
        'w_in': nrm((DEPTH, D_MODEL, IN_COLS), D_MODEL ** -0.5),
        'q_gain': 1.0 + nrm((DEPTH, N_GROUPS, HEAD_DIM), 0.1),
        'k_gain': 1.0 + nrm((DEPTH, N_GROUPS, HEAD_DIM), 0.1),
        'rel_bias': nrm((N_BUCKETS, N_ATTN_HEADS), 0.5),
        'pool_w': nrm((DEPTH, len(POOL_WINDOWS), POOL_GROUP, POOL_GROUP), POOL_GROUP ** -0.5),
        'pool_scale': 1.0 + nrm((DEPTH, POOL_WIDTH), 0.1),
        'w_br_a': nrm((DEPTH, ATTN_WIDTH, D_MODEL), ATTN_WIDTH ** -0.5),
        'w_br_b': nrm((DEPTH, POOL_WIDTH, D_MODEL), POOL_WIDTH ** -0.5),
        'w_out': nrm((DEPTH, D_MODEL, D_MODEL), D_MODEL ** -0.5),
        'router_w': nrm((DEPTH, D_MODEL, N_EXPERTS), D_MODEL ** -0.5),
        'router_bias': nrm((DEPTH, N_EXPERTS), 0.01),
        'exp_w_gate': nrm((DEPTH, N_EXPERTS, D_MODEL, EXPERT_HIDDEN), D_MODEL ** -0.5),
        'exp_w_up': nrm((DEPTH, N_EXPERTS, D_MODEL, EXPERT_HIDDEN), D_MODEL ** -0.5),
        'exp_w_down': nrm((DEPTH, N_EXPERTS, EXPERT_HIDDEN, D_MODEL), EXPERT_HIDDEN ** -0.5),
        'sh_w_gate': nrm((DEPTH, D_MODEL, SHARED_HIDDEN), D_MODEL ** -0.5),
        'sh_w_up': nrm((DEPTH, D_MODEL, SHARED_HIDDEN), D_MODEL ** -0.5),
        'sh_w_down': nrm((DEPTH, SHARED_HIDDEN, D_MODEL), SHARED_HIDDEN ** -0.5),
    }


def reference(x_prompt, x_sample, cache_k_w128, cache_v_w128, cache_k_w512, cache_v_w512, cache_k_w2048, cache_v_w2048,
              state_pool, c_prompt, c_sample, ada_w, ada_b, norm1, norm2, w_in, q_gain, k_gain, rel_bias, pool_w,
              pool_scale, w_br_a, w_br_b, w_out, router_w, router_bias, exp_w_gate, exp_w_up, exp_w_down,
              sh_w_gate, sh_w_up, sh_w_down):
    bias_js = [group_bias(rel_bias, gi) for gi in range(N_GROUPS)]
    k_caches = (cache_k_w128, cache_k_w512, cache_k_w2048)
    v_caches = (cache_v_w128, cache_v_w512, cache_v_w2048)
    xp, xs = x_prompt, x_sample
    n_prompt = xp.shape[0] * xp.shape[1]
    pk = [[] for _ in range(N_GROUPS)]
    pv = [[] for _ in range(N_GROUPS)]
    sk = [[] for _ in range(N_GROUPS)]
    sv = [[] for _ in range(N_GROUPS)]
    ppool, spool = [], []
    for l in range(DEPTH):
        mod_p = adaln(c_prompt, ada_w[l], ada_b[l])
        mod_s = adaln(c_sample, ada_w[l], ada_b[l])
        mix_w = (w_in[l], q_gain[l], k_gain[l], bias_js, pool_w[l], pool_scale[l], w_br_a[l], w_br_b[l], w_out[l])
        hp = modulate(rmsnorm(xp, norm1[l]), mod_p[0], mod_p[1])
        yp, nk, nv, npool = token_mixer(hp, 0, None, None, None, *mix_w)
        for gi in range(N_GROUPS):
            pk[gi].append(nk[gi])
            pv[gi].append(nv[gi])
        ppool.append(npool)
        hs = modulate(rmsnorm(xs, norm1[l]), mod_s[0], mod_s[1])
        ys, nk, nv, npool = token_mixer(hs, PAST_LEN, [c[l] for c in k_caches], [c[l] for c in v_caches],
                                        state_pool[l], *mix_w)
        for gi in range(N_GROUPS):
            sk[gi].append(nk[gi])
            sv[gi].append(nv[gi])
        spool.append(npool)
        xp = xp + mod_p[2] * yp
        xs = xs + mod_s[2] * ys
        h2 = jnp.concatenate([
            modulate(rmsnorm(xp, norm2[l]), mod_p[3], mod_p[4]).reshape(-1, D_MODEL),
            modulate(rmsnorm(xs, norm2[l]), mod_s[3], mod_s[4]).reshape(-1, D_MODEL)], axis=0)
        f = moe(h2, router_w[l], router_bias[l], exp_w_gate[l], exp_w_up[l], exp_w_down[l],
                sh_w_gate[l], sh_w_up[l], sh_w_down[l])
        xp = xp + mod_p[5] * f[:n_prompt].reshape(xp.shape)
        xs = xs + mod_s[5] * f[n_prompt:].reshape(xs.shape)
    p_k_w128, p_k_w512, p_k_w2048 = (jnp.stack(a) for a in pk)
    p_v_w128, p_v_w512, p_v_w2048 = (jnp.stack(a) for a in pv)
    s_k_w128, s_k_w512, s_k_w2048 = (jnp.stack(a) for a in sk)
    s_v_w128, s_v_w512, s_v_w2048 = (jnp.stack(a) for a in sv)
    p_pool = jnp.stack(ppool)
    s_pool = jnp.stack(spool)
    return (xp, xs, p_k_w128, p_v_w128, p_k_w512, p_v_w512, p_k_w2048, p_v_w2048, p_pool,
            s_k_w128, s_v_w128, s_k_w512, s_v_w512, s_k_w2048, s_v_w2048, s_pool)
```

```python
import contextlib
import os as _os
import numpy as np
import concourse.bass as bass
import concourse.mybir as mybir
from concourse.bass_utils import run_bass_kernel_spmd

F32 = mybir.dt.float32
BF16 = mybir.dt.bfloat16
AF = mybir.ActivationFunctionType
ALU = mybir.AluOpType
AX = mybir.AxisListType

NCORES = 8
D = 1024
S = 4096
NT = S // 128
NS = 4
NTOK = S + NS
INC = 7168
QW = 1536
EPS = 1e-6
GROUPS = ((128, 1), (512, 4), (2048, 16))
NE = 256
EH = 256
PAST = 16384
VS = 68


class Buf:
    def __init__(self, t, name=""):
        self.t = t
        self.name = name
        self.w = {}
        self.r = {}
        self.excl = name.startswith("P:")

    def __getitem__(self, idx):
        return self.t[idx]


class G:
    def __init__(self, nc, es, n_dma_sems=40):
        self.nc = nc
        self.es = es
        self.eng = {"pe": nc.tensor, "act": nc.scalar, "dve": nc.vector, "pool": nc.gpsimd, "sp": nc.sync}
        self.sem = {}
        self.cnt = {}
        self.seen = {e: {} for e in self.eng}
        for e in self.eng:
            self.sem[e] = es.enter_context(nc.semaphore("sem_" + e))
            self.cnt[e] = 0
        self.dsem = []
        for i in range(n_dma_sems):
            self.dsem.append([es.enter_context(nc.semaphore("dsem%d" % i)), 0])
        self.dnext = 0
        self.out_tickets = []

    def _semof(self, key):
        if isinstance(key, tuple):
            return self.dsem[key[1]][0]
        return self.sem[key]

    def wait(self, e, ticket):
        key, val = ticket
        if key == e and e == "pe":
            return
        if self.seen[e].get(key, 0) >= val:
            return
        self.eng[e].wait_ge(self._semof(key), val)
        self.seen[e][key] = val

    def _deps(self, reads, writes):
        deps = {}
        for b in reads:
            for k, v in b.w.items():
                deps[k] = max(deps.get(k, 0), v)
            if b.excl:
                for k, v in b.r.items():
                    deps[k] = max(deps.get(k, 0), v)
        for b in writes:
            for k, v in b.w.items():
                deps[k] = max(deps.get(k, 0), v)
            for k, v in b.r.items():
                deps[k] = max(deps.get(k, 0), v)
        return deps

    def _mark(self, t, reads, writes):
        for b in reads:
            b.r[t[0]] = max(b.r.get(t[0], 0), t[1])
        for b in writes:
            b.w = {t[0]: t[1]}
            b.r = {}

    def barrier(self):
        for e in self.eng:
            for i, (sem, n) in enumerate(self.dsem):
                if n > 0:
                    self.wait(e, (("d", i), 16 * n))
            for e2 in self.eng:
                if e2 != e and self.cnt[e2] > 0:
                    self.wait(e, (e2, self.cnt[e2]))

    def op(self, e, fn, reads=(), writes=()):
        for k, v in self._deps(reads, writes).items():
            self.wait(e, (k, v))
        inst = fn()
        self.cnt[e] += 1
        inst.then_inc(self.sem[e], 1)
        t = (e, self.cnt[e])
        self._mark(t, reads, writes)
        return t

    def mm(self, mms, reads, out):
        for k, v in self._deps(reads, [out]).items():
            self.wait("pe", (k, v))
        n = len(mms)
        inst = None
        for i, (o, l, r) in enumerate(mms):
            inst = self.nc.tensor.matmul(o, lhsT=l, rhs=r, start=(i == 0), stop=(i == n - 1))
        self.cnt["pe"] += 1
        inst.then_inc(self.sem["pe"], 1)
        t = ("pe", self.cnt["pe"])
        self._mark(t, reads, [out])
        return t

    def tr(self, trs, reads, out):
        for k, v in self._deps(reads, [out]).items():
            self.wait("pe", (k, v))
        inst = None
        for (o, i_, idn) in trs:
            inst = self.nc.tensor.transpose(o, i_, idn)
        self.cnt["pe"] += 1
        inst.then_inc(self.sem["pe"], 1)
        t = ("pe", self.cnt["pe"])
        self._mark(t, reads, [out])
        return t

    def dma(self, q, out, in_, reads=(), writes=(), is_output=False, **kw):
        for k, v in self._deps(reads, writes).items():
            self.wait(q, (k, v))
        i = self.dnext
        self.dnext = (self.dnext + 1) % len(self.dsem)
        sem, n = self.dsem[i]
        if n > 0:
            self.wait(q, (("d", i), 16 * n))
        self.eng[q].dma_start(out=out, in_=in_, **kw).then_inc(sem, 16)
        self.dsem[i][1] = n + 1
        t = (("d", i), 16 * (n + 1))
        self._mark(t, reads, writes)
        if is_output:
            self.out_tickets.append(t)
        return t

    def finish(self):
        for i, (sem, n) in enumerate(self.dsem):
            if n > 0:
                self.wait("sp", (("d", i), 16 * n))
        for e in ("pe", "act", "dve", "pool"):
            if self.cnt[e] > 0:
                self.wait("sp", (e, self.cnt[e]))


def t5_bucket(dist):
    exact = 16
    d = np.asarray(dist)
    large = exact + (np.log(np.maximum(d, 1) / exact) / np.log(2048 / exact) * (32 - exact)).astype(np.int32)
    large = np.minimum(large, 31)
    return np.where(d < exact, d, large).astype(np.int32)


def static_tables():
    tb = {}
    k = np.arange(128)[:, None]
    qq = np.arange(256)[None, :]
    j = qq - k
    valid = (j >= 0) & (j <= 128)
    tb["maskT"] = valid.astype(np.float32)
    tb["jT"] = np.clip(j, 0, 128)
    tb["bkt"] = [t5_bucket(np.arange(129) * dil) for (_, dil) in GROUPS]
    tb["ident"] = np.eye(128, dtype=np.float32)
    bands = np.zeros((3, 4, 128, 128), np.float32)
    for gi, w in enumerate((2, 4, 8, 16)):
        for t in range(128):
            for tp in range(t - w + 1, t + 1):
                if tp >= 0:
                    bands[0, gi, tp, t] += 1.0 / w
                    bands[2, gi, tp, t] += 1.0 / min(t + 1, w)
                else:
                    bands[1, gi, 128 + tp, t] += 1.0 / w
            bands[0, gi, t, t] -= 1.0
            bands[2, gi, t, t] -= 1.0
    tb["bands"] = bands
    bd = np.zeros((8, 8, 65), np.float32)
    for h in range(8):
        bd[h, h, :] = 1.0
    tb["bd"] = bd
    bandS = np.zeros((4, 60, NS), np.float32)
    diagS = np.zeros((4, NS, NS), np.float32)
    for gi, w in enumerate((2, 4, 8, 16)):
        for b in range(NS):
            for i in range(15 - (w - 1), 15):
                bandS[gi, b * 15 + i, b] = 1.0 / w
            diagS[gi, b, b] = 1.0 / w - 1.0
    tb["bandS"] = bandS
    tb["diagS"] = diagS
    return tb


TB = static_tables()


def build_nc(stages=("all",), debug=False):
    nc = bass.Bass("TRN2", target_bir_lowering=False)
    es = contextlib.ExitStack()
    dr = {}

    def din(name, shape, dt=F32):
        dr[name] = nc.dram_tensor(name, list(shape), dt, kind="ExternalInput").ap()
        return dr[name]

    def dout(name, shape, dt=F32):
        dr[name] = nc.dram_tensor(name, list(shape), dt, kind="ExternalOutput").ap()
        return dr[name]

    def dscr(name, shape, dt=F32):
        kind = "ExternalOutput" if debug else "Internal"
        dr[name] = nc.dram_tensor(name, list(shape), dt, kind=kind).ap()
        return dr[name]

    x = din("x", [S, D])
    xs = din("xs", [NS, D])
    cin = din("cin", [128, 8, 5])
    ada_w = din("ada_w", [D, 6 * D])
    ada_b = din("ada_b", [1, 6 * D])
    norm1 = din("norm1", [1, D])
    norm2 = din("norm2", [1, D])
    w_in = din("w_in", [D, INC])
    qg = din("qg", [1, QW])
    kg = din("kg", [1, QW])
    ident_d = din("ident", [128, 128])
    bands_d = din("bands", [3, 4, 128, 128])
    pool_w = din("pool_w", [4, 128, 128])
    pool_sc = din("pool_sc", [128, 4])
    y = dout("y", [S, D])
    ys = dout("ys", [NS, D])
    pk = [dout("pk%d" % w, [w, 512]) for (w, _) in GROUPS]
    pv = [dout("pv%d" % w, [w, 512]) for (w, _) in GROUPS]
    ppool = dout("ppool", [15, 512])
    mod_d = dscr("mod_d", [5, 6 * D])
    QT_d = dscr("QT_d", [QW, S], BF16)
    KT_d = dscr("KT_d", [QW, S], BF16)
    V1_d = dscr("V1_d", [S, 24 * VS], BF16)
    qkvs_d = dscr("qkvs_d", [NS, 3 * QW])
    gates_d = dscr("gates_d", [NTOK, 2048], BF16)
    ybT_d = dscr("ybT_d", [NT + 1, 128, 512], BF16)
    us_d = dscr("us_d", [NS, 512])
    biasT_d = din("biasT", [24, 128, 256])
    maskT_d = din("maskT", [128, 256])
    A_d = dscr("A_d", [3, S, 520])
    w_br_a = din("w_br_a", [512, D])
    w_br_b = din("w_br_b", [512, D])
    w_out = din("w_out", [D, D])
    router_w = din("router_w", [D, NE])
    router_b = din("router_b", [1, NE])
    eg = din("eg", [NE, D, EH])
    eu = din("eu", [NE, D, EH])
    ed = din("ed", [NE, EH, D])
    shg = din("shg", [D, EH])
    shu = din("shu", [D, EH])
    shd = din("shd", [EH, D])
    x1_d = dscr("x1_d", [NTOK, D])
    h2T_d = dscr("h2T_d", [NT + 1, 128, 8 * 128], BF16)
    Wd_d = dscr("Wd_d", [NT + 1, 128, NE])
    oas_d = dscr("oas_d", [NS, 512])
    As_d = dscr("As_d", [NS, 3, 520])
    ck = [din("ck%d" % w, [NS, w, 512]) for (w, _) in GROUPS]
    cv = [din("cv%d" % w, [NS, w, 512]) for (w, _) in GROUPS]
    stp = din("stp", [NS, 15, 512])
    bias0_d = din("bias0", [1, 24])
    bias_s_d = din("bias_s", [3, 128, 8])
    bd_d = din("bd", [8, 8, 65])
    bandS_d = din("bandS", [4, 60, NS])
    diagS_d = din("diagS", [4, NS, NS])
    sk = [dout("sk%d" % w, [NS, w, 512]) for (w, _) in GROUPS]
    sv = [dout("sv%d" % w, [NS, w, 512]) for (w, _) in GROUPS]
    spool = dout("spool", [NS, 15, 512])

    with es:
        g = G(nc, es)

        def sb(name, shape, dt=F32):
            return Buf(es.enter_context(nc.sbuf_tensor("s_" + name, list(shape), dt)), name)

        def ps(name, shape, dt=F32):
            return Buf(es.enter_context(nc.psum_tensor("p_" + name, list(shape), dt)), name)

        ident_f = sb("ident_f", [128, 128])
        g.dma("sp", ident_f[:], ident_d, writes=[ident_f])
        nhalf = sb("nhalf", [128, 8])
        g.op("dve", lambda: nc.vector.memset(nhalf[:], -0.5), [], [nhalf])
        ident_b = sb("ident_b", [128, 128], BF16)
        g.dma("pool", ident_b[:], ident_d, writes=[ident_b])

        if True:
            p0 = contextlib.ExitStack()
            with p0:
                def sb0(name, shape, dt=F32):
                    return Buf(p0.enter_context(nc.sbuf_tensor("s_" + name, list(shape), dt)), name)
                cT = sb0("cT", [128, 8, 5])
                sg = sb0("sg", [128, 8, 5])
                g.dma("sp", cT[:], cin, writes=[cT])
                g.op("act", lambda: nc.scalar.activation(out=sg[:], in_=cT[:], func=AF.Sigmoid), [cT], [sg])
                g.op("dve", lambda: nc.vector.tensor_mul(out=sg[:], in0=cT[:], in1=sg[:]), [cT, sg], [sg])
                adab = sb0("adab", [5, 6 * D])
                g.dma("act", adab[:], ada_b.to_broadcast([5, 6 * D]), writes=[adab])
                modsb = sb0("modsb", [5, 6 * D])
                wbuf = [sb0("adaw%d" % i, [128, 8, 512]) for i in range(2)]
                pmod = [Buf(p0.enter_context(nc.psum_tensor("pmod%d" % i, [5, 512], F32)), "P:pmod") for i in range(2)]
                for cg in range(12):
                    wb = wbuf[cg % 2]
                    g.dma("sp" if cg % 2 == 0 else "act", wb[:],
                          ada_w[:, cg * 512:(cg + 1) * 512].rearrange("(k p) n -> p k n", p=128), writes=[wb])
                    pm = pmod[cg % 2]
                    g.mm([(pm[:], sg[:, k, :], wb[:, k, :]) for k in range(8)], [sg, wb], pm)
                    g.op("dve", lambda pm=pm, cg=cg: nc.vector.tensor_add(
                        out=modsb[:, cg * 512:(cg + 1) * 512], in0=pm[:], in1=adab[:, cg * 512:(cg + 1) * 512]),
                        [pm, adab], [modsb])
                g.dma("sp", mod_d, modsb[:], reads=[modsb])
                g.barrier()

        if "p1" in stages or "all" in stages:
            p1 = contextlib.ExitStack()
            with p1:
                def sb1(name, shape, dt=F32):
                    return Buf(p1.enter_context(nc.sbuf_tensor("s_" + name, list(shape), dt)), name)

                def ps1(name, shape, dt=F32):
                    return Buf(p1.enter_context(nc.psum_tensor("p_" + name, list(shape), dt)), "P:" + name)

                w_in_sb = sb1("w_in_sb", [128, 8, INC], BF16)
                wchunks = [Buf(None, "wc%d" % i) for i in range(14)]
                _skip = _os.environ.get('SKIP', '').split(',')
                for cg in range(14 if 'win' not in _skip else 0):
                    g.dma("pool", w_in_sb[:, :, cg * 512:(cg + 1) * 512],
                          w_in[:, cg * 512:(cg + 1) * 512].rearrange("(k p) n -> p k n", p=128),
                          writes=[wchunks[cg]])
                G1 = sb1("G1", [128, D]); SH1 = sb1("SH1", [128, D])
                G1s = sb1("G1s", [NS, D]); SH1s = sb1("SH1s", [NS, D])
                n1b = sb1("n1b", [128, D])
                g.dma("sp", n1b[:], norm1.to_broadcast([128, D]), writes=[n1b])
                g.dma("sp", G1[:], mod_d[0:1, D:2 * D].to_broadcast([128, D]), writes=[G1])
                g.dma("sp", SH1[:], mod_d[0:1, 0:D].to_broadcast([128, D]), writes=[SH1])
                g.dma("sp", G1s[:], mod_d[1:5, D:2 * D], writes=[G1s])
                g.dma("sp", SH1s[:], mod_d[1:5, 0:D], writes=[SH1s])
                g.op("dve", lambda: nc.vector.scalar_tensor_tensor(
                    out=G1[:], in0=G1[:], scalar=1.0, in1=n1b[:], op0=ALU.add, op1=ALU.mult), [G1, n1b], [G1])
                g.op("dve", lambda: nc.vector.scalar_tensor_tensor(
                    out=G1s[:], in0=G1s[:], scalar=1.0, in1=n1b[:NS, :], op0=ALU.add, op1=ALU.mult), [G1s, n1b], [G1s])
                qgb = sb1("qgb", [128, QW]); kgb = sb1("kgb", [128, QW])
                g.dma("sp", qgb[:], qg.to_broadcast([128, QW]), writes=[qgb])
                g.dma("sp", kgb[:], kg.to_broadcast([128, QW]), writes=[kgb])
                g.op("dve", lambda: nc.vector.tensor_scalar_mul(out=qgb[:], in0=qgb[:], scalar1=0.125), [qgb], [qgb])
                bands = sb1("bands", [128, 12, 128])
                if 'bands' not in _skip:
                    g.dma("sp", bands[:], bands_d.rearrange("a g p t -> p (a g) t"), writes=[bands])
                pw_sb = sb1("pw_sb", [128, 4, 128], BF16)
                if 'poolw' not in _skip:
                    g.dma("pool", pw_sb[:], pool_w.rearrange("g c d -> c g d"), writes=[pw_sb])
                psc = sb1("psc", [128, 4])
                g.dma("sp", psc[:], pool_sc, writes=[psc])

                xt = [sb1("xt%d" % i, [128, D]) for i in range(2)]
                sq = sb1("sq", [128, D])
                hb = sb1("hb", [128, D], BF16)
                hT = sb1("hT", [128, 8, 128], BF16)
                ssq = sb1("ssq", [128, 1]); rstd = sb1("rstd", [128, 1])
                ss8 = sb1("ss8", [128, 8]); rs8 = sb1("rs8", [128, 8])
                qf = [sb1("qf%d" % i, [128, 512]) for i in range(2)]
                kf = [sb1("kf%d" % i, [128, 512]) for i in range(2)]
                vf = [sb1("vf%d" % i, [128, 512]) for i in range(2)]
                V1 = sb1("V1", [128, 24, VS], BF16)
                if 'memv1' not in _skip:
                    g.op("dve", lambda: nc.vector.memset(V1[:], 1.0), [], [V1])
                QTst = sb1("QTst", [128, 12, 128], BF16)
                KTst = sb1("KTst", [128, 12, 128], BF16)
                ut = [sb1("ut%d" % i, [128, 512]) for i in range(2)]
                gts = sb1("gts", [128, 2048], BF16)
                pooledT = sb1("pooledT", [128, 4, 128], BF16)
                ybT = sb1("ybT", [128, 4, 128], BF16)
                pz = [ps1("pz%d" % i, [128, 512]) for i in range(3)]
                ptr = ps1("ptr", [128, 8, 128], BF16)
                ptq = ps1("ptq", [128, 4, 128])
                ppl = ps1("ppl", [128, 4, 128])
                pmx = ps1("pmx", [128, 4, 128])

                def load_x(i):
                    if i < NT:
                        g.dma("sp", xt[i % 2][:], x[i * 128:(i + 1) * 128, :], writes=[xt[i % 2]])
                    else:
                        g.dma("sp", xt[i % 2][:NS, :], xs, writes=[xt[i % 2]])

                load_x(0)
                pzi = 0
                _tl = _os.environ.get('P1_TILES')
                _tiles = list(range(NT + 1)) if _tl is None else [int(v) for v in _tl.split(',') if v != '']
                for i in _tiles:
                    n = 128 if i < NT else NS
                    samp = (i == NT)
                    if i + 1 <= NT and _tl is None:
                        load_x(i + 1)
                    if _tl is not None and i != 0:
                        load_x(i)
                    xb = xt[i % 2]
                    Gm, Sm = (G1s, SH1s) if samp else (G1, SH1)
                    g.op("act", lambda: nc.scalar.activation(out=sq[:n, :], in_=xb[:n, :], func=AF.Square,
                                                             accum_out=ssq[:n, :]), [xb], [sq, ssq])
                    g.op("dve", lambda: nc.vector.tensor_scalar(out=rstd[:n, :], in0=ssq[:n, :], scalar1=1.0 / D,
                                                                scalar2=EPS, op0=ALU.mult, op1=ALU.add), [ssq], [rstd])
                    g.op("pool", lambda: nc.gpsimd.tensor_tensor(out=rstd[:n, :], in0=rstd[:n, :], in1=nhalf[:n, 0:1],
                                                                 op=ALU.pow), [rstd, nhalf], [rstd])
                    g.op("dve", lambda: nc.vector.scalar_tensor_tensor(
                        out=sq[:n, :], in0=xb[:n, :], scalar=rstd[:n, :], in1=Gm[:n, :], op0=ALU.mult, op1=ALU.mult),
                        [xb, rstd, Gm], [sq])
                    g.op("dve", lambda: nc.vector.tensor_add(out=hb[:n, :], in0=sq[:n, :], in1=Sm[:n, :]), [sq, Sm], [hb])
                    LVL = int(_os.environ.get('P1_LVL', '99'))
                    if LVL < 2:
                        continue
                    g.tr([(ptr[:, k, :n], hb[:n, k * 128:(k + 1) * 128], ident_b[:n, :n]) for k in range(8)],
                         [hb, ident_b], ptr)
                    g.op("act", lambda: nc.scalar.copy(out=hT[:, :, :n], in_=ptr[:, :, :n]), [ptr], [hT])
                    if LVL < 3:
                        continue
                    ucur = ut[i % 2]
                    for cg in range(14):
                        pzb = pz[pzi % 3]
                        pzi += 1
                        g.mm([(pzb[:n, :], hT[:, k, :n], w_in_sb[:, k, cg * 512:(cg + 1) * 512]) for k in range(8)],
                             [hT, wchunks[cg]], pzb)
                        if LVL < 4:
                            continue
                        if cg < 6:
                            gi = cg % 3
                            isq = cg < 3
                            dst = (qf if isq else kf)[gi % 2]
                            gain = qgb if isq else kgb
                            g.op("act", lambda: nc.scalar.activation(out=sq[:n, :512], in_=pzb[:n, :], func=AF.Square),
                                 [pzb], [sq])
                            g.op("dve", lambda: nc.vector.tensor_reduce(
                                out=ss8[:n, :], in_=sq[:n, :512].rearrange("p (h e) -> p h e", e=64),
                                axis=AX.X, op=ALU.add), [sq], [ss8])
                            g.op("dve", lambda: nc.vector.tensor_scalar(out=rs8[:n, :], in0=ss8[:n, :], scalar1=1.0 / 64,
                                                                        scalar2=EPS, op0=ALU.mult, op1=ALU.add), [ss8], [rs8])
                            g.op("pool", lambda: nc.gpsimd.tensor_tensor(out=rs8[:n, :], in0=rs8[:n, :], in1=nhalf[:n, :],
                                                                         op=ALU.pow), [rs8, nhalf], [rs8])
                            g.op("dve", lambda: nc.vector.tensor_tensor(
                                out=dst[:n, :].rearrange("p (h e) -> p h e", e=64),
                                in0=pzb[:n, :].rearrange("p (h e) -> p h e", e=64),
                                in1=rs8[:n, :].unsqueeze(2).to_broadcast([n, 8, 64]), op=ALU.mult), [pzb, rs8], [dst])
                            g.op("dve", lambda: nc.vector.tensor_mul(out=dst[:n, :], in0=dst[:n, :],
                                                                     in1=gain[:n, gi * 512:(gi + 1) * 512]), [dst, gain], [dst])
                            if LVL < 5:
                                continue
                            if not samp:
                                g.tr([(ptq[:, j, :n], dst[:n, j * 128:(j + 1) * 128], ident_f[:n, :n]) for j in range(4)],
                                     [dst, ident_f], ptq)
                                st = QTst if isq else KTst
                                g.op("act", lambda: nc.scalar.copy(out=st[:, gi * 4:(gi + 1) * 4, :], in_=ptq[:]), [ptq], [st])
                                if not isq:
                                    W = GROUPS[gi][0]
                                    r0 = i * 128 - (S - W)
                                    if r0 >= 0:
                                        g.dma("sp", pk[gi][r0:r0 + 128, :], dst[:], reads=[dst], is_output=True)
                            else:
                                off = (0 if isq else QW) + gi * 512
                                g.dma("sp", qkvs_d[:, off:off + 512], dst[:NS, :], reads=[dst])
                        elif LVL < 6:
                            continue
                        elif cg < 9:
                            gi = cg - 6
                            dst = vf[gi % 2]
                            if 'vact' not in _skip:
                                g.op("dve", lambda: nc.vector.tensor_copy(out=dst[:n, :], in_=pzb[:n, :]), [pzb], [dst])
                            if not samp and 'vcopy' not in _skip:
                                g.op("act", lambda: nc.scalar.copy(
                                    out=V1[:, gi * 8:(gi + 1) * 8, 0:64], in_=dst[:, :].rearrange("p (h e) -> p h e", e=64)),
                                    [dst], [V1])
                                W = GROUPS[gi][0]
                                r0 = i * 128 - (S - W)
                                if r0 >= 0:
                                    g.dma("sp", pv[gi][r0:r0 + 128, :], dst[:], reads=[dst], is_output=True)
                            else:
                                off = 2 * QW + gi * 512
                                g.dma("sp", qkvs_d[:, off:off + 512], dst[:NS, :], reads=[dst])
                        elif LVL < 7:
                            continue
                        elif cg == 9:
                            g.op("act", lambda: nc.scalar.copy(out=ucur[:n, :], in_=pzb[:n, :]), [pzb], [ucur])
                        else:
                            c0 = (cg - 10) * 512
                            g.op("act", lambda: nc.scalar.activation(out=gts[:n, c0:c0 + 512], in_=pzb[:n, :],
                                                                     func=AF.Sigmoid), [pzb], [gts])
                    if LVL < 8:
                        continue
                    if not samp:
                        g.dma("sp", QT_d[:, i * 128:(i + 1) * 128].rearrange("(j p) t -> p j t", p=128), QTst[:], reads=[QTst])
                        g.dma("sp", KT_d[:, i * 128:(i + 1) * 128].rearrange("(j p) t -> p j t", p=128), KTst[:], reads=[KTst])
                        g.dma("sp", V1_d[i * 128:(i + 1) * 128, :], V1[:].rearrange("p a b -> p (a b)"), reads=[V1])
                        g.dma("sp", gates_d[i * 128:(i + 1) * 128, :], gts[:], reads=[gts])
                        if i == NT - 1:
                            g.dma("sp", ppool, ucur[113:128, :], reads=[ucur], is_output=True)
                        if LVL < 9:
                            continue
                        uprev = ut[(i + 1) % 2]
                        for gi in range(4):
                            cs = slice(gi * 128, (gi + 1) * 128)
                            if i == 0:
                                mms = [(ppl[:, gi, :], ucur[:, cs], bands[:, 8 + gi, :])]
                            else:
                                mms = [(ppl[:, gi, :], ucur[:, cs], bands[:, gi, :]),
                                       (ppl[:, gi, :], uprev[:, cs], bands[:, 4 + gi, :])]
                            g.mm(mms, [ucur, uprev, bands], ppl)
                        g.op("dve", lambda: nc.vector.tensor_copy(out=pooledT[:], in_=ppl[:]), [ppl], [pooledT])
                        for gi in range(4):
                            g.mm([(pmx[:, gi, :], pw_sb[:, gi, :], pooledT[:, gi, :])], [pw_sb, pooledT], pmx)
                        for gi in range(4):
                            g.op("act", lambda gi=gi: nc.scalar.activation(out=ybT[:, gi, :], in_=pmx[:, gi, :], func=AF.Copy,
                                                                          scale=psc[:, gi:gi + 1]), [pmx, psc], [ybT])
                        g.dma("sp", ybT_d[i], ybT[:].rearrange("p a b -> p (a b)"), reads=[ybT])
                    else:
                        g.dma("sp", gates_d[S:S + NS, :], gts[:NS, :], reads=[gts])
                        g.dma("sp", us_d, ucur[:NS, :], reads=[ucur])
                g.barrier()
        if "p2" in stages or "all" in stages:
            p2 = contextlib.ExitStack()
            with p2:
                def sb2(name, shape, dt=F32):
                    return Buf(p2.enter_context(nc.sbuf_tensor("s2_" + name, list(shape), dt)), name)

                def ps2(name, shape, dt=F32):
                    return Buf(p2.enter_context(nc.psum_tensor("p2_" + name, list(shape), dt)), "P:" + name)

                Eb = sb2("Eb", [128, 24, 256], BF16)
                mk = sb2("mk", [128, 256])
                g.dma("sp", mk[:], maskT_d, writes=[mk])
                for c4 in range(6):
                    bt = sb2("bt%d" % c4, [128, 4, 256])
                    g.dma("sp", bt[:], biasT_d[c4 * 4:(c4 + 1) * 4].rearrange("a k q -> k a q"), writes=[bt])
                    g.op("act", lambda: nc.scalar.activation(out=bt[:], in_=bt[:], func=AF.Exp), [bt], [bt])
                    g.op("dve", lambda: nc.vector.tensor_tensor(
                        out=Eb[:, c4 * 4:(c4 + 1) * 4, :], in0=bt[:], in1=mk[:].unsqueeze(1).to_broadcast([128, 4, 256]),
                        op=ALU.mult), [bt, mk], [Eb])
                QTg = sb2("QTg", [128, 4, S], BF16)
                KTg = sb2("KTg", [128, 4, S], BF16)
                V1g = sb2("V1g", [128, 32, 8 * VS], BF16)
                PT = [[sb2("PT%d_%d" % (h, j), [128, 256], BF16) for j in range(2)] for h in range(8)]
                pe32 = [sb2("pe32_%d" % j, [128, 256]) for j in range(2)]
                oacc = [sb2("oacc%d" % j, [128, 8, 65]) for j in range(2)]
                pS = [ps2("pS%d" % j, [128, 512]) for j in range(4)]
                pO = [[ps2("pO%d_%d" % (j, hh), [128, 512]) for hh in range(2)] for j in range(2)]

                def sl(s0, c, st):
                    return slice(s0, s0 + st * (c - 1) + 1, st)

                si = 0
                oi = 0
                for gi, (W, dil) in enumerate(GROUPS):
                    L = S // dil
                    nb = L // 128
                    g.dma("sp", QTg[:], QT_d[gi * 512:(gi + 1) * 512, :].rearrange("(j p) t -> p j t", p=128), writes=[QTg])
                    g.dma("act", KTg[:], KT_d[gi * 512:(gi + 1) * 512, :].rearrange("(j p) t -> p j t", p=128), writes=[KTg])
                    vsrc = V1_d[:, gi * 8 * VS:(gi + 1) * 8 * VS].rearrange("(cb a r) c -> a r cb c", a=128, r=dil)
                    vdst = V1g[:].rearrange("p (r cb) c -> p r cb c", r=dil)
                    nsplit = max(1, 4 // dil)
                    cbs = nb // nsplit
                    wt = []
                    for r in range(dil):
                        for sp_ in range(nsplit):
                            g.dma("sp", vdst[:, r, sp_ * cbs:(sp_ + 1) * cbs, :], vsrc[:, r, sp_ * cbs:(sp_ + 1) * cbs, :],
                                  writes=[V1g] if (r == 0 and sp_ == 0) else [])
                    V1g.w = {(("d", i)): 16 * n for i, (s_, n) in enumerate(g.dsem) if n > 0}
                    Adst = A_d[gi].rearrange("(cb a r) c -> r cb a c", a=128, r=dil)
                    for r in range(dil):
                        for kb in range(nb):
                            nq = 256 if kb < nb - 1 else 128
                            for h in range(8):
                                j = h // 2
                                rows = slice((h % 2) * 64, (h % 2) * 64 + 64)
                                psb = pS[si % 4]
                                si += 1
                                g.mm([(psb[:, :nq], KTg[rows, j, sl(r + dil * kb * 128, 128, dil)],
                                       QTg[rows, j, sl(r + dil * kb * 128, nq, dil)])], [KTg, QTg], psb)
                                e32 = pe32[si % 2]
                                g.op("act", lambda: nc.scalar.activation(out=e32[:, :nq], in_=psb[:, :nq], func=AF.Exp),
                                     [psb], [e32])
                                ptb = PT[h][kb % 2]
                                g.op("dve", lambda: nc.vector.tensor_tensor(out=ptb[:, :nq], in0=e32[:, :nq],
                                                                            in1=Eb[:, gi * 8 + h, :nq], op=ALU.mult),
                                     [e32, Eb], [ptb])
                            po = pO[oi % 2]
                            ob = oacc[oi % 2]
                            oi += 1
                            bi = r * nb + kb
                            for h in range(8):
                                pob = po[h // 4]
                                mms = []
                                if kb > 0:
                                    mms.append((pob[:, (h % 4) * 65:(h % 4) * 65 + 65], PT[h][(kb - 1) % 2][:, 128:256], V1g[:, bi - 1, h * VS:h * VS + 65]))
                                mms.append((pob[:, (h % 4) * 65:(h % 4) * 65 + 65], PT[h][kb % 2][:, 0:128], V1g[:, bi, h * VS:h * VS + 65]))
                                g.mm(mms, [PT[h][0], PT[h][1], V1g], pob)
                            g.op("act", lambda: nc.scalar.copy(out=ob[:, 0:4, :].rearrange("p a b -> p (a b)"), in_=po[0][:, 0:260]), [po[0]], [ob])
                            g.op("dve", lambda: nc.vector.tensor_copy(out=ob[:, 4:8, :].rearrange("p a b -> p (a b)"), in_=po[1][:, 0:260]), [po[1]], [ob])
                            g.dma("sp", Adst[r, kb], ob[:].rearrange("p a b -> p (a b)"), reads=[ob])
                g.barrier()
        if "p2b" in stages or "all" in stages:
            for gi, (W, dil) in enumerate(GROUPS):
                for b in range(NS):
                    for (src, dst, off) in ((ck[gi], sk[gi], QW), (cv[gi], sv[gi], 2 * QW)):
                        for r0 in range(1, W, 512):
                            r1 = min(W, r0 + 512)
                            g.dma("pool", dst[b, r0 - 1:r1 - 1, :], src[b, r0:r1, :], is_output=True)
                        g.dma("pool", dst[b, W - 1:W, :], qkvs_d[b:b + 1, off + gi * 512:off + (gi + 1) * 512], is_output=True)
            for b in range(NS):
                g.dma("pool", spool[b, 0:14, :], stp[b, 1:15, :], is_output=True)
                g.dma("pool", spool[b, 14:15, :], us_d[b:b + 1, :], is_output=True)
            pb = contextlib.ExitStack()
            with pb:
                def sbb(name, shape, dt=F32):
                    return Buf(pb.enter_context(nc.sbuf_tensor("sb_" + name, list(shape), dt)), name)

                def psb_(name, shape, dt=F32):
                    return Buf(pb.enter_context(nc.psum_tensor("pb_" + name, list(shape), dt)), "P:" + name)

                qs = sbb("qs", [NS, QW]); ks = sbb("ks", [NS, QW]); vs_ = sbb("vs", [NS, QW])
                g.dma("sp", qs[:], qkvs_d[:, 0:QW], writes=[qs])
                g.dma("sp", ks[:], qkvs_d[:, QW:2 * QW], writes=[ks])
                g.dma("sp", vs_[:], qkvs_d[:, 2 * QW:3 * QW], writes=[vs_])
                prod = sbb("prod", [NS, QW])
                s0 = sbb("s0", [NS, 24]); b0 = sbb("b0", [NS, 24]); p0 = sbb("p0", [NS, 24])
                num0 = sbb("num0", [NS, 24, 64])
                g.dma("sp", b0[:], bias0_d.to_broadcast([NS, 24]), writes=[b0])
                g.op("dve", lambda: nc.vector.tensor_mul(out=prod[:], in0=qs[:], in1=ks[:]), [qs, ks], [prod])
                g.op("dve", lambda: nc.vector.tensor_reduce(out=s0[:], in_=prod[:].rearrange("p (h e) -> p h e", e=64),
                                                            axis=AX.X, op=ALU.add), [prod], [s0])
                g.op("dve", lambda: nc.vector.tensor_add(out=s0[:], in0=s0[:], in1=b0[:]), [s0, b0], [s0])
                g.op("act", lambda: nc.scalar.activation(out=p0[:], in_=s0[:], func=AF.Exp), [s0], [p0])
                g.op("dve", lambda: nc.vector.tensor_tensor(
                    out=num0[:], in0=vs_[:].rearrange("p (h e) -> p h e", e=64),
                    in1=p0[:].unsqueeze(2).to_broadcast([NS, 24, 64]), op=ALU.mult), [vs_, p0], [num0])
                BD = sbb("BD", [8, 8, 65])
                g.dma("sp", BD[:], bd_d, writes=[BD])
                ones8 = sbb("ones8", [8, 1])
                g.op("dve", lambda: nc.vector.memset(ones8[:], 1.0), [], [ones8])
                bsm = sbb("bsm", [128, 3, 8])
                g.dma("sp", bsm[:], bias_s_d.rearrange("g k h -> k g h"), writes=[bsm])
                Ksel = [sbb("Ksel%d" % j, [128, 512]) for j in range(2)]
                V1s = [sbb("V1s%d" % j, [128, 8, 65]) for j in range(2)]
                for j in range(2):
                    g.op("dve", lambda j=j: nc.vector.memset(V1s[j][:], 1.0), [], [V1s[j]])
                qbc = [sbb("qbc%d" % j, [128, 512]) for j in range(2)]
                pr2 = sbb("pr2", [128, 512])
                sc_ = sbb("sc_", [128, 8]); pp = sbb("pp", [128, 8])
                m1 = sbb("m1", [8, 8, 65])
                arow = sbb("arow", [1, 520])
                po1 = [psb_("po1_%d" % j, [128, 512]) for j in range(2)]
                po2 = [psb_("po2_%d" % j, [128, 512]) for j in range(2)]
                it = 0
                for b in range(NS):
                    for gi, (W, dil) in enumerate(GROUPS):
                        kb_ = Ksel[it % 2]; vb_ = V1s[it % 2]; qb_ = qbc[it % 2]
                        it += 1
                        g.dma("sp", kb_[:], ck[gi][b, 0:W:dil, :], writes=[kb_])
                        g.dma("act", vb_[:, :, 0:64], cv[gi][b, 0:W:dil, :].rearrange("r (h e) -> r h e", e=64), writes=[vb_])
                        g.dma("sp", qb_[:], qkvs_d[b:b + 1, gi * 512:(gi + 1) * 512].to_broadcast([128, 512]), writes=[qb_])
                        g.op("dve", lambda: nc.vector.tensor_mul(out=pr2[:], in0=kb_[:], in1=qb_[:]), [kb_, qb_], [pr2])
                        g.op("dve", lambda: nc.vector.tensor_reduce(out=sc_[:], in_=pr2[:].rearrange("p (h e) -> p h e", e=64),
                                                                    axis=AX.X, op=ALU.add), [pr2], [sc_])
                        g.op("dve", lambda: nc.vector.tensor_add(out=sc_[:], in0=sc_[:], in1=bsm[:, gi, :]), [sc_, bsm], [sc_])
                        g.op("act", lambda: nc.scalar.activation(out=pp[:], in_=sc_[:], func=AF.Exp), [sc_], [pp])
                        for hf in range(2):
                            g.mm([(po1[hf][0:8, 0:260], pp[:, :], vb_[:, hf * 4:(hf + 1) * 4, :].rearrange("p a b -> p (a b)"))],
                                 [pp, vb_], po1[hf])
                            g.op("dve", lambda: nc.vector.tensor_tensor(
                                out=m1[:, hf * 4:(hf + 1) * 4, :].rearrange("p a b -> p (a b)"), in0=po1[hf][0:8, 0:260],
                                in1=BD[:, hf * 4:(hf + 1) * 4, :].rearrange("p a b -> p (a b)"), op=ALU.mult), [po1[hf], BD], [m1])
                        for hf in range(2):
                            g.mm([(po2[hf][0:1, 0:260], ones8[:, :], m1[:, hf * 4:(hf + 1) * 4, :].rearrange("p a b -> p (a b)"))],
                                 [ones8, m1], po2[hf])
                            g.op("act", lambda: nc.scalar.copy(out=arow[:, hf * 260:(hf + 1) * 260], in_=po2[hf][0:1, 0:260]),
                                 [po2[hf]], [arow])
                        g.dma("sp", As_d[b, gi:gi + 1, :], arow[:], reads=[arow])
                st = sbb("st", [60, 512]); us = sbb("us", [NS, 512])
                g.dma("sp", st[:], stp.rearrange("b r c -> (b r) c"), writes=[st])
                g.dma("sp", us[:], us_d, writes=[us])
                bS = sbb("bS", [60, 4, NS]); dS = sbb("dS", [NS, 4, NS])
                g.dma("sp", bS[:], bandS_d.rearrange("g k b -> k g b"), writes=[bS])
                g.dma("sp", dS[:], diagS_d.rearrange("g k b -> k g b"), writes=[dS])
                pw2 = sbb("pw2", [128, 4, 128], BF16)
                g.dma("pool", pw2[:], pool_w.rearrange("g c d -> c g d"), writes=[pw2])
                psc2 = sbb("psc2", [128, 4])
                g.dma("sp", psc2[:], pool_sc, writes=[psc2])
                pps = psb_("pps", [128, 512]); pmxs = psb_("pmxs", [128, 512])
                for gq in range(4):
                    cs = slice(gq * 128, (gq + 1) * 128)
                    g.mm([(pps[:, gq * NS:(gq + 1) * NS], st[:, cs], bS[:, gq, :]),
                          (pps[:, gq * NS:(gq + 1) * NS], us[:, cs], dS[:, gq, :])], [st, us, bS, dS], pps)
                pTs = sbb("pTs", [128, 4 * NS], BF16)
                g.op("dve", lambda: nc.vector.tensor_copy(out=pTs[:], in_=pps[:, 0:4 * NS]), [pps], [pTs])
                for gq in range(4):
                    g.mm([(pmxs[:, gq * NS:(gq + 1) * NS], pw2[:, gq, :], pTs[:, gq * NS:(gq + 1) * NS])], [pw2, pTs], pmxs)
                ybs = sbb("ybs", [128, 4, 128], BF16)
                g.op("dve", lambda: nc.vector.memset(ybs[:], 0.0), [], [ybs])
                for gq in range(4):
                    g.op("act", lambda gq=gq: nc.scalar.activation(out=ybs[:, gq, 0:NS], in_=pmxs[:, gq * NS:(gq + 1) * NS], func=AF.Copy,
                                                                  scale=psc2[:, gq:gq + 1]), [pmxs, psc2], [ybs])
                g.dma("sp", ybT_d[NT], ybs[:].rearrange("p a b -> p (a b)"), reads=[ybs])
                g.barrier()
                As = sbb("As", [NS, 3, 8, 65])
                g.dma("sp", As[:].rearrange("p a b c -> p (a b c)"), As_d.rearrange("b g c -> b (g c)"), writes=[As])
                numt = sbb("numt", [NS, 8, 64]); lt = sbb("lt", [NS, 8])
                g.op("dve", lambda: nc.vector.tensor_add(out=numt[:], in0=As[:, 0, :, 0:64], in1=As[:, 1, :, 0:64]), [As], [numt])
                g.op("dve", lambda: nc.vector.tensor_add(out=numt[:], in0=numt[:], in1=As[:, 2, :, 0:64]), [As, numt], [numt])
                g.op("dve", lambda: nc.vector.tensor_add(out=lt[:], in0=As[:, 0, :, 64], in1=As[:, 1, :, 64]), [As], [lt])
                g.op("dve", lambda: nc.vector.tensor_add(out=lt[:], in0=lt[:], in1=As[:, 2, :, 64]), [As, lt], [lt])
                for gi in range(3):
                    g.op("dve", lambda gi=gi: nc.vector.tensor_add(out=numt[:], in0=numt[:], in1=num0[:, gi * 8:(gi + 1) * 8, :]),
                         [numt, num0], [numt])
                    g.op("dve", lambda gi=gi: nc.vector.tensor_add(out=lt[:], in0=lt[:], in1=p0[:, gi * 8:(gi + 1) * 8]), [lt, p0], [lt])
                g.op("dve", lambda: nc.vector.reciprocal(out=lt[:], in_=lt[:]), [lt], [lt])
                oas = sbb("oas", [NS, 512])
                g.op("dve", lambda: nc.vector.tensor_tensor(out=oas[:].rearrange("p (h e) -> p h e", e=64), in0=numt[:],
                                                            in1=lt[:].unsqueeze(2).to_broadcast([NS, 8, 64]), op=ALU.mult),
                     [numt, lt], [oas])
                g.dma("sp", oas_d, oas[:], reads=[oas])
                g.barrier()
        if "p3" in stages or "all" in stages:
            p3 = contextlib.ExitStack()
            with p3:
                def sb3(name, shape, dt=F32):
                    return Buf(p3.enter_context(nc.sbuf_tensor("s3_" + name, list(shape), dt)), name)

                def ps3(name, shape, dt=F32):
                    return Buf(p3.enter_context(nc.psum_tensor("p3_" + name, list(shape), dt)), "P:" + name)

                wa_sb = sb3("wa", [128, 4, D], BF16)
                wb_sb = sb3("wb", [128, 4, D], BF16)
                wo_sb = sb3("wo", [128, 8, D], BF16)
                g.dma("pool", wa_sb[:], w_br_a.rearrange("(k p) n -> p k n", p=128), writes=[wa_sb])
                g.dma("pool", wb_sb[:], w_br_b.rearrange("(k p) n -> p k n", p=128), writes=[wb_sb])
                for k2 in range(2):
                    g.dma("pool", wo_sb[:, k2 * 4:(k2 + 1) * 4, :],
                          w_out[k2 * 512:(k2 + 1) * 512, :].rearrange("(k p) n -> p k n", p=128), writes=[wo_sb] if k2 == 0 else [])
                wo_sb.w = {(("d", i)): 16 * n for i, (s_, n) in enumerate(g.dsem) if n > 0}
                rw_sb = sb3("rw", [128, 8, NE])
                g.dma("sp", rw_sb[:], router_w.rearrange("(k p) n -> p k n", p=128), writes=[rw_sb])
                rbias = sb3("rbias", [128, NE])
                g.dma("sp", rbias[:], router_b.to_broadcast([128, NE]), writes=[rbias])
                GT1 = sb3("GT1", [128, D]); G2 = sb3("G2", [128, D]); SH2 = sb3("SH2", [128, D]); n2b = sb3("n2b", [128, D])
                GT1s = sb3("GT1s", [NS, D]); G2s = sb3("G2s", [NS, D]); SH2s = sb3("SH2s", [NS, D])
                g.dma("sp", n2b[:], norm2.to_broadcast([128, D]), writes=[n2b])
                g.dma("sp", GT1[:], mod_d[0:1, 2 * D:3 * D].to_broadcast([128, D]), writes=[GT1])
                g.dma("sp", SH2[:], mod_d[0:1, 3 * D:4 * D].to_broadcast([128, D]), writes=[SH2])
                g.dma("sp", G2[:], mod_d[0:1, 4 * D:5 * D].to_broadcast([128, D]), writes=[G2])
                g.dma("sp", GT1s[:], mod_d[1:5, 2 * D:3 * D], writes=[GT1s])
                g.dma("sp", SH2s[:], mod_d[1:5, 3 * D:4 * D], writes=[SH2s])
                g.dma("sp", G2s[:], mod_d[1:5, 4 * D:5 * D], writes=[G2s])
                g.op("dve", lambda: nc.vector.scalar_tensor_tensor(
                    out=G2[:], in0=G2[:], scalar=1.0, in1=n2b[:], op0=ALU.add, op1=ALU.mult), [G2, n2b], [G2])
                g.op("dve", lambda: nc.vector.scalar_tensor_tensor(
                    out=G2s[:], in0=G2s[:], scalar=1.0, in1=n2b[:NS, :], op0=ALU.add, op1=ALU.mult), [G2s, n2b], [G2s])

                A0 = sb3("A0", [128, 8, 65]); A1 = sb3("A1", [128, 8, 65]); A2 = sb3("A2", [128, 8, 65])
                rl = sb3("rl", [128, 8])
                oaf = sb3("oaf", [128, 512])
                oab = sb3("oab", [128, 512], BF16)
                oaT = sb3("oaT", [128, 4, 128], BF16)
                ybt = sb3("ybt", [128, 4, 128], BF16)
                gt = sb3("gt", [128, 2048], BF16)
                xt3 = sb3("xt3", [128, D])
                t1_ = sb3("t1_", [128, D]); t2_ = sb3("t2_", [128, D])
                mgb = sb3("mgb", [128, D], BF16)
                mT = sb3("mT", [128, 8, 128], BF16)
                x1 = sb3("x1", [128, D])
                h2 = sb3("h2", [128, D])
                sq3 = sb3("sq3", [128, D])
                ssq3 = sb3("ssq3", [128, 1]); rstd3 = sb3("rstd3", [128, 1])
                h2T32 = sb3("h2T32", [128, 8, 128])
                h2Tb = sb3("h2Tb", [128, 8, 128], BF16)
                sc = sb3("sc", [128, NE]); sel = sb3("sel", [128, NE]); selm = sb3("selm", [128, NE])
                mx8 = sb3("mx8", [128, 8, 8]); gsc = sb3("gsc", [128, 8]); gtop = sb3("gtop", [128, 8])
                gmask = sb3("gmask", [128, 8]); top8 = sb3("top8", [128, 8])
                Mk = sb3("Mk", [128, NE]); Wd = sb3("Wd", [128, NE]); den = sb3("den", [128, 1])
                ptr3 = ps3("ptr3", [128, 8, 128], BF16)
                pbr = [ps3("pbr%d" % j, [128, 512]) for j in range(2)]
                pym = [ps3("pym%d" % j, [128, 512]) for j in range(2)]
                pt32 = [ps3("pt32_%d" % j, [128, 4, 128]) for j in range(2)]
                prt = ps3("prt", [128, 512])

                for i in range(NT + 1):
                    n = 128 if i < NT else NS
                    samp = (i == NT)
                    rows = slice(i * 128, i * 128 + n)
                    gt1, g2m, sh2m = (GT1s, G2s, SH2s) if samp else (GT1, G2, SH2)
                    g.dma("sp", xt3[:n, :], xs if samp else x[rows, :], writes=[xt3])
                    g.dma("act", gt[:n, :], gates_d[rows, :], writes=[gt])
                    g.dma("act", ybt[:].rearrange("p a b -> p (a b)"), ybT_d[i], writes=[ybt])
                    if not samp:
                        g.dma("sp", A0[:].rearrange("p a b -> p (a b)"), A_d[0][rows, :], writes=[A0])
                        g.dma("sp", A1[:].rearrange("p a b -> p (a b)"), A_d[1][rows, :], writes=[A1])
                        g.dma("sp", A2[:].rearrange("p a b -> p (a b)"), A_d[2][rows, :], writes=[A2])
                        g.op("dve", lambda: nc.vector.tensor_add(out=A0[:], in0=A0[:], in1=A1[:]), [A0, A1], [A0])
                        g.op("dve", lambda: nc.vector.tensor_add(out=A0[:], in0=A0[:], in1=A2[:]), [A0, A2], [A0])
                        g.op("dve", lambda: nc.vector.reciprocal(out=rl[:], in_=A0[:, :, 64]), [A0], [rl])
                        g.op("dve", lambda: nc.vector.tensor_tensor(
                            out=oab[:].rearrange("p (h e) -> p h e", e=64), in0=A0[:, :, 0:64],
                            in1=rl[:].unsqueeze(2).to_broadcast([128, 8, 64]), op=ALU.mult), [A0, rl], [oab])
                    else:
                        g.dma("sp", oaf[:NS, :], oas_d, writes=[oaf])
                        g.op("dve", lambda: nc.vector.tensor_copy(out=oab[:NS, :], in_=oaf[:NS, :]), [oaf], [oab])
                    g.tr([(ptr3[:, k, :n], oab[:n, k * 128:(k + 1) * 128], ident_b[:n, :n]) for k in range(4)], [oab, ident_b], ptr3)
                    g.op("act", lambda: nc.scalar.copy(out=oaT[:, :, :n], in_=ptr3[:, 0:4, :n]), [ptr3], [oaT])
                    for half in range(2):
                        cs = slice(half * 512, (half + 1) * 512)
                        g.mm([(pbr[0][:n, :], oaT[:, k, :n], wa_sb[:, k, cs]) for k in range(4)], [oaT, wa_sb], pbr[0])
                        g.mm([(pbr[1][:n, :], ybt[:, k, :n], wb_sb[:, k, cs]) for k in range(4)], [ybt, wb_sb], pbr[1])
                        g.op("dve", lambda: nc.vector.tensor_tensor(out=t1_[:n, cs], in0=pbr[0][:n, :], in1=gt[:n, cs], op=ALU.mult),
                             [pbr[0], gt], [t1_])
                        g.op("dve", lambda: nc.vector.tensor_tensor(out=t2_[:n, cs], in0=pbr[1][:n, :],
                                                                    in1=gt[:n, 1024 + half * 512:1024 + (half + 1) * 512], op=ALU.mult),
                             [pbr[1], gt], [t2_])
                    g.op("dve", lambda: nc.vector.tensor_add(out=mgb[:n, :], in0=t1_[:n, :], in1=t2_[:n, :]), [t1_, t2_], [mgb])
                    g.tr([(ptr3[:, k, :n], mgb[:n, k * 128:(k + 1) * 128], ident_b[:n, :n]) for k in range(8)], [mgb, ident_b], ptr3)
                    g.op("act", lambda: nc.scalar.copy(out=mT[:, :, :n], in_=ptr3[:, :, :n]), [ptr3], [mT])
                    for half in range(2):
                        cs = slice(half * 512, (half + 1) * 512)
                        g.mm([(pym[half][:n, :], mT[:, k, :n], wo_sb[:, k, cs]) for k in range(8)], [mT, wo_sb], pym[half])
                        g.op("dve", lambda: nc.vector.tensor_tensor(out=t1_[:n, cs], in0=pym[half][:n, :], in1=gt1[:n, cs], op=ALU.mult),
                             [pym[half], gt1], [t1_])
                    g.op("dve", lambda: nc.vector.tensor_add(out=x1[:n, :], in0=t1_[:n, :], in1=xt3[:n, :]), [t1_, xt3], [x1])
                    g.dma("sp", x1_d[rows, :], x1[:n, :], reads=[x1])
                    g.op("act", lambda: nc.scalar.activation(out=sq3[:n, :], in_=x1[:n, :], func=AF.Square, accum_out=ssq3[:n, :]),
                         [x1], [sq3, ssq3])
                    g.op("dve", lambda: nc.vector.tensor_scalar(out=rstd3[:n, :], in0=ssq3[:n, :], scalar1=1.0 / D, scalar2=EPS,
                                                                op0=ALU.mult, op1=ALU.add), [ssq3], [rstd3])
                    g.op("pool", lambda: nc.gpsimd.tensor_tensor(out=rstd3[:n, :], in0=rstd3[:n, :], in1=nhalf[:n, 0:1], op=ALU.pow),
                         [rstd3, nhalf], [rstd3])
                    g.op("dve", lambda: nc.vector.scalar_tensor_tensor(out=sq3[:n, :], in0=x1[:n, :], scalar=rstd3[:n, :],
                                                                       in1=g2m[:n, :], op0=ALU.mult, op1=ALU.mult),
                         [x1, rstd3, g2m], [sq3])
                    g.op("dve", lambda: nc.vector.tensor_add(out=h2[:n, :], in0=sq3[:n, :], in1=sh2m[:n, :]), [sq3, sh2m], [h2])
                    for hf in range(2):
                        g.tr([(pt32[hf][:, k, :n], h2[:n, (hf * 4 + k) * 128:(hf * 4 + k + 1) * 128], ident_f[:n, :n]) for k in range(4)],
                             [h2, ident_f], pt32[hf])
                        g.op("act", lambda: nc.scalar.copy(out=h2T32[:, hf * 4:(hf + 1) * 4, :n], in_=pt32[hf][:, :, :n]),
                             [pt32[hf]], [h2T32])
                    if samp:
                        g.op("dve", lambda: nc.vector.memset(h2Tb[:], 0.0), [], [h2Tb])
                    g.op("dve", lambda: nc.vector.tensor_copy(out=h2Tb[:, :, :n], in_=h2T32[:, :, :n]), [h2T32], [h2Tb])
                    g.dma("sp", h2T_d[i], h2Tb[:].rearrange("p a b -> p (a b)"), reads=[h2Tb])
                    g.mm([(prt[:n, :NE], h2T32[:, k, :n], rw_sb[:, k, :]) for k in range(8)], [h2T32, rw_sb], prt)
                    g.op("act", lambda: nc.scalar.activation(out=sc[:n, :], in_=prt[:n, :NE], func=AF.Sigmoid), [prt], [sc])
                    g.op("dve", lambda: nc.vector.tensor_add(out=sel[:n, :], in0=sc[:n, :], in1=rbias[:n, :]), [sc, rbias], [sel])
                    for gq in range(8):
                        g.op("dve", lambda: nc.vector.max(out=mx8[:n, gq, :], in_=sel[:n, gq * 32:(gq + 1) * 32]), [sel], [mx8])
                    g.op("dve", lambda: nc.vector.tensor_add(out=gsc[:n, :], in0=mx8[:n, :, 0], in1=mx8[:n, :, 1]), [mx8], [gsc])
                    g.op("dve", lambda: nc.vector.max(out=gtop[:n, :], in_=gsc[:n, :]), [gsc], [gtop])
                    g.op("dve", lambda: nc.vector.tensor_scalar(out=gmask[:n, :], in0=gsc[:n, :], scalar1=gtop[:n, 3:4], scalar2=None,
                                                                op0=ALU.is_ge), [gsc, gtop], [gmask])
                    g.op("dve", lambda: nc.vector.tensor_scalar(out=gmask[:n, :], in0=gmask[:n, :], scalar1=-1.0, scalar2=1e9,
                                                                op0=ALU.add, op1=ALU.mult), [gmask], [gmask])
                    g.op("dve", lambda: nc.vector.tensor_tensor(
                        out=selm[:n, :].rearrange("p (a b) -> p a b", b=32), in0=sel[:n, :].rearrange("p (a b) -> p a b", b=32),
                        in1=gmask[:n, :].unsqueeze(2).to_broadcast([n, 8, 32]), op=ALU.add), [sel, gmask], [selm])
                    g.op("dve", lambda: nc.vector.max(out=top8[:n, :], in_=selm[:n, :]), [selm], [top8])
                    g.op("dve", lambda: nc.vector.tensor_scalar(out=Mk[:n, :], in0=selm[:n, :], scalar1=top8[:n, 7:8], scalar2=None,
                                                                op0=ALU.is_ge), [selm, top8], [Mk])
                    g.op("dve", lambda: nc.vector.tensor_tensor(out=Wd[:n, :], in0=Mk[:n, :], in1=sc[:n, :], op=ALU.mult), [Mk, sc], [Wd])
                    g.op("dve", lambda: nc.vector.tensor_reduce(out=den[:n, :], in_=Wd[:n, :], axis=AX.X, op=ALU.add), [Wd], [den])
                    g.op("dve", lambda: nc.vector.reciprocal(out=den[:n, :], in_=den[:n, :]), [den], [den])
                    g.op("dve", lambda: nc.vector.tensor_scalar(out=Wd[:n, :], in0=Wd[:n, :], scalar1=den[:n, :], scalar2=2.5,
                                                                op0=ALU.mult, op1=ALU.mult), [Wd, den], [Wd])
                    g.dma("sp", Wd_d[i, 0:n, :], Wd[:n, :], reads=[Wd])
                g.barrier()
        if "p4" in stages or "all" in stages:
            NG = 3
            tiles_all = list(range(NT + 1))
            per = (len(tiles_all) + NG - 1) // NG
            groups4 = [tiles_all[a:a + per] for a in range(0, len(tiles_all), per)]
            n_exp = int(_os.environ.get("P4_NEXP", str(NE)))
            for tg in groups4:
                p4 = contextlib.ExitStack()
                with p4:
                    def sb4(name, shape, dt=F32):
                        return Buf(p4.enter_context(nc.sbuf_tensor("s4_%d_" % tg[0] + name, list(shape), dt)), name)

                    def ps4(name, shape, dt=F32):
                        return Buf(p4.enter_context(nc.psum_tensor("p4_%d_" % tg[0] + name, list(shape), dt)), "P:" + name)

                    ntl = len(tg)
                    hT4 = sb4("hT4", [128, 8, ntl * 128], BF16)
                    for ti, i in enumerate(tg):
                        g.dma("sp", hT4[:, :, ti * 128:(ti + 1) * 128], h2T_d[i].rearrange("p (k t) -> p k t", k=8),
                              writes=[hT4] if ti == 0 else [])
                    Wg = sb4("Wg", [128, ntl, NE])
                    for ti, i in enumerate(tg):
                        nn = 128 if i < NT else NS
                        g.dma("sp", Wg[:nn, ti, :], Wd_d[i, 0:nn, :], writes=[])
                    acc = sb4("acc", [128, ntl, D])
                    g.op("pool", lambda: nc.gpsimd.memset(acc[:], 0.0), [], [acc])
                    allw = {(("d", i_)): 16 * n_ for i_, (s_, n_) in enumerate(g.dsem) if n_ > 0}
                    hT4.w = dict(allw)
                    Wg.w = dict(allw)
                    wgs = [sb4("wg%d" % j, [128, 8, EH], BF16) for j in range(2)]
                    wus = [sb4("wu%d" % j, [128, 8, EH], BF16) for j in range(2)]
                    wds = [sb4("wd%d" % j, [128, 2, D], BF16) for j in range(2)]
                    sgt = [sb4("sgt%d" % j, [128, 512]) for j in range(2)]
                    act = [sb4("act%d" % j, [128, 512], BF16) for j in range(2)]
                    ph = [[ps4("ph%d_%d" % (a, b), [128, 512]) for b in range(2)] for a in range(2)]
                    py = [[ps4("py%d_%d" % (a, b), [128, 512]) for b in range(2)] for a in range(2)]
                    blocks = [list(range(a, min(a + 4, ntl))) for a in range(0, ntl, 4)]
                    yi = 0
                    elist = list(range(n_exp)) + [NE]
                    for ei, e in enumerate(elist):
                        j2 = ei % 2
                        if e < NE:
                            srcs = (eg[e], eu[e], ed[e])
                        else:
                            srcs = (shg, shu, shd)
                        g.dma("pool", wgs[j2][:], srcs[0].rearrange("(k p) n -> p k n", p=128), writes=[wgs[j2]])
                        g.dma("pool", wus[j2][:], srcs[1].rearrange("(k p) n -> p k n", p=128), writes=[wus[j2]])
                        g.dma("pool", wds[j2][:], srcs[2].rearrange("(k p) n -> p k n", p=128), writes=[wds[j2]])
                        for blk in blocks:
                            t0 = blk[0] * 128
                            ntb = sum(128 if tg[ti] < NT else NS for ti in blk)
                            for hh in range(2):
                                hs = slice(hh * 128, (hh + 1) * 128)
                                g.mm([(ph[0][hh][:, :ntb], wgs[j2][:, k, hs], hT4[:, k, t0:t0 + ntb]) for k in range(8)],
                                     [wgs[j2], hT4], ph[0][hh])
                                g.mm([(ph[1][hh][:, :ntb], wus[j2][:, k, hs], hT4[:, k, t0:t0 + ntb]) for k in range(8)],
                                     [wus[j2], hT4], ph[1][hh])
                                g.op("act", lambda: nc.scalar.activation(out=sgt[hh][:, :ntb], in_=ph[0][hh][:, :ntb], func=AF.Silu),
                                     [ph[0][hh]], [sgt[hh]])
                                g.op("dve", lambda: nc.vector.tensor_tensor(out=act[hh][:, :ntb], in0=ph[1][hh][:, :ntb],
                                                                            in1=sgt[hh][:, :ntb], op=ALU.mult),
                                     [ph[1][hh], sgt[hh]], [act[hh]])
                            for ti in blk:
                                nn = 128 if tg[ti] < NT else NS
                                c0 = (ti - blk[0]) * 128
                                pyy = py[yi % 2]
                                yi += 1
                                for half in range(2):
                                    cs = slice(half * 512, (half + 1) * 512)
                                    g.mm([(pyy[half][:nn, :], act[hh2][:, c0:c0 + nn], wds[j2][:, hh2, cs]) for hh2 in range(2)],
                                         [act[0], act[1], wds[j2]], pyy[half])
                                    if e < NE:
                                        g.op("dve", lambda: nc.vector.scalar_tensor_tensor(
                                            out=acc[:nn, ti, cs], in0=pyy[half][:nn, :], scalar=Wg[:nn, ti, e:e + 1],
                                            in1=acc[:nn, ti, cs], op0=ALU.mult, op1=ALU.add), [pyy[half], Wg, acc], [acc])
                                    else:
                                        g.op("dve", lambda: nc.vector.tensor_tensor(
                                            out=acc[:nn, ti, cs], in0=pyy[half][:nn, :], in1=acc[:nn, ti, cs], op=ALU.add),
                                            [pyy[half], acc], [acc])
                    GT2 = sb4("GT2", [128, D]); GT2s = sb4("GT2s", [NS, D])
                    g.dma("sp", GT2[:], mod_d[0:1, 5 * D:6 * D].to_broadcast([128, D]), writes=[GT2])
                    g.dma("sp", GT2s[:], mod_d[1:5, 5 * D:6 * D], writes=[GT2s])
                    xo = [sb4("xo%d" % j, [128, D]) for j in range(2)]
                    for ti, i in enumerate(tg):
                        nn = 128 if i < NT else NS
                        samp = (i == NT)
                        rows = slice(i * 128, i * 128 + nn)
                        xb_ = xo[ti % 2]
                        gt2 = GT2s if samp else GT2
                        g.dma("sp", xb_[:nn, :], x1_d[rows, :], writes=[xb_])
                        g.op("dve", lambda: nc.vector.tensor_tensor(out=acc[:nn, ti, :], in0=acc[:nn, ti, :], in1=gt2[:nn, :], op=ALU.mult),
                             [acc, gt2], [acc])
                        g.op("dve", lambda: nc.vector.tensor_add(out=xb_[:nn, :], in0=xb_[:nn, :], in1=acc[:nn, ti, :]), [xb_, acc], [xb_])
                        g.dma("sp", ys if samp else y[rows, :], xb_[:nn, :], reads=[xb_], is_output=True)
                    g.barrier()
        g.finish()
    return nc, dr


def core_inputs(inp, c):
    m = {}
    m["x"] = np.ascontiguousarray(inp["x_prompt"][c])
    m["xs"] = np.ascontiguousarray(inp["x_sample"][NS * c:NS * c + NS, 0])
    call = np.concatenate([inp["c_prompt"][c:c + 1], inp["c_sample"][NS * c:NS * c + NS]], axis=0)
    m["cin"] = np.ascontiguousarray(call.T.reshape(8, 128, 5).transpose(1, 0, 2))
    m["ada_w"] = inp["ada_w"][0]
    m["ada_b"] = inp["ada_b"]
    m["norm1"] = inp["norm1"]
    m["norm2"] = inp["norm2"]
    m["w_in"] = inp["w_in"][0]
    m["qg"] = np.ascontiguousarray(np.broadcast_to(inp["q_gain"][0][:, None, :], (3, 8, 64)).reshape(1, QW))
    m["kg"] = np.ascontiguousarray(np.broadcast_to(inp["k_gain"][0][:, None, :], (3, 8, 64)).reshape(1, QW))
    m["ident"] = TB["ident"]
    m["bands"] = TB["bands"]
    m["pool_w"] = inp["pool_w"][0]
    m["pool_sc"] = np.ascontiguousarray(inp["pool_scale"][0].reshape(4, 128).T)
    rb = inp["rel_bias"]
    bT = np.zeros((24, 128, 256), np.float32)
    for gi in range(3):
        idx = TB["bkt"][gi][TB["jT"]]
        for h in range(8):
            bT[gi * 8 + h] = rb[idx, gi * 8 + h]
    m["biasT"] = bT
    m["maskT"] = TB["maskT"]
    for gi, (w, dil) in enumerate(GROUPS):
        m["ck%d" % w] = np.ascontiguousarray(inp["cache_k_w%d" % w][0, NS * c:NS * c + NS].reshape(NS, w, 512))
        m["cv%d" % w] = np.ascontiguousarray(inp["cache_v_w%d" % w][0, NS * c:NS * c + NS].reshape(NS, w, 512))
    m["stp"] = np.ascontiguousarray(inp["state_pool"][0, NS * c:NS * c + NS])
    m["bias0"] = np.ascontiguousarray(rb[0:1, :])
    bs = np.zeros((3, 128, 8), np.float32)
    for gi in range(3):
        bs[gi] = rb[TB["bkt"][gi][128 - np.arange(128)], gi * 8:(gi + 1) * 8]
    m["bias_s"] = bs
    m["bd"] = TB["bd"]
    m["bandS"] = TB["bandS"]
    m["diagS"] = TB["diagS"]
    m["w_br_a"] = inp["w_br_a"][0]
    m["w_br_b"] = inp["w_br_b"][0]
    m["w_out"] = inp["w_out"][0]
    m["router_w"] = inp["router_w"][0]
    m["router_b"] = inp["router_bias"]
    m["eg"] = inp["exp_w_gate"][0]
    m["eu"] = inp["exp_w_up"][0]
    m["ed"] = inp["exp_w_down"][0]
    m["shg"] = inp["sh_w_gate"][0]
    m["shu"] = inp["sh_w_up"][0]
    m["shd"] = inp["sh_w_down"][0]
    return m


_NC_CACHE = {}


def kernel(**inputs):
    inp = {k: np.asarray(v) for k, v in inputs.items()}
    if "nc" not in _NC_CACHE:
        _NC_CACHE["nc"] = build_nc(stages=("all",), debug=False)
    nc, dr = _NC_CACHE["nc"]
    in_maps = []
    for c in range(NCORES):
        m = core_inputs(inp, c)
        in_maps.append({k: np.ascontiguousarray(v, dtype=np.float32) for k, v in m.items() if k in dr})
    res = run_bass_kernel_spmd(nc, in_maps, core_ids=list(range(NCORES)))
    R = res.results
    f = np.float32
    y_prompt = np.stack([R[c]["y"] for c in range(NCORES)], 0).astype(f)
    y_sample = np.concatenate([R[c]["ys"] for c in range(NCORES)], 0).reshape(NCORES * NS, 1, D).astype(f)
    outs = [y_prompt, y_sample]
    for (w, _) in GROUPS:
        for nm in ("pk", "pv"):
            outs.append(np.stack([R[c]["%s%d" % (nm, w)] for c in range(NCORES)], 0).reshape(1, NCORES, w, 8, 64).astype(f))
    outs.append(np.stack([R[c]["ppool"] for c in range(NCORES)], 0).reshape(1, NCORES, 15, 512).astype(f))
    for (w, _) in GROUPS:
        for nm in ("sk", "sv"):
            outs.append(np.concatenate([R[c]["%s%d" % (nm, w)] for c in range(NCORES)], 0).reshape(1, NCORES * NS, w, 8, 64).astype(f))
    outs.append(np.concatenate([R[c]["spool"] for c in range(NCORES)], 0).reshape(1, NCORES * NS, 15, 512).astype(f))
    return tuple(outs)
```

```python
import contextlib
import os as _os
import numpy as np
import concourse.bass as bass
import concourse.mybir as mybir
from concourse.bass_utils import run_bass_kernel_spmd

F32 = mybir.dt.float32
BF16 = mybir.dt.bfloat16
AF = mybir.ActivationFunctionType
ALU = mybir.AluOpType
AX = mybir.AxisListType

NCORES = 8
D = 1024
S = 4096
NT = S // 128
NS = 4
NTOK = S + NS
INC = 7168
QW = 1536
EPS = 1e-6
GROUPS = ((128, 1), (512, 4), (2048, 16))
NE = 256
EH = 256
PAST = 16384
NBLK = 513
NSLOT = NBLK * 128
SPARSE = _os.environ.get('MOE_DENSE') is None
I32 = mybir.dt.int32
VS = 68


class Buf:
    def __init__(self, t, name=""):
        self.t = t
        self.name = name
        self.w = {}
        self.r = {}
        self.excl = name.startswith("P:")

    def __getitem__(self, idx):
        return self.t[idx]


class G:
    def __init__(self, nc, es, n_dma_sems=40):
        self.nc = nc
        self.es = es
        self.eng = {"pe": nc.tensor, "act": nc.scalar, "dve": nc.vector, "pool": nc.gpsimd, "sp": nc.sync}
        self.sem = {}
        self.cnt = {}
        self.seen = {e: {} for e in self.eng}
        for e in self.eng:
            self.sem[e] = es.enter_context(nc.semaphore("sem_" + e))
            self.cnt[e] = 0
        self.dsem = []
        for i in range(n_dma_sems):
            self.dsem.append([es.enter_context(nc.semaphore("dsem%d" % i)), 0])
        self.dnext = 0
        self.out_tickets = []

    def _semof(self, key):
        if isinstance(key, tuple):
            return self.dsem[key[1]][0]
        return self.sem[key]

    def wait(self, e, ticket):
        key, val = ticket
        if key == e and e == "pe":
            return
        if self.seen[e].get(key, 0) >= val:
            return
        self.eng[e].wait_ge(self._semof(key), val)
        self.seen[e][key] = val

    def _deps(self, reads, writes):
        deps = {}
        for b in reads:
            for k, v in b.w.items():
                deps[k] = max(deps.get(k, 0), v)
            if b.excl:
                for k, v in b.r.items():
                    deps[k] = max(deps.get(k, 0), v)
        for b in writes:
            for k, v in b.w.items():
                deps[k] = max(deps.get(k, 0), v)
            for k, v in b.r.items():
                deps[k] = max(deps.get(k, 0), v)
        return deps

    def _mark(self, t, reads, writes):
        for b in reads:
            b.r[t[0]] = max(b.r.get(t[0], 0), t[1])
        for b in writes:
            b.w = {t[0]: t[1]}
            b.r = {}

    def barrier(self):
        for e in self.eng:
            for i, (sem, n) in enumerate(self.dsem):
                if n > 0:
                    self.wait(e, (("d", i), 16 * n))
            for e2 in self.eng:
                if e2 != e and self.cnt[e2] > 0:
                    self.wait(e, (e2, self.cnt[e2]))

    def op(self, e, fn, reads=(), writes=()):
        for k, v in self._deps(reads, writes).items():
            self.wait(e, (k, v))
        inst = fn()
        self.cnt[e] += 1
        inst.then_inc(self.sem[e], 1)
        t = (e, self.cnt[e])
        self._mark(t, reads, writes)
        return t

    def mm(self, mms, reads, out):
        for k, v in self._deps(reads, [out]).items():
            self.wait("pe", (k, v))
        n = len(mms)
        inst = None
        for i, (o, l, r) in enumerate(mms):
            inst = self.nc.tensor.matmul(o, lhsT=l, rhs=r, start=(i == 0), stop=(i == n - 1))
        self.cnt["pe"] += 1
        inst.then_inc(self.sem["pe"], 1)
        t = ("pe", self.cnt["pe"])
        self._mark(t, reads, [out])
        return t

    def tr(self, trs, reads, out):
        for k, v in self._deps(reads, [out]).items():
            self.wait("pe", (k, v))
        inst = None
        for (o, i_, idn) in trs:
            inst = self.nc.tensor.transpose(o, i_, idn)
        self.cnt["pe"] += 1
        inst.then_inc(self.sem["pe"], 1)
        t = ("pe", self.cnt["pe"])
        self._mark(t, reads, [out])
        return t

    def dma(self, q, out, in_, reads=(), writes=(), is_output=False, **kw):
        for k, v in self._deps(reads, writes).items():
            self.wait(q, (k, v))
        i = self.dnext
        self.dnext = (self.dnext + 1) % len(self.dsem)
        sem, n = self.dsem[i]
        if n > 0:
            self.wait(q, (("d", i), 16 * n))
        self.eng[q].dma_start(out=out, in_=in_, **kw).then_inc(sem, 16)
        self.dsem[i][1] = n + 1
        t = (("d", i), 16 * (n + 1))
        self._mark(t, reads, writes)
        if is_output:
            self.out_tickets.append(t)
        return t

    def idma(self, out, out_off, in_, in_off, reads=(), writes=()):
        q = "pool"
        for k, v in self._deps(reads, writes).items():
            self.wait(q, (k, v))
        i = self.dnext
        self.dnext = (self.dnext + 1) % len(self.dsem)
        sem, n = self.dsem[i]
        if n > 0:
            self.wait(q, (("d", i), 16 * n))
        self.nc.gpsimd.indirect_dma_start(out=out, out_offset=out_off, in_=in_, in_offset=in_off).then_inc(sem, 16)
        self.dsem[i][1] = n + 1
        t = (("d", i), 16 * (n + 1))
        self._mark(t, reads, writes)
        return t

    def finish(self):
        for i, (sem, n) in enumerate(self.dsem):
            if n > 0:
                self.wait("sp", (("d", i), 16 * n))
        for e in ("pe", "act", "dve", "pool"):
            if self.cnt[e] > 0:
                self.wait("sp", (e, self.cnt[e]))


def t5_bucket(dist):
    exact = 16
    d = np.asarray(dist)
    large = exact + (np.log(np.maximum(d, 1) / exact) / np.log(2048 / exact) * (32 - exact)).astype(np.int32)
    large = np.minimum(large, 31)
    return np.where(d < exact, d, large).astype(np.int32)


def static_tables():
    tb = {}
    k = np.arange(128)[:, None]
    qq = np.arange(256)[None, :]
    j = qq - k
    valid = (j >= 0) & (j <= 128)
    tb["maskT"] = valid.astype(np.float32)
    tb["jT"] = np.clip(j, 0, 128)
    tb["bkt"] = [t5_bucket(np.arange(129) * dil) for (_, dil) in GROUPS]
    tb["ident"] = np.eye(128, dtype=np.float32)
    bands = np.zeros((3, 4, 128, 128), np.float32)
    for gi, w in enumerate((2, 4, 8, 16)):
        for t in range(128):
            for tp in range(t - w + 1, t + 1):
                if tp >= 0:
                    bands[0, gi, tp, t] += 1.0 / w
                    bands[2, gi, tp, t] += 1.0 / min(t + 1, w)
                else:
                    bands[1, gi, 128 + tp, t] += 1.0 / w
            bands[0, gi, t, t] -= 1.0
            bands[2, gi, t, t] -= 1.0
    tb["bands"] = bands
    tb["UT"] = np.triu(np.ones((128, 128), np.float32), 1)
    tb["trash"] = np.repeat((NSLOT + 1.0 + np.arange(128, dtype=np.float32))[:, None], 8, 1)
    ee = np.arange(NE)[None, None, :]
    tb["tri"] = ((np.arange(2)[:, None, None] * 128 + np.arange(128)[None, :, None]) <= ee).astype(np.float32)
    tb["bstart"] = ((np.arange(5)[None, :] * 128 + np.arange(128)[:, None]) * 128).astype(np.float32)
    tb["piota"] = np.arange(128, dtype=np.float32).reshape(128, 1)
    bd = np.zeros((8, 8, 65), np.float32)
    for h in range(8):
        bd[h, h, :] = 1.0
    tb["bd"] = bd
    bandS = np.zeros((4, 60, NS), np.float32)
    diagS = np.zeros((4, NS, NS), np.float32)
    for gi, w in enumerate((2, 4, 8, 16)):
        for b in range(NS):
            for i in range(15 - (w - 1), 15):
                bandS[gi, b * 15 + i, b] = 1.0 / w
            diagS[gi, b, b] = 1.0 / w - 1.0
    tb["bandS"] = bandS
    tb["diagS"] = diagS
    return tb


TB = static_tables()


def build_nc(stages=("all",), debug=False):
    nc = bass.Bass("TRN2", target_bir_lowering=False)
    es = contextlib.ExitStack()
    dr = {}

    def din(name, shape, dt=F32):
        dr[name] = nc.dram_tensor(name, list(shape), dt, kind="ExternalInput").ap()
        return dr[name]

    def dout(name, shape, dt=F32):
        dr[name] = nc.dram_tensor(name, list(shape), dt, kind="ExternalOutput").ap()
        return dr[name]

    def dscr(name, shape, dt=F32):
        kind = "ExternalOutput" if debug else "Internal"
        dr[name] = nc.dram_tensor(name, list(shape), dt, kind=kind).ap()
        return dr[name]

    x = din("x", [S, D])
    xs = din("xs", [NS, D])
    cin = din("cin", [128, 8, 5])
    ada_w = din("ada_w", [D, 6 * D])
    ada_b = din("ada_b", [1, 6 * D])
    norm1 = din("norm1", [1, D])
    norm2 = din("norm2", [1, D])
    w_in = din("w_in", [D, INC])
    qg = din("qg", [1, QW])
    kg = din("kg", [1, QW])
    ident_d = din("ident", [128, 128])
    bands_d = din("bands", [3, 4, 128, 128])
    pool_w = din("pool_w", [4, 128, 128])
    pool_sc = din("pool_sc", [128, 4])
    y = dout("y", [S, D])
    ys = dout("ys", [NS, D])
    pk = [dout("pk%d" % w, [w, 512]) for (w, _) in GROUPS]
    pv = [dout("pv%d" % w, [w, 512]) for (w, _) in GROUPS]
    ppool = dout("ppool", [15, 512])
    mod_d = dscr("mod_d", [5, 6 * D])
    QT_d = dscr("QT_d", [QW, S], BF16)
    KT_d = dscr("KT_d", [QW, S], BF16)
    V1_d = dscr("V1_d", [S, 24 * VS], BF16)
    qkvs_d = dscr("qkvs_d", [NS, 3 * QW])
    gates_d = dscr("gates_d", [NTOK, 2048], BF16)
    ybT_d = dscr("ybT_d", [NT + 1, 128, 512], BF16)
    us_d = dscr("us_d", [NS, 512])
    biasT_d = din("biasT", [24, 128, 256])
    maskT_d = din("maskT", [128, 256])
    A_d = dscr("A_d", [3, S, 520])
    w_br_a = din("w_br_a", [512, D])
    w_br_b = din("w_br_b", [512, D])
    w_out = din("w_out", [D, D])
    router_w = din("router_w", [D, NE])
    router_b = din("router_b", [1, NE])
    if not SPARSE:
        eg = din("eg", [NE, D, EH])
        eu = din("eu", [NE, D, EH])
        ed = din("ed", [NE, EH, D])
    shg = din("shg", [D, EH])
    shu = din("shu", [D, EH])
    shd = din("shd", [EH, D])
    x1_d = dscr("x1_d", [NTOK, D])
    h2T_d = dscr("h2T_d", [NT + 1, 128, 8 * 128], BF16)
    Wd_d = dscr("Wd_d", [NT + 1, 128, NE])
    oas_d = dscr("oas_d", [NS, 512])
    Xs_d = dscr("Xs_d", [NSLOT + 128, D], BF16)
    Ys_d = dscr("Ys_d", [NSLOT + 128, D], BF16)
    Ysh_d = dscr("Ysh_d", [NTOK, D])
    slot_d = dscr("slot_d", [NT + 1, 128, 8], I32)
    w8_d = dscr("w8_d", [NT + 1, 128, 8])
    pos_d = dscr("pos_d", [NT + 1, 128, NE])
    h2b_d = dscr("h2b_d", [NT + 1, 128, D], BF16)
    be_d = dscr("be_d", [1, 640])
    tri_d = din("tri", [2, 128, NE])
    bstart_d = din("bstart", [128, 5])
    piota_d = din("piota", [128, 1])
    egl = din("egl", [NE * 128, 8 * EH])
    eul = din("eul", [NE * 128, 8 * EH])
    edl = din("edl", [NE * 128, 2 * D])
    UT_d = din("UT", [128, 128])
    trash_d = din("trash", [128, 8])
    As_d = dscr("As_d", [NS, 3, 520])
    ck = [din("ck%d" % w, [NS, w, 512]) for (w, _) in GROUPS]
    cv = [din("cv%d" % w, [NS, w, 512]) for (w, _) in GROUPS]
    stp = din("stp", [NS, 15, 512])
    bias0_d = din("bias0", [1, 24])
    bias_s_d = din("bias_s", [3, 128, 8])
    bd_d = din("bd", [8, 8, 65])
    bandS_d = din("bandS", [4, 60, NS])
    diagS_d = din("diagS", [4, NS, NS])
    sk = [dout("sk%d" % w, [NS, w, 512]) for (w, _) in GROUPS]
    sv = [dout("sv%d" % w, [NS, w, 512]) for (w, _) in GROUPS]
    spool = dout("spool", [NS, 15, 512])

    with es:
        g = G(nc, es)

        def sb(name, shape, dt=F32):
            return Buf(es.enter_context(nc.sbuf_tensor("s_" + name, list(shape), dt)), name)

        def ps(name, shape, dt=F32):
            return Buf(es.enter_context(nc.psum_tensor("p_" + name, list(shape), dt)), name)

        ident_f = sb("ident_f", [128, 128])
        g.dma("sp", ident_f[:], ident_d, writes=[ident_f])
        nhalf = sb("nhalf", [128, 8])
        g.op("dve", lambda: nc.vector.memset(nhalf[:], -0.5), [], [nhalf])
        ident_b = sb("ident_b", [128, 128], BF16)
        g.dma("pool", ident_b[:], ident_d, writes=[ident_b])

        if True:
            p0 = contextlib.ExitStack()
            with p0:
                def sb0(name, shape, dt=F32):
                    return Buf(p0.enter_context(nc.sbuf_tensor("s_" + name, list(shape), dt)), name)
                cT = sb0("cT", [128, 8, 5])
                sg = sb0("sg", [128, 8, 5])
                g.dma("sp", cT[:], cin, writes=[cT])
                g.op("act", lambda: nc.scalar.activation(out=sg[:], in_=cT[:], func=AF.Sigmoid), [cT], [sg])
                g.op("dve", lambda: nc.vector.tensor_mul(out=sg[:], in0=cT[:], in1=sg[:]), [cT, sg], [sg])
                adab = sb0("adab", [5, 6 * D])
                g.dma("act", adab[:], ada_b.to_broadcast([5, 6 * D]), writes=[adab])
                modsb = sb0("modsb", [5, 6 * D])
                wbuf = [sb0("adaw%d" % i, [128, 8, 512]) for i in range(2)]
                pmod = [Buf(p0.enter_context(nc.psum_tensor("pmod%d" % i, [5, 512], F32)), "P:pmod") for i in range(2)]
                for cg in range(12):
                    wb = wbuf[cg % 2]
                    g.dma("sp" if cg % 2 == 0 else "act", wb[:],
                          ada_w[:, cg * 512:(cg + 1) * 512].rearrange("(k p) n -> p k n", p=128), writes=[wb])
                    pm = pmod[cg % 2]
                    g.mm([(pm[:], sg[:, k, :], wb[:, k, :]) for k in range(8)], [sg, wb], pm)
                    g.op("dve", lambda pm=pm, cg=cg: nc.vector.tensor_add(
                        out=modsb[:, cg * 512:(cg + 1) * 512], in0=pm[:], in1=adab[:, cg * 512:(cg + 1) * 512]),
                        [pm, adab], [modsb])
                g.dma("sp", mod_d, modsb[:], reads=[modsb])
                g.barrier()

        if "p1" in stages or "all" in stages:
            p1 = contextlib.ExitStack()
            with p1:
                def sb1(name, shape, dt=F32):
                    return Buf(p1.enter_context(nc.sbuf_tensor("s_" + name, list(shape), dt)), name)

                def ps1(name, shape, dt=F32):
                    return Buf(p1.enter_context(nc.psum_tensor("p_" + name, list(shape), dt)), "P:" + name)

                w_in_sb = sb1("w_in_sb", [128, 8, INC], BF16)
                wchunks = [Buf(None, "wc%d" % i) for i in range(14)]
                _skip = _os.environ.get('SKIP', '').split(',')
                for cg in range(14 if 'win' not in _skip else 0):
                    g.dma("pool", w_in_sb[:, :, cg * 512:(cg + 1) * 512],
                          w_in[:, cg * 512:(cg + 1) * 512].rearrange("(k p) n -> p k n", p=128),
                          writes=[wchunks[cg]])
                G1 = sb1("G1", [128, D]); SH1 = sb1("SH1", [128, D])
                G1s = sb1("G1s", [NS, D]); SH1s = sb1("SH1s", [NS, D])
                n1b = sb1("n1b", [128, D])
                g.dma("sp", n1b[:], norm1.to_broadcast([128, D]), writes=[n1b])
                g.dma("sp", G1[:], mod_d[0:1, D:2 * D].to_broadcast([128, D]), writes=[G1])
                g.dma("sp", SH1[:], mod_d[0:1, 0:D].to_broadcast([128, D]), writes=[SH1])
                g.dma("sp", G1s[:], mod_d[1:5, D:2 * D], writes=[G1s])
                g.dma("sp", SH1s[:], mod_d[1:5, 0:D], writes=[SH1s])
                g.op("dve", lambda: nc.vector.scalar_tensor_tensor(
                    out=G1[:], in0=G1[:], scalar=1.0, in1=n1b[:], op0=ALU.add, op1=ALU.mult), [G1, n1b], [G1])
                g.op("dve", lambda: nc.vector.scalar_tensor_tensor(
                    out=G1s[:], in0=G1s[:], scalar=1.0, in1=n1b[:NS, :], op0=ALU.add, op1=ALU.mult), [G1s, n1b], [G1s])
                qgb = sb1("qgb", [128, QW]); kgb = sb1("kgb", [128, QW])
                g.dma("sp", qgb[:], qg.to_broadcast([128, QW]), writes=[qgb])
                g.dma("sp", kgb[:], kg.to_broadcast([128, QW]), writes=[kgb])
                g.op("dve", lambda: nc.vector.tensor_scalar_mul(out=qgb[:], in0=qgb[:], scalar1=0.125), [qgb], [qgb])
                bands = sb1("bands", [128, 12, 128])
                if 'bands' not in _skip:
                    g.dma("sp", bands[:], bands_d.rearrange("a g p t -> p (a g) t"), writes=[bands])
                pw_sb = sb1("pw_sb", [128, 4, 128], BF16)
                if 'poolw' not in _skip:
                    g.dma("pool", pw_sb[:], pool_w.rearrange("g c d -> c g d"), writes=[pw_sb])
                psc = sb1("psc", [128, 4])
                g.dma("sp", psc[:], pool_sc, writes=[psc])

                xt = [sb1("xt%d" % i, [128, D]) for i in range(2)]
                sq = sb1("sq", [128, D])
                hb = sb1("hb", [128, D], BF16)
                hT = sb1("hT", [128, 8, 128], BF16)
                ssq = sb1("ssq", [128, 1]); rstd = sb1("rstd", [128, 1])
                ss8 = sb1("ss8", [128, 8]); rs8 = sb1("rs8", [128, 8])
                qf = [sb1("qf%d" % i, [128, 512]) for i in range(2)]
                kf = [sb1("kf%d" % i, [128, 512]) for i in range(2)]
                vf = [sb1("vf%d" % i, [128, 512]) for i in range(2)]
                V1 = sb1("V1", [128, 24, VS], BF16)
                if 'memv1' not in _skip:
                    g.op("dve", lambda: nc.vector.memset(V1[:], 1.0), [], [V1])
                QTst = sb1("QTst", [128, 12, 128], BF16)
                KTst = sb1("KTst", [128, 12, 128], BF16)
                ut = [sb1("ut%d" % i, [128, 512]) for i in range(2)]
                gts = sb1("gts", [128, 2048], BF16)
                pooledT = sb1("pooledT", [128, 4, 128], BF16)
                ybT = sb1("ybT", [128, 4, 128], BF16)
                pz = [ps1("pz%d" % i, [128, 512]) for i in range(3)]
                ptr = ps1("ptr", [128, 8, 128], BF16)
                ptq = ps1("ptq", [128, 4, 128])
                ppl = ps1("ppl", [128, 4, 128])
                pmx = ps1("pmx", [128, 4, 128])

                def load_x(i):
                    if i < NT:
                        g.dma("sp", xt[i % 2][:], x[i * 128:(i + 1) * 128, :], writes=[xt[i % 2]])
                    else:
                        g.dma("sp", xt[i % 2][:NS, :], xs, writes=[xt[i % 2]])

                load_x(0)
                pzi = 0
                _tl = _os.environ.get('P1_TILES')
                _tiles = list(range(NT + 1)) if _tl is None else [int(v) for v in _tl.split(',') if v != '']
                for i in _tiles:
                    n = 128 if i < NT else NS
                    samp = (i == NT)
                    if i + 1 <= NT and _tl is None:
                        load_x(i + 1)
                    if _tl is not None and i != 0:
                        load_x(i)
                    xb = xt[i % 2]
                    Gm, Sm = (G1s, SH1s) if samp else (G1, SH1)
                    g.op("act", lambda: nc.scalar.activation(out=sq[:n, :], in_=xb[:n, :], func=AF.Square,
                                                             accum_out=ssq[:n, :]), [xb], [sq, ssq])
                    g.op("dve", lambda: nc.vector.tensor_scalar(out=rstd[:n, :], in0=ssq[:n, :], scalar1=1.0 / D,
                                                                scalar2=EPS, op0=ALU.mult, op1=ALU.add), [ssq], [rstd])
                    g.op("pool", lambda: nc.gpsimd.tensor_tensor(out=rstd[:n, :], in0=rstd[:n, :], in1=nhalf[:n, 0:1],
                                                                 op=ALU.pow), [rstd, nhalf], [rstd])
                    g.op("dve", lambda: nc.vector.scalar_tensor_tensor(
                        out=sq[:n, :], in0=xb[:n, :], scalar=rstd[:n, :], in1=Gm[:n, :], op0=ALU.mult, op1=ALU.mult),
                        [xb, rstd, Gm], [sq])
                    g.op("dve", lambda: nc.vector.tensor_add(out=hb[:n, :], in0=sq[:n, :], in1=Sm[:n, :]), [sq, Sm], [hb])
                    LVL = int(_os.environ.get('P1_LVL', '99'))
                    if LVL < 2:
                        continue
                    g.tr([(ptr[:, k, :n], hb[:n, k * 128:(k + 1) * 128], ident_b[:n, :n]) for k in range(8)],
                         [hb, ident_b], ptr)
                    g.op("act", lambda: nc.scalar.copy(out=hT[:, :, :n], in_=ptr[:, :, :n]), [ptr], [hT])
                    if LVL < 3:
                        continue
                    ucur = ut[i % 2]
                    for cg in range(14):
                        pzb = pz[pzi % 3]
                        pzi += 1
                        g.mm([(pzb[:n, :], hT[:, k, :n], w_in_sb[:, k, cg * 512:(cg + 1) * 512]) for k in range(8)],
                             [hT, wchunks[cg]], pzb)
                        if LVL < 4:
                            continue
                        if cg < 6:
                            gi = cg % 3
                            isq = cg < 3
                            dst = (qf if isq else kf)[gi % 2]
                            gain = qgb if isq else kgb
                            g.op("act", lambda: nc.scalar.activation(out=sq[:n, :512], in_=pzb[:n, :], func=AF.Square),
                                 [pzb], [sq])
                            g.op("dve", lambda: nc.vector.tensor_reduce(
                                out=ss8[:n, :], in_=sq[:n, :512].rearrange("p (h e) -> p h e", e=64),
                                axis=AX.X, op=ALU.add), [sq], [ss8])
                            g.op("dve", lambda: nc.vector.tensor_scalar(out=rs8[:n, :], in0=ss8[:n, :], scalar1=1.0 / 64,
                                                                        scalar2=EPS, op0=ALU.mult, op1=ALU.add), [ss8], [rs8])
                            g.op("pool", lambda: nc.gpsimd.tensor_tensor(out=rs8[:n, :], in0=rs8[:n, :], in1=nhalf[:n, :],
                                                                         op=ALU.pow), [rs8, nhalf], [rs8])
                            g.op("dve", lambda: nc.vector.tensor_tensor(
                                out=dst[:n, :].rearrange("p (h e) -> p h e", e=64),
                                in0=pzb[:n, :].rearrange("p (h e) -> p h e", e=64),
                                in1=rs8[:n, :].unsqueeze(2).to_broadcast([n, 8, 64]), op=ALU.mult), [pzb, rs8], [dst])
                            g.op("dve", lambda: nc.vector.tensor_mul(out=dst[:n, :], in0=dst[:n, :],
                                                                     in1=gain[:n, gi * 512:(gi + 1) * 512]), [dst, gain], [dst])
                            if LVL < 5:
                                continue
                            if not samp:
                                g.tr([(ptq[:, j, :n], dst[:n, j * 128:(j + 1) * 128], ident_f[:n, :n]) for j in range(4)],
                                     [dst, ident_f], ptq)
                                st = QTst if isq else KTst
                                g.op("act", lambda: nc.scalar.copy(out=st[:, gi * 4:(gi + 1) * 4, :], in_=ptq[:]), [ptq], [st])
                                if not isq:
                                    W = GROUPS[gi][0]
                                    r0 = i * 128 - (S - W)
                                    if r0 >= 0:
                                        g.dma("sp", pk[gi][r0:r0 + 128, :], dst[:], reads=[dst], is_output=True)
                            else:
                                off = (0 if isq else QW) + gi * 512
                                g.dma("sp", qkvs_d[:, off:off + 512], dst[:NS, :], reads=[dst])
                        elif LVL < 6:
                            continue
                        elif cg < 9:
                            gi = cg - 6
                            dst = vf[gi % 2]
                            if 'vact' not in _skip:
                                g.op("dve", lambda: nc.vector.tensor_copy(out=dst[:n, :], in_=pzb[:n, :]), [pzb], [dst])
                            if not samp and 'vcopy' not in _skip:
                                g.op("act", lambda: nc.scalar.copy(
                                    out=V1[:, gi * 8:(gi + 1) * 8, 0:64], in_=dst[:, :].rearrange("p (h e) -> p h e", e=64)),
                                    [dst], [V1])
                                W = GROUPS[gi][0]
                                r0 = i * 128 - (S - W)
                                if r0 >= 0:
                                    g.dma("sp", pv[gi][r0:r0 + 128, :], dst[:], reads=[dst], is_output=True)
                            else:
                                off = 2 * QW + gi * 512
                                g.dma("sp", qkvs_d[:, off:off + 512], dst[:NS, :], reads=[dst])
                        elif LVL < 7:
                            continue
                        elif cg == 9:
                            g.op("act", lambda: nc.scalar.copy(out=ucur[:n, :], in_=pzb[:n, :]), [pzb], [ucur])
                        else:
                            c0 = (cg - 10) * 512
                            g.op("act", lambda: nc.scalar.activation(out=gts[:n, c0:c0 + 512], in_=pzb[:n, :],
                                                                     func=AF.Sigmoid), [pzb], [gts])
                    if LVL < 8:
                        continue
                    if not samp:
                        g.dma("sp", QT_d[:, i * 128:(i + 1) * 128].rearrange("(j p) t -> p j t", p=128), QTst[:], reads=[QTst])
                        g.dma("sp", KT_d[:, i * 128:(i + 1) * 128].rearrange("(j p) t -> p j t", p=128), KTst[:], reads=[KTst])
                        g.dma("sp", V1_d[i * 128:(i + 1) * 128, :], V1[:].rearrange("p a b -> p (a b)"), reads=[V1])
                        g.dma("sp", gates_d[i * 128:(i + 1) * 128, :], gts[:], reads=[gts])
                        if i == NT - 1:
                            g.dma("sp", ppool, ucur[113:128, :], reads=[ucur], is_output=True)
                        if LVL < 9:
                            continue
                        uprev = ut[(i + 1) % 2]
                        for gi in range(4):
                            cs = slice(gi * 128, (gi + 1) * 128)
                            if i == 0:
                                mms = [(ppl[:, gi, :], ucur[:, cs], bands[:, 8 + gi, :])]
                            else:
                                mms = [(ppl[:, gi, :], ucur[:, cs], bands[:, gi, :]),
                                       (ppl[:, gi, :], uprev[:, cs], bands[:, 4 + gi, :])]
                            g.mm(mms, [ucur, uprev, bands], ppl)
                        g.op("dve", lambda: nc.vector.tensor_copy(out=pooledT[:], in_=ppl[:]), [ppl], [pooledT])
                        for gi in range(4):
                            g.mm([(pmx[:, gi, :], pw_sb[:, gi, :], pooledT[:, gi, :])], [pw_sb, pooledT], pmx)
                        for gi in range(4):
                            g.op("act", lambda gi=gi: nc.scalar.activation(out=ybT[:, gi, :], in_=pmx[:, gi, :], func=AF.Copy,
                                                                          scale=psc[:, gi:gi + 1]), [pmx, psc], [ybT])
                        g.dma("sp", ybT_d[i], ybT[:].rearrange("p a b -> p (a b)"), reads=[ybT])
                    else:
                        g.dma("sp", gates_d[S:S + NS, :], gts[:NS, :], reads=[gts])
                        g.dma("sp", us_d, ucur[:NS, :], reads=[ucur])
                g.barrier()
        if "p2" in stages or "all" in stages:
            p2 = contextlib.ExitStack()
            with p2:
                def sb2(name, shape, dt=F32):
                    return Buf(p2.enter_context(nc.sbuf_tensor("s2_" + name, list(shape), dt)), name)

                def ps2(name, shape, dt=F32):
                    return Buf(p2.enter_context(nc.psum_tensor("p2_" + name, list(shape), dt)), "P:" + name)

                Eb = sb2("Eb", [128, 24, 256], BF16)
                mk = sb2("mk", [128, 256])
                g.dma("sp", mk[:], maskT_d, writes=[mk])
                for c4 in range(6):
                    bt = sb2("bt%d" % c4, [128, 4, 256])
                    g.dma("sp", bt[:], biasT_d[c4 * 4:(c4 + 1) * 4].rearrange("a k q -> k a q"), writes=[bt])
                    g.op("act", lambda: nc.scalar.activation(out=bt[:], in_=bt[:], func=AF.Exp), [bt], [bt])
                    g.op("dve", lambda: nc.vector.tensor_tensor(
                        out=Eb[:, c4 * 4:(c4 + 1) * 4, :], in0=bt[:], in1=mk[:].unsqueeze(1).to_broadcast([128, 4, 256]),
                        op=ALU.mult), [bt, mk], [Eb])
                QTg = sb2("QTg", [128, 4, S], BF16)
                KTg = sb2("KTg", [128, 4, S], BF16)
                V1g = sb2("V1g", [128, 32, 8 * VS], BF16)
                PT = [[sb2("PT%d_%d" % (h, j), [128, 256], BF16) for j in range(2)] for h in range(8)]
                pe32 = [sb2("pe32_%d" % j, [128, 256]) for j in range(2)]
                oacc = [sb2("oacc%d" % j, [128, 8, 65]) for j in range(2)]
                pS = [ps2("pS%d" % j, [128, 512]) for j in range(4)]
                pO = [[ps2("pO%d_%d" % (j, hh), [128, 512]) for hh in range(2)] for j in range(2)]

                def sl(s0, c, st):
                    return slice(s0, s0 + st * (c - 1) + 1, st)

                si = 0
                oi = 0
                for gi, (W, dil) in enumerate(GROUPS):
                    L = S // dil
                    nb = L // 128
                    g.dma("sp", QTg[:], QT_d[gi * 512:(gi + 1) * 512, :].rearrange("(j p) t -> p j t", p=128), writes=[QTg])
                    g.dma("act", KTg[:], KT_d[gi * 512:(gi + 1) * 512, :].rearrange("(j p) t -> p j t", p=128), writes=[KTg])
                    vsrc = V1_d[:, gi * 8 * VS:(gi + 1) * 8 * VS].rearrange("(cb a r) c -> a r cb c", a=128, r=dil)
                    vdst = V1g[:].rearrange("p (r cb) c -> p r cb c", r=dil)
                    nsplit = max(1, 4 // dil)
                    cbs = nb // nsplit
                    wt = []
                    for r in range(dil):
                        for sp_ in range(nsplit):
                            g.dma("sp", vdst[:, r, sp_ * cbs:(sp_ + 1) * cbs, :], vsrc[:, r, sp_ * cbs:(sp_ + 1) * cbs, :],
                                  writes=[V1g] if (r == 0 and sp_ == 0) else [])
                    V1g.w = {(("d", i)): 16 * n for i, (s_, n) in enumerate(g.dsem) if n > 0}
                    Adst = A_d[gi].rearrange("(cb a r) c -> r cb a c", a=128, r=dil)
                    for r in range(dil):
                        for kb in range(nb):
                            nq = 256 if kb < nb - 1 else 128
                            for h in range(8):
                                j = h // 2
                                rows = slice((h % 2) * 64, (h % 2) * 64 + 64)
                                psb = pS[si % 4]
                                si += 1
                                g.mm([(psb[:, :nq], KTg[rows, j, sl(r + dil * kb * 128, 128, dil)],
                                       QTg[rows, j, sl(r + dil * kb * 128, nq, dil)])], [KTg, QTg], psb)
                                e32 = pe32[si % 2]
                                g.op("act", lambda: nc.scalar.activation(out=e32[:, :nq], in_=psb[:, :nq], func=AF.Exp),
                                     [psb], [e32])
                                ptb = PT[h][kb % 2]
                                g.op("dve", lambda: nc.vector.tensor_tensor(out=ptb[:, :nq], in0=e32[:, :nq],
                                                                            in1=Eb[:, gi * 8 + h, :nq], op=ALU.mult),
                                     [e32, Eb], [ptb])
                            po = pO[oi % 2]
                            ob = oacc[oi % 2]
                            oi += 1
                            bi = r * nb + kb
                            for h in range(8):
                                pob = po[h // 4]
                                mms = []
                                if kb > 0:
                                    mms.append((pob[:, (h % 4) * 65:(h % 4) * 65 + 65], PT[h][(kb - 1) % 2][:, 128:256], V1g[:, bi - 1, h * VS:h * VS + 65]))
                                mms.append((pob[:, (h % 4) * 65:(h % 4) * 65 + 65], PT[h][kb % 2][:, 0:128], V1g[:, bi, h * VS:h * VS + 65]))
                                g.mm(mms, [PT[h][0], PT[h][1], V1g], pob)
                            g.op("act", lambda: nc.scalar.copy(out=ob[:, 0:4, :].rearrange("p a b -> p (a b)"), in_=po[0][:, 0:260]), [po[0]], [ob])
                            g.op("dve", lambda: nc.vector.tensor_copy(out=ob[:, 4:8, :].rearrange("p a b -> p (a b)"), in_=po[1][:, 0:260]), [po[1]], [ob])
                            g.dma("sp", Adst[r, kb], ob[:].rearrange("p a b -> p (a b)"), reads=[ob])
                g.barrier()
        if "p2b" in stages or "all" in stages:
            for gi, (W, dil) in enumerate(GROUPS):
                for b in range(NS):
                    for (src, dst, off) in ((ck[gi], sk[gi], QW), (cv[gi], sv[gi], 2 * QW)):
                        for r0 in range(1, W, 512):
                            r1 = min(W, r0 + 512)
                            g.dma("pool", dst[b, r0 - 1:r1 - 1, :], src[b, r0:r1, :], is_output=True)
                        g.dma("pool", dst[b, W - 1:W, :], qkvs_d[b:b + 1, off + gi * 512:off + (gi + 1) * 512], is_output=True)
            for b in range(NS):
                g.dma("pool", spool[b, 0:14, :], stp[b, 1:15, :], is_output=True)
                g.dma("pool", spool[b, 14:15, :], us_d[b:b + 1, :], is_output=True)
            pb = contextlib.ExitStack()
            with pb:
                def sbb(name, shape, dt=F32):
                    return Buf(pb.enter_context(nc.sbuf_tensor("sb_" + name, list(shape), dt)), name)

                def psb_(name, shape, dt=F32):
                    return Buf(pb.enter_context(nc.psum_tensor("pb_" + name, list(shape), dt)), "P:" + name)

                qs = sbb("qs", [NS, QW]); ks = sbb("ks", [NS, QW]); vs_ = sbb("vs", [NS, QW])
                g.dma("sp", qs[:], qkvs_d[:, 0:QW], writes=[qs])
                g.dma("sp", ks[:], qkvs_d[:, QW:2 * QW], writes=[ks])
                g.dma("sp", vs_[:], qkvs_d[:, 2 * QW:3 * QW], writes=[vs_])
                prod = sbb("prod", [NS, QW])
                s0 = sbb("s0", [NS, 24]); b0 = sbb("b0", [NS, 24]); p0 = sbb("p0", [NS, 24])
                num0 = sbb("num0", [NS, 24, 64])
                g.dma("sp", b0[:], bias0_d.to_broadcast([NS, 24]), writes=[b0])
                g.op("dve", lambda: nc.vector.tensor_mul(out=prod[:], in0=qs[:], in1=ks[:]), [qs, ks], [prod])
                g.op("dve", lambda: nc.vector.tensor_reduce(out=s0[:], in_=prod[:].rearrange("p (h e) -> p h e", e=64),
                                                            axis=AX.X, op=ALU.add), [prod], [s0])
                g.op("dve", lambda: nc.vector.tensor_add(out=s0[:], in0=s0[:], in1=b0[:]), [s0, b0], [s0])
                g.op("act", lambda: nc.scalar.activation(out=p0[:], in_=s0[:], func=AF.Exp), [s0], [p0])
                g.op("dve", lambda: nc.vector.tensor_tensor(
                    out=num0[:], in0=vs_[:].rearrange("p (h e) -> p h e", e=64),
                    in1=p0[:].unsqueeze(2).to_broadcast([NS, 24, 64]), op=ALU.mult), [vs_, p0], [num0])
                BD = sbb("BD", [8, 8, 65])
                g.dma("sp", BD[:], bd_d, writes=[BD])
                ones8 = sbb("ones8", [8, 1])
                g.op("dve", lambda: nc.vector.memset(ones8[:], 1.0), [], [ones8])
                bsm = sbb("bsm", [128, 3, 8])
                g.dma("sp", bsm[:], bias_s_d.rearrange("g k h -> k g h"), writes=[bsm])
                Ksel = [sbb("Ksel%d" % j, [128, 512]) for j in range(2)]
                V1s = [sbb("V1s%d" % j, [128, 8, 65]) for j in range(2)]
                for j in range(2):
                    g.op("dve", lambda j=j: nc.vector.memset(V1s[j][:], 1.0), [], [V1s[j]])
                qbc = [sbb("qbc%d" % j, [128, 512]) for j in range(2)]
                pr2 = sbb("pr2", [128, 512])
                sc_ = sbb("sc_", [128, 8]); pp = sbb("pp", [128, 8])
                m1 = sbb("m1", [8, 8, 65])
                arow = sbb("arow", [1, 520])
                po1 = [psb_("po1_%d" % j, [128, 512]) for j in range(2)]
                po2 = [psb_("po2_%d" % j, [128, 512]) for j in range(2)]
                it = 0
                for b in range(NS):
                    for gi, (W, dil) in enumerate(GROUPS):
                        kb_ = Ksel[it % 2]; vb_ = V1s[it % 2]; qb_ = qbc[it % 2]
                        it += 1
                        g.dma("sp", kb_[:], ck[gi][b, 0:W:dil, :], writes=[kb_])
                        g.dma("act", vb_[:, :, 0:64], cv[gi][b, 0:W:dil, :].rearrange("r (h e) -> r h e", e=64), writes=[vb_])
                        g.dma("sp", qb_[:], qkvs_d[b:b + 1, gi * 512:(gi + 1) * 512].to_broadcast([128, 512]), writes=[qb_])
                        g.op("dve", lambda: nc.vector.tensor_mul(out=pr2[:], in0=kb_[:], in1=qb_[:]), [kb_, qb_], [pr2])
                        g.op("dve", lambda: nc.vector.tensor_reduce(out=sc_[:], in_=pr2[:].rearrange("p (h e) -> p h e", e=64),
                                                                    axis=AX.X, op=ALU.add), [pr2], [sc_])
                        g.op("dve", lambda: nc.vector.tensor_add(out=sc_[:], in0=sc_[:], in1=bsm[:, gi, :]), [sc_, bsm], [sc_])
                        g.op("act", lambda: nc.scalar.activation(out=pp[:], in_=sc_[:], func=AF.Exp), [sc_], [pp])
                        for hf in range(2):
                            g.mm([(po1[hf][0:8, 0:260], pp[:, :], vb_[:, hf * 4:(hf + 1) * 4, :].rearrange("p a b -> p (a b)"))],
                                 [pp, vb_], po1[hf])
                            g.op("dve", lambda: nc.vector.tensor_tensor(
                                out=m1[:, hf * 4:(hf + 1) * 4, :].rearrange("p a b -> p (a b)"), in0=po1[hf][0:8, 0:260],
                                in1=BD[:, hf * 4:(hf + 1) * 4, :].rearrange("p a b -> p (a b)"), op=ALU.mult), [po1[hf], BD], [m1])
                        for hf in range(2):
                            g.mm([(po2[hf][0:1, 0:260], ones8[:, :], m1[:, hf * 4:(hf + 1) * 4, :].rearrange("p a b -> p (a b)"))],
                                 [ones8, m1], po2[hf])
                            g.op("act", lambda: nc.scalar.copy(out=arow[:, hf * 260:(hf + 1) * 260], in_=po2[hf][0:1, 0:260]),
                                 [po2[hf]], [arow])
                        g.dma("sp", As_d[b, gi:gi + 1, :], arow[:], reads=[arow])
                st = sbb("st", [60, 512]); us = sbb("us", [NS, 512])
                g.dma("sp", st[:], stp.rearrange("b r c -> (b r) c"), writes=[st])
                g.dma("sp", us[:], us_d, writes=[us])
                bS = sbb("bS", [60, 4, NS]); dS = sbb("dS", [NS, 4, NS])
                g.dma("sp", bS[:], bandS_d.rearrange("g k b -> k g b"), writes=[bS])
                g.dma("sp", dS[:], diagS_d.rearrange("g k b -> k g b"), writes=[dS])
                pw2 = sbb("pw2", [128, 4, 128], BF16)
                g.dma("pool", pw2[:], pool_w.rearrange("g c d -> c g d"), writes=[pw2])
                psc2 = sbb("psc2", [128, 4])
                g.dma("sp", psc2[:], pool_sc, writes=[psc2])
                pps = psb_("pps", [128, 512]); pmxs = psb_("pmxs", [128, 512])
                for gq in range(4):
                    cs = slice(gq * 128, (gq + 1) * 128)
                    g.mm([(pps[:, gq * NS:(gq + 1) * NS], st[:, cs], bS[:, gq, :]),
                          (pps[:, gq * NS:(gq + 1) * NS], us[:, cs], dS[:, gq, :])], [st, us, bS, dS], pps)
                pTs = sbb("pTs", [128, 4 * NS], BF16)
                g.op("dve", lambda: nc.vector.tensor_copy(out=pTs[:], in_=pps[:, 0:4 * NS]), [pps], [pTs])
                for gq in range(4):
                    g.mm([(pmxs[:, gq * NS:(gq + 1) * NS], pw2[:, gq, :], pTs[:, gq * NS:(gq + 1) * NS])], [pw2, pTs], pmxs)
                ybs = sbb("ybs", [128, 4, 128], BF16)
                g.op("dve", lambda: nc.vector.memset(ybs[:], 0.0), [], [ybs])
                for gq in range(4):
                    g.op("act", lambda gq=gq: nc.scalar.activation(out=ybs[:, gq, 0:NS], in_=pmxs[:, gq * NS:(gq + 1) * NS], func=AF.Copy,
                                                                  scale=psc2[:, gq:gq + 1]), [pmxs, psc2], [ybs])
                g.dma("sp", ybT_d[NT], ybs[:].rearrange("p a b -> p (a b)"), reads=[ybs])
                g.barrier()
                As = sbb("As", [NS, 3, 8, 65])
                g.dma("sp", As[:].rearrange("p a b c -> p (a b c)"), As_d.rearrange("b g c -> b (g c)"), writes=[As])
                numt = sbb("numt", [NS, 8, 64]); lt = sbb("lt", [NS, 8])
                g.op("dve", lambda: nc.vector.tensor_add(out=numt[:], in0=As[:, 0, :, 0:64], in1=As[:, 1, :, 0:64]), [As], [numt])
                g.op("dve", lambda: nc.vector.tensor_add(out=numt[:], in0=numt[:], in1=As[:, 2, :, 0:64]), [As, numt], [numt])
                g.op("dve", lambda: nc.vector.tensor_add(out=lt[:], in0=As[:, 0, :, 64], in1=As[:, 1, :, 64]), [As], [lt])
                g.op("dve", lambda: nc.vector.tensor_add(out=lt[:], in0=lt[:], in1=As[:, 2, :, 64]), [As, lt], [lt])
                for gi in range(3):
                    g.op("dve", lambda gi=gi: nc.vector.tensor_add(out=numt[:], in0=numt[:], in1=num0[:, gi * 8:(gi + 1) * 8, :]),
                         [numt, num0], [numt])
                    g.op("dve", lambda gi=gi: nc.vector.tensor_add(out=lt[:], in0=lt[:], in1=p0[:, gi * 8:(gi + 1) * 8]), [lt, p0], [lt])
                g.op("dve", lambda: nc.vector.reciprocal(out=lt[:], in_=lt[:]), [lt], [lt])
                oas = sbb("oas", [NS, 512])
                g.op("dve", lambda: nc.vector.tensor_tensor(out=oas[:].rearrange("p (h e) -> p h e", e=64), in0=numt[:],
                                                            in1=lt[:].unsqueeze(2).to_broadcast([NS, 8, 64]), op=ALU.mult),
                     [numt, lt], [oas])
                g.dma("sp", oas_d, oas[:], reads=[oas])
                g.barrier()
        if "p3" in stages or "all" in stages:
            p3 = contextlib.ExitStack()
            with p3:
                def sb3(name, shape, dt=F32):
                    return Buf(p3.enter_context(nc.sbuf_tensor("s3_" + name, list(shape), dt)), name)

                def ps3(name, shape, dt=F32):
                    return Buf(p3.enter_context(nc.psum_tensor("p3_" + name, list(shape), dt)), "P:" + name)

                wa_sb = sb3("wa", [128, 4, D], BF16)
                wb_sb = sb3("wb", [128, 4, D], BF16)
                wo_sb = sb3("wo", [128, 8, D], BF16)
                g.dma("pool", wa_sb[:], w_br_a.rearrange("(k p) n -> p k n", p=128), writes=[wa_sb])
                g.dma("pool", wb_sb[:], w_br_b.rearrange("(k p) n -> p k n", p=128), writes=[wb_sb])
                for k2 in range(2):
                    g.dma("pool", wo_sb[:, k2 * 4:(k2 + 1) * 4, :],
                          w_out[k2 * 512:(k2 + 1) * 512, :].rearrange("(k p) n -> p k n", p=128), writes=[wo_sb] if k2 == 0 else [])
                wo_sb.w = {(("d", i)): 16 * n for i, (s_, n) in enumerate(g.dsem) if n > 0}
                rw_sb = sb3("rw", [128, 8, NE])
                g.dma("sp", rw_sb[:], router_w.rearrange("(k p) n -> p k n", p=128), writes=[rw_sb])
                rbias = sb3("rbias", [128, NE])
                g.dma("sp", rbias[:], router_b.to_broadcast([128, NE]), writes=[rbias])
                GT1 = sb3("GT1", [128, D]); G2 = sb3("G2", [128, D]); SH2 = sb3("SH2", [128, D]); n2b = sb3("n2b", [128, D])
                GT1s = sb3("GT1s", [NS, D]); G2s = sb3("G2s", [NS, D]); SH2s = sb3("SH2s", [NS, D])
                g.dma("sp", n2b[:], norm2.to_broadcast([128, D]), writes=[n2b])
                g.dma("sp", GT1[:], mod_d[0:1, 2 * D:3 * D].to_broadcast([128, D]), writes=[GT1])
                g.dma("sp", SH2[:], mod_d[0:1, 3 * D:4 * D].to_broadcast([128, D]), writes=[SH2])
                g.dma("sp", G2[:], mod_d[0:1, 4 * D:5 * D].to_broadcast([128, D]), writes=[G2])
                g.dma("sp", GT1s[:], mod_d[1:5, 2 * D:3 * D], writes=[GT1s])
                g.dma("sp", SH2s[:], mod_d[1:5, 3 * D:4 * D], writes=[SH2s])
                g.dma("sp", G2s[:], mod_d[1:5, 4 * D:5 * D], writes=[G2s])
                g.op("dve", lambda: nc.vector.scalar_tensor_tensor(
                    out=G2[:], in0=G2[:], scalar=1.0, in1=n2b[:], op0=ALU.add, op1=ALU.mult), [G2, n2b], [G2])
                g.op("dve", lambda: nc.vector.scalar_tensor_tensor(
                    out=G2s[:], in0=G2s[:], scalar=1.0, in1=n2b[:NS, :], op0=ALU.add, op1=ALU.mult), [G2s, n2b], [G2s])

                A0 = sb3("A0", [128, 8, 65]); A1 = sb3("A1", [128, 8, 65]); A2 = sb3("A2", [128, 8, 65])
                rl = sb3("rl", [128, 8])
                oaf = sb3("oaf", [128, 512])
                oab = sb3("oab", [128, 512], BF16)
                oaT = sb3("oaT", [128, 4, 128], BF16)
                ybt = sb3("ybt", [128, 4, 128], BF16)
                gt = sb3("gt", [128, 2048], BF16)
                xt3 = sb3("xt3", [128, D])
                t1_ = sb3("t1_", [128, D]); t2_ = sb3("t2_", [128, D])
                mgb = sb3("mgb", [128, D], BF16)
                mT = sb3("mT", [128, 8, 128], BF16)
                x1 = sb3("x1", [128, D])
                h2 = sb3("h2", [128, D])
                sq3 = sb3("sq3", [128, D])
                ssq3 = sb3("ssq3", [128, 1]); rstd3 = sb3("rstd3", [128, 1])
                h2T32 = sb3("h2T32", [128, 8, 128])
                h2Tb = sb3("h2Tb", [128, 8, 128], BF16)
                sc = sb3("sc", [128, NE]); sel = sb3("sel", [128, NE]); selm = sb3("selm", [128, NE])
                mx8 = sb3("mx8", [128, 8, 8]); gsc = sb3("gsc", [128, 8]); gtop = sb3("gtop", [128, 8])
                gmask = sb3("gmask", [128, 8]); top8 = sb3("top8", [128, 8])
                Mk = sb3("Mk", [128, NE]); Wd = sb3("Wd", [128, NE]); den = sb3("den", [128, 1])
                UT = sb3("UT", [128, 128])
                g.dma("sp", UT[:], UT_d, writes=[UT])
                ones_r = sb3("ones_r", [1, 128]); ones_c = sb3("ones_c", [128, 1]); carry = sb3("carry", [1, NE])
                g.op("dve", lambda: nc.vector.memset(ones_r[:], 1.0), [], [ones_r])
                g.op("dve", lambda: nc.vector.memset(ones_c[:], 1.0), [], [ones_c])
                g.op("dve", lambda: nc.vector.memset(carry[:], 0.0), [], [carry])
                pos1t = sb3("pos1t", [128, NE]); key_ = sb3("key_", [128, NE]); junk = sb3("junk", [128, NE])
                h2b = sb3("h2b", [128, D], BF16)
                ptr3 = ps3("ptr3", [128, 8, 128], BF16)
                pbr = [ps3("pbr%d" % j, [128, 512]) for j in range(2)]
                pym = [ps3("pym%d" % j, [128, 512]) for j in range(2)]
                pt32 = [ps3("pt32_%d" % j, [128, 4, 128]) for j in range(2)]
                prt = ps3("prt", [128, 512])

                for i in range(NT + 1):
                    n = 128 if i < NT else NS
                    samp = (i == NT)
                    rows = slice(i * 128, i * 128 + n)
                    gt1, g2m, sh2m = (GT1s, G2s, SH2s) if samp else (GT1, G2, SH2)
                    g.dma("sp", xt3[:n, :], xs if samp else x[rows, :], writes=[xt3])
                    g.dma("act", gt[:n, :], gates_d[rows, :], writes=[gt])
                    g.dma("act", ybt[:].rearrange("p a b -> p (a b)"), ybT_d[i], writes=[ybt])
                    if not samp:
                        g.dma("sp", A0[:].rearrange("p a b -> p (a b)"), A_d[0][rows, :], writes=[A0])
                        g.dma("sp", A1[:].rearrange("p a b -> p (a b)"), A_d[1][rows, :], writes=[A1])
                        g.dma("sp", A2[:].rearrange("p a b -> p (a b)"), A_d[2][rows, :], writes=[A2])
                        g.op("dve", lambda: nc.vector.tensor_add(out=A0[:], in0=A0[:], in1=A1[:]), [A0, A1], [A0])
                        g.op("dve", lambda: nc.vector.tensor_add(out=A0[:], in0=A0[:], in1=A2[:]), [A0, A2], [A0])
                        g.op("dve", lambda: nc.vector.reciprocal(out=rl[:], in_=A0[:, :, 64]), [A0], [rl])
                        g.op("dve", lambda: nc.vector.tensor_tensor(
                            out=oab[:].rearrange("p (h e) -> p h e", e=64), in0=A0[:, :, 0:64],
                            in1=rl[:].unsqueeze(2).to_broadcast([128, 8, 64]), op=ALU.mult), [A0, rl], [oab])
                    else:
                        g.dma("sp", oaf[:NS, :], oas_d, writes=[oaf])
                        g.op("dve", lambda: nc.vector.tensor_copy(out=oab[:NS, :], in_=oaf[:NS, :]), [oaf], [oab])
                    g.tr([(ptr3[:, k, :n], oab[:n, k * 128:(k + 1) * 128], ident_b[:n, :n]) for k in range(4)], [oab, ident_b], ptr3)
                    g.op("act", lambda: nc.scalar.copy(out=oaT[:, :, :n], in_=ptr3[:, 0:4, :n]), [ptr3], [oaT])
                    for half in range(2):
                        cs = slice(half * 512, (half + 1) * 512)
                        g.mm([(pbr[0][:n, :], oaT[:, k, :n], wa_sb[:, k, cs]) for k in range(4)], [oaT, wa_sb], pbr[0])
                        g.mm([(pbr[1][:n, :], ybt[:, k, :n], wb_sb[:, k, cs]) for k in range(4)], [ybt, wb_sb], pbr[1])
                        g.op("dve", lambda: nc.vector.tensor_tensor(out=t1_[:n, cs], in0=pbr[0][:n, :], in1=gt[:n, cs], op=ALU.mult),
                             [pbr[0], gt], [t1_])
                        g.op("dve", lambda: nc.vector.tensor_tensor(out=t2_[:n, cs], in0=pbr[1][:n, :],
                                                                    in1=gt[:n, 1024 + half * 512:1024 + (half + 1) * 512], op=ALU.mult),
                             [pbr[1], gt], [t2_])
                    g.op("dve", lambda: nc.vector.tensor_add(out=mgb[:n, :], in0=t1_[:n, :], in1=t2_[:n, :]), [t1_, t2_], [mgb])
                    g.tr([(ptr3[:, k, :n], mgb[:n, k * 128:(k + 1) * 128], ident_b[:n, :n]) for k in range(8)], [mgb, ident_b], ptr3)
                    g.op("act", lambda: nc.scalar.copy(out=mT[:, :, :n], in_=ptr3[:, :, :n]), [ptr3], [mT])
                    for half in range(2):
                        cs = slice(half * 512, (half + 1) * 512)
                        g.mm([(pym[half][:n, :], mT[:, k, :n], wo_sb[:, k, cs]) for k in range(8)], [mT, wo_sb], pym[half])
                        g.op("dve", lambda: nc.vector.tensor_tensor(out=t1_[:n, cs], in0=pym[half][:n, :], in1=gt1[:n, cs], op=ALU.mult),
                             [pym[half], gt1], [t1_])
                    g.op("dve", lambda: nc.vector.tensor_add(out=x1[:n, :], in0=t1_[:n, :], in1=xt3[:n, :]), [t1_, xt3], [x1])
                    g.dma("sp", x1_d[rows, :], x1[:n, :], reads=[x1])
                    g.op("act", lambda: nc.scalar.activation(out=sq3[:n, :], in_=x1[:n, :], func=AF.Square, accum_out=ssq3[:n, :]),
                         [x1], [sq3, ssq3])
                    g.op("dve", lambda: nc.vector.tensor_scalar(out=rstd3[:n, :], in0=ssq3[:n, :], scalar1=1.0 / D, scalar2=EPS,
                                                                op0=ALU.mult, op1=ALU.add), [ssq3], [rstd3])
                    g.op("pool", lambda: nc.gpsimd.tensor_tensor(out=rstd3[:n, :], in0=rstd3[:n, :], in1=nhalf[:n, 0:1], op=ALU.pow),
                         [rstd3, nhalf], [rstd3])
                    g.op("dve", lambda: nc.vector.scalar_tensor_tensor(out=sq3[:n, :], in0=x1[:n, :], scalar=rstd3[:n, :],
                                                                       in1=g2m[:n, :], op0=ALU.mult, op1=ALU.mult),
                         [x1, rstd3, g2m], [sq3])
                    g.op("dve", lambda: nc.vector.tensor_add(out=h2[:n, :], in0=sq3[:n, :], in1=sh2m[:n, :]), [sq3, sh2m], [h2])
                    for hf in range(2):
                        g.tr([(pt32[hf][:, k, :n], h2[:n, (hf * 4 + k) * 128:(hf * 4 + k + 1) * 128], ident_f[:n, :n]) for k in range(4)],
                             [h2, ident_f], pt32[hf])
                        g.op("act", lambda: nc.scalar.copy(out=h2T32[:, hf * 4:(hf + 1) * 4, :n], in_=pt32[hf][:, :, :n]),
                             [pt32[hf]], [h2T32])
                    if samp:
                        g.op("dve", lambda: nc.vector.memset(h2Tb[:], 0.0), [], [h2Tb])
                    g.op("dve", lambda: nc.vector.tensor_copy(out=h2Tb[:, :, :n], in_=h2T32[:, :, :n]), [h2T32], [h2Tb])
                    g.dma("sp", h2T_d[i], h2Tb[:].rearrange("p a b -> p (a b)"), reads=[h2Tb])
                    g.mm([(prt[:n, :NE], h2T32[:, k, :n], rw_sb[:, k, :]) for k in range(8)], [h2T32, rw_sb], prt)
                    g.op("act", lambda: nc.scalar.activation(out=sc[:n, :], in_=prt[:n, :NE], func=AF.Sigmoid), [prt], [sc])
                    g.op("dve", lambda: nc.vector.tensor_add(out=sel[:n, :], in0=sc[:n, :], in1=rbias[:n, :]), [sc, rbias], [sel])
                    for gq in range(8):
                        g.op("dve", lambda: nc.vector.max(out=mx8[:n, gq, :], in_=sel[:n, gq * 32:(gq + 1) * 32]), [sel], [mx8])
                    g.op("dve", lambda: nc.vector.tensor_add(out=gsc[:n, :], in0=mx8[:n, :, 0], in1=mx8[:n, :, 1]), [mx8], [gsc])
                    g.op("dve", lambda: nc.vector.max(out=gtop[:n, :], in_=gsc[:n, :]), [gsc], [gtop])
                    g.op("dve", lambda: nc.vector.tensor_scalar(out=gmask[:n, :], in0=gsc[:n, :], scalar1=gtop[:n, 3:4], scalar2=None,
                                                                op0=ALU.is_ge), [gsc, gtop], [gmask])
                    g.op("dve", lambda: nc.vector.tensor_scalar(out=gmask[:n, :], in0=gmask[:n, :], scalar1=-1.0, scalar2=1e9,
                                                                op0=ALU.add, op1=ALU.mult), [gmask], [gmask])
                    g.op("dve", lambda: nc.vector.tensor_tensor(
                        out=selm[:n, :].rearrange("p (a b) -> p a b", b=32), in0=sel[:n, :].rearrange("p (a b) -> p a b", b=32),
                        in1=gmask[:n, :].unsqueeze(2).to_broadcast([n, 8, 32]), op=ALU.add), [sel, gmask], [selm])
                    g.op("dve", lambda: nc.vector.max(out=top8[:n, :], in_=selm[:n, :]), [selm], [top8])
                    g.op("dve", lambda: nc.vector.tensor_scalar(out=Mk[:n, :], in0=selm[:n, :], scalar1=top8[:n, 7:8], scalar2=None,
                                                                op0=ALU.is_ge), [selm, top8], [Mk])
                    g.op("dve", lambda: nc.vector.tensor_tensor(out=Wd[:n, :], in0=Mk[:n, :], in1=sc[:n, :], op=ALU.mult), [Mk, sc], [Wd])
                    g.op("dve", lambda: nc.vector.tensor_reduce(out=den[:n, :], in_=Wd[:n, :], axis=AX.X, op=ALU.add), [Wd], [den])
                    g.op("dve", lambda: nc.vector.reciprocal(out=den[:n, :], in_=den[:n, :]), [den], [den])
                    g.op("dve", lambda: nc.vector.tensor_scalar(out=Wd[:n, :], in0=Wd[:n, :], scalar1=den[:n, :], scalar2=2.5,
                                                                op0=ALU.mult, op1=ALU.mult), [Wd, den], [Wd])
                    g.dma("sp", Wd_d[i, 0:n, :], Wd[:n, :], reads=[Wd])
                    if SPARSE:
                        pps_ = pbr[0]; pcs_ = pbr[1]
                        g.mm([(pps_[:n, :NE], UT[:n, :n], Mk[:n, :]), (pps_[:n, :NE], ones_r[0:1, :n], carry[0:1, :])],
                             [UT, Mk, ones_r, carry], pps_)
                        g.op("dve", lambda: nc.vector.tensor_scalar(out=pos1t[:n, :], in0=pps_[:n, :NE], scalar1=1.0, scalar2=None,
                                                                    op0=ALU.add), [pps_], [pos1t])
                        g.dma("sp", pos_d[i, 0:n, :], pos1t[:n, :], reads=[pos1t])
                        g.mm([(pcs_[0:1, :NE], ones_c[:n, 0:1], Mk[:n, :])], [ones_c, Mk], pcs_)
                        g.op("dve", lambda: nc.vector.tensor_add(out=carry[0:1, :], in0=carry[0:1, :], in1=pcs_[0:1, :NE]),
                             [carry, pcs_], [carry])
                        g.op("act", lambda: nc.scalar.copy(out=h2b[:n, :], in_=h2[:n, :]), [h2], [h2b])
                        g.dma("sp", h2b_d[i, 0:n, :], h2b[:n, :], reads=[h2b])
                if SPARSE:
                    ci_ = sb3("ci_", [1, NE], I32)
                    pc = sb3("pc", [1, NE]); pcT = sb3("pcT", [128, 2]); pend = sb3("pend", [1, NE]); ps1r = sb3("ps1r", [1, NE])
                    tri = sb3("tri", [128, 2, NE]); bstart = sb3("bstart", [128, 5])
                    g.dma("sp", tri[:], tri_d.rearrange("c p e -> p c e"), writes=[tri])
                    g.dma("sp", bstart[:], bstart_d, writes=[bstart])
                    g.op("dve", lambda: nc.vector.tensor_scalar(out=pc[:], in0=carry[:], scalar1=127.0, scalar2=None, op0=ALU.add),
                         [carry], [pc])
                    g.op("dve", lambda: nc.vector.tensor_copy(out=ci_[:], in_=pc[:]), [pc], [ci_])
                    g.op("dve", lambda: nc.vector.tensor_single_scalar(out=ci_[:], in_=ci_[:], scalar=7, op=ALU.arith_shift_right),
                         [ci_], [ci_])
                    g.op("dve", lambda: nc.vector.tensor_single_scalar(out=ci_[:], in_=ci_[:], scalar=7, op=ALU.logical_shift_left),
                         [ci_], [ci_])
                    g.op("dve", lambda: nc.vector.tensor_copy(out=pc[:], in_=ci_[:]), [ci_], [pc])
                    g.tr([(pt32[0][:, 0, 0:1], pc[0:1, 0:128], ident_f[0:1, 0:1]), (pt32[0][:, 1, 0:1], pc[0:1, 128:256], ident_f[0:1, 0:1])],
                         [pc, ident_f], pt32[0])
                    g.op("dve", lambda: nc.vector.tensor_copy(out=pcT[:], in_=pt32[0][:, 0:2, 0]), [pt32[0]], [pcT])
                    g.mm([(prt[0:1, :NE], pcT[:, c2:c2 + 1], tri[:, c2, :]) for c2 in range(2)], [pcT, tri], prt)
                    g.op("dve", lambda: nc.vector.tensor_copy(out=pend[:], in_=prt[0:1, :NE]), [prt], [pend])
                    g.op("dve", lambda: nc.vector.tensor_sub(out=ps1r[:], in0=pend[:], in1=pc[:]), [pend, pc], [ps1r])
                    PSb = sb3("PSb", [128, NE]); PEb = sb3("PEb", [128, NE])
                    g.mm([(pbr[0][:, :NE], ones_r[0:1, :], ps1r[0:1, :])], [ones_r, ps1r], pbr[0])
                    g.op("dve", lambda: nc.vector.tensor_copy(out=PSb[:], in_=pbr[0][:, :NE]), [pbr[0]], [PSb])
                    g.mm([(pbr[1][:, :NE], ones_r[0:1, :], pend[0:1, :])], [ones_r, pend], pbr[1])
                    g.op("dve", lambda: nc.vector.tensor_copy(out=PEb[:], in_=pbr[1][:, :NE]), [pbr[1]], [PEb])
                    be = sb3("be", [128, 8])
                    g.op("dve", lambda: nc.vector.memset(be[:], 0.0), [], [be])
                    for j5 in range(5):
                        g.op("dve", lambda j5=j5: nc.vector.tensor_scalar(
                            out=key_[:, :], in0=PEb[:, :], scalar1=bstart[:, j5:j5 + 1], scalar2=None, op0=ALU.is_le, op1=ALU.add,
                            accum_out=be[:, j5:j5 + 1]), [PEb, bstart], [key_, be])
                    g.op("dve", lambda: nc.vector.tensor_scalar_min(out=be[:], in0=be[:], scalar1=float(NE - 1)), [be], [be])
                    g.tr([(pt32[1][0:8, 0, :], be[:, 0:8], ident_f[:, :])], [be, ident_f], pt32[1])
                    beT = sb3("beT", [8, 128])
                    g.op("dve", lambda: nc.vector.tensor_copy(out=beT[:], in_=pt32[1][0:8, 0, :]), [pt32[1]], [beT])
                    g.dma("sp", be_d.rearrange("o (j b) -> (o j) b", j=5), beT[0:5, :], reads=[beT])
                    g.barrier()
                    trashf = sb3("trashf", [128, 8])
                    g.dma("sp", trashf[:], trash_d, writes=[trashf])
                    s8f = sb3("s8f", [128, 8]); w8 = sb3("w8", [128, 8]); s8i = sb3("s8i", [128, 8], I32)
                    g.op("dve", lambda: nc.vector.memset(h2b[:], 0.0), [], [h2b])
                    for i in range(NT + 1):
                        n = 128 if i < NT else NS
                        samp = (i == NT)
                        g.dma("sp", pos1t[:n, :], pos_d[i, 0:n, :], writes=[pos1t])
                        g.dma("act", Wd[:n, :], Wd_d[i, 0:n, :], writes=[Wd])
                        g.dma("act", h2b[:n, :], h2b_d[i, 0:n, :], writes=[h2b])
                        g.op("dve", lambda: nc.vector.tensor_single_scalar(out=Mk[:n, :], in_=Wd[:n, :], scalar=0.0, op=ALU.is_gt),
                             [Wd], [Mk])
                        g.op("dve", lambda: nc.vector.tensor_add(out=key_[:n, :], in0=pos1t[:n, :], in1=PSb[:n, :]), [pos1t, PSb], [key_])
                        g.op("dve", lambda: nc.vector.tensor_mul(out=key_[:n, :], in0=key_[:n, :], in1=Mk[:n, :]), [key_, Mk], [key_])
                        if samp:
                            g.op("dve", lambda: nc.vector.tensor_copy(out=s8f[:], in_=trashf[:]), [trashf], [s8f])
                        g.op("dve", lambda: nc.vector.max(out=s8f[:n, :], in_=key_[:n, :]), [key_], [s8f])
                        for k8 in range(8):
                            g.op("dve", lambda k8=k8: nc.vector.scalar_tensor_tensor(
                                out=junk[:n, :], in0=key_[:n, :], scalar=s8f[:n, k8:k8 + 1], in1=Wd[:n, :],
                                op0=ALU.is_equal, op1=ALU.mult, accum_out=w8[:n, k8:k8 + 1]), [key_, s8f, Wd], [junk, w8])
                        g.op("dve", lambda: nc.vector.tensor_scalar(out=s8f[:, :], in0=s8f[:, :], scalar1=-1.0, scalar2=None,
                                                                    op0=ALU.add), [s8f], [s8f])
                        g.op("dve", lambda: nc.vector.tensor_copy(out=s8i[:, :], in_=s8f[:, :]), [s8f], [s8i])
                        g.dma("sp", slot_d[i], s8i[:, :], reads=[s8i])
                        g.dma("sp", w8_d[i, 0:n, :], w8[:n, :], reads=[w8])
                        for k8 in range(8):
                            g.idma(Xs_d[:, :], bass.IndirectOffsetOnAxis(ap=s8i[:, k8:k8 + 1], axis=0), h2b[:, :], None,
                                   reads=[s8i, h2b])
                g.barrier()
        if ("p4" in stages or "all" in stages) and not SPARSE:
            NG = 3
            tiles_all = list(range(NT + 1))
            per = (len(tiles_all) + NG - 1) // NG
            groups4 = [tiles_all[a:a + per] for a in range(0, len(tiles_all), per)]
            n_exp = int(_os.environ.get("P4_NEXP", str(NE)))
            for tg in groups4:
                p4 = contextlib.ExitStack()
                with p4:
                    def sb4(name, shape, dt=F32):
                        return Buf(p4.enter_context(nc.sbuf_tensor("s4_%d_" % tg[0] + name, list(shape), dt)), name)

                    def ps4(name, shape, dt=F32):
                        return Buf(p4.enter_context(nc.psum_tensor("p4_%d_" % tg[0] + name, list(shape), dt)), "P:" + name)

                    ntl = len(tg)
                    hT4 = sb4("hT4", [128, 8, ntl * 128], BF16)
                    for ti, i in enumerate(tg):
                        g.dma("sp", hT4[:, :, ti * 128:(ti + 1) * 128], h2T_d[i].rearrange("p (k t) -> p k t", k=8),
                              writes=[hT4] if ti == 0 else [])
                    Wg = sb4("Wg", [128, ntl, NE])
                    for ti, i in enumerate(tg):
                        nn = 128 if i < NT else NS
                        g.dma("sp", Wg[:nn, ti, :], Wd_d[i, 0:nn, :], writes=[])
                    acc = sb4("acc", [128, ntl, D])
                    g.op("pool", lambda: nc.gpsimd.memset(acc[:], 0.0), [], [acc])
                    allw = {(("d", i_)): 16 * n_ for i_, (s_, n_) in enumerate(g.dsem) if n_ > 0}
                    hT4.w = dict(allw)
                    Wg.w = dict(allw)
                    wgs = [sb4("wg%d" % j, [128, 8, EH], BF16) for j in range(2)]
                    wus = [sb4("wu%d" % j, [128, 8, EH], BF16) for j in range(2)]
                    wds = [sb4("wd%d" % j, [128, 2, D], BF16) for j in range(2)]
                    sgt = [sb4("sgt%d" % j, [128, 512]) for j in range(2)]
                    act = [sb4("act%d" % j, [128, 512], BF16) for j in range(2)]
                    ph = [[ps4("ph%d_%d" % (a, b), [128, 512]) for b in range(2)] for a in range(2)]
                    py = [[ps4("py%d_%d" % (a, b), [128, 512]) for b in range(2)] for a in range(2)]
                    blocks = [list(range(a, min(a + 4, ntl))) for a in range(0, ntl, 4)]
                    yi = 0
                    elist = list(range(n_exp)) + [NE]
                    for ei, e in enumerate(elist):
                        j2 = ei % 2
                        if e < NE:
                            srcs = (eg[e], eu[e], ed[e])
                        else:
                            srcs = (shg, shu, shd)
                        g.dma("pool", wgs[j2][:], srcs[0].rearrange("(k p) n -> p k n", p=128), writes=[wgs[j2]])
                        g.dma("pool", wus[j2][:], srcs[1].rearrange("(k p) n -> p k n", p=128), writes=[wus[j2]])
                        g.dma("pool", wds[j2][:], srcs[2].rearrange("(k p) n -> p k n", p=128), writes=[wds[j2]])
                        for blk in blocks:
                            t0 = blk[0] * 128
                            ntb = sum(128 if tg[ti] < NT else NS for ti in blk)
                            for hh in range(2):
                                hs = slice(hh * 128, (hh + 1) * 128)
                                g.mm([(ph[0][hh][:, :ntb], wgs[j2][:, k, hs], hT4[:, k, t0:t0 + ntb]) for k in range(8)],
                                     [wgs[j2], hT4], ph[0][hh])
                                g.mm([(ph[1][hh][:, :ntb], wus[j2][:, k, hs], hT4[:, k, t0:t0 + ntb]) for k in range(8)],
                                     [wus[j2], hT4], ph[1][hh])
                                g.op("act", lambda: nc.scalar.activation(out=sgt[hh][:, :ntb], in_=ph[0][hh][:, :ntb], func=AF.Silu),
                                     [ph[0][hh]], [sgt[hh]])
                                g.op("dve", lambda: nc.vector.tensor_tensor(out=act[hh][:, :ntb], in0=ph[1][hh][:, :ntb],
                                                                            in1=sgt[hh][:, :ntb], op=ALU.mult),
                                     [ph[1][hh], sgt[hh]], [act[hh]])
                            for ti in blk:
                                nn = 128 if tg[ti] < NT else NS
                                c0 = (ti - blk[0]) * 128
                                pyy = py[yi % 2]
                                yi += 1
                                for half in range(2):
                                    cs = slice(half * 512, (half + 1) * 512)
                                    g.mm([(pyy[half][:nn, :], act[hh2][:, c0:c0 + nn], wds[j2][:, hh2, cs]) for hh2 in range(2)],
                                         [act[0], act[1], wds[j2]], pyy[half])
                                    if e < NE:
                                        g.op("dve", lambda: nc.vector.scalar_tensor_tensor(
                                            out=acc[:nn, ti, cs], in0=pyy[half][:nn, :], scalar=Wg[:nn, ti, e:e + 1],
                                            in1=acc[:nn, ti, cs], op0=ALU.mult, op1=ALU.add), [pyy[half], Wg, acc], [acc])
                                    else:
                                        g.op("dve", lambda: nc.vector.tensor_tensor(
                                            out=acc[:nn, ti, cs], in0=pyy[half][:nn, :], in1=acc[:nn, ti, cs], op=ALU.add),
                                            [pyy[half], acc], [acc])
                    GT2 = sb4("GT2", [128, D]); GT2s = sb4("GT2s", [NS, D])
                    g.dma("sp", GT2[:], mod_d[0:1, 5 * D:6 * D].to_broadcast([128, D]), writes=[GT2])
                    g.dma("sp", GT2s[:], mod_d[1:5, 5 * D:6 * D], writes=[GT2s])
                    xo = [sb4("xo%d" % j, [128, D]) for j in range(2)]
                    for ti, i in enumerate(tg):
                        nn = 128 if i < NT else NS
                        samp = (i == NT)
                        rows = slice(i * 128, i * 128 + nn)
                        xb_ = xo[ti % 2]
                        gt2 = GT2s if samp else GT2
                        g.dma("sp", xb_[:nn, :], x1_d[rows, :], writes=[xb_])
                        g.op("dve", lambda: nc.vector.tensor_tensor(out=acc[:nn, ti, :], in0=acc[:nn, ti, :], in1=gt2[:nn, :], op=ALU.mult),
                             [acc, gt2], [acc])
                        g.op("dve", lambda: nc.vector.tensor_add(out=xb_[:nn, :], in0=xb_[:nn, :], in1=acc[:nn, ti, :]), [xb_, acc], [xb_])
                        g.dma("sp", ys if samp else y[rows, :], xb_[:nn, :], reads=[xb_], is_output=True)
                    g.barrier()
        if ("p4" in stages or "all" in stages) and SPARSE:
            p4 = contextlib.ExitStack()
            with p4:
                def sb4(name, shape, dt=F32):
                    return Buf(p4.enter_context(nc.sbuf_tensor("s4_" + name, list(shape), dt)), name)

                def ps4(name, shape, dt=F32):
                    return Buf(p4.enter_context(nc.psum_tensor("p4_" + name, list(shape), dt)), "P:" + name)

                NW = 3
                wgs = [sb4("wg%d" % j, [128, 8, EH], BF16) for j in range(NW)]
                wus = [sb4("wu%d" % j, [128, 8, EH], BF16) for j in range(NW)]
                wds = [sb4("wd%d" % j, [128, 2, D], BF16) for j in range(NW)]
                Xsb = [sb4("Xs%d" % j, [128, D], BF16) for j in range(3)]
                XTb = [sb4("XT%d" % j, [128, 8, 512], BF16) for j in range(2)]
                sgt = [sb4("sgt%d" % j, [128, 512]) for j in range(2)]
                act = [sb4("act%d" % j, [128, 512], BF16) for j in range(2)]
                Ysb = [sb4("Ys%d" % j, [128, D], BF16) for j in range(2)]
                Yshb = [sb4("Ysh%d" % j, [128, D]) for j in range(2)]
                ptx = [ps4("ptx%d" % j, [128, 8, 128], BF16) for j in range(2)]
                ph = [[ps4("ph%d_%d" % (a_, b_), [128, 512]) for b_ in range(2)] for a_ in range(2)]
                py = [ps4("py%d" % a_, [128, 512]) for a_ in range(2)]
                BEb = sb4("BEb", [128, 640]); piota = sb4("piota", [128, 1]); widx = sb4("widx", [128, 640], I32)
                g.dma("sp", BEb[:], be_d.to_broadcast([128, 640]), writes=[BEb])
                g.dma("sp", piota[:], piota_d, writes=[piota])
                g.op("dve", lambda: nc.vector.tensor_scalar(out=BEb[:], in0=BEb[:], scalar1=128.0, scalar2=piota[:, 0:1],
                                                            op0=ALU.mult, op1=ALU.add), [BEb, piota], [BEb])
                g.op("dve", lambda: nc.vector.tensor_copy(out=widx[:], in_=BEb[:]), [BEb], [widx])
                nblk = int(_os.environ.get("P4_NBLK", str(NBLK)))
                yi = 0
                ci = 0

                def expert_block(j2, XT, ntb):
                    for hh in range(2):
                        hs = slice(hh * 128, (hh + 1) * 128)
                        g.mm([(ph[0][hh][:, :ntb], wgs[j2][:, k, hs], XT[:, k, :ntb]) for k in range(8)], [wgs[j2], XT], ph[0][hh])
                        g.mm([(ph[1][hh][:, :ntb], wus[j2][:, k, hs], XT[:, k, :ntb]) for k in range(8)], [wus[j2], XT], ph[1][hh])
                        g.op("act", lambda: nc.scalar.activation(out=sgt[hh][:, :ntb], in_=ph[0][hh][:, :ntb], func=AF.Silu),
                             [ph[0][hh]], [sgt[hh]])
                        g.op("dve", lambda: nc.vector.tensor_tensor(out=act[hh][:, :ntb], in0=ph[1][hh][:, :ntb],
                                                                    in1=sgt[hh][:, :ntb], op=ALU.mult),
                             [ph[1][hh], sgt[hh]], [act[hh]])

                def down(j2, c0, nn):
                    for half in range(2):
                        cs = slice(half * 512, (half + 1) * 512)
                        g.mm([(py[half][:nn, :], act[hh2][:, c0:c0 + nn], wds[j2][:, hh2, cs]) for hh2 in range(2)],
                             [act[0], act[1], wds[j2]], py[half])

                for b4 in range(nblk):
                    j2 = b4 % NW
                    off = bass.IndirectOffsetOnAxis(ap=widx[:, b4:b4 + 1], axis=0)
                    g.idma(wgs[j2][:].rearrange("p k n -> p (k n)"), None, egl[:, :], off, reads=[widx], writes=[wgs[j2]])
                    g.idma(wus[j2][:].rearrange("p k n -> p (k n)"), None, eul[:, :], off, reads=[widx], writes=[wus[j2]])
                    g.idma(wds[j2][:].rearrange("p k n -> p (k n)"), None, edl[:, :], off, reads=[widx], writes=[wds[j2]])
                    Xs = Xsb[b4 % 3]
                    pt_ = ptx[b4 % 2]
                    XT = XTb[b4 % 2]
                    g.dma("sp", Xs[:], Xs_d[b4 * 128:(b4 + 1) * 128, :], writes=[Xs])
                    g.tr([(pt_[:, k, :], Xs[:, k * 128:(k + 1) * 128], ident_b[:, :]) for k in range(8)], [Xs, ident_b], pt_)
                    g.op("act", lambda: nc.scalar.copy(out=XT[:, 0:4, 0:128], in_=pt_[:, 0:4, :]), [pt_], [XT])
                    g.op("dve", lambda: nc.vector.tensor_copy(out=XT[:, 4:8, 0:128], in_=pt_[:, 4:8, :]), [pt_], [XT])
                    expert_block(j2, XT, 128)
                    Ys = Ysb[b4 % 2]
                    down(j2, 0, 128)
                    g.op("act", lambda: nc.scalar.copy(out=Ys[:, 0:512], in_=py[0][:, :]), [py[0]], [Ys])
                    g.op("dve", lambda: nc.vector.tensor_copy(out=Ys[:, 512:1024], in_=py[1][:, :]), [py[1]], [Ys])
                    g.dma("act", Ys_d[b4 * 128:(b4 + 1) * 128, :], Ys[:], reads=[Ys])
                j2 = 0
                g.dma("pool", wgs[j2][:], shg.rearrange("(k p) n -> p k n", p=128), writes=[wgs[j2]])
                g.dma("pool", wus[j2][:], shu.rearrange("(k p) n -> p k n", p=128), writes=[wus[j2]])
                g.dma("pool", wds[j2][:], shd.rearrange("(k p) n -> p k n", p=128), writes=[wds[j2]])
                for t0 in range(0, NT + 1, 4):
                    tl = list(range(t0, min(t0 + 4, NT + 1)))
                    XT = XTb[(t0 // 4) % 2]
                    ntb = sum(128 if i < NT else NS for i in tl)
                    for ti, i in enumerate(tl):
                        g.dma("sp", XT[:, :, ti * 128:(ti + 1) * 128], h2T_d[i].rearrange("p (k t) -> p k t", k=8),
                              writes=[XT] if ti == 0 else [])
                    XT.w = {(("d", i_)): 16 * n_ for i_, (s_, n_) in enumerate(g.dsem) if n_ > 0}
                    expert_block(j2, XT, ntb)
                    for ti, i in enumerate(tl):
                        nn = 128 if i < NT else NS
                        Ysh = Yshb[yi % 2]
                        yi += 1
                        down(j2, ti * 128, nn)
                        g.op("act", lambda: nc.scalar.copy(out=Ysh[:nn, 0:512], in_=py[0][:nn, :]), [py[0]], [Ysh])
                        g.op("dve", lambda: nc.vector.tensor_copy(out=Ysh[:nn, 512:1024], in_=py[1][:nn, :]), [py[1]], [Ysh])
                        g.dma("act", Ysh_d[i * 128:i * 128 + nn, :], Ysh[:nn, :], reads=[Ysh])
                g.barrier()
            p5 = contextlib.ExitStack()
            with p5:
                def sb5(name, shape, dt=F32):
                    return Buf(p5.enter_context(nc.sbuf_tensor("s5_" + name, list(shape), dt)), name)
                GT2 = sb5("GT2", [128, D]); GT2s = sb5("GT2s", [NS, D])
                g.dma("sp", GT2[:], mod_d[0:1, 5 * D:6 * D].to_broadcast([128, D]), writes=[GT2])
                g.dma("sp", GT2s[:], mod_d[1:5, 5 * D:6 * D], writes=[GT2s])
                s8t = [sb5("s8t%d" % j, [128, 8], I32) for j in range(2)]
                w8t = [sb5("w8t%d" % j, [128, 8]) for j in range(2)]
                accf = [sb5("accf%d" % j, [128, D]) for j in range(2)]
                xo = [sb5("xo%d" % j, [128, D]) for j in range(2)]
                Yg = [sb5("Yg%d" % j, [128, D], BF16) for j in range(4)]
                gi_ = 0
                for i in range(NT + 1):
                    nn = 128 if i < NT else NS
                    samp = (i == NT)
                    rows = slice(i * 128, i * 128 + nn)
                    s8 = s8t[i % 2]; w8_ = w8t[i % 2]; af = accf[i % 2]; xb_ = xo[i % 2]
                    gt2 = GT2s if samp else GT2
                    g.dma("sp", s8[:], slot_d[i], writes=[s8])
                    g.dma("sp", w8_[:nn, :], w8_d[i, 0:nn, :], writes=[w8_])
                    g.dma("sp", af[:nn, :], Ysh_d[rows, :], writes=[af])
                    g.dma("sp", xb_[:nn, :], x1_d[rows, :], writes=[xb_])
                    for k8 in range(8):
                        yg = Yg[gi_ % 4]
                        gi_ += 1
                        g.idma(yg[:, :], None, Ys_d[:, :], bass.IndirectOffsetOnAxis(ap=s8[:, k8:k8 + 1], axis=0),
                               reads=[s8], writes=[yg])
                        g.op("dve", lambda: nc.vector.scalar_tensor_tensor(
                            out=af[:nn, :], in0=yg[:nn, :], scalar=w8_[:nn, k8:k8 + 1], in1=af[:nn, :],
                            op0=ALU.mult, op1=ALU.add), [yg, w8_, af], [af])
                    g.op("dve", lambda: nc.vector.tensor_tensor(out=af[:nn, :], in0=af[:nn, :], in1=gt2[:nn, :], op=ALU.mult),
                         [af, gt2], [af])
                    g.op("dve", lambda: nc.vector.tensor_add(out=xb_[:nn, :], in0=xb_[:nn, :], in1=af[:nn, :]), [xb_, af], [xb_])
                    g.dma("sp", ys if samp else y[rows, :], xb_[:nn, :], reads=[xb_], is_output=True)
                g.barrier()
        g.finish()
    return nc, dr


def core_inputs(inp, c):
    m = {}
    m["x"] = np.ascontiguousarray(inp["x_prompt"][c])
    m["xs"] = np.ascontiguousarray(inp["x_sample"][NS * c:NS * c + NS, 0])
    call = np.concatenate([inp["c_prompt"][c:c + 1], inp["c_sample"][NS * c:NS * c + NS]], axis=0)
    m["cin"] = np.ascontiguousarray(call.T.reshape(8, 128, 5).transpose(1, 0, 2))
    m["ada_w"] = inp["ada_w"][0]
    m["ada_b"] = inp["ada_b"]
    m["norm1"] = inp["norm1"]
    m["norm2"] = inp["norm2"]
    m["w_in"] = inp["w_in"][0]
    m["qg"] = np.ascontiguousarray(np.broadcast_to(inp["q_gain"][0][:, None, :], (3, 8, 64)).reshape(1, QW))
    m["kg"] = np.ascontiguousarray(np.broadcast_to(inp["k_gain"][0][:, None, :], (3, 8, 64)).reshape(1, QW))
    m["ident"] = TB["ident"]
    m["bands"] = TB["bands"]
    m["pool_w"] = inp["pool_w"][0]
    m["pool_sc"] = np.ascontiguousarray(inp["pool_scale"][0].reshape(4, 128).T)
    rb = inp["rel_bias"]
    bT = np.zeros((24, 128, 256), np.float32)
    for gi in range(3):
        idx = TB["bkt"][gi][TB["jT"]]
        for h in range(8):
            bT[gi * 8 + h] = rb[idx, gi * 8 + h]
    m["biasT"] = bT
    m["maskT"] = TB["maskT"]
    for gi, (w, dil) in enumerate(GROUPS):
        m["ck%d" % w] = np.ascontiguousarray(inp["cache_k_w%d" % w][0, NS * c:NS * c + NS].reshape(NS, w, 512))
        m["cv%d" % w] = np.ascontiguousarray(inp["cache_v_w%d" % w][0, NS * c:NS * c + NS].reshape(NS, w, 512))
    m["stp"] = np.ascontiguousarray(inp["state_pool"][0, NS * c:NS * c + NS])
    m["bias0"] = np.ascontiguousarray(rb[0:1, :])
    bs = np.zeros((3, 128, 8), np.float32)
    for gi in range(3):
        bs[gi] = rb[TB["bkt"][gi][128 - np.arange(128)], gi * 8:(gi + 1) * 8]
    m["bias_s"] = bs
    m["bd"] = TB["bd"]
    m["bandS"] = TB["bandS"]
    m["diagS"] = TB["diagS"]
    m["UT"] = TB["UT"]
    m["trash"] = TB["trash"]
    m["tri"] = TB["tri"]
    m["bstart"] = TB["bstart"]
    m["piota"] = TB["piota"]
    m["w_br_a"] = inp["w_br_a"][0]
    m["w_br_b"] = inp["w_br_b"][0]
    m["w_out"] = inp["w_out"][0]
    m["router_w"] = inp["router_w"][0]
    m["router_b"] = inp["router_bias"]
    if SPARSE:
        m["egl"] = inp["_egl"]
        m["eul"] = inp["_eul"]
        m["edl"] = inp["_edl"]
    else:
        m["eg"] = inp["exp_w_gate"][0]
        m["eu"] = inp["exp_w_up"][0]
        m["ed"] = inp["exp_w_down"][0]
    m["shg"] = inp["sh_w_gate"][0]
    m["shu"] = inp["sh_w_up"][0]
    m["shd"] = inp["sh_w_down"][0]
    return m


_NC_CACHE = {}


def prep_experts(inp):
    if SPARSE and "_egl" not in inp:
        inp["_egl"] = np.ascontiguousarray(inp["exp_w_gate"][0].reshape(NE, 8, 128, EH).transpose(0, 2, 1, 3)).reshape(NE * 128, 8 * EH)
        inp["_eul"] = np.ascontiguousarray(inp["exp_w_up"][0].reshape(NE, 8, 128, EH).transpose(0, 2, 1, 3)).reshape(NE * 128, 8 * EH)
        inp["_edl"] = np.ascontiguousarray(inp["exp_w_down"][0].reshape(NE, 2, 128, D).transpose(0, 2, 1, 3)).reshape(NE * 128, 2 * D)


def kernel(**inputs):
    inp = {k: np.asarray(v) for k, v in inputs.items()}
    prep_experts(inp)
    if "nc" not in _NC_CACHE:
        _NC_CACHE["nc"] = build_nc(stages=("all",), debug=False)
    nc, dr = _NC_CACHE["nc"]
    in_maps = []
    for c in range(NCORES):
        m = core_inputs(inp, c)
        in_maps.append({k: np.ascontiguousarray(v, dtype=np.float32) for k, v in m.items() if k in dr})
    res = run_bass_kernel_spmd(nc, in_maps, core_ids=list(range(NCORES)))
    R = res.results
    f = np.float32
    y_prompt = np.stack([R[c]["y"] for c in range(NCORES)], 0).astype(f)
    y_sample = np.concatenate([R[c]["ys"] for c in range(NCORES)], 0).reshape(NCORES * NS, 1, D).astype(f)
    outs = [y_prompt, y_sample]
    for (w, _) in GROUPS:
        for nm in ("pk", "pv"):
            outs.append(np.stack([R[c]["%s%d" % (nm, w)] for c in range(NCORES)], 0).reshape(1, NCORES, w, 8, 64).astype(f))
    outs.append(np.stack([R[c]["ppool"] for c in range(NCORES)], 0).reshape(1, NCORES, 15, 512).astype(f))
    for (w, _) in GROUPS:
        for nm in ("sk", "sv"):
            outs.append(np.concatenate([R[c]["%s%d" % (nm, w)] for c in range(NCORES)], 0).reshape(1, NCORES * NS, w, 8, 64).astype(f))
    outs.append(np.concatenate([R[c]["spool"] for c in range(NCORES)], 0).reshape(1, NCORES * NS, 15, 512).astype(f))
    return tuple(outs)
```

```python
import contextlib
import os as _os
import numpy as np
import concourse.bass as bass
import concourse.mybir as mybir
from concourse.bass_utils import run_bass_kernel_spmd

F32 = mybir.dt.float32
BF16 = mybir.dt.bfloat16
AF = mybir.ActivationFunctionType
ALU = mybir.AluOpType
AX = mybir.AxisListType

NCORES = 8
D = 1024
S = 4096
NT = S // 128
NS = 4
NTOK = S + NS
INC = 7168
QW = 1536
EPS = 1e-6
GROUPS = ((128, 1), (512, 4), (2048, 16))
NE = 256
EH = 256
PAST = 16384
NBLK = 513
NSLOT = NBLK * 128
SPARSE = _os.environ.get('MOE_DENSE') is None
I32 = mybir.dt.int32
VS = 68


class Buf:
    def __init__(self, t, name=""):
        self.t = t
        self.name = name
        self.w = {}
        self.r = {}
        self.excl = name.startswith("P:")

    def __getitem__(self, idx):
        return self.t[idx]


class G:
    def __init__(self, nc, es, n_dma_sems=40):
        self.nc = nc
        self.es = es
        self.eng = {"pe": nc.tensor, "act": nc.scalar, "dve": nc.vector, "pool": nc.gpsimd, "sp": nc.sync}
        self.sem = {}
        self.cnt = {}
        self.seen = {e: {} for e in self.eng}
        for e in self.eng:
            self.sem[e] = es.enter_context(nc.semaphore("sem_" + e))
            self.cnt[e] = 0
        self.dsem = []
        for i in range(n_dma_sems):
            self.dsem.append([es.enter_context(nc.semaphore("dsem%d" % i)), 0])
        self.dnext = 0
        self.out_tickets = []

    def _semof(self, key):
        if isinstance(key, tuple):
            return self.dsem[key[1]][0]
        return self.sem[key]

    def wait(self, e, ticket):
        key, val = ticket
        if key == e and e == "pe":
            return
        if self.seen[e].get(key, 0) >= val:
            return
        self.eng[e].wait_ge(self._semof(key), val)
        self.seen[e][key] = val

    def _deps(self, reads, writes):
        deps = {}
        for b in reads:
            for k, v in b.w.items():
                deps[k] = max(deps.get(k, 0), v)
            if b.excl:
                for k, v in b.r.items():
                    deps[k] = max(deps.get(k, 0), v)
        for b in writes:
            for k, v in b.w.items():
                deps[k] = max(deps.get(k, 0), v)
            for k, v in b.r.items():
                deps[k] = max(deps.get(k, 0), v)
        return deps

    def _mark(self, t, reads, writes):
        for b in reads:
            b.r[t[0]] = max(b.r.get(t[0], 0), t[1])
        for b in writes:
            b.w = {t[0]: t[1]}
            b.r = {}

    def barrier(self):
        for e in self.eng:
            for i, (sem, n) in enumerate(self.dsem):
                if n > 0:
                    self.wait(e, (("d", i), 16 * n))
            for e2 in self.eng:
                if e2 != e and self.cnt[e2] > 0:
                    self.wait(e, (e2, self.cnt[e2]))

    def op(self, e, fn, reads=(), writes=()):
        for k, v in self._deps(reads, writes).items():
            self.wait(e, (k, v))
        inst = fn()
        self.cnt[e] += 1
        inst.then_inc(self.sem[e], 1)
        t = (e, self.cnt[e])
        self._mark(t, reads, writes)
        return t

    def mm(self, mms, reads, out):
        for k, v in self._deps(reads, [out]).items():
            self.wait("pe", (k, v))
        n = len(mms)
        inst = None
        for i, (o, l, r) in enumerate(mms):
            inst = self.nc.tensor.matmul(o, lhsT=l, rhs=r, start=(i == 0), stop=(i == n - 1))
        self.cnt["pe"] += 1
        inst.then_inc(self.sem["pe"], 1)
        t = ("pe", self.cnt["pe"])
        self._mark(t, reads, [out])
        return t

    def tr(self, trs, reads, out):
        for k, v in self._deps(reads, [out]).items():
            self.wait("pe", (k, v))
        inst = None
        for (o, i_, idn) in trs:
            inst = self.nc.tensor.transpose(o, i_, idn)
        self.cnt["pe"] += 1
        inst.then_inc(self.sem["pe"], 1)
        t = ("pe", self.cnt["pe"])
        self._mark(t, reads, [out])
        return t

    def dma(self, q, out, in_, reads=(), writes=(), is_output=False, **kw):
        for k, v in self._deps(reads, writes).items():
            self.wait(q, (k, v))
        i = self.dnext
        self.dnext = (self.dnext + 1) % len(self.dsem)
        sem, n = self.dsem[i]
        if n > 0:
            self.wait(q, (("d", i), 16 * n))
        self.eng[q].dma_start(out=out, in_=in_, **kw).then_inc(sem, 16)
        self.dsem[i][1] = n + 1
        t = (("d", i), 16 * (n + 1))
        self._mark(t, reads, writes)
        if is_output:
            self.out_tickets.append(t)
        return t

    def idma(self, out, out_off, in_, in_off, reads=(), writes=()):
        q = "pool"
        for k, v in self._deps(reads, writes).items():
            self.wait(q, (k, v))
        i = self.dnext
        self.dnext = (self.dnext + 1) % len(self.dsem)
        sem, n = self.dsem[i]
        if n > 0:
            self.wait(q, (("d", i), 16 * n))
        self.nc.gpsimd.indirect_dma_start(out=out, out_offset=out_off, in_=in_, in_offset=in_off).then_inc(sem, 16)
        self.dsem[i][1] = n + 1
        t = (("d", i), 16 * (n + 1))
        self._mark(t, reads, writes)
        return t

    def finish(self):
        for i, (sem, n) in enumerate(self.dsem):
            if n > 0:
                self.wait("sp", (("d", i), 16 * n))
        for e in ("pe", "act", "dve", "pool"):
            if self.cnt[e] > 0:
                self.wait("sp", (e, self.cnt[e]))


def t5_bucket(dist):
    exact = 16
    d = np.asarray(dist)
    large = exact + (np.log(np.maximum(d, 1) / exact) / np.log(2048 / exact) * (32 - exact)).astype(np.int32)
    large = np.minimum(large, 31)
    return np.where(d < exact, d, large).astype(np.int32)


def static_tables():
    tb = {}
    k = np.arange(128)[:, None]
    qq = np.arange(256)[None, :]
    j = qq - k
    valid = (j >= 0) & (j <= 128)
    tb["maskT"] = valid.astype(np.float32)
    tb["jT"] = np.clip(j, 0, 128)
    tb["bkt"] = [t5_bucket(np.arange(129) * dil) for (_, dil) in GROUPS]
    tb["ident"] = np.eye(128, dtype=np.float32)
    bands = np.zeros((3, 4, 128, 128), np.float32)
    for gi, w in enumerate((2, 4, 8, 16)):
        for t in range(128):
            for tp in range(t - w + 1, t + 1):
                if tp >= 0:
                    bands[0, gi, tp, t] += 1.0 / w
                    bands[2, gi, tp, t] += 1.0 / min(t + 1, w)
                else:
                    bands[1, gi, 128 + tp, t] += 1.0 / w
            bands[0, gi, t, t] -= 1.0
            bands[2, gi, t, t] -= 1.0
    tb["bands"] = bands
    tb["UT"] = np.triu(np.ones((128, 128), np.float32), 1)
    tb["trash"] = np.repeat((NSLOT + 1.0 + np.arange(128, dtype=np.float32))[:, None], 8, 1)
    ee = np.arange(NE)[None, None, :]
    tb["tri"] = ((np.arange(2)[:, None, None] * 128 + np.arange(128)[None, :, None]) <= ee).astype(np.float32)
    tb["bstart"] = ((np.arange(5)[None, :] * 128 + np.arange(128)[:, None]) * 128).astype(np.float32)
    tb["piota"] = np.arange(128, dtype=np.float32).reshape(128, 1)
    bd = np.zeros((8, 8, 65), np.float32)
    for h in range(8):
        bd[h, h, :] = 1.0
    tb["bd"] = bd
    bandS = np.zeros((4, 60, NS), np.float32)
    diagS = np.zeros((4, NS, NS), np.float32)
    for gi, w in enumerate((2, 4, 8, 16)):
        for b in range(NS):
            for i in range(15 - (w - 1), 15):
                bandS[gi, b * 15 + i, b] = 1.0 / w
            diagS[gi, b, b] = 1.0 / w - 1.0
    tb["bandS"] = bandS
    tb["diagS"] = diagS
    return tb


TB = static_tables()


def build_nc(stages=("all",), debug=False):
    nc = bass.Bass("TRN2", target_bir_lowering=False)
    es = contextlib.ExitStack()
    dr = {}

    def din(name, shape, dt=F32):
        dr[name] = nc.dram_tensor(name, list(shape), dt, kind="ExternalInput").ap()
        return dr[name]

    def dout(name, shape, dt=F32):
        dr[name] = nc.dram_tensor(name, list(shape), dt, kind="ExternalOutput").ap()
        return dr[name]

    def dscr(name, shape, dt=F32):
        kind = "ExternalOutput" if debug else "Internal"
        dr[name] = nc.dram_tensor(name, list(shape), dt, kind=kind).ap()
        return dr[name]

    x = din("x", [S, D])
    xs = din("xs", [NS, D])
    cin = din("cin", [128, 8, 5])
    ada_w = din("ada_w", [D, 6 * D])
    ada_b = din("ada_b", [1, 6 * D])
    norm1 = din("norm1", [1, D])
    norm2 = din("norm2", [1, D])
    w_in = din("w_in", [D, INC])
    qg = din("qg", [1, QW])
    kg = din("kg", [1, QW])
    ident_d = din("ident", [128, 128])
    bands_d = din("bands", [3, 4, 128, 128])
    pool_w = din("pool_w", [4, 128, 128])
    pool_sc = din("pool_sc", [128, 4])
    y = dout("y", [S, D])
    ys = dout("ys", [NS, D])
    pk = [dout("pk%d" % w, [w, 512]) for (w, _) in GROUPS]
    pv = [dout("pv%d" % w, [w, 512]) for (w, _) in GROUPS]
    ppool = dout("ppool", [15, 512])
    mod_d = dscr("mod_d", [5, 6 * D])
    QT_d = dscr("QT_d", [QW, S], BF16)
    KT_d = dscr("KT_d", [QW, S], BF16)
    V1_d = dscr("V1_d", [S, 24 * VS], BF16)
    qkvs_d = dscr("qkvs_d", [NS, 3 * QW])
    gates_d = dscr("gates_d", [NTOK, 2048], BF16)
    ybT_d = dscr("ybT_d", [NT + 1, 128, 512], BF16)
    us_d = dscr("us_d", [NS, 512])
    biasT_d = din("biasT", [24, 128, 256])
    maskT_d = din("maskT", [128, 256])
    A_d = dscr("A_d", [3, S, 520])
    w_br_a = din("w_br_a", [512, D])
    w_br_b = din("w_br_b", [512, D])
    w_out = din("w_out", [D, D])
    router_w = din("router_w", [D, NE])
    router_b = din("router_b", [1, NE])
    if not SPARSE:
        eg = din("eg", [NE, D, EH])
        eu = din("eu", [NE, D, EH])
        ed = din("ed", [NE, EH, D])
    shg = din("shg", [D, EH])
    shu = din("shu", [D, EH])
    shd = din("shd", [EH, D])
    x1_d = dscr("x1_d", [NTOK, D])
    h2T_d = dscr("h2T_d", [NT + 1, 128, 8 * 128], BF16)
    Wd_d = dscr("Wd_d", [NT + 1, 128, NE])
    oas_d = dscr("oas_d", [NS, 512])
    Xs_d = dscr("Xs_d", [NSLOT + 128, D], BF16)
    Ys_d = dscr("Ys_d", [NSLOT + 128, D], BF16)
    Ysh_d = dscr("Ysh_d", [NTOK, D])
    slot_d = dscr("slot_d", [NT + 1, 128, 8], I32)
    w8_d = dscr("w8_d", [NT + 1, 128, 8])
    pos_d = dscr("pos_d", [NT + 1, 128, NE])
    h2b_d = dscr("h2b_d", [NT + 1, 128, D], BF16)
    be_d = dscr("be_d", [1, 640])
    egb = dscr("egb", [NE * 128, 8 * EH], BF16)
    eub = dscr("eub", [NE * 128, 8 * EH], BF16)
    edb = dscr("edb", [NE * 128, 2 * D], BF16)
    tri_d = din("tri", [2, 128, NE])
    bstart_d = din("bstart", [128, 5])
    piota_d = din("piota", [128, 1])
    egl = din("egl", [NE * 128, 8 * EH])
    eul = din("eul", [NE * 128, 8 * EH])
    edl = din("edl", [NE * 128, 2 * D])
    UT_d = din("UT", [128, 128])
    trash_d = din("trash", [128, 8])
    As_d = dscr("As_d", [NS, 3, 520])
    ck = [din("ck%d" % w, [NS, w, 512]) for (w, _) in GROUPS]
    cv = [din("cv%d" % w, [NS, w, 512]) for (w, _) in GROUPS]
    stp = din("stp", [NS, 15, 512])
    bias0_d = din("bias0", [1, 24])
    bias_s_d = din("bias_s", [3, 128, 8])
    bd_d = din("bd", [8, 8, 65])
    bandS_d = din("bandS", [4, 60, NS])
    diagS_d = din("diagS", [4, NS, NS])
    sk = [dout("sk%d" % w, [NS, w, 512]) for (w, _) in GROUPS]
    sv = [dout("sv%d" % w, [NS, w, 512]) for (w, _) in GROUPS]
    spool = dout("spool", [NS, 15, 512])

    with es:
        g = G(nc, es)
        cvt_sem = es.enter_context(nc.semaphore("cvt_sem"))
        cvt_list = []
        if SPARSE:
            for (src_, dst_) in ((egl, egb), (eul, eub), (edl, edb)):
                for r0_ in range(0, NE * 128, 1024):
                    cvt_list.append((src_[r0_:r0_ + 1024, :], dst_[r0_:r0_ + 1024, :]))
        cvt_total = len(cvt_list)

        def cvt_emit(nmax):
            for _ in range(nmax):
                if cvt_list:
                    s_, d_ = cvt_list.pop(0)
                    nc.gpsimd.dma_start(out=d_, in_=s_).then_inc(cvt_sem, 16)

        def sb(name, shape, dt=F32):
            return Buf(es.enter_context(nc.sbuf_tensor("s_" + name, list(shape), dt)), name)

        def ps(name, shape, dt=F32):
            return Buf(es.enter_context(nc.psum_tensor("p_" + name, list(shape), dt)), name)

        ident_f = sb("ident_f", [128, 128])
        g.dma("sp", ident_f[:], ident_d, writes=[ident_f])
        nhalf = sb("nhalf", [128, 8])
        g.op("dve", lambda: nc.vector.memset(nhalf[:], -0.5), [], [nhalf])
        ident_b = sb("ident_b", [128, 128], BF16)
        g.dma("pool", ident_b[:], ident_d, writes=[ident_b])

        if True:
            p0 = contextlib.ExitStack()
            with p0:
                def sb0(name, shape, dt=F32):
                    return Buf(p0.enter_context(nc.sbuf_tensor("s_" + name, list(shape), dt)), name)
                cT = sb0("cT", [128, 8, 5])
                sg = sb0("sg", [128, 8, 5])
                g.dma("sp", cT[:], cin, writes=[cT])
                g.op("act", lambda: nc.scalar.activation(out=sg[:], in_=cT[:], func=AF.Sigmoid), [cT], [sg])
                g.op("dve", lambda: nc.vector.tensor_mul(out=sg[:], in0=cT[:], in1=sg[:]), [cT, sg], [sg])
                adab = sb0("adab", [5, 6 * D])
                g.dma("act", adab[:], ada_b.to_broadcast([5, 6 * D]), writes=[adab])
                modsb = sb0("modsb", [5, 6 * D])
                wbuf = [sb0("adaw%d" % i, [128, 8, 512]) for i in range(2)]
                pmod = [Buf(p0.enter_context(nc.psum_tensor("pmod%d" % i, [5, 512], F32)), "P:pmod") for i in range(2)]
                for cg in range(12):
                    wb = wbuf[cg % 2]
                    g.dma("sp" if cg % 2 == 0 else "act", wb[:],
                          ada_w[:, cg * 512:(cg + 1) * 512].rearrange("(k p) n -> p k n", p=128), writes=[wb])
                    pm = pmod[cg % 2]
                    g.mm([(pm[:], sg[:, k, :], wb[:, k, :]) for k in range(8)], [sg, wb], pm)
                    g.op("dve", lambda pm=pm, cg=cg: nc.vector.tensor_add(
                        out=modsb[:, cg * 512:(cg + 1) * 512], in0=pm[:], in1=adab[:, cg * 512:(cg + 1) * 512]),
                        [pm, adab], [modsb])
                g.dma("sp", mod_d, modsb[:], reads=[modsb])
                g.barrier()

        if "p1" in stages or "all" in stages:
            p1 = contextlib.ExitStack()
            with p1:
                def sb1(name, shape, dt=F32):
                    return Buf(p1.enter_context(nc.sbuf_tensor("s_" + name, list(shape), dt)), name)

                def ps1(name, shape, dt=F32):
                    return Buf(p1.enter_context(nc.psum_tensor("p_" + name, list(shape), dt)), "P:" + name)

                w_in_sb = sb1("w_in_sb", [128, 8, INC], BF16)
                wchunks = [Buf(None, "wc%d" % i) for i in range(14)]
                _skip = _os.environ.get('SKIP', '').split(',')
                for cg in range(14 if 'win' not in _skip else 0):
                    g.dma("pool", w_in_sb[:, :, cg * 512:(cg + 1) * 512],
                          w_in[:, cg * 512:(cg + 1) * 512].rearrange("(k p) n -> p k n", p=128),
                          writes=[wchunks[cg]])
                G1 = sb1("G1", [128, D]); SH1 = sb1("SH1", [128, D])
                G1s = sb1("G1s", [NS, D]); SH1s = sb1("SH1s", [NS, D])
                n1b = sb1("n1b", [128, D])
                g.dma("sp", n1b[:], norm1.to_broadcast([128, D]), writes=[n1b])
                g.dma("sp", G1[:], mod_d[0:1, D:2 * D].to_broadcast([128, D]), writes=[G1])
                g.dma("sp", SH1[:], mod_d[0:1, 0:D].to_broadcast([128, D]), writes=[SH1])
                g.dma("sp", G1s[:], mod_d[1:5, D:2 * D], writes=[G1s])
                g.dma("sp", SH1s[:], mod_d[1:5, 0:D], writes=[SH1s])
                g.op("dve", lambda: nc.vector.scalar_tensor_tensor(
                    out=G1[:], in0=G1[:], scalar=1.0, in1=n1b[:], op0=ALU.add, op1=ALU.mult), [G1, n1b], [G1])
                g.op("dve", lambda: nc.vector.scalar_tensor_tensor(
                    out=G1s[:], in0=G1s[:], scalar=1.0, in1=n1b[:NS, :], op0=ALU.add, op1=ALU.mult), [G1s, n1b], [G1s])
                qgb = sb1("qgb", [128, QW]); kgb = sb1("kgb", [128, QW])
                g.dma("sp", qgb[:], qg.to_broadcast([128, QW]), writes=[qgb])
                g.dma("sp", kgb[:], kg.to_broadcast([128, QW]), writes=[kgb])
                g.op("dve", lambda: nc.vector.tensor_scalar_mul(out=qgb[:], in0=qgb[:], scalar1=0.125), [qgb], [qgb])
                bands = sb1("bands", [128, 12, 128])
                if 'bands' not in _skip:
                    g.dma("sp", bands[:], bands_d.rearrange("a g p t -> p (a g) t"), writes=[bands])
                pw_sb = sb1("pw_sb", [128, 4, 128], BF16)
                if 'poolw' not in _skip:
                    g.dma("pool", pw_sb[:], pool_w.rearrange("g c d -> c g d"), writes=[pw_sb])
                psc = sb1("psc", [128, 4])
                g.dma("sp", psc[:], pool_sc, writes=[psc])

                xt = [sb1("xt%d" % i, [128, D]) for i in range(2)]
                sq = sb1("sq", [128, D])
                hb = sb1("hb", [128, D], BF16)
                hT = sb1("hT", [128, 8, 128], BF16)
                ssq = sb1("ssq", [128, 1]); rstd = sb1("rstd", [128, 1])
                ss8 = sb1("ss8", [128, 8]); rs8 = sb1("rs8", [128, 8])
                qf = [sb1("qf%d" % i, [128, 512]) for i in range(2)]
                kf = [sb1("kf%d" % i, [128, 512]) for i in range(2)]
                vf = [sb1("vf%d" % i, [128, 512]) for i in range(2)]
                V1 = sb1("V1", [128, 24, VS], BF16)
                if 'memv1' not in _skip:
                    g.op("dve", lambda: nc.vector.memset(V1[:], 1.0), [], [V1])
                QTst = sb1("QTst", [128, 12, 128], BF16)
                KTst = sb1("KTst", [128, 12, 128], BF16)
                ut = [sb1("ut%d" % i, [128, 512]) for i in range(2)]
                gts = sb1("gts", [128, 2048], BF16)
                pooledT = sb1("pooledT", [128, 4, 128], BF16)
                ybT = sb1("ybT", [128, 4, 128], BF16)
                pz = [ps1("pz%d" % i, [128, 512]) for i in range(3)]
                ptr = ps1("ptr", [128, 8, 128], BF16)
                ptq = ps1("ptq", [128, 4, 128])
                ppl = ps1("ppl", [128, 4, 128])
                pmx = ps1("pmx", [128, 4, 128])

                def load_x(i):
                    if i < NT:
                        g.dma("sp", xt[i % 2][:], x[i * 128:(i + 1) * 128, :], writes=[xt[i % 2]])
                    else:
                        g.dma("sp", xt[i % 2][:NS, :], xs, writes=[xt[i % 2]])

                load_x(0)
                pzi = 0
                _tl = _os.environ.get('P1_TILES')
                _tiles = list(range(NT + 1)) if _tl is None else [int(v) for v in _tl.split(',') if v != '']
                for i in _tiles:
                    n = 128 if i < NT else NS
                    samp = (i == NT)
                    if i + 1 <= NT and _tl is None:
                        load_x(i + 1)
                    if _tl is not None and i != 0:
                        load_x(i)
                    xb = xt[i % 2]
                    Gm, Sm = (G1s, SH1s) if samp else (G1, SH1)
                    g.op("act", lambda: nc.scalar.activation(out=sq[:n, :], in_=xb[:n, :], func=AF.Square,
                                                             accum_out=ssq[:n, :]), [xb], [sq, ssq])
                    g.op("dve", lambda: nc.vector.tensor_scalar(out=rstd[:n, :], in0=ssq[:n, :], scalar1=1.0 / D,
                                                                scalar2=EPS, op0=ALU.mult, op1=ALU.add), [ssq], [rstd])
                    g.op("pool", lambda: nc.gpsimd.tensor_tensor(out=rstd[:n, :], in0=rstd[:n, :], in1=nhalf[:n, 0:1],
                                                                 op=ALU.pow), [rstd, nhalf], [rstd])
                    g.op("dve", lambda: nc.vector.scalar_tensor_tensor(
                        out=sq[:n, :], in0=xb[:n, :], scalar=rstd[:n, :], in1=Gm[:n, :], op0=ALU.mult, op1=ALU.mult),
                        [xb, rstd, Gm], [sq])
                    g.op("dve", lambda: nc.vector.tensor_add(out=hb[:n, :], in0=sq[:n, :], in1=Sm[:n, :]), [sq, Sm], [hb])
                    LVL = int(_os.environ.get('P1_LVL', '99'))
                    if LVL < 2:
                        continue
                    g.tr([(ptr[:, k, :n], hb[:n, k * 128:(k + 1) * 128], ident_b[:n, :n]) for k in range(8)],
                         [hb, ident_b], ptr)
                    g.op("act", lambda: nc.scalar.copy(out=hT[:, :, :n], in_=ptr[:, :, :n]), [ptr], [hT])
                    if LVL < 3:
                        continue
                    ucur = ut[i % 2]
                    for cg in range(14):
                        pzb = pz[pzi % 3]
                        pzi += 1
                        g.mm([(pzb[:n, :], hT[:, k, :n], w_in_sb[:, k, cg * 512:(cg + 1) * 512]) for k in range(8)],
                             [hT, wchunks[cg]], pzb)
                        if LVL < 4:
                            continue
                        if cg < 6:
                            gi = cg % 3
                            isq = cg < 3
                            dst = (qf if isq else kf)[gi % 2]
                            gain = qgb if isq else kgb
                            g.op("act", lambda: nc.scalar.activation(out=sq[:n, :512], in_=pzb[:n, :], func=AF.Square),
                                 [pzb], [sq])
                            g.op("dve", lambda: nc.vector.tensor_reduce(
                                out=ss8[:n, :], in_=sq[:n, :512].rearrange("p (h e) -> p h e", e=64),
                                axis=AX.X, op=ALU.add), [sq], [ss8])
                            g.op("dve", lambda: nc.vector.tensor_scalar(out=rs8[:n, :], in0=ss8[:n, :], scalar1=1.0 / 64,
                                                                        scalar2=EPS, op0=ALU.mult, op1=ALU.add), [ss8], [rs8])
                            g.op("pool", lambda: nc.gpsimd.tensor_tensor(out=rs8[:n, :], in0=rs8[:n, :], in1=nhalf[:n, :],
                                                                         op=ALU.pow), [rs8, nhalf], [rs8])
                            g.op("dve", lambda: nc.vector.tensor_tensor(
                                out=dst[:n, :].rearrange("p (h e) -> p h e", e=64),
                                in0=pzb[:n, :].rearrange("p (h e) -> p h e", e=64),
                                in1=rs8[:n, :].unsqueeze(2).to_broadcast([n, 8, 64]), op=ALU.mult), [pzb, rs8], [dst])
                            g.op("dve", lambda: nc.vector.tensor_mul(out=dst[:n, :], in0=dst[:n, :],
                                                                     in1=gain[:n, gi * 512:(gi + 1) * 512]), [dst, gain], [dst])
                            if LVL < 5:
                                continue
                            if not samp:
                                g.tr([(ptq[:, j, :n], dst[:n, j * 128:(j + 1) * 128], ident_f[:n, :n]) for j in range(4)],
                                     [dst, ident_f], ptq)
                                st = QTst if isq else KTst
                                g.op("act", lambda: nc.scalar.copy(out=st[:, gi * 4:(gi + 1) * 4, :], in_=ptq[:]), [ptq], [st])
                                if not isq:
                                    W = GROUPS[gi][0]
                                    r0 = i * 128 - (S - W)
                                    if r0 >= 0:
                                        g.dma("sp", pk[gi][r0:r0 + 128, :], dst[:], reads=[dst], is_output=True)
                            else:
                                off = (0 if isq else QW) + gi * 512
                                g.dma("sp", qkvs_d[:, off:off + 512], dst[:NS, :], reads=[dst])
                        elif LVL < 6:
                            continue
                        elif cg < 9:
                            gi = cg - 6
                            dst = vf[gi % 2]
                            if 'vact' not in _skip:
                                g.op("dve", lambda: nc.vector.tensor_copy(out=dst[:n, :], in_=pzb[:n, :]), [pzb], [dst])
                            if not samp and 'vcopy' not in _skip:
                                g.op("act", lambda: nc.scalar.copy(
                                    out=V1[:, gi * 8:(gi + 1) * 8, 0:64], in_=dst[:, :].rearrange("p (h e) -> p h e", e=64)),
                                    [dst], [V1])
                                W = GROUPS[gi][0]
                                r0 = i * 128 - (S - W)
                                if r0 >= 0:
                                    g.dma("sp", pv[gi][r0:r0 + 128, :], dst[:], reads=[dst], is_output=True)
                            else:
                                off = 2 * QW + gi * 512
                                g.dma("sp", qkvs_d[:, off:off + 512], dst[:NS, :], reads=[dst])
                        elif LVL < 7:
                            continue
                        elif cg == 9:
                            g.op("act", lambda: nc.scalar.copy(out=ucur[:n, :], in_=pzb[:n, :]), [pzb], [ucur])
                        else:
                            c0 = (cg - 10) * 512
                            g.op("act", lambda: nc.scalar.activation(out=gts[:n, c0:c0 + 512], in_=pzb[:n, :],
                                                                     func=AF.Sigmoid), [pzb], [gts])
                    if LVL < 8:
                        continue
                    if not samp:
                        g.dma("sp", QT_d[:, i * 128:(i + 1) * 128].rearrange("(j p) t -> p j t", p=128), QTst[:], reads=[QTst])
                        g.dma("sp", KT_d[:, i * 128:(i + 1) * 128].rearrange("(j p) t -> p j t", p=128), KTst[:], reads=[KTst])
                        g.dma("sp", V1_d[i * 128:(i + 1) * 128, :], V1[:].rearrange("p a b -> p (a b)"), reads=[V1])
                        g.dma("sp", gates_d[i * 128:(i + 1) * 128, :], gts[:], reads=[gts])
                        if i == NT - 1:
                            g.dma("sp", ppool, ucur[113:128, :], reads=[ucur], is_output=True)
                        if LVL < 9:
                            continue
                        uprev = ut[(i + 1) % 2]
                        for gi in range(4):
                            cs = slice(gi * 128, (gi + 1) * 128)
                            if i == 0:
                                mms = [(ppl[:, gi, :], ucur[:, cs], bands[:, 8 + gi, :])]
                            else:
                                mms = [(ppl[:, gi, :], ucur[:, cs], bands[:, gi, :]),
                                       (ppl[:, gi, :], uprev[:, cs], bands[:, 4 + gi, :])]
                            g.mm(mms, [ucur, uprev, bands], ppl)
                        g.op("dve", lambda: nc.vector.tensor_copy(out=pooledT[:], in_=ppl[:]), [ppl], [pooledT])
                        for gi in range(4):
                            g.mm([(pmx[:, gi, :], pw_sb[:, gi, :], pooledT[:, gi, :])], [pw_sb, pooledT], pmx)
                        for gi in range(4):
                            g.op("act", lambda gi=gi: nc.scalar.activation(out=ybT[:, gi, :], in_=pmx[:, gi, :], func=AF.Copy,
                                                                          scale=psc[:, gi:gi + 1]), [pmx, psc], [ybT])
                        g.dma("sp", ybT_d[i], ybT[:].rearrange("p a b -> p (a b)"), reads=[ybT])
                        cvt_emit(3)
                    else:
                        g.dma("sp", gates_d[S:S + NS, :], gts[:NS, :], reads=[gts])
                        g.dma("sp", us_d, ucur[:NS, :], reads=[ucur])
                g.barrier()
        if "p2" in stages or "all" in stages:
            p2 = contextlib.ExitStack()
            with p2:
                def sb2(name, shape, dt=F32):
                    return Buf(p2.enter_context(nc.sbuf_tensor("s2_" + name, list(shape), dt)), name)

                def ps2(name, shape, dt=F32):
                    return Buf(p2.enter_context(nc.psum_tensor("p2_" + name, list(shape), dt)), "P:" + name)

                Eb = sb2("Eb", [128, 24, 256], BF16)
                mk = sb2("mk", [128, 256])
                g.dma("sp", mk[:], maskT_d, writes=[mk])
                for c4 in range(6):
                    bt = sb2("bt%d" % c4, [128, 4, 256])
                    g.dma("sp", bt[:], biasT_d[c4 * 4:(c4 + 1) * 4].rearrange("a k q -> k a q"), writes=[bt])
                    g.op("act", lambda: nc.scalar.activation(out=bt[:], in_=bt[:], func=AF.Exp), [bt], [bt])
                    g.op("dve", lambda: nc.vector.tensor_tensor(
                        out=Eb[:, c4 * 4:(c4 + 1) * 4, :], in0=bt[:], in1=mk[:].unsqueeze(1).to_broadcast([128, 4, 256]),
                        op=ALU.mult), [bt, mk], [Eb])
                QTg = sb2("QTg", [128, 4, S], BF16)
                KTg = sb2("KTg", [128, 4, S], BF16)
                V1g = sb2("V1g", [128, 32, 8 * VS], BF16)
                PT = [[sb2("PT%d_%d" % (h, j), [128, 256], BF16) for j in range(2)] for h in range(8)]
                pe32 = [sb2("pe32_%d" % j, [128, 256]) for j in range(2)]
                oacc = [sb2("oacc%d" % j, [128, 8, 65]) for j in range(2)]
                pS = [ps2("pS%d" % j, [128, 512]) for j in range(4)]
                pO = [[ps2("pO%d_%d" % (j, hh), [128, 512]) for hh in range(2)] for j in range(2)]

                def sl(s0, c, st):
                    return slice(s0, s0 + st * (c - 1) + 1, st)

                si = 0
                oi = 0
                for gi, (W, dil) in enumerate(GROUPS):
                    L = S // dil
                    nb = L // 128
                    g.dma("sp", QTg[:], QT_d[gi * 512:(gi + 1) * 512, :].rearrange("(j p) t -> p j t", p=128), writes=[QTg])
                    g.dma("act", KTg[:], KT_d[gi * 512:(gi + 1) * 512, :].rearrange("(j p) t -> p j t", p=128), writes=[KTg])
                    vsrc = V1_d[:, gi * 8 * VS:(gi + 1) * 8 * VS].rearrange("(cb a r) c -> a r cb c", a=128, r=dil)
                    vdst = V1g[:].rearrange("p (r cb) c -> p r cb c", r=dil)
                    nsplit = max(1, 4 // dil)
                    cbs = nb // nsplit
                    wt = []
                    for r in range(dil):
                        for sp_ in range(nsplit):
                            g.dma("sp", vdst[:, r, sp_ * cbs:(sp_ + 1) * cbs, :], vsrc[:, r, sp_ * cbs:(sp_ + 1) * cbs, :],
                                  writes=[V1g] if (r == 0 and sp_ == 0) else [])
                    V1g.w = {(("d", i)): 16 * n for i, (s_, n) in enumerate(g.dsem) if n > 0}
                    Adst = A_d[gi].rearrange("(cb a r) c -> r cb a c", a=128, r=dil)
                    for r in range(dil):
                        for kb in range(nb):
                            nq = 256 if kb < nb - 1 else 128
                            for h in range(8):
                                j = h // 2
                                rows = slice((h % 2) * 64, (h % 2) * 64 + 64)
                                psb = pS[si % 4]
                                si += 1
                                g.mm([(psb[:, :nq], KTg[rows, j, sl(r + dil * kb * 128, 128, dil)],
                                       QTg[rows, j, sl(r + dil * kb * 128, nq, dil)])], [KTg, QTg], psb)
                                e32 = pe32[si % 2]
                                g.op("act", lambda: nc.scalar.activation(out=e32[:, :nq], in_=psb[:, :nq], func=AF.Exp),
                                     [psb], [e32])
                                ptb = PT[h][kb % 2]
                                g.op("dve", lambda: nc.vector.tensor_tensor(out=ptb[:, :nq], in0=e32[:, :nq],
                                                                            in1=Eb[:, gi * 8 + h, :nq], op=ALU.mult),
                                     [e32, Eb], [ptb])
                            po = pO[oi % 2]
                            ob = oacc[oi % 2]
                            oi += 1
                            bi = r * nb + kb
                            for h in range(8):
                                pob = po[h // 4]
                                mms = []
                                if kb > 0:
                                    mms.append((pob[:, (h % 4) * 65:(h % 4) * 65 + 65], PT[h][(kb - 1) % 2][:, 128:256], V1g[:, bi - 1, h * VS:h * VS + 65]))
                                mms.append((pob[:, (h % 4) * 65:(h % 4) * 65 + 65], PT[h][kb % 2][:, 0:128], V1g[:, bi, h * VS:h * VS + 65]))
                                g.mm(mms, [PT[h][0], PT[h][1], V1g], pob)
                            g.op("act", lambda: nc.scalar.copy(out=ob[:, 0:4, :].rearrange("p a b -> p (a b)"), in_=po[0][:, 0:260]), [po[0]], [ob])
                            g.op("dve", lambda: nc.vector.tensor_copy(out=ob[:, 4:8, :].rearrange("p a b -> p (a b)"), in_=po[1][:, 0:260]), [po[1]], [ob])
                            g.dma("sp", Adst[r, kb], ob[:].rearrange("p a b -> p (a b)"), reads=[ob])
                g.barrier()
        if "p2b" in stages or "all" in stages:
            for gi, (W, dil) in enumerate(GROUPS):
                for b in range(NS):
                    for (src, dst, off) in ((ck[gi], sk[gi], QW), (cv[gi], sv[gi], 2 * QW)):
                        for r0 in range(1, W, 512):
                            r1 = min(W, r0 + 512)
                            g.dma("act", dst[b, r0 - 1:r1 - 1, :], src[b, r0:r1, :], is_output=True)
                        g.dma("act", dst[b, W - 1:W, :], qkvs_d[b:b + 1, off + gi * 512:off + (gi + 1) * 512], is_output=True)
            for b in range(NS):
                g.dma("act", spool[b, 0:14, :], stp[b, 1:15, :], is_output=True)
                g.dma("act", spool[b, 14:15, :], us_d[b:b + 1, :], is_output=True)
            pb = contextlib.ExitStack()
            with pb:
                def sbb(name, shape, dt=F32):
                    return Buf(pb.enter_context(nc.sbuf_tensor("sb_" + name, list(shape), dt)), name)

                def psb_(name, shape, dt=F32):
                    return Buf(pb.enter_context(nc.psum_tensor("pb_" + name, list(shape), dt)), "P:" + name)

                qs = sbb("qs", [NS, QW]); ks = sbb("ks", [NS, QW]); vs_ = sbb("vs", [NS, QW])
                g.dma("sp", qs[:], qkvs_d[:, 0:QW], writes=[qs])
                g.dma("sp", ks[:], qkvs_d[:, QW:2 * QW], writes=[ks])
                g.dma("sp", vs_[:], qkvs_d[:, 2 * QW:3 * QW], writes=[vs_])
                prod = sbb("prod", [NS, QW])
                s0 = sbb("s0", [NS, 24]); b0 = sbb("b0", [NS, 24]); p0 = sbb("p0", [NS, 24])
                num0 = sbb("num0", [NS, 24, 64])
                g.dma("sp", b0[:], bias0_d.to_broadcast([NS, 24]), writes=[b0])
                g.op("dve", lambda: nc.vector.tensor_mul(out=prod[:], in0=qs[:], in1=ks[:]), [qs, ks], [prod])
                g.op("dve", lambda: nc.vector.tensor_reduce(out=s0[:], in_=prod[:].rearrange("p (h e) -> p h e", e=64),
                                                            axis=AX.X, op=ALU.add), [prod], [s0])
                g.op("dve", lambda: nc.vector.tensor_add(out=s0[:], in0=s0[:], in1=b0[:]), [s0, b0], [s0])
                g.op("act", lambda: nc.scalar.activation(out=p0[:], in_=s0[:], func=AF.Exp), [s0], [p0])
                g.op("dve", lambda: nc.vector.tensor_tensor(
                    out=num0[:], in0=vs_[:].rearrange("p (h e) -> p h e", e=64),
                    in1=p0[:].unsqueeze(2).to_broadcast([NS, 24, 64]), op=ALU.mult), [vs_, p0], [num0])
                BD = sbb("BD", [8, 8, 65])
                g.dma("sp", BD[:], bd_d, writes=[BD])
                ones8 = sbb("ones8", [8, 1])
                g.op("dve", lambda: nc.vector.memset(ones8[:], 1.0), [], [ones8])
                bsm = sbb("bsm", [128, 3, 8])
                g.dma("sp", bsm[:], bias_s_d.rearrange("g k h -> k g h"), writes=[bsm])
                Ksel = [sbb("Ksel%d" % j, [128, 512]) for j in range(2)]
                V1s = [sbb("V1s%d" % j, [128, 8, 65]) for j in range(2)]
                for j in range(2):
                    g.op("dve", lambda j=j: nc.vector.memset(V1s[j][:], 1.0), [], [V1s[j]])
                qbc = [sbb("qbc%d" % j, [128, 512]) for j in range(2)]
                pr2 = sbb("pr2", [128, 512])
                sc_ = sbb("sc_", [128, 8]); pp = sbb("pp", [128, 8])
                m1 = sbb("m1", [8, 8, 65])
                arow = sbb("arow", [1, 520])
                po1 = [psb_("po1_%d" % j, [128, 512]) for j in range(2)]
                po2 = [psb_("po2_%d" % j, [128, 512]) for j in range(2)]
                it = 0
                for b in range(NS):
                    for gi, (W, dil) in enumerate(GROUPS):
                        kb_ = Ksel[it % 2]; vb_ = V1s[it % 2]; qb_ = qbc[it % 2]
                        it += 1
                        g.dma("sp", kb_[:], ck[gi][b, 0:W:dil, :], writes=[kb_])
                        g.dma("act", vb_[:, :, 0:64], cv[gi][b, 0:W:dil, :].rearrange("r (h e) -> r h e", e=64), writes=[vb_])
                        g.dma("sp", qb_[:], qkvs_d[b:b + 1, gi * 512:(gi + 1) * 512].to_broadcast([128, 512]), writes=[qb_])
                        g.op("dve", lambda: nc.vector.tensor_mul(out=pr2[:], in0=kb_[:], in1=qb_[:]), [kb_, qb_], [pr2])
                        g.op("dve", lambda: nc.vector.tensor_reduce(out=sc_[:], in_=pr2[:].rearrange("p (h e) -> p h e", e=64),
                                                                    axis=AX.X, op=ALU.add), [pr2], [sc_])
                        g.op("dve", lambda: nc.vector.tensor_add(out=sc_[:], in0=sc_[:], in1=bsm[:, gi, :]), [sc_, bsm], [sc_])
                        g.op("act", lambda: nc.scalar.activation(out=pp[:], in_=sc_[:], func=AF.Exp), [sc_], [pp])
                        for hf in range(2):
                            g.mm([(po1[hf][0:8, 0:260], pp[:, :], vb_[:, hf * 4:(hf + 1) * 4, :].rearrange("p a b -> p (a b)"))],
                                 [pp, vb_], po1[hf])
                            g.op("dve", lambda: nc.vector.tensor_tensor(
                                out=m1[:, hf * 4:(hf + 1) * 4, :].rearrange("p a b -> p (a b)"), in0=po1[hf][0:8, 0:260],
                                in1=BD[:, hf * 4:(hf + 1) * 4, :].rearrange("p a b -> p (a b)"), op=ALU.mult), [po1[hf], BD], [m1])
                        for hf in range(2):
                            g.mm([(po2[hf][0:1, 0:260], ones8[:, :], m1[:, hf * 4:(hf + 1) * 4, :].rearrange("p a b -> p (a b)"))],
                                 [ones8, m1], po2[hf])
                            g.op("act", lambda: nc.scalar.copy(out=arow[:, hf * 260:(hf + 1) * 260], in_=po2[hf][0:1, 0:260]),
                                 [po2[hf]], [arow])
                        g.dma("sp", As_d[b, gi:gi + 1, :], arow[:], reads=[arow])
                st = sbb("st", [60, 512]); us = sbb("us", [NS, 512])
                g.dma("sp", st[:], stp.rearrange("b r c -> (b r) c"), writes=[st])
                g.dma("sp", us[:], us_d, writes=[us])
                bS = sbb("bS", [60, 4, NS]); dS = sbb("dS", [NS, 4, NS])
                g.dma("sp", bS[:], bandS_d.rearrange("g k b -> k g b"), writes=[bS])
                g.dma("sp", dS[:], diagS_d.rearrange("g k b -> k g b"), writes=[dS])
                pw2 = sbb("pw2", [128, 4, 128], BF16)
                pw2f = sbb("pw2f", [128, 4, 128])
                g.dma("sp", pw2f[:], pool_w.rearrange("g c d -> c g d"), writes=[pw2f])
                g.op("dve", lambda: nc.vector.tensor_copy(out=pw2[:], in_=pw2f[:]), [pw2f], [pw2])
                psc2 = sbb("psc2", [128, 4])
                g.dma("sp", psc2[:], pool_sc, writes=[psc2])
                pps = psb_("pps", [128, 512]); pmxs = psb_("pmxs", [128, 512])
                for gq in range(4):
                    cs = slice(gq * 128, (gq + 1) * 128)
                    g.mm([(pps[:, gq * NS:(gq + 1) * NS], st[:, cs], bS[:, gq, :]),
                          (pps[:, gq * NS:(gq + 1) * NS], us[:, cs], dS[:, gq, :])], [st, us, bS, dS], pps)
                pTs = sbb("pTs", [128, 4 * NS], BF16)
                g.op("dve", lambda: nc.vector.tensor_copy(out=pTs[:], in_=pps[:, 0:4 * NS]), [pps], [pTs])
                for gq in range(4):
                    g.mm([(pmxs[:, gq * NS:(gq + 1) * NS], pw2[:, gq, :], pTs[:, gq * NS:(gq + 1) * NS])], [pw2, pTs], pmxs)
                ybs = sbb("ybs", [128, 4, 128], BF16)
                g.op("dve", lambda: nc.vector.memset(ybs[:], 0.0), [], [ybs])
                for gq in range(4):
                    g.op("act", lambda gq=gq: nc.scalar.activation(out=ybs[:, gq, 0:NS], in_=pmxs[:, gq * NS:(gq + 1) * NS], func=AF.Copy,
                                                                  scale=psc2[:, gq:gq + 1]), [pmxs, psc2], [ybs])
                g.dma("sp", ybT_d[NT], ybs[:].rearrange("p a b -> p (a b)"), reads=[ybs])
                g.barrier()
                As = sbb("As", [NS, 3, 8, 65])
                g.dma("sp", As[:].rearrange("p a b c -> p (a b c)"), As_d.rearrange("b g c -> b (g c)"), writes=[As])
                numt = sbb("numt", [NS, 8, 64]); lt = sbb("lt", [NS, 8])
                g.op("dve", lambda: nc.vector.tensor_add(out=numt[:], in0=As[:, 0, :, 0:64], in1=As[:, 1, :, 0:64]), [As], [numt])
                g.op("dve", lambda: nc.vector.tensor_add(out=numt[:], in0=numt[:], in1=As[:, 2, :, 0:64]), [As, numt], [numt])
                g.op("dve", lambda: nc.vector.tensor_add(out=lt[:], in0=As[:, 0, :, 64], in1=As[:, 1, :, 64]), [As], [lt])
                g.op("dve", lambda: nc.vector.tensor_add(out=lt[:], in0=lt[:], in1=As[:, 2, :, 64]), [As, lt], [lt])
                for gi in range(3):
                    g.op("dve", lambda gi=gi: nc.vector.tensor_add(out=numt[:], in0=numt[:], in1=num0[:, gi * 8:(gi + 1) * 8, :]),
                         [numt, num0], [numt])
                    g.op("dve", lambda gi=gi: nc.vector.tensor_add(out=lt[:], in0=lt[:], in1=p0[:, gi * 8:(gi + 1) * 8]), [lt, p0], [lt])
                g.op("dve", lambda: nc.vector.reciprocal(out=lt[:], in_=lt[:]), [lt], [lt])
                oas = sbb("oas", [NS, 512])
                g.op("dve", lambda: nc.vector.tensor_tensor(out=oas[:].rearrange("p (h e) -> p h e", e=64), in0=numt[:],
                                                            in1=lt[:].unsqueeze(2).to_broadcast([NS, 8, 64]), op=ALU.mult),
                     [numt, lt], [oas])
                g.dma("sp", oas_d, oas[:], reads=[oas])
                g.barrier()
        if "p3" in stages or "all" in stages:
            p3 = contextlib.ExitStack()
            with p3:
                def sb3(name, shape, dt=F32):
                    return Buf(p3.enter_context(nc.sbuf_tensor("s3_" + name, list(shape), dt)), name)

                def ps3(name, shape, dt=F32):
                    return Buf(p3.enter_context(nc.psum_tensor("p3_" + name, list(shape), dt)), "P:" + name)

                wa_sb = sb3("wa", [128, 4, D], BF16)
                wb_sb = sb3("wb", [128, 4, D], BF16)
                wo_sb = sb3("wo", [128, 8, D], BF16)
                stg = sb3("stg", [128, 8, D])
                g.dma("sp", stg[:, 0:4, :], w_br_a.rearrange("(k p) n -> p k n", p=128), writes=[stg])
                g.op("act", lambda: nc.scalar.copy(out=wa_sb[:], in_=stg[:, 0:4, :]), [stg], [wa_sb])
                stg2 = sb3("stg2", [128, 4, D])
                g.dma("act", stg2[:], w_br_b.rearrange("(k p) n -> p k n", p=128), writes=[stg2])
                g.op("dve", lambda: nc.vector.tensor_copy(out=wb_sb[:], in_=stg2[:]), [stg2], [wb_sb])
                g.dma("sp", stg[:], w_out.rearrange("(k p) n -> p k n", p=128), writes=[stg])
                g.op("act", lambda: nc.scalar.copy(out=wo_sb[:, 0:4, :], in_=stg[:, 0:4, :]), [stg], [wo_sb])
                g.op("dve", lambda: nc.vector.tensor_copy(out=wo_sb[:, 4:8, :], in_=stg[:, 4:8, :]), [stg], [wo_sb])
                rw_sb = sb3("rw", [128, 8, NE])
                g.dma("sp", rw_sb[:], router_w.rearrange("(k p) n -> p k n", p=128), writes=[rw_sb])
                rbias = sb3("rbias", [128, NE])
                g.dma("sp", rbias[:], router_b.to_broadcast([128, NE]), writes=[rbias])
                GT1 = sb3("GT1", [128, D]); G2 = sb3("G2", [128, D]); SH2 = sb3("SH2", [128, D]); n2b = sb3("n2b", [128, D])
                GT1s = sb3("GT1s", [NS, D]); G2s = sb3("G2s", [NS, D]); SH2s = sb3("SH2s", [NS, D])
                g.dma("sp", n2b[:], norm2.to_broadcast([128, D]), writes=[n2b])
                g.dma("sp", GT1[:], mod_d[0:1, 2 * D:3 * D].to_broadcast([128, D]), writes=[GT1])
                g.dma("sp", SH2[:], mod_d[0:1, 3 * D:4 * D].to_broadcast([128, D]), writes=[SH2])
                g.dma("sp", G2[:], mod_d[0:1, 4 * D:5 * D].to_broadcast([128, D]), writes=[G2])
                g.dma("sp", GT1s[:], mod_d[1:5, 2 * D:3 * D], writes=[GT1s])
                g.dma("sp", SH2s[:], mod_d[1:5, 3 * D:4 * D], writes=[SH2s])
                g.dma("sp", G2s[:], mod_d[1:5, 4 * D:5 * D], writes=[G2s])
                g.op("dve", lambda: nc.vector.scalar_tensor_tensor(
                    out=G2[:], in0=G2[:], scalar=1.0, in1=n2b[:], op0=ALU.add, op1=ALU.mult), [G2, n2b], [G2])
                g.op("dve", lambda: nc.vector.scalar_tensor_tensor(
                    out=G2s[:], in0=G2s[:], scalar=1.0, in1=n2b[:NS, :], op0=ALU.add, op1=ALU.mult), [G2s, n2b], [G2s])

                A0 = sb3("A0", [128, 8, 65]); A1 = sb3("A1", [128, 8, 65]); A2 = sb3("A2", [128, 8, 65])
                rl = sb3("rl", [128, 8])
                oaf = sb3("oaf", [128, 512])
                oab = sb3("oab", [128, 512], BF16)
                oaT = sb3("oaT", [128, 4, 128], BF16)
                ybt = sb3("ybt", [128, 4, 128], BF16)
                gt = sb3("gt", [128, 2048], BF16)
                xt3 = sb3("xt3", [128, D])
                t1_ = sb3("t1_", [128, D]); t2_ = sb3("t2_", [128, D])
                mgb = sb3("mgb", [128, D], BF16)
                mT = sb3("mT", [128, 8, 128], BF16)
                x1 = sb3("x1", [128, D])
                h2 = sb3("h2", [128, D])
                sq3 = sb3("sq3", [128, D])
                ssq3 = sb3("ssq3", [128, 1]); rstd3 = sb3("rstd3", [128, 1])
                h2T32 = sb3("h2T32", [128, 8, 128])
                h2Tb = sb3("h2Tb", [128, 8, 128], BF16)
                sc = sb3("sc", [128, NE]); sel = sb3("sel", [128, NE]); selm = sb3("selm", [128, NE])
                mx8 = sb3("mx8", [128, 8, 8]); gsc = sb3("gsc", [128, 8]); gtop = sb3("gtop", [128, 8])
                gmask = sb3("gmask", [128, 8]); top8 = sb3("top8", [128, 8])
                Mk = sb3("Mk", [128, NE]); Wd = sb3("Wd", [128, NE]); den = sb3("den", [128, 1])
                UT = sb3("UT", [128, 128])
                g.dma("sp", UT[:], UT_d, writes=[UT])
                ones_r = sb3("ones_r", [1, 128]); ones_c = sb3("ones_c", [128, 1]); carry = sb3("carry", [1, NE])
                g.op("dve", lambda: nc.vector.memset(ones_r[:], 1.0), [], [ones_r])
                g.op("dve", lambda: nc.vector.memset(ones_c[:], 1.0), [], [ones_c])
                g.op("dve", lambda: nc.vector.memset(carry[:], 0.0), [], [carry])
                pos1t = sb3("pos1t", [128, NE]); key_ = sb3("key_", [128, NE]); junk = sb3("junk", [128, NE])
                h2b = sb3("h2b", [128, D], BF16)
                ptr3 = ps3("ptr3", [128, 8, 128], BF16)
                pbr = [ps3("pbr%d" % j, [128, 512]) for j in range(2)]
                pym = [ps3("pym%d" % j, [128, 512]) for j in range(2)]
                pt32 = [ps3("pt32_%d" % j, [128, 4, 128]) for j in range(2)]
                prt = ps3("prt", [128, 512])

                for i in range(NT + 1):
                    n = 128 if i < NT else NS
                    samp = (i == NT)
                    rows = slice(i * 128, i * 128 + n)
                    gt1, g2m, sh2m = (GT1s, G2s, SH2s) if samp else (GT1, G2, SH2)
                    g.dma("sp", xt3[:n, :], xs if samp else x[rows, :], writes=[xt3])
                    g.dma("act", gt[:n, :], gates_d[rows, :], writes=[gt])
                    g.dma("act", ybt[:].rearrange("p a b -> p (a b)"), ybT_d[i], writes=[ybt])
                    if not samp:
                        g.dma("sp", A0[:].rearrange("p a b -> p (a b)"), A_d[0][rows, :], writes=[A0])
                        g.dma("sp", A1[:].rearrange("p a b -> p (a b)"), A_d[1][rows, :], writes=[A1])
                        g.dma("sp", A2[:].rearrange("p a b -> p (a b)"), A_d[2][rows, :], writes=[A2])
                        g.op("dve", lambda: nc.vector.tensor_add(out=A0[:], in0=A0[:], in1=A1[:]), [A0, A1], [A0])
                        g.op("dve", lambda: nc.vector.tensor_add(out=A0[:], in0=A0[:], in1=A2[:]), [A0, A2], [A0])
                        g.op("dve", lambda: nc.vector.reciprocal(out=rl[:], in_=A0[:, :, 64]), [A0], [rl])
                        g.op("dve", lambda: nc.vector.tensor_tensor(
                            out=oab[:].rearrange("p (h e) -> p h e", e=64), in0=A0[:, :, 0:64],
                            in1=rl[:].unsqueeze(2).to_broadcast([128, 8, 64]), op=ALU.mult), [A0, rl], [oab])
                    else:
                        g.dma("sp", oaf[:NS, :], oas_d, writes=[oaf])
                        g.op("dve", lambda: nc.vector.tensor_copy(out=oab[:NS, :], in_=oaf[:NS, :]), [oaf], [oab])
                    g.tr([(ptr3[:, k, :n], oab[:n, k * 128:(k + 1) * 128], ident_b[:n, :n]) for k in range(4)], [oab, ident_b], ptr3)
                    g.op("act", lambda: nc.scalar.copy(out=oaT[:, :, :n], in_=ptr3[:, 0:4, :n]), [ptr3], [oaT])
                    for half in range(2):
                        cs = slice(half * 512, (half + 1) * 512)
                        g.mm([(pbr[0][:n, :], oaT[:, k, :n], wa_sb[:, k, cs]) for k in range(4)], [oaT, wa_sb], pbr[0])
                        g.mm([(pbr[1][:n, :], ybt[:, k, :n], wb_sb[:, k, cs]) for k in range(4)], [ybt, wb_sb], pbr[1])
                        g.op("dve", lambda: nc.vector.tensor_tensor(out=t1_[:n, cs], in0=pbr[0][:n, :], in1=gt[:n, cs], op=ALU.mult),
                             [pbr[0], gt], [t1_])
                        g.op("dve", lambda: nc.vector.tensor_tensor(out=t2_[:n, cs], in0=pbr[1][:n, :],
                                                                    in1=gt[:n, 1024 + half * 512:1024 + (half + 1) * 512], op=ALU.mult),
                             [pbr[1], gt], [t2_])
                    g.op("dve", lambda: nc.vector.tensor_add(out=mgb[:n, :], in0=t1_[:n, :], in1=t2_[:n, :]), [t1_, t2_], [mgb])
                    g.tr([(ptr3[:, k, :n], mgb[:n, k * 128:(k + 1) * 128], ident_b[:n, :n]) for k in range(8)], [mgb, ident_b], ptr3)
                    g.op("act", lambda: nc.scalar.copy(out=mT[:, :, :n], in_=ptr3[:, :, :n]), [ptr3], [mT])
                    for half in range(2):
                        cs = slice(half * 512, (half + 1) * 512)
                        g.mm([(pym[half][:n, :], mT[:, k, :n], wo_sb[:, k, cs]) for k in range(8)], [mT, wo_sb], pym[half])
                        g.op("dve", lambda: nc.vector.tensor_tensor(out=t1_[:n, cs], in0=pym[half][:n, :], in1=gt1[:n, cs], op=ALU.mult),
                             [pym[half], gt1], [t1_])
                    g.op("dve", lambda: nc.vector.tensor_add(out=x1[:n, :], in0=t1_[:n, :], in1=xt3[:n, :]), [t1_, xt3], [x1])
                    g.dma("sp", x1_d[rows, :], x1[:n, :], reads=[x1])
                    g.op("act", lambda: nc.scalar.activation(out=sq3[:n, :], in_=x1[:n, :], func=AF.Square, accum_out=ssq3[:n, :]),
                         [x1], [sq3, ssq3])
                    g.op("dve", lambda: nc.vector.tensor_scalar(out=rstd3[:n, :], in0=ssq3[:n, :], scalar1=1.0 / D, scalar2=EPS,
                                                                op0=ALU.mult, op1=ALU.add), [ssq3], [rstd3])
                    g.op("pool", lambda: nc.gpsimd.tensor_tensor(out=rstd3[:n, :], in0=rstd3[:n, :], in1=nhalf[:n, 0:1], op=ALU.pow),
                         [rstd3, nhalf], [rstd3])
                    g.op("dve", lambda: nc.vector.scalar_tensor_tensor(out=sq3[:n, :], in0=x1[:n, :], scalar=rstd3[:n, :],
                                                                       in1=g2m[:n, :], op0=ALU.mult, op1=ALU.mult),
                         [x1, rstd3, g2m], [sq3])
                    g.op("dve", lambda: nc.vector.tensor_add(out=h2[:n, :], in0=sq3[:n, :], in1=sh2m[:n, :]), [sq3, sh2m], [h2])
                    for hf in range(2):
                        g.tr([(pt32[hf][:, k, :n], h2[:n, (hf * 4 + k) * 128:(hf * 4 + k + 1) * 128], ident_f[:n, :n]) for k in range(4)],
                             [h2, ident_f], pt32[hf])
                        g.op("act", lambda: nc.scalar.copy(out=h2T32[:, hf * 4:(hf + 1) * 4, :n], in_=pt32[hf][:, :, :n]),
                             [pt32[hf]], [h2T32])
                    if samp:
                        g.op("dve", lambda: nc.vector.memset(h2Tb[:], 0.0), [], [h2Tb])
                    g.op("dve", lambda: nc.vector.tensor_copy(out=h2Tb[:, :, :n], in_=h2T32[:, :, :n]), [h2T32], [h2Tb])
                    g.dma("sp", h2T_d[i], h2Tb[:].rearrange("p a b -> p (a b)"), reads=[h2Tb])
                    g.mm([(prt[:n, :NE], h2T32[:, k, :n], rw_sb[:, k, :]) for k in range(8)], [h2T32, rw_sb], prt)
                    g.op("act", lambda: nc.scalar.activation(out=sc[:n, :], in_=prt[:n, :NE], func=AF.Sigmoid), [prt], [sc])
                    g.op("dve", lambda: nc.vector.tensor_add(out=sel[:n, :], in0=sc[:n, :], in1=rbias[:n, :]), [sc, rbias], [sel])
                    for gq in range(8):
                        g.op("dve", lambda: nc.vector.max(out=mx8[:n, gq, :], in_=sel[:n, gq * 32:(gq + 1) * 32]), [sel], [mx8])
                    g.op("dve", lambda: nc.vector.tensor_add(out=gsc[:n, :], in0=mx8[:n, :, 0], in1=mx8[:n, :, 1]), [mx8], [gsc])
                    g.op("dve", lambda: nc.vector.max(out=gtop[:n, :], in_=gsc[:n, :]), [gsc], [gtop])
                    g.op("dve", lambda: nc.vector.tensor_scalar(out=gmask[:n, :], in0=gsc[:n, :], scalar1=gtop[:n, 3:4], scalar2=None,
                                                                op0=ALU.is_ge), [gsc, gtop], [gmask])
                    g.op("dve", lambda: nc.vector.tensor_scalar(out=gmask[:n, :], in0=gmask[:n, :], scalar1=-1.0, scalar2=1e9,
                                                                op0=ALU.add, op1=ALU.mult), [gmask], [gmask])
                    g.op("dve", lambda: nc.vector.tensor_tensor(
                        out=selm[:n, :].rearrange("p (a b) -> p a b", b=32), in0=sel[:n, :].rearrange("p (a b) -> p a b", b=32),
                        in1=gmask[:n, :].unsqueeze(2).to_broadcast([n, 8, 32]), op=ALU.add), [sel, gmask], [selm])
                    g.op("dve", lambda: nc.vector.max(out=top8[:n, :], in_=selm[:n, :]), [selm], [top8])
                    g.op("dve", lambda: nc.vector.tensor_scalar(out=Mk[:n, :], in0=selm[:n, :], scalar1=top8[:n, 7:8], scalar2=None,
                                                                op0=ALU.is_ge), [selm, top8], [Mk])
                    g.op("dve", lambda: nc.vector.tensor_tensor(out=Wd[:n, :], in0=Mk[:n, :], in1=sc[:n, :], op=ALU.mult), [Mk, sc], [Wd])
                    g.op("dve", lambda: nc.vector.tensor_reduce(out=den[:n, :], in_=Wd[:n, :], axis=AX.X, op=ALU.add), [Wd], [den])
                    g.op("dve", lambda: nc.vector.reciprocal(out=den[:n, :], in_=den[:n, :]), [den], [den])
                    g.op("dve", lambda: nc.vector.tensor_scalar(out=Wd[:n, :], in0=Wd[:n, :], scalar1=den[:n, :], scalar2=2.5,
                                                                op0=ALU.mult, op1=ALU.mult), [Wd, den], [Wd])
                    g.dma("sp", Wd_d[i, 0:n, :], Wd[:n, :], reads=[Wd])
                    if SPARSE:
                        pps_ = pbr[0]; pcs_ = pbr[1]
                        g.mm([(pps_[:n, :NE], UT[:n, :n], Mk[:n, :]), (pps_[:n, :NE], ones_r[0:1, :n], carry[0:1, :])],
                             [UT, Mk, ones_r, carry], pps_)
                        g.op("dve", lambda: nc.vector.tensor_scalar(out=pos1t[:n, :], in0=pps_[:n, :NE], scalar1=1.0, scalar2=None,
                                                                    op0=ALU.add), [pps_], [pos1t])
                        g.dma("sp", pos_d[i, 0:n, :], pos1t[:n, :], reads=[pos1t])
                        g.mm([(pcs_[0:1, :NE], ones_c[:n, 0:1], Mk[:n, :])], [ones_c, Mk], pcs_)
                        g.op("dve", lambda: nc.vector.tensor_add(out=carry[0:1, :], in0=carry[0:1, :], in1=pcs_[0:1, :NE]),
                             [carry, pcs_], [carry])
                        g.op("act", lambda: nc.scalar.copy(out=h2b[:n, :], in_=h2[:n, :]), [h2], [h2b])
                        g.dma("sp", h2b_d[i, 0:n, :], h2b[:n, :], reads=[h2b])
                if SPARSE:
                    ci_ = sb3("ci_", [1, NE], I32)
                    pc = sb3("pc", [1, NE]); pcT = sb3("pcT", [128, 2]); pend = sb3("pend", [1, NE]); ps1r = sb3("ps1r", [1, NE])
                    tri = sb3("tri", [128, 2, NE]); bstart = sb3("bstart", [128, 5])
                    g.dma("sp", tri[:], tri_d.rearrange("c p e -> p c e"), writes=[tri])
                    g.dma("sp", bstart[:], bstart_d, writes=[bstart])
                    g.op("dve", lambda: nc.vector.tensor_scalar(out=pc[:], in0=carry[:], scalar1=127.0, scalar2=None, op0=ALU.add),
                         [carry], [pc])
                    g.op("dve", lambda: nc.vector.tensor_copy(out=ci_[:], in_=pc[:]), [pc], [ci_])
                    g.op("dve", lambda: nc.vector.tensor_single_scalar(out=ci_[:], in_=ci_[:], scalar=7, op=ALU.arith_shift_right),
                         [ci_], [ci_])
                    g.op("dve", lambda: nc.vector.tensor_single_scalar(out=ci_[:], in_=ci_[:], scalar=7, op=ALU.logical_shift_left),
                         [ci_], [ci_])
                    g.op("dve", lambda: nc.vector.tensor_copy(out=pc[:], in_=ci_[:]), [ci_], [pc])
                    g.tr([(pt32[0][:, 0, 0:1], pc[0:1, 0:128], ident_f[0:1, 0:1]), (pt32[0][:, 1, 0:1], pc[0:1, 128:256], ident_f[0:1, 0:1])],
                         [pc, ident_f], pt32[0])
                    g.op("dve", lambda: nc.vector.tensor_copy(out=pcT[:], in_=pt32[0][:, 0:2, 0]), [pt32[0]], [pcT])
                    g.mm([(prt[0:1, :NE], pcT[:, c2:c2 + 1], tri[:, c2, :]) for c2 in range(2)], [pcT, tri], prt)
                    g.op("dve", lambda: nc.vector.tensor_copy(out=pend[:], in_=prt[0:1, :NE]), [prt], [pend])
                    g.op("dve", lambda: nc.vector.tensor_sub(out=ps1r[:], in0=pend[:], in1=pc[:]), [pend, pc], [ps1r])
                    PSb = sb3("PSb", [128, NE]); PEb = sb3("PEb", [128, NE])
                    g.mm([(pbr[0][:, :NE], ones_r[0:1, :], ps1r[0:1, :])], [ones_r, ps1r], pbr[0])
                    g.op("dve", lambda: nc.vector.tensor_copy(out=PSb[:], in_=pbr[0][:, :NE]), [pbr[0]], [PSb])
                    g.mm([(pbr[1][:, :NE], ones_r[0:1, :], pend[0:1, :])], [ones_r, pend], pbr[1])
                    g.op("dve", lambda: nc.vector.tensor_copy(out=PEb[:], in_=pbr[1][:, :NE]), [pbr[1]], [PEb])
                    be = sb3("be", [128, 8])
                    g.op("dve", lambda: nc.vector.memset(be[:], 0.0), [], [be])
                    for j5 in range(5):
                        g.op("dve", lambda j5=j5: nc.vector.tensor_scalar(
                            out=key_[:, :], in0=PEb[:, :], scalar1=bstart[:, j5:j5 + 1], scalar2=None, op0=ALU.is_le, op1=ALU.add,
                            accum_out=be[:, j5:j5 + 1]), [PEb, bstart], [key_, be])
                    g.op("dve", lambda: nc.vector.tensor_scalar_min(out=be[:], in0=be[:], scalar1=float(NE - 1)), [be], [be])
                    g.tr([(pt32[1][0:8, 0, :], be[:, 0:8], ident_f[:, :])], [be, ident_f], pt32[1])
                    beT = sb3("beT", [8, 128])
                    g.op("dve", lambda: nc.vector.tensor_copy(out=beT[:], in_=pt32[1][0:8, 0, :]), [pt32[1]], [beT])
                    g.dma("sp", be_d.rearrange("o (j b) -> (o j) b", j=5), beT[0:5, :], reads=[beT])
                    g.barrier()
                    trashf = sb3("trashf", [128, 8])
                    g.dma("sp", trashf[:], trash_d, writes=[trashf])
                    s8f = sb3("s8f", [128, 8]); w8 = sb3("w8", [128, 8]); s8i = sb3("s8i", [128, 8], I32)
                    g.op("dve", lambda: nc.vector.memset(h2b[:], 0.0), [], [h2b])
                    for i in range(NT + 1):
                        n = 128 if i < NT else NS
                        samp = (i == NT)
                        g.dma("sp", pos1t[:n, :], pos_d[i, 0:n, :], writes=[pos1t])
                        g.dma("act", Wd[:n, :], Wd_d[i, 0:n, :], writes=[Wd])
                        g.dma("act", h2b[:n, :], h2b_d[i, 0:n, :], writes=[h2b])
                        g.op("dve", lambda: nc.vector.tensor_single_scalar(out=Mk[:n, :], in_=Wd[:n, :], scalar=0.0, op=ALU.is_gt),
                             [Wd], [Mk])
                        g.op("dve", lambda: nc.vector.tensor_add(out=key_[:n, :], in0=pos1t[:n, :], in1=PSb[:n, :]), [pos1t, PSb], [key_])
                        g.op("dve", lambda: nc.vector.tensor_mul(out=key_[:n, :], in0=key_[:n, :], in1=Mk[:n, :]), [key_, Mk], [key_])
                        if samp:
                            g.op("dve", lambda: nc.vector.tensor_copy(out=s8f[:], in_=trashf[:]), [trashf], [s8f])
                        g.op("dve", lambda: nc.vector.max(out=s8f[:n, :], in_=key_[:n, :]), [key_], [s8f])
                        for k8 in range(8):
                            g.op("dve", lambda k8=k8: nc.vector.scalar_tensor_tensor(
                                out=junk[:n, :], in0=key_[:n, :], scalar=s8f[:n, k8:k8 + 1], in1=Wd[:n, :],
                                op0=ALU.is_equal, op1=ALU.mult, accum_out=w8[:n, k8:k8 + 1]), [key_, s8f, Wd], [junk, w8])
                        g.op("dve", lambda: nc.vector.tensor_scalar(out=s8f[:, :], in0=s8f[:, :], scalar1=-1.0, scalar2=None,
                                                                    op0=ALU.add), [s8f], [s8f])
                        g.op("dve", lambda: nc.vector.tensor_copy(out=s8i[:, :], in_=s8f[:, :]), [s8f], [s8i])
                        g.dma("sp", slot_d[i], s8i[:, :], reads=[s8i])
                        g.dma("sp", w8_d[i, 0:n, :], w8[:n, :], reads=[w8])
                        for k8 in range(8):
                            g.idma(Xs_d[:, :], bass.IndirectOffsetOnAxis(ap=s8i[:, k8:k8 + 1], axis=0), h2b[:, :], None,
                                   reads=[s8i, h2b])
                g.barrier()
        if ("p4" in stages or "all" in stages) and not SPARSE:
            NG = 3
            tiles_all = list(range(NT + 1))
            per = (len(tiles_all) + NG - 1) // NG
            groups4 = [tiles_all[a:a + per] for a in range(0, len(tiles_all), per)]
            n_exp = int(_os.environ.get("P4_NEXP", str(NE)))
            for tg in groups4:
                p4 = contextlib.ExitStack()
                with p4:
                    def sb4(name, shape, dt=F32):
                        return Buf(p4.enter_context(nc.sbuf_tensor("s4_%d_" % tg[0] + name, list(shape), dt)), name)

                    def ps4(name, shape, dt=F32):
                        return Buf(p4.enter_context(nc.psum_tensor("p4_%d_" % tg[0] + name, list(shape), dt)), "P:" + name)

                    ntl = len(tg)
                    hT4 = sb4("hT4", [128, 8, ntl * 128], BF16)
                    for ti, i in enumerate(tg):
                        g.dma("sp", hT4[:, :, ti * 128:(ti + 1) * 128], h2T_d[i].rearrange("p (k t) -> p k t", k=8),
                              writes=[hT4] if ti == 0 else [])
                    Wg = sb4("Wg", [128, ntl, NE])
                    for ti, i in enumerate(tg):
                        nn = 128 if i < NT else NS
                        g.dma("sp", Wg[:nn, ti, :], Wd_d[i, 0:nn, :], writes=[])
                    acc = sb4("acc", [128, ntl, D])
                    g.op("pool", lambda: nc.gpsimd.memset(acc[:], 0.0), [], [acc])
                    allw = {(("d", i_)): 16 * n_ for i_, (s_, n_) in enumerate(g.dsem) if n_ > 0}
                    hT4.w = dict(allw)
                    Wg.w = dict(allw)
                    wgs = [sb4("wg%d" % j, [128, 8, EH], BF16) for j in range(2)]
                    wus = [sb4("wu%d" % j, [128, 8, EH], BF16) for j in range(2)]
                    wds = [sb4("wd%d" % j, [128, 2, D], BF16) for j in range(2)]
                    sgt = [sb4("sgt%d" % j, [128, 512]) for j in range(2)]
                    act = [sb4("act%d" % j, [128, 512], BF16) for j in range(2)]
                    ph = [[ps4("ph%d_%d" % (a, b), [128, 512]) for b in range(2)] for a in range(2)]
                    py = [[ps4("py%d_%d" % (a, b), [128, 512]) for b in range(2)] for a in range(2)]
                    blocks = [list(range(a, min(a + 4, ntl))) for a in range(0, ntl, 4)]
                    yi = 0
                    elist = list(range(n_exp)) + [NE]
                    for ei, e in enumerate(elist):
                        j2 = ei % 2
                        if e < NE:
                            srcs = (eg[e], eu[e], ed[e])
                        else:
                            srcs = (shg, shu, shd)
                        g.dma("pool", wgs[j2][:], srcs[0].rearrange("(k p) n -> p k n", p=128), writes=[wgs[j2]])
                        g.dma("pool", wus[j2][:], srcs[1].rearrange("(k p) n -> p k n", p=128), writes=[wus[j2]])
                        g.dma("pool", wds[j2][:], srcs[2].rearrange("(k p) n -> p k n", p=128), writes=[wds[j2]])
                        for blk in blocks:
                            t0 = blk[0] * 128
                            ntb = sum(128 if tg[ti] < NT else NS for ti in blk)
                            for hh in range(2):
                                hs = slice(hh * 128, (hh + 1) * 128)
                                g.mm([(ph[0][hh][:, :ntb], wgs[j2][:, k, hs], hT4[:, k, t0:t0 + ntb]) for k in range(8)],
                                     [wgs[j2], hT4], ph[0][hh])
                                g.mm([(ph[1][hh][:, :ntb], wus[j2][:, k, hs], hT4[:, k, t0:t0 + ntb]) for k in range(8)],
                                     [wus[j2], hT4], ph[1][hh])
                                g.op("act", lambda: nc.scalar.activation(out=sgt[hh][:, :ntb], in_=ph[0][hh][:, :ntb], func=AF.Silu),
                                     [ph[0][hh]], [sgt[hh]])
                                g.op("dve", lambda: nc.vector.tensor_tensor(out=act[hh][:, :ntb], in0=ph[1][hh][:, :ntb],
                                                                            in1=sgt[hh][:, :ntb], op=ALU.mult),
                                     [ph[1][hh], sgt[hh]], [act[hh]])
                            for ti in blk:
                                nn = 128 if tg[ti] < NT else NS
                                c0 = (ti - blk[0]) * 128
                                pyy = py[yi % 2]
                                yi += 1
                                for half in range(2):
                                    cs = slice(half * 512, (half + 1) * 512)
                                    g.mm([(pyy[half][:nn, :], act[hh2][:, c0:c0 + nn], wds[j2][:, hh2, cs]) for hh2 in range(2)],
                                         [act[0], act[1], wds[j2]], pyy[half])
                                    if e < NE:
                                        g.op("dve", lambda: nc.vector.scalar_tensor_tensor(
                                            out=acc[:nn, ti, cs], in0=pyy[half][:nn, :], scalar=Wg[:nn, ti, e:e + 1],
                                            in1=acc[:nn, ti, cs], op0=ALU.mult, op1=ALU.add), [pyy[half], Wg, acc], [acc])
                                    else:
                                        g.op("dve", lambda: nc.vector.tensor_tensor(
                                            out=acc[:nn, ti, cs], in0=pyy[half][:nn, :], in1=acc[:nn, ti, cs], op=ALU.add),
                                            [pyy[half], acc], [acc])
                    GT2 = sb4("GT2", [128, D]); GT2s = sb4("GT2s", [NS, D])
                    g.dma("sp", GT2[:], mod_d[0:1, 5 * D:6 * D].to_broadcast([128, D]), writes=[GT2])
                    g.dma("sp", GT2s[:], mod_d[1:5, 5 * D:6 * D], writes=[GT2s])
                    xo = [sb4("xo%d" % j, [128, D]) for j in range(2)]
                    for ti, i in enumerate(tg):
                        nn = 128 if i < NT else NS
                        samp = (i == NT)
                        rows = slice(i * 128, i * 128 + nn)
                        xb_ = xo[ti % 2]
                        gt2 = GT2s if samp else GT2
                        g.dma("sp", xb_[:nn, :], x1_d[rows, :], writes=[xb_])
                        g.op("dve", lambda: nc.vector.tensor_tensor(out=acc[:nn, ti, :], in0=acc[:nn, ti, :], in1=gt2[:nn, :], op=ALU.mult),
                             [acc, gt2], [acc])
                        g.op("dve", lambda: nc.vector.tensor_add(out=xb_[:nn, :], in0=xb_[:nn, :], in1=acc[:nn, ti, :]), [xb_, acc], [xb_])
                        g.dma("sp", ys if samp else y[rows, :], xb_[:nn, :], reads=[xb_], is_output=True)
                    g.barrier()
        if ("p4" in stages or "all" in stages) and SPARSE:
            p4 = contextlib.ExitStack()
            with p4:
                def sb4(name, shape, dt=F32):
                    return Buf(p4.enter_context(nc.sbuf_tensor("s4_" + name, list(shape), dt)), name)

                def ps4(name, shape, dt=F32):
                    return Buf(p4.enter_context(nc.psum_tensor("p4_" + name, list(shape), dt)), "P:" + name)

                NW = 3
                wgs = [sb4("wg%d" % j, [128, 8, EH], BF16) for j in range(NW)]
                wus = [sb4("wu%d" % j, [128, 8, EH], BF16) for j in range(NW)]
                wds = [sb4("wd%d" % j, [128, 2, D], BF16) for j in range(NW)]
                Xsb = [sb4("Xs%d" % j, [128, D], BF16) for j in range(3)]
                XTb = [sb4("XT%d" % j, [128, 8, 512], BF16) for j in range(2)]
                sgt = [sb4("sgt%d" % j, [128, 512]) for j in range(2)]
                act = [sb4("act%d" % j, [128, 512], BF16) for j in range(2)]
                Ysb = [sb4("Ys%d" % j, [128, D], BF16) for j in range(2)]
                Yshb = [sb4("Ysh%d" % j, [128, D]) for j in range(2)]
                ptx = [ps4("ptx%d" % j, [128, 8, 128], BF16) for j in range(2)]
                ph = [[ps4("ph%d_%d" % (a_, b_), [128, 512]) for b_ in range(2)] for a_ in range(2)]
                py = [ps4("py%d" % a_, [128, 512]) for a_ in range(2)]
                cvt_emit(1000)
                for e_ in g.eng:
                    g.eng[e_].wait_ge(cvt_sem, 16 * cvt_total)
                BEb = sb4("BEb", [128, 640]); piota = sb4("piota", [128, 1]); widx = sb4("widx", [128, 640], I32)
                g.dma("sp", BEb[:], be_d.to_broadcast([128, 640]), writes=[BEb])
                g.dma("sp", piota[:], piota_d, writes=[piota])
                g.op("dve", lambda: nc.vector.tensor_scalar(out=BEb[:], in0=BEb[:], scalar1=128.0, scalar2=piota[:, 0:1],
                                                            op0=ALU.mult, op1=ALU.add), [BEb, piota], [BEb])
                g.op("dve", lambda: nc.vector.tensor_copy(out=widx[:], in_=BEb[:]), [BEb], [widx])
                nblk = int(_os.environ.get("P4_NBLK", str(NBLK)))
                yi = 0
                ci = 0

                def expert_block(j2, XT, ntb):
                    for hh in range(2):
                        hs = slice(hh * 128, (hh + 1) * 128)
                        g.mm([(ph[0][hh][:, :ntb], wgs[j2][:, k, hs], XT[:, k, :ntb]) for k in range(8)], [wgs[j2], XT], ph[0][hh])
                        g.mm([(ph[1][hh][:, :ntb], wus[j2][:, k, hs], XT[:, k, :ntb]) for k in range(8)], [wus[j2], XT], ph[1][hh])
                        g.op("act", lambda: nc.scalar.activation(out=sgt[hh][:, :ntb], in_=ph[0][hh][:, :ntb], func=AF.Silu),
                             [ph[0][hh]], [sgt[hh]])
                        g.op("dve", lambda: nc.vector.tensor_tensor(out=act[hh][:, :ntb], in0=ph[1][hh][:, :ntb],
                                                                    in1=sgt[hh][:, :ntb], op=ALU.mult),
                             [ph[1][hh], sgt[hh]], [act[hh]])

                def down(j2, c0, nn):
                    for half in range(2):
                        cs = slice(half * 512, (half + 1) * 512)
                        g.mm([(py[half][:nn, :], act[hh2][:, c0:c0 + nn], wds[j2][:, hh2, cs]) for hh2 in range(2)],
                             [act[0], act[1], wds[j2]], py[half])

                for b4 in range(nblk):
                    j2 = b4 % NW
                    off = bass.IndirectOffsetOnAxis(ap=widx[:, b4:b4 + 1], axis=0)
                    g.idma(wgs[j2][:].rearrange("p k n -> p (k n)"), None, egb[:, :], off, reads=[widx], writes=[wgs[j2]])
                    g.idma(wus[j2][:].rearrange("p k n -> p (k n)"), None, eub[:, :], off, reads=[widx], writes=[wus[j2]])
                    g.idma(wds[j2][:].rearrange("p k n -> p (k n)"), None, edb[:, :], off, reads=[widx], writes=[wds[j2]])
                    Xs = Xsb[b4 % 3]
                    pt_ = ptx[b4 % 2]
                    XT = XTb[b4 % 2]
                    g.dma("sp", Xs[:], Xs_d[b4 * 128:(b4 + 1) * 128, :], writes=[Xs])
                    g.tr([(pt_[:, k, :], Xs[:, k * 128:(k + 1) * 128], ident_b[:, :]) for k in range(8)], [Xs, ident_b], pt_)
                    g.op("act", lambda: nc.scalar.copy(out=XT[:, 0:4, 0:128], in_=pt_[:, 0:4, :]), [pt_], [XT])
                    g.op("dve", lambda: nc.vector.tensor_copy(out=XT[:, 4:8, 0:128], in_=pt_[:, 4:8, :]), [pt_], [XT])
                    expert_block(j2, XT, 128)
                    Ys = Ysb[b4 % 2]
                    down(j2, 0, 128)
                    g.op("act", lambda: nc.scalar.copy(out=Ys[:, 0:512], in_=py[0][:, :]), [py[0]], [Ys])
                    g.op("dve", lambda: nc.vector.tensor_copy(out=Ys[:, 512:1024], in_=py[1][:, :]), [py[1]], [Ys])
                    g.dma("act", Ys_d[b4 * 128:(b4 + 1) * 128, :], Ys[:], reads=[Ys])
                j2 = 0
                g.dma("pool", wgs[j2][:], shg.rearrange("(k p) n -> p k n", p=128), writes=[wgs[j2]])
                g.dma("pool", wus[j2][:], shu.rearrange("(k p) n -> p k n", p=128), writes=[wus[j2]])
                g.dma("pool", wds[j2][:], shd.rearrange("(k p) n -> p k n", p=128), writes=[wds[j2]])
                for t0 in range(0, NT + 1, 4):
                    tl = list(range(t0, min(t0 + 4, NT + 1)))
                    XT = XTb[(t0 // 4) % 2]
                    ntb = sum(128 if i < NT else NS for i in tl)
                    for ti, i in enumerate(tl):
                        g.dma("sp", XT[:, :, ti * 128:(ti + 1) * 128], h2T_d[i].rearrange("p (k t) -> p k t", k=8),
                              writes=[XT] if ti == 0 else [])
                    XT.w = {(("d", i_)): 16 * n_ for i_, (s_, n_) in enumerate(g.dsem) if n_ > 0}
                    expert_block(j2, XT, ntb)
                    for ti, i in enumerate(tl):
                        nn = 128 if i < NT else NS
                        Ysh = Yshb[yi % 2]
                        yi += 1
                        down(j2, ti * 128, nn)
                        g.op("act", lambda: nc.scalar.copy(out=Ysh[:nn, 0:512], in_=py[0][:nn, :]), [py[0]], [Ysh])
                        g.op("dve", lambda: nc.vector.tensor_copy(out=Ysh[:nn, 512:1024], in_=py[1][:nn, :]), [py[1]], [Ysh])
                        g.dma("act", Ysh_d[i * 128:i * 128 + nn, :], Ysh[:nn, :], reads=[Ysh])
                g.barrier()
            p5 = contextlib.ExitStack()
            with p5:
                def sb5(name, shape, dt=F32):
                    return Buf(p5.enter_context(nc.sbuf_tensor("s5_" + name, list(shape), dt)), name)
                GT2 = sb5("GT2", [128, D]); GT2s = sb5("GT2s", [NS, D])
                g.dma("sp", GT2[:], mod_d[0:1, 5 * D:6 * D].to_broadcast([128, D]), writes=[GT2])
                g.dma("sp", GT2s[:], mod_d[1:5, 5 * D:6 * D], writes=[GT2s])
                s8t = [sb5("s8t%d" % j, [128, 8], I32) for j in range(2)]
                w8t = [sb5("w8t%d" % j, [128, 8]) for j in range(2)]
                accf = [sb5("accf%d" % j, [128, D]) for j in range(2)]
                xo = [sb5("xo%d" % j, [128, D]) for j in range(2)]
                Yg = [sb5("Yg%d" % j, [128, D], BF16) for j in range(4)]
                gi_ = 0
                for i in range(NT + 1):
                    nn = 128 if i < NT else NS
                    samp = (i == NT)
                    rows = slice(i * 128, i * 128 + nn)
                    s8 = s8t[i % 2]; w8_ = w8t[i % 2]; af = accf[i % 2]; xb_ = xo[i % 2]
                    gt2 = GT2s if samp else GT2
                    g.dma("sp", s8[:], slot_d[i], writes=[s8])
                    g.dma("sp", w8_[:nn, :], w8_d[i, 0:nn, :], writes=[w8_])
                    g.dma("sp", af[:nn, :], Ysh_d[rows, :], writes=[af])
                    g.dma("sp", xb_[:nn, :], x1_d[rows, :], writes=[xb_])
                    for k8 in range(8):
                        yg = Yg[gi_ % 4]
                        gi_ += 1
                        g.idma(yg[:, :], None, Ys_d[:, :], bass.IndirectOffsetOnAxis(ap=s8[:, k8:k8 + 1], axis=0),
                               reads=[s8], writes=[yg])
                        g.op("dve", lambda: nc.vector.scalar_tensor_tensor(
                            out=af[:nn, :], in0=yg[:nn, :], scalar=w8_[:nn, k8:k8 + 1], in1=af[:nn, :],
                            op0=ALU.mult, op1=ALU.add), [yg, w8_, af], [af])
                    g.op("dve", lambda: nc.vector.tensor_tensor(out=af[:nn, :], in0=af[:nn, :], in1=gt2[:nn, :], op=ALU.mult),
                         [af, gt2], [af])
                    g.op("dve", lambda: nc.vector.tensor_add(out=xb_[:nn, :], in0=xb_[:nn, :], in1=af[:nn, :]), [xb_, af], [xb_])
                    g.dma("sp", ys if samp else y[rows, :], xb_[:nn, :], reads=[xb_], is_output=True)
                g.barrier()
        g.finish()
    return nc, dr


def core_inputs(inp, c):
    m = {}
    m["x"] = np.ascontiguousarray(inp["x_prompt"][c])
    m["xs"] = np.ascontiguousarray(inp["x_sample"][NS * c:NS * c + NS, 0])
    call = np.concatenate([inp["c_prompt"][c:c + 1], inp["c_sample"][NS * c:NS * c + NS]], axis=0)
    m["cin"] = np.ascontiguousarray(call.T.reshape(8, 128, 5).transpose(1, 0, 2))
    m["ada_w"] = inp["ada_w"][0]
    m["ada_b"] = inp["ada_b"]
    m["norm1"] = inp["norm1"]
    m["norm2"] = inp["norm2"]
    m["w_in"] = inp["w_in"][0]
    m["qg"] = np.ascontiguousarray(np.broadcast_to(inp["q_gain"][0][:, None, :], (3, 8, 64)).reshape(1, QW))
    m["kg"] = np.ascontiguousarray(np.broadcast_to(inp["k_gain"][0][:, None, :], (3, 8, 64)).reshape(1, QW))
    m["ident"] = TB["ident"]
    m["bands"] = TB["bands"]
    m["pool_w"] = inp["pool_w"][0]
    m["pool_sc"] = np.ascontiguousarray(inp["pool_scale"][0].reshape(4, 128).T)
    rb = inp["rel_bias"]
    bT = np.zeros((24, 128, 256), np.float32)
    for gi in range(3):
        idx = TB["bkt"][gi][TB["jT"]]
        for h in range(8):
            bT[gi * 8 + h] = rb[idx, gi * 8 + h]
    m["biasT"] = bT
    m["maskT"] = TB["maskT"]
    caches = {"ck128": inp["cache_k_w128"], "cv128": inp["cache_v_w128"], "ck512": inp["cache_k_w512"],
              "cv512": inp["cache_v_w512"], "ck2048": inp["cache_k_w2048"], "cv2048": inp["cache_v_w2048"]}
    for nm, arr in caches.items():
        m[nm] = np.ascontiguousarray(arr[0, NS * c:NS * c + NS].reshape(NS, arr.shape[2], 512))
    m["stp"] = np.ascontiguousarray(inp["state_pool"][0, NS * c:NS * c + NS])
    m["bias0"] = np.ascontiguousarray(rb[0:1, :])
    bs = np.zeros((3, 128, 8), np.float32)
    for gi in range(3):
        bs[gi] = rb[TB["bkt"][gi][128 - np.arange(128)], gi * 8:(gi + 1) * 8]
    m["bias_s"] = bs
    m["bd"] = TB["bd"]
    m["bandS"] = TB["bandS"]
    m["diagS"] = TB["diagS"]
    m["UT"] = TB["UT"]
    m["trash"] = TB["trash"]
    m["tri"] = TB["tri"]
    m["bstart"] = TB["bstart"]
    m["piota"] = TB["piota"]
    m["w_br_a"] = inp["w_br_a"][0]
    m["w_br_b"] = inp["w_br_b"][0]
    m["w_out"] = inp["w_out"][0]
    m["router_w"] = inp["router_w"][0]
    m["router_b"] = inp["router_bias"]
    if SPARSE:
        m["egl"] = inp["_egl"]
        m["eul"] = inp["_eul"]
        m["edl"] = inp["_edl"]
    else:
        m["eg"] = inp["exp_w_gate"][0]
        m["eu"] = inp["exp_w_up"][0]
        m["ed"] = inp["exp_w_down"][0]
    m["shg"] = inp["sh_w_gate"][0]
    m["shu"] = inp["sh_w_up"][0]
    m["shd"] = inp["sh_w_down"][0]
    return m


_NC_CACHE = {}


def prep_experts(inp):
    if SPARSE and "_egl" not in inp:
        inp["_egl"] = np.ascontiguousarray(inp["exp_w_gate"][0].reshape(NE, 8, 128, EH).transpose(0, 2, 1, 3)).reshape(NE * 128, 8 * EH)
        inp["_eul"] = np.ascontiguousarray(inp["exp_w_up"][0].reshape(NE, 8, 128, EH).transpose(0, 2, 1, 3)).reshape(NE * 128, 8 * EH)
        inp["_edl"] = np.ascontiguousarray(inp["exp_w_down"][0].reshape(NE, 2, 128, D).transpose(0, 2, 1, 3)).reshape(NE * 128, 2 * D)


def kernel(**inputs):
    inp = {k: np.asarray(v) for k, v in inputs.items()}
    prep_experts(inp)
    if "nc" not in _NC_CACHE:
        _NC_CACHE["nc"] = build_nc(stages=("all",), debug=False)
    nc, dr = _NC_CACHE["nc"]
    in_maps = []
    for c in range(NCORES):
        m = core_inputs(inp, c)
        in_maps.append({k: np.ascontiguousarray(v, dtype=np.float32) for k, v in m.items() if k in dr})
    res = run_bass_kernel_spmd(nc, in_maps, core_ids=list(range(NCORES)))
    R = res.results
    f = np.float32
    y_prompt = np.stack([R[c]["y"] for c in range(NCORES)], 0).astype(f)
    y_sample = np.concatenate([R[c]["ys"] for c in range(NCORES)], 0).reshape(NCORES * NS, 1, D).astype(f)
    outs = [y_prompt, y_sample]
    for (w, _) in GROUPS:
        for nm in ("pk", "pv"):
            outs.append(np.stack([R[c]["%s%d" % (nm, w)] for c in range(NCORES)], 0).reshape(1, NCORES, w, 8, 64).astype(f))
    outs.append(np.stack([R[c]["ppool"] for c in range(NCORES)], 0).reshape(1, NCORES, 15, 512).astype(f))
    for (w, _) in GROUPS:
        for nm in ("sk", "sv"):
            outs.append(np.concatenate([R[c]["%s%d" % (nm, w)] for c in range(NCORES)], 0).reshape(1, NCORES * NS, w, 8, 64).astype(f))
    outs.append(np.concatenate([R[c]["spool"] for c in range(NCORES)], 0).reshape(1, NCORES * NS, 15, 512).astype(f))
    return tuple(outs)
```

```python
import contextlib
import os as _os
import numpy as np
import concourse.bass as bass
import concourse.mybir as mybir
from concourse.bass_utils import run_bass_kernel_spmd

F32 = mybir.dt.float32
BF16 = mybir.dt.bfloat16
AF = mybir.ActivationFunctionType
ALU = mybir.AluOpType
AX = mybir.AxisListType

NCORES = 8
D = 1024
S = 4096
NT = S // 128
NS = 4
NTOK = S + NS
INC = 7168
QW = 1536
EPS = 1e-6
GROUPS = ((128, 1), (512, 4), (2048, 16))
NE = 256
EH = 256
PAST = 16384
NBLK = 513
NSLOT = NBLK * 128
SPARSE = _os.environ.get('MOE_DENSE') is None
I32 = mybir.dt.int32
VS = 68


class Buf:
    def __init__(self, t, name=""):
        self.t = t
        self.name = name
        self.w = {}
        self.r = {}
        self.excl = name.startswith("P:")

    def __getitem__(self, idx):
        return self.t[idx]


class G:
    def __init__(self, nc, es, n_dma_sems=40):
        self.nc = nc
        self.es = es
        self.eng = {"pe": nc.tensor, "act": nc.scalar, "dve": nc.vector, "pool": nc.gpsimd, "sp": nc.sync}
        self.sem = {}
        self.cnt = {}
        self.seen = {e: {} for e in self.eng}
        for e in self.eng:
            self.sem[e] = es.enter_context(nc.semaphore("sem_" + e))
            self.cnt[e] = 0
        self.dsem = []
        for i in range(n_dma_sems):
            self.dsem.append([es.enter_context(nc.semaphore("dsem%d" % i)), 0])
        self.dnext = 0
        self.out_tickets = []

    def _semof(self, key):
        if isinstance(key, tuple):
            return self.dsem[key[1]][0]
        return self.sem[key]

    def wait(self, e, ticket):
        key, val = ticket
        if key == e and e == "pe":
            return
        if self.seen[e].get(key, 0) >= val:
            return
        self.eng[e].wait_ge(self._semof(key), val)
        self.seen[e][key] = val

    def _deps(self, reads, writes):
        deps = {}
        for b in reads:
            for k, v in b.w.items():
                deps[k] = max(deps.get(k, 0), v)
            if b.excl:
                for k, v in b.r.items():
                    deps[k] = max(deps.get(k, 0), v)
        for b in writes:
            for k, v in b.w.items():
                deps[k] = max(deps.get(k, 0), v)
            for k, v in b.r.items():
                deps[k] = max(deps.get(k, 0), v)
        return deps

    def _mark(self, t, reads, writes):
        for b in reads:
            b.r[t[0]] = max(b.r.get(t[0], 0), t[1])
        for b in writes:
            b.w = {t[0]: t[1]}
            b.r = {}

    def barrier(self):
        for e in self.eng:
            for i, (sem, n) in enumerate(self.dsem):
                if n > 0:
                    self.wait(e, (("d", i), 16 * n))
            for e2 in self.eng:
                if e2 != e and self.cnt[e2] > 0:
                    self.wait(e, (e2, self.cnt[e2]))

    def op(self, e, fn, reads=(), writes=()):
        for k, v in self._deps(reads, writes).items():
            self.wait(e, (k, v))
        inst = fn()
        self.cnt[e] += 1
        inst.then_inc(self.sem[e], 1)
        t = (e, self.cnt[e])
        self._mark(t, reads, writes)
        return t

    def mm(self, mms, reads, out):
        for k, v in self._deps(reads, [out]).items():
            self.wait("pe", (k, v))
        n = len(mms)
        inst = None
        for i, (o, l, r) in enumerate(mms):
            inst = self.nc.tensor.matmul(o, lhsT=l, rhs=r, start=(i == 0), stop=(i == n - 1))
        self.cnt["pe"] += 1
        inst.then_inc(self.sem["pe"], 1)
        t = ("pe", self.cnt["pe"])
        self._mark(t, reads, [out])
        return t

    def tr(self, trs, reads, out):
        for k, v in self._deps(reads, [out]).items():
            self.wait("pe", (k, v))
        inst = None
        for (o, i_, idn) in trs:
            inst = self.nc.tensor.transpose(o, i_, idn)
        self.cnt["pe"] += 1
        inst.then_inc(self.sem["pe"], 1)
        t = ("pe", self.cnt["pe"])
        self._mark(t, reads, [out])
        return t

    def dma(self, q, out, in_, reads=(), writes=(), is_output=False, **kw):
        for k, v in self._deps(reads, writes).items():
            self.wait(q, (k, v))
        i = self.dnext
        self.dnext = (self.dnext + 1) % len(self.dsem)
        sem, n = self.dsem[i]
        if n > 0:
            self.wait(q, (("d", i), 16 * n))
        self.eng[q].dma_start(out=out, in_=in_, **kw).then_inc(sem, 16)
        self.dsem[i][1] = n + 1
        t = (("d", i), 16 * (n + 1))
        self._mark(t, reads, writes)
        if is_output:
            self.out_tickets.append(t)
        return t

    def idma(self, out, out_off, in_, in_off, reads=(), writes=()):
        q = "pool"
        for k, v in self._deps(reads, writes).items():
            self.wait(q, (k, v))
        i = self.dnext
        self.dnext = (self.dnext + 1) % len(self.dsem)
        sem, n = self.dsem[i]
        if n > 0:
            self.wait(q, (("d", i), 16 * n))
        self.nc.gpsimd.indirect_dma_start(out=out, out_offset=out_off, in_=in_, in_offset=in_off).then_inc(sem, 16)
        self.dsem[i][1] = n + 1
        t = (("d", i), 16 * (n + 1))
        self._mark(t, reads, writes)
        return t

    def finish(self):
        for i, (sem, n) in enumerate(self.dsem):
            if n > 0:
                self.wait("sp", (("d", i), 16 * n))
        for e in ("pe", "act", "dve", "pool"):
            if self.cnt[e] > 0:
                self.wait("sp", (e, self.cnt[e]))


def t5_bucket(dist):
    exact = 16
    d = np.asarray(dist)
    large = exact + (np.log(np.maximum(d, 1) / exact) / np.log(2048 / exact) * (32 - exact)).astype(np.int32)
    large = np.minimum(large, 31)
    return np.where(d < exact, d, large).astype(np.int32)


def static_tables():
    tb = {}
    k = np.arange(128)[:, None]
    qq = np.arange(256)[None, :]
    j = qq - k
    valid = (j >= 0) & (j <= 128)
    tb["maskT"] = valid.astype(np.float32)
    tb["jT"] = np.clip(j, 0, 128)
    tb["bkt"] = [t5_bucket(np.arange(129) * dil) for (_, dil) in GROUPS]
    tb["ident"] = np.eye(128, dtype=np.float32)
    bands = np.zeros((3, 4, 128, 128), np.float32)
    for gi, w in enumerate((2, 4, 8, 16)):
        for t in range(128):
            for tp in range(t - w + 1, t + 1):
                if tp >= 0:
                    bands[0, gi, tp, t] += 1.0 / w
                    bands[2, gi, tp, t] += 1.0 / min(t + 1, w)
                else:
                    bands[1, gi, 128 + tp, t] += 1.0 / w
            bands[0, gi, t, t] -= 1.0
            bands[2, gi, t, t] -= 1.0
    tb["bands"] = bands
    tb["UT"] = np.triu(np.ones((128, 128), np.float32), 1)
    tb["trash"] = np.repeat((NSLOT + 1.0 + np.arange(128, dtype=np.float32))[:, None], 8, 1)
    ee = np.arange(NE)[None, None, :]
    tb["tri"] = ((np.arange(2)[:, None, None] * 128 + np.arange(128)[None, :, None]) <= ee).astype(np.float32)
    tb["bstart"] = ((np.arange(5)[None, :] * 128 + np.arange(128)[:, None]) * 128).astype(np.float32)
    tb["piota"] = np.arange(128, dtype=np.float32).reshape(128, 1)
    bd = np.zeros((8, 8, 65), np.float32)
    for h in range(8):
        bd[h, h, :] = 1.0
    tb["bd"] = bd
    bandS = np.zeros((4, 60, NS), np.float32)
    diagS = np.zeros((4, NS, NS), np.float32)
    for gi, w in enumerate((2, 4, 8, 16)):
        for b in range(NS):
            for i in range(15 - (w - 1), 15):
                bandS[gi, b * 15 + i, b] = 1.0 / w
            diagS[gi, b, b] = 1.0 / w - 1.0
    tb["bandS"] = bandS
    tb["diagS"] = diagS
    return tb


TB = static_tables()


def build_nc(stages=("all",), debug=False):
    nc = bass.Bass("TRN2", target_bir_lowering=False)
    es = contextlib.ExitStack()
    dr = {}

    def din(name, shape, dt=F32):
        dr[name] = nc.dram_tensor(name, list(shape), dt, kind="ExternalInput").ap()
        return dr[name]

    def dout(name, shape, dt=F32):
        dr[name] = nc.dram_tensor(name, list(shape), dt, kind="ExternalOutput").ap()
        return dr[name]

    def dscr(name, shape, dt=F32):
        kind = "ExternalOutput" if debug else "Internal"
        dr[name] = nc.dram_tensor(name, list(shape), dt, kind=kind).ap()
        return dr[name]

    x = din("x", [S, D])
    xs = din("xs", [NS, D])
    cin = din("cin", [128, 8, 5])
    ada_w = din("ada_w", [D, 6 * D])
    ada_b = din("ada_b", [1, 6 * D])
    norm1 = din("norm1", [1, D])
    norm2 = din("norm2", [1, D])
    w_in = din("w_in", [D, INC])
    qg = din("qg", [1, QW])
    kg = din("kg", [1, QW])
    ident_d = din("ident", [128, 128])
    bands_d = din("bands", [3, 4, 128, 128])
    pool_w = din("pool_w", [4, 128, 128])
    pool_sc = din("pool_sc", [128, 4])
    y = dout("y", [S, D])
    ys = dout("ys", [NS, D])
    pk = [dout("pk%d" % w, [w, 512]) for (w, _) in GROUPS]
    pv = [dout("pv%d" % w, [w, 512]) for (w, _) in GROUPS]
    ppool = dout("ppool", [15, 512])
    mod_d = dscr("mod_d", [5, 6 * D])
    QT_d = dscr("QT_d", [QW, S], BF16)
    KT_d = dscr("KT_d", [QW, S], BF16)
    V1_d = dscr("V1_d", [S, 24 * VS], BF16)
    qkvs_d = dscr("qkvs_d", [NS, 3 * QW])
    gates_d = dscr("gates_d", [NTOK, 2048], BF16)
    ybT_d = dscr("ybT_d", [NT + 1, 128, 512], BF16)
    us_d = dscr("us_d", [NS, 512])
    biasT_d = din("biasT", [24, 128, 256])
    maskT_d = din("maskT", [128, 256])
    A_d = dscr("A_d", [3, S, 520])
    w_br_a = din("w_br_a", [512, D])
    w_br_b = din("w_br_b", [512, D])
    w_out = din("w_out", [D, D])
    router_w = din("router_w", [D, NE])
    router_b = din("router_b", [1, NE])
    if not SPARSE:
        eg = din("eg", [NE, D, EH])
        eu = din("eu", [NE, D, EH])
        ed = din("ed", [NE, EH, D])
    shg = din("shg", [D, EH])
    shu = din("shu", [D, EH])
    shd = din("shd", [EH, D])
    x1_d = dscr("x1_d", [NTOK, D])
    h2T_d = dscr("h2T_d", [NT + 1, 128, 8 * 128], BF16)
    Wd_d = dscr("Wd_d", [NT + 1, 128, NE])
    oas_d = dscr("oas_d", [NS, 512])
    Xs_d = dscr("Xs_d", [NSLOT + 128, D], BF16)
    Ys_d = dscr("Ys_d", [NSLOT + 128, D], BF16)
    Ysh_d = dscr("Ysh_d", [NTOK, D])
    slot_d = dscr("slot_d", [NT + 1, 128, 8], I32)
    w8_d = dscr("w8_d", [NT + 1, 128, 8])
    pos_d = dscr("pos_d", [NT + 1, 128, NE])
    h2b_d = dscr("h2b_d", [NT + 1, 128, D], BF16)
    be_d = dscr("be_d", [1, 640])
    egb = dscr("egb", [NE * 128, 8 * EH], BF16)
    eub = dscr("eub", [NE * 128, 8 * EH], BF16)
    edb = dscr("edb", [NE * 128, 2 * D], BF16)
    tri_d = din("tri", [2, 128, NE])
    bstart_d = din("bstart", [128, 5])
    piota_d = din("piota", [128, 1])
    egl = din("egl", [NE * 128, 8 * EH])
    eul = din("eul", [NE * 128, 8 * EH])
    edl = din("edl", [NE * 128, 2 * D])
    UT_d = din("UT", [128, 128])
    trash_d = din("trash", [128, 8])
    As_d = dscr("As_d", [NS, 3, 520])
    ck = [din("ck%d" % w, [NS, w, 512]) for (w, _) in GROUPS]
    cv = [din("cv%d" % w, [NS, w, 512]) for (w, _) in GROUPS]
    stp = din("stp", [NS, 15, 512])
    bias0_d = din("bias0", [1, 24])
    bias_s_d = din("bias_s", [3, 128, 8])
    bd_d = din("bd", [8, 8, 65])
    bandS_d = din("bandS", [4, 60, NS])
    diagS_d = din("diagS", [4, NS, NS])
    sk = [dout("sk%d" % w, [NS, w, 512]) for (w, _) in GROUPS]
    sv = [dout("sv%d" % w, [NS, w, 512]) for (w, _) in GROUPS]
    spool = dout("spool", [NS, 15, 512])

    with es:
        g = G(nc, es)
        cvt_sem = es.enter_context(nc.semaphore("cvt_sem"))
        cvt_list = []
        if SPARSE:
            for (src_, dst_) in ((egl, egb), (eul, eub), (edl, edb)):
                for r0_ in range(0, NE * 128, 1024):
                    cvt_list.append((src_[r0_:r0_ + 1024, :], dst_[r0_:r0_ + 1024, :]))
        cvt_total = len(cvt_list)

        def cvt_emit(nmax):
            for _ in range(nmax):
                if cvt_list:
                    s_, d_ = cvt_list.pop(0)
                    nc.gpsimd.dma_start(out=d_, in_=s_).then_inc(cvt_sem, 16)

        def sb(name, shape, dt=F32):
            return Buf(es.enter_context(nc.sbuf_tensor("s_" + name, list(shape), dt)), name)

        def ps(name, shape, dt=F32):
            return Buf(es.enter_context(nc.psum_tensor("p_" + name, list(shape), dt)), name)

        ident_f = sb("ident_f", [128, 128])
        g.dma("sp", ident_f[:], ident_d, writes=[ident_f])
        nhalf = sb("nhalf", [128, 8])
        g.op("dve", lambda: nc.vector.memset(nhalf[:], -0.5), [], [nhalf])
        ident_b = sb("ident_b", [128, 128], BF16)
        g.dma("pool", ident_b[:], ident_d, writes=[ident_b])

        if True:
            p0 = contextlib.ExitStack()
            with p0:
                def sb0(name, shape, dt=F32):
                    return Buf(p0.enter_context(nc.sbuf_tensor("s_" + name, list(shape), dt)), name)
                cT = sb0("cT", [128, 8, 5])
                sg = sb0("sg", [128, 8, 5])
                g.dma("sp", cT[:], cin, writes=[cT])
                g.op("act", lambda: nc.scalar.activation(out=sg[:], in_=cT[:], func=AF.Sigmoid), [cT], [sg])
                g.op("dve", lambda: nc.vector.tensor_mul(out=sg[:], in0=cT[:], in1=sg[:]), [cT, sg], [sg])
                adab = sb0("adab", [5, 6 * D])
                g.dma("act", adab[:], ada_b.to_broadcast([5, 6 * D]), writes=[adab])
                modsb = sb0("modsb", [5, 6 * D])
                wbuf = [sb0("adaw%d" % i, [128, 8, 512]) for i in range(2)]
                pmod = [Buf(p0.enter_context(nc.psum_tensor("pmod%d" % i, [5, 512], F32)), "P:pmod") for i in range(2)]
                for cg in range(12):
                    wb = wbuf[cg % 2]
                    g.dma("sp" if cg % 2 == 0 else "act", wb[:],
                          ada_w[:, cg * 512:(cg + 1) * 512].rearrange("(k p) n -> p k n", p=128), writes=[wb])
                    pm = pmod[cg % 2]
                    g.mm([(pm[:], sg[:, k, :], wb[:, k, :]) for k in range(8)], [sg, wb], pm)
                    g.op("dve", lambda pm=pm, cg=cg: nc.vector.tensor_add(
                        out=modsb[:, cg * 512:(cg + 1) * 512], in0=pm[:], in1=adab[:, cg * 512:(cg + 1) * 512]),
                        [pm, adab], [modsb])
                g.dma("sp", mod_d, modsb[:], reads=[modsb])
                g.barrier()

        if "p1" in stages or "all" in stages:
            p1 = contextlib.ExitStack()
            with p1:
                def sb1(name, shape, dt=F32):
                    return Buf(p1.enter_context(nc.sbuf_tensor("s_" + name, list(shape), dt)), name)

                def ps1(name, shape, dt=F32):
                    return Buf(p1.enter_context(nc.psum_tensor("p_" + name, list(shape), dt)), "P:" + name)

                w_in_sb = sb1("w_in_sb", [128, 8, INC], BF16)
                wchunks = [Buf(None, "wc%d" % i) for i in range(14)]
                _skip = _os.environ.get('SKIP', '').split(',')
                for cg in range(14 if 'win' not in _skip else 0):
                    g.dma("pool", w_in_sb[:, :, cg * 512:(cg + 1) * 512],
                          w_in[:, cg * 512:(cg + 1) * 512].rearrange("(k p) n -> p k n", p=128),
                          writes=[wchunks[cg]])
                G1 = sb1("G1", [128, D]); SH1 = sb1("SH1", [128, D])
                G1s = sb1("G1s", [NS, D]); SH1s = sb1("SH1s", [NS, D])
                n1b = sb1("n1b", [128, D])
                g.dma("sp", n1b[:], norm1.to_broadcast([128, D]), writes=[n1b])
                g.dma("sp", G1[:], mod_d[0:1, D:2 * D].to_broadcast([128, D]), writes=[G1])
                g.dma("sp", SH1[:], mod_d[0:1, 0:D].to_broadcast([128, D]), writes=[SH1])
                g.dma("sp", G1s[:], mod_d[1:5, D:2 * D], writes=[G1s])
                g.dma("sp", SH1s[:], mod_d[1:5, 0:D], writes=[SH1s])
                g.op("dve", lambda: nc.vector.scalar_tensor_tensor(
                    out=G1[:], in0=G1[:], scalar=1.0, in1=n1b[:], op0=ALU.add, op1=ALU.mult), [G1, n1b], [G1])
                g.op("dve", lambda: nc.vector.scalar_tensor_tensor(
                    out=G1s[:], in0=G1s[:], scalar=1.0, in1=n1b[:NS, :], op0=ALU.add, op1=ALU.mult), [G1s, n1b], [G1s])
                qgb = sb1("qgb", [128, QW]); kgb = sb1("kgb", [128, QW])
                g.dma("sp", qgb[:], qg.to_broadcast([128, QW]), writes=[qgb])
                g.dma("sp", kgb[:], kg.to_broadcast([128, QW]), writes=[kgb])
                g.op("dve", lambda: nc.vector.tensor_scalar_mul(out=qgb[:], in0=qgb[:], scalar1=0.125), [qgb], [qgb])
                bands = sb1("bands", [128, 12, 128])
                if 'bands' not in _skip:
                    g.dma("sp", bands[:], bands_d.rearrange("a g p t -> p (a g) t"), writes=[bands])
                pw_sb = sb1("pw_sb", [128, 4, 128], BF16)
                if 'poolw' not in _skip:
                    g.dma("pool", pw_sb[:], pool_w.rearrange("g c d -> c g d"), writes=[pw_sb])
                psc = sb1("psc", [128, 4])
                g.dma("sp", psc[:], pool_sc, writes=[psc])

                xt = [sb1("xt%d" % i, [128, D]) for i in range(2)]
                sq = sb1("sq", [128, D])
                hb = sb1("hb", [128, D], BF16)
                hT = sb1("hT", [128, 8, 128], BF16)
                ssq = sb1("ssq", [128, 1]); rstd = sb1("rstd", [128, 1])
                ss8 = sb1("ss8", [128, 8]); rs8 = sb1("rs8", [128, 8])
                qf = [sb1("qf%d" % i, [128, 512]) for i in range(2)]
                kf = [sb1("kf%d" % i, [128, 512]) for i in range(2)]
                vf = [sb1("vf%d" % i, [128, 512]) for i in range(2)]
                V1 = sb1("V1", [128, 24, VS], BF16)
                if 'memv1' not in _skip:
                    g.op("dve", lambda: nc.vector.memset(V1[:], 1.0), [], [V1])
                QTst = sb1("QTst", [128, 12, 128], BF16)
                KTst = sb1("KTst", [128, 12, 128], BF16)
                ut = [sb1("ut%d" % i, [128, 512]) for i in range(2)]
                gts = sb1("gts", [128, 2048], BF16)
                pooledT = sb1("pooledT", [128, 4, 128], BF16)
                ybT = sb1("ybT", [128, 4, 128], BF16)
                pz = [ps1("pz%d" % i, [128, 512]) for i in range(3)]
                ptr = ps1("ptr", [128, 8, 128], BF16)
                ptq = ps1("ptq", [128, 4, 128])
                ppl = ps1("ppl", [128, 4, 128])
                pmx = ps1("pmx", [128, 4, 128])

                def load_x(i):
                    if i < NT:
                        g.dma("sp", xt[i % 2][:], x[i * 128:(i + 1) * 128, :], writes=[xt[i % 2]])
                    else:
                        g.dma("sp", xt[i % 2][:NS, :], xs, writes=[xt[i % 2]])

                load_x(0)
                pzi = 0
                _tl = _os.environ.get('P1_TILES')
                _tiles = list(range(NT + 1)) if _tl is None else [int(v) for v in _tl.split(',') if v != '']
                for i in _tiles:
                    n = 128 if i < NT else NS
                    samp = (i == NT)
                    if i + 1 <= NT and _tl is None:
                        load_x(i + 1)
                    if _tl is not None and i != 0:
                        load_x(i)
                    xb = xt[i % 2]
                    Gm, Sm = (G1s, SH1s) if samp else (G1, SH1)
                    g.op("act", lambda: nc.scalar.activation(out=sq[:n, :], in_=xb[:n, :], func=AF.Square,
                                                             accum_out=ssq[:n, :]), [xb], [sq, ssq])
                    g.op("dve", lambda: nc.vector.tensor_scalar(out=rstd[:n, :], in0=ssq[:n, :], scalar1=1.0 / D,
                                                                scalar2=EPS, op0=ALU.mult, op1=ALU.add), [ssq], [rstd])
                    g.op("pool", lambda: nc.gpsimd.tensor_tensor(out=rstd[:n, :], in0=rstd[:n, :], in1=nhalf[:n, 0:1],
                                                                 op=ALU.pow), [rstd, nhalf], [rstd])
                    g.op("dve", lambda: nc.vector.scalar_tensor_tensor(
                        out=sq[:n, :], in0=xb[:n, :], scalar=rstd[:n, :], in1=Gm[:n, :], op0=ALU.mult, op1=ALU.mult),
                        [xb, rstd, Gm], [sq])
                    g.op("dve", lambda: nc.vector.tensor_add(out=hb[:n, :], in0=sq[:n, :], in1=Sm[:n, :]), [sq, Sm], [hb])
                    LVL = int(_os.environ.get('P1_LVL', '99'))
                    if LVL < 2:
                        continue
                    g.tr([(ptr[:, k, :n], hb[:n, k * 128:(k + 1) * 128], ident_b[:n, :n]) for k in range(8)],
                         [hb, ident_b], ptr)
                    g.op("act", lambda: nc.scalar.copy(out=hT[:, :, :n], in_=ptr[:, :, :n]), [ptr], [hT])
                    if LVL < 3:
                        continue
                    ucur = ut[i % 2]
                    pend = [None]
                    for cg in range(14):
                        pzb = pz[pzi % 3]
                        pzi += 1
                        g.mm([(pzb[:n, :], hT[:, k, :n], w_in_sb[:, k, cg * 512:(cg + 1) * 512]) for k in range(8)],
                             [hT, wchunks[cg]], pzb)
                        if LVL < 4:
                            continue
                        if cg < 6:
                            gi = cg % 3
                            isq = cg < 3
                            dst = (qf if isq else kf)[gi % 2]
                            gain = qgb if isq else kgb
                            g.op("act", lambda: nc.scalar.copy(out=dst[:n, :], in_=pzb[:n, :]), [pzb], [dst])
                            g.op("act", lambda: nc.scalar.activation(out=sq[:n, :512], in_=dst[:n, :], func=AF.Square),
                                 [dst], [sq])
                            g.op("dve", lambda: nc.vector.tensor_reduce(
                                out=ss8[:n, :], in_=sq[:n, :512].rearrange("p (h e) -> p h e", e=64),
                                axis=AX.X, op=ALU.add), [sq], [ss8])
                            g.op("dve", lambda: nc.vector.tensor_scalar(out=rs8[:n, :], in0=ss8[:n, :], scalar1=1.0 / 64,
                                                                        scalar2=EPS, op0=ALU.mult, op1=ALU.add), [ss8], [rs8])
                            g.op("pool", lambda: nc.gpsimd.tensor_tensor(out=rs8[:n, :], in0=rs8[:n, :], in1=nhalf[:n, :],
                                                                         op=ALU.pow), [rs8, nhalf], [rs8])
                            g.op("dve", lambda: nc.vector.tensor_tensor(
                                out=dst[:n, :].rearrange("p (h e) -> p h e", e=64),
                                in0=dst[:n, :].rearrange("p (h e) -> p h e", e=64),
                                in1=rs8[:n, :].unsqueeze(2).to_broadcast([n, 8, 64]), op=ALU.mult), [dst, rs8], [dst])
                            g.op("dve", lambda: nc.vector.tensor_mul(out=dst[:n, :], in0=dst[:n, :],
                                                                     in1=gain[:n, gi * 512:(gi + 1) * 512]), [dst, gain], [dst])

                            def finish_qk(dst=dst, isq=isq, gi=gi, n=n, samp=samp, i=i):
                                if not samp:
                                    g.tr([(ptq[:, j, :n], dst[:n, j * 128:(j + 1) * 128], ident_f[:n, :n]) for j in range(4)],
                                         [dst, ident_f], ptq)
                                    st = QTst if isq else KTst
                                    g.op("act", lambda: nc.scalar.copy(out=st[:, gi * 4:(gi + 1) * 4, :], in_=ptq[:]), [ptq], [st])
                                    if not isq:
                                        W = GROUPS[gi][0]
                                        r0 = i * 128 - (S - W)
                                        if r0 >= 0:
                                            g.dma("sp", pk[gi][r0:r0 + 128, :], dst[:], reads=[dst], is_output=True)
                                else:
                                    off = (0 if isq else QW) + gi * 512
                                    g.dma("sp", qkvs_d[:, off:off + 512], dst[:NS, :], reads=[dst])
                            if pend[0] is not None:
                                pend[0]()
                            pend[0] = finish_qk
                            continue
                        if pend[0] is not None:
                            pend[0]()
                            pend[0] = None
                        if LVL < 6:
                            continue
                        if cg < 9:
                            gi = cg - 6
                            dst = vf[gi % 2]
                            if 'vact' not in _skip:
                                g.op("dve", lambda: nc.vector.tensor_copy(out=dst[:n, :], in_=pzb[:n, :]), [pzb], [dst])
                            if not samp and 'vcopy' not in _skip:
                                g.op("act", lambda: nc.scalar.copy(
                                    out=V1[:, gi * 8:(gi + 1) * 8, 0:64], in_=dst[:, :].rearrange("p (h e) -> p h e", e=64)),
                                    [dst], [V1])
                                W = GROUPS[gi][0]
                                r0 = i * 128 - (S - W)
                                if r0 >= 0:
                                    g.dma("sp", pv[gi][r0:r0 + 128, :], dst[:], reads=[dst], is_output=True)
                            else:
                                off = 2 * QW + gi * 512
                                g.dma("sp", qkvs_d[:, off:off + 512], dst[:NS, :], reads=[dst])
                        elif LVL < 7:
                            continue
                        elif cg == 9:
                            g.op("act", lambda: nc.scalar.copy(out=ucur[:n, :], in_=pzb[:n, :]), [pzb], [ucur])
                        else:
                            c0 = (cg - 10) * 512
                            g.op("act", lambda: nc.scalar.activation(out=gts[:n, c0:c0 + 512], in_=pzb[:n, :],
                                                                     func=AF.Sigmoid), [pzb], [gts])
                    if LVL < 8:
                        continue
                    if not samp:
                        g.dma("sp", QT_d[:, i * 128:(i + 1) * 128].rearrange("(j p) t -> p j t", p=128), QTst[:], reads=[QTst])
                        g.dma("sp", KT_d[:, i * 128:(i + 1) * 128].rearrange("(j p) t -> p j t", p=128), KTst[:], reads=[KTst])
                        g.dma("sp", V1_d[i * 128:(i + 1) * 128, :], V1[:].rearrange("p a b -> p (a b)"), reads=[V1])
                        g.dma("sp", gates_d[i * 128:(i + 1) * 128, :], gts[:], reads=[gts])
                        if i == NT - 1:
                            g.dma("sp", ppool, ucur[113:128, :], reads=[ucur], is_output=True)
                        if LVL < 9:
                            continue
                        uprev = ut[(i + 1) % 2]
                        for gi in range(4):
                            cs = slice(gi * 128, (gi + 1) * 128)
                            if i == 0:
                                mms = [(ppl[:, gi, :], ucur[:, cs], bands[:, 8 + gi, :])]
                            else:
                                mms = [(ppl[:, gi, :], ucur[:, cs], bands[:, gi, :]),
                                       (ppl[:, gi, :], uprev[:, cs], bands[:, 4 + gi, :])]
                            g.mm(mms, [ucur, uprev, bands], ppl)
                        g.op("dve", lambda: nc.vector.tensor_copy(out=pooledT[:], in_=ppl[:]), [ppl], [pooledT])
                        for gi in range(4):
                            g.mm([(pmx[:, gi, :], pw_sb[:, gi, :], pooledT[:, gi, :])], [pw_sb, pooledT], pmx)
                        for gi in range(4):
                            g.op("act", lambda gi=gi: nc.scalar.activation(out=ybT[:, gi, :], in_=pmx[:, gi, :], func=AF.Copy,
                                                                          scale=psc[:, gi:gi + 1]), [pmx, psc], [ybT])
                        g.dma("sp", ybT_d[i], ybT[:].rearrange("p a b -> p (a b)"), reads=[ybT])
                        cvt_emit(3)
                    else:
                        g.dma("sp", gates_d[S:S + NS, :], gts[:NS, :], reads=[gts])
                        g.dma("sp", us_d, ucur[:NS, :], reads=[ucur])
                g.barrier()
        if "p2" in stages or "all" in stages:
            p2 = contextlib.ExitStack()
            with p2:
                def sb2(name, shape, dt=F32):
                    return Buf(p2.enter_context(nc.sbuf_tensor("s2_" + name, list(shape), dt)), name)

                def ps2(name, shape, dt=F32):
                    return Buf(p2.enter_context(nc.psum_tensor("p2_" + name, list(shape), dt)), "P:" + name)

                Eb = sb2("Eb", [128, 24, 256], BF16)
                mk = sb2("mk", [128, 256])
                g.dma("sp", mk[:], maskT_d, writes=[mk])
                for c4 in range(6):
                    bt = sb2("bt%d" % c4, [128, 4, 256])
                    g.dma("sp", bt[:], biasT_d[c4 * 4:(c4 + 1) * 4].rearrange("a k q -> k a q"), writes=[bt])
                    g.op("act", lambda: nc.scalar.activation(out=bt[:], in_=bt[:], func=AF.Exp), [bt], [bt])
                    g.op("dve", lambda: nc.vector.tensor_tensor(
                        out=Eb[:, c4 * 4:(c4 + 1) * 4, :], in0=bt[:], in1=mk[:].unsqueeze(1).to_broadcast([128, 4, 256]),
                        op=ALU.mult), [bt, mk], [Eb])
                QTg = sb2("QTg", [128, 4, S], BF16)
                KTg = sb2("KTg", [128, 4, S], BF16)
                V1g = sb2("V1g", [128, 32, 8 * VS], BF16)
                PT = [[sb2("PT%d_%d" % (h, j), [128, 256], BF16) for j in range(2)] for h in range(8)]
                pe32 = [sb2("pe32_%d" % j, [128, 256]) for j in range(2)]
                oacc = [sb2("oacc%d" % j, [128, 8, 65]) for j in range(2)]
                pS = [ps2("pS%d" % j, [128, 512]) for j in range(4)]
                pO = [[ps2("pO%d_%d" % (j, hh), [128, 512]) for hh in range(2)] for j in range(2)]

                def sl(s0, c, st):
                    return slice(s0, s0 + st * (c - 1) + 1, st)

                si = 0
                oi = 0
                for gi, (W, dil) in enumerate(GROUPS):
                    L = S // dil
                    nb = L // 128
                    g.dma("sp", QTg[:], QT_d[gi * 512:(gi + 1) * 512, :].rearrange("(j p) t -> p j t", p=128), writes=[QTg])
                    g.dma("act", KTg[:], KT_d[gi * 512:(gi + 1) * 512, :].rearrange("(j p) t -> p j t", p=128), writes=[KTg])
                    vsrc = V1_d[:, gi * 8 * VS:(gi + 1) * 8 * VS].rearrange("(cb a r) c -> a r cb c", a=128, r=dil)
                    vdst = V1g[:].rearrange("p (r cb) c -> p r cb c", r=dil)
                    nsplit = max(1, 4 // dil)
                    cbs = nb // nsplit
                    wt = []
                    for r in range(dil):
                        for sp_ in range(nsplit):
                            g.dma("sp", vdst[:, r, sp_ * cbs:(sp_ + 1) * cbs, :], vsrc[:, r, sp_ * cbs:(sp_ + 1) * cbs, :],
                                  writes=[V1g] if (r == 0 and sp_ == 0) else [])
                    V1g.w = {(("d", i)): 16 * n for i, (s_, n) in enumerate(g.dsem) if n > 0}
                    Adst = A_d[gi].rearrange("(cb a r) c -> r cb a c", a=128, r=dil)
                    for r in range(dil):
                        for kb in range(nb):
                            nq = 256 if kb < nb - 1 else 128
                            for h in range(8):
                                j = h // 2
                                rows = slice((h % 2) * 64, (h % 2) * 64 + 64)
                                psb = pS[si % 4]
                                si += 1
                                g.mm([(psb[:, :nq], KTg[rows, j, sl(r + dil * kb * 128, 128, dil)],
                                       QTg[rows, j, sl(r + dil * kb * 128, nq, dil)])], [KTg, QTg], psb)
                                e32 = pe32[si % 2]
                                g.op("act", lambda: nc.scalar.activation(out=e32[:, :nq], in_=psb[:, :nq], func=AF.Exp),
                                     [psb], [e32])
                                ptb = PT[h][kb % 2]
                                g.op("dve", lambda: nc.vector.tensor_tensor(out=ptb[:, :nq], in0=e32[:, :nq],
                                                                            in1=Eb[:, gi * 8 + h, :nq], op=ALU.mult),
                                     [e32, Eb], [ptb])
                            po = pO[oi % 2]
                            ob = oacc[oi % 2]
                            oi += 1
                            bi = r * nb + kb
                            for h in range(8):
                                pob = po[h // 4]
                                mms = []
                                if kb > 0:
                                    mms.append((pob[:, (h % 4) * 65:(h % 4) * 65 + 65], PT[h][(kb - 1) % 2][:, 128:256], V1g[:, bi - 1, h * VS:h * VS + 65]))
                                mms.append((pob[:, (h % 4) * 65:(h % 4) * 65 + 65], PT[h][kb % 2][:, 0:128], V1g[:, bi, h * VS:h * VS + 65]))
                                g.mm(mms, [PT[h][0], PT[h][1], V1g], pob)
                            g.op("act", lambda: nc.scalar.copy(out=ob[:, 0:4, :].rearrange("p a b -> p (a b)"), in_=po[0][:, 0:260]), [po[0]], [ob])
                            g.op("dve", lambda: nc.vector.tensor_copy(out=ob[:, 4:8, :].rearrange("p a b -> p (a b)"), in_=po[1][:, 0:260]), [po[1]], [ob])
                            g.dma("sp", Adst[r, kb], ob[:].rearrange("p a b -> p (a b)"), reads=[ob])
                g.barrier()
        if "p2b" in stages or "all" in stages:
            for gi, (W, dil) in enumerate(GROUPS):
                for b in range(NS):
                    for (src, dst, off) in ((ck[gi], sk[gi], QW), (cv[gi], sv[gi], 2 * QW)):
                        for r0 in range(1, W, 512):
                            r1 = min(W, r0 + 512)
                            g.dma("act", dst[b, r0 - 1:r1 - 1, :], src[b, r0:r1, :], is_output=True)
                        g.dma("act", dst[b, W - 1:W, :], qkvs_d[b:b + 1, off + gi * 512:off + (gi + 1) * 512], is_output=True)
            for b in range(NS):
                g.dma("act", spool[b, 0:14, :], stp[b, 1:15, :], is_output=True)
                g.dma("act", spool[b, 14:15, :], us_d[b:b + 1, :], is_output=True)
            pb = contextlib.ExitStack()
            with pb:
                def sbb(name, shape, dt=F32):
                    return Buf(pb.enter_context(nc.sbuf_tensor("sb_" + name, list(shape), dt)), name)

                def psb_(name, shape, dt=F32):
                    return Buf(pb.enter_context(nc.psum_tensor("pb_" + name, list(shape), dt)), "P:" + name)

                qs = sbb("qs", [NS, QW]); ks = sbb("ks", [NS, QW]); vs_ = sbb("vs", [NS, QW])
                g.dma("sp", qs[:], qkvs_d[:, 0:QW], writes=[qs])
                g.dma("sp", ks[:], qkvs_d[:, QW:2 * QW], writes=[ks])
                g.dma("sp", vs_[:], qkvs_d[:, 2 * QW:3 * QW], writes=[vs_])
                prod = sbb("prod", [NS, QW])
                s0 = sbb("s0", [NS, 24]); b0 = sbb("b0", [NS, 24]); p0 = sbb("p0", [NS, 24])
                num0 = sbb("num0", [NS, 24, 64])
                g.dma("sp", b0[:], bias0_d.to_broadcast([NS, 24]), writes=[b0])
                g.op("dve", lambda: nc.vector.tensor_mul(out=prod[:], in0=qs[:], in1=ks[:]), [qs, ks], [prod])
                g.op("dve", lambda: nc.vector.tensor_reduce(out=s0[:], in_=prod[:].rearrange("p (h e) -> p h e", e=64),
                                                            axis=AX.X, op=ALU.add), [prod], [s0])
                g.op("dve", lambda: nc.vector.tensor_add(out=s0[:], in0=s0[:], in1=b0[:]), [s0, b0], [s0])
                g.op("act", lambda: nc.scalar.activation(out=p0[:], in_=s0[:], func=AF.Exp), [s0], [p0])
                g.op("dve", lambda: nc.vector.tensor_tensor(
                    out=num0[:], in0=vs_[:].rearrange("p (h e) -> p h e", e=64),
                    in1=p0[:].unsqueeze(2).to_broadcast([NS, 24, 64]), op=ALU.mult), [vs_, p0], [num0])
                BD = sbb("BD", [8, 8, 65])
                g.dma("sp", BD[:], bd_d, writes=[BD])
                ones8 = sbb("ones8", [8, 1])
                g.op("dve", lambda: nc.vector.memset(ones8[:], 1.0), [], [ones8])
                bsm = sbb("bsm", [128, 3, 8])
                g.dma("sp", bsm[:], bias_s_d.rearrange("g k h -> k g h"), writes=[bsm])
                Ksel = [sbb("Ksel%d" % j, [128, 512]) for j in range(2)]
                V1s = [sbb("V1s%d" % j, [128, 8, 65]) for j in range(2)]
                for j in range(2):
                    g.op("dve", lambda j=j: nc.vector.memset(V1s[j][:], 1.0), [], [V1s[j]])
                qbc = [sbb("qbc%d" % j, [128, 512]) for j in range(2)]
                pr2 = sbb("pr2", [128, 512])
                sc_ = sbb("sc_", [128, 8]); pp = sbb("pp", [128, 8])
                m1 = sbb("m1", [8, 8, 65])
                arow = sbb("arow", [1, 520])
                po1 = [psb_("po1_%d" % j, [128, 512]) for j in range(2)]
                po2 = [psb_("po2_%d" % j, [128, 512]) for j in range(2)]
                it = 0
                for b in range(NS):
                    for gi, (W, dil) in enumerate(GROUPS):
                        kb_ = Ksel[it % 2]; vb_ = V1s[it % 2]; qb_ = qbc[it % 2]
                        it += 1
                        g.dma("sp", kb_[:], ck[gi][b, 0:W:dil, :], writes=[kb_])
                        g.dma("act", vb_[:, :, 0:64], cv[gi][b, 0:W:dil, :].rearrange("r (h e) -> r h e", e=64), writes=[vb_])
                        g.dma("sp", qb_[:], qkvs_d[b:b + 1, gi * 512:(gi + 1) * 512].to_broadcast([128, 512]), writes=[qb_])
                        g.op("dve", lambda: nc.vector.tensor_mul(out=pr2[:], in0=kb_[:], in1=qb_[:]), [kb_, qb_], [pr2])
                        g.op("dve", lambda: nc.vector.tensor_reduce(out=sc_[:], in_=pr2[:].rearrange("p (h e) -> p h e", e=64),
                                                                    axis=AX.X, op=ALU.add), [pr2], [sc_])
                        g.op("dve", lambda: nc.vector.tensor_add(out=sc_[:], in0=sc_[:], in1=bsm[:, gi, :]), [sc_, bsm], [sc_])
                        g.op("act", lambda: nc.scalar.activation(out=pp[:], in_=sc_[:], func=AF.Exp), [sc_], [pp])
                        for hf in range(2):
                            g.mm([(po1[hf][0:8, 0:260], pp[:, :], vb_[:, hf * 4:(hf + 1) * 4, :].rearrange("p a b -> p (a b)"))],
                                 [pp, vb_], po1[hf])
                            g.op("dve", lambda: nc.vector.tensor_tensor(
                                out=m1[:, hf * 4:(hf + 1) * 4, :].rearrange("p a b -> p (a b)"), in0=po1[hf][0:8, 0:260],
                                in1=BD[:, hf * 4:(hf + 1) * 4, :].rearrange("p a b -> p (a b)"), op=ALU.mult), [po1[hf], BD], [m1])
                        for hf in range(2):
                            g.mm([(po2[hf][0:1, 0:260], ones8[:, :], m1[:, hf * 4:(hf + 1) * 4, :].rearrange("p a b -> p (a b)"))],
                                 [ones8, m1], po2[hf])
                            g.op("act", lambda: nc.scalar.copy(out=arow[:, hf * 260:(hf + 1) * 260], in_=po2[hf][0:1, 0:260]),
                                 [po2[hf]], [arow])
                        g.dma("sp", As_d[b, gi:gi + 1, :], arow[:], reads=[arow])
                st = sbb("st", [60, 512]); us = sbb("us", [NS, 512])
                g.dma("sp", st[:], stp.rearrange("b r c -> (b r) c"), writes=[st])
                g.dma("sp", us[:], us_d, writes=[us])
                bS = sbb("bS", [60, 4, NS]); dS = sbb("dS", [NS, 4, NS])
                g.dma("sp", bS[:], bandS_d.rearrange("g k b -> k g b"), writes=[bS])
                g.dma("sp", dS[:], diagS_d.rearrange("g k b -> k g b"), writes=[dS])
                pw2 = sbb("pw2", [128, 4, 128], BF16)
                pw2f = sbb("pw2f", [128, 4, 128])
                g.dma("sp", pw2f[:], pool_w.rearrange("g c d -> c g d"), writes=[pw2f])
                g.op("dve", lambda: nc.vector.tensor_copy(out=pw2[:], in_=pw2f[:]), [pw2f], [pw2])
                psc2 = sbb("psc2", [128, 4])
                g.dma("sp", psc2[:], pool_sc, writes=[psc2])
                pps = psb_("pps", [128, 512]); pmxs = psb_("pmxs", [128, 512])
                for gq in range(4):
                    cs = slice(gq * 128, (gq + 1) * 128)
                    g.mm([(pps[:, gq * NS:(gq + 1) * NS], st[:, cs], bS[:, gq, :]),
                          (pps[:, gq * NS:(gq + 1) * NS], us[:, cs], dS[:, gq, :])], [st, us, bS, dS], pps)
                pTs = sbb("pTs", [128, 4 * NS], BF16)
                g.op("dve", lambda: nc.vector.tensor_copy(out=pTs[:], in_=pps[:, 0:4 * NS]), [pps], [pTs])
                for gq in range(4):
                    g.mm([(pmxs[:, gq * NS:(gq + 1) * NS], pw2[:, gq, :], pTs[:, gq * NS:(gq + 1) * NS])], [pw2, pTs], pmxs)
                ybs = sbb("ybs", [128, 4, 128], BF16)
                g.op("dve", lambda: nc.vector.memset(ybs[:], 0.0), [], [ybs])
                for gq in range(4):
                    g.op("act", lambda gq=gq: nc.scalar.activation(out=ybs[:, gq, 0:NS], in_=pmxs[:, gq * NS:(gq + 1) * NS], func=AF.Copy,
                                                                  scale=psc2[:, gq:gq + 1]), [pmxs, psc2], [ybs])
                g.dma("sp", ybT_d[NT], ybs[:].rearrange("p a b -> p (a b)"), reads=[ybs])
                g.barrier()
                As = sbb("As", [NS, 3, 8, 65])
                g.dma("sp", As[:].rearrange("p a b c -> p (a b c)"), As_d.rearrange("b g c -> b (g c)"), writes=[As])
                numt = sbb("numt", [NS, 8, 64]); lt = sbb("lt", [NS, 8])
                g.op("dve", lambda: nc.vector.tensor_add(out=numt[:], in0=As[:, 0, :, 0:64], in1=As[:, 1, :, 0:64]), [As], [numt])
                g.op("dve", lambda: nc.vector.tensor_add(out=numt[:], in0=numt[:], in1=As[:, 2, :, 0:64]), [As, numt], [numt])
                g.op("dve", lambda: nc.vector.tensor_add(out=lt[:], in0=As[:, 0, :, 64], in1=As[:, 1, :, 64]), [As], [lt])
                g.op("dve", lambda: nc.vector.tensor_add(out=lt[:], in0=lt[:], in1=As[:, 2, :, 64]), [As, lt], [lt])
                for gi in range(3):
                    g.op("dve", lambda gi=gi: nc.vector.tensor_add(out=numt[:], in0=numt[:], in1=num0[:, gi * 8:(gi + 1) * 8, :]),
                         [numt, num0], [numt])
                    g.op("dve", lambda gi=gi: nc.vector.tensor_add(out=lt[:], in0=lt[:], in1=p0[:, gi * 8:(gi + 1) * 8]), [lt, p0], [lt])
                g.op("dve", lambda: nc.vector.reciprocal(out=lt[:], in_=lt[:]), [lt], [lt])
                oas = sbb("oas", [NS, 512])
                g.op("dve", lambda: nc.vector.tensor_tensor(out=oas[:].rearrange("p (h e) -> p h e", e=64), in0=numt[:],
                                                            in1=lt[:].unsqueeze(2).to_broadcast([NS, 8, 64]), op=ALU.mult),
                     [numt, lt], [oas])
                g.dma("sp", oas_d, oas[:], reads=[oas])
                g.barrier()
        if "p3" in stages or "all" in stages:
            p3 = contextlib.ExitStack()
            with p3:
                def sb3(name, shape, dt=F32):
                    return Buf(p3.enter_context(nc.sbuf_tensor("s3_" + name, list(shape), dt)), name)

                def ps3(name, shape, dt=F32):
                    return Buf(p3.enter_context(nc.psum_tensor("p3_" + name, list(shape), dt)), "P:" + name)

                wa_sb = sb3("wa", [128, 4, D], BF16)
                wb_sb = sb3("wb", [128, 4, D], BF16)
                wo_sb = sb3("wo", [128, 8, D], BF16)
                stg = sb3("stg", [128, 8, D])
                g.dma("sp", stg[:, 0:4, :], w_br_a.rearrange("(k p) n -> p k n", p=128), writes=[stg])
                g.op("act", lambda: nc.scalar.copy(out=wa_sb[:], in_=stg[:, 0:4, :]), [stg], [wa_sb])
                stg2 = sb3("stg2", [128, 4, D])
                g.dma("act", stg2[:], w_br_b.rearrange("(k p) n -> p k n", p=128), writes=[stg2])
                g.op("dve", lambda: nc.vector.tensor_copy(out=wb_sb[:], in_=stg2[:]), [stg2], [wb_sb])
                g.dma("sp", stg[:], w_out.rearrange("(k p) n -> p k n", p=128), writes=[stg])
                g.op("act", lambda: nc.scalar.copy(out=wo_sb[:, 0:4, :], in_=stg[:, 0:4, :]), [stg], [wo_sb])
                g.op("dve", lambda: nc.vector.tensor_copy(out=wo_sb[:, 4:8, :], in_=stg[:, 4:8, :]), [stg], [wo_sb])
                rw_sb = sb3("rw", [128, 8, NE])
                g.dma("sp", rw_sb[:], router_w.rearrange("(k p) n -> p k n", p=128), writes=[rw_sb])
                rbias = sb3("rbias", [128, NE])
                g.dma("sp", rbias[:], router_b.to_broadcast([128, NE]), writes=[rbias])
                GT1 = sb3("GT1", [128, D]); G2 = sb3("G2", [128, D]); SH2 = sb3("SH2", [128, D]); n2b = sb3("n2b", [128, D])
                GT1s = sb3("GT1s", [NS, D]); G2s = sb3("G2s", [NS, D]); SH2s = sb3("SH2s", [NS, D])
                g.dma("sp", n2b[:], norm2.to_broadcast([128, D]), writes=[n2b])
                g.dma("sp", GT1[:], mod_d[0:1, 2 * D:3 * D].to_broadcast([128, D]), writes=[GT1])
                g.dma("sp", SH2[:], mod_d[0:1, 3 * D:4 * D].to_broadcast([128, D]), writes=[SH2])
                g.dma("sp", G2[:], mod_d[0:1, 4 * D:5 * D].to_broadcast([128, D]), writes=[G2])
                g.dma("sp", GT1s[:], mod_d[1:5, 2 * D:3 * D], writes=[GT1s])
                g.dma("sp", SH2s[:], mod_d[1:5, 3 * D:4 * D], writes=[SH2s])
                g.dma("sp", G2s[:], mod_d[1:5, 4 * D:5 * D], writes=[G2s])
                g.op("dve", lambda: nc.vector.scalar_tensor_tensor(
                    out=G2[:], in0=G2[:], scalar=1.0, in1=n2b[:], op0=ALU.add, op1=ALU.mult), [G2, n2b], [G2])
                g.op("dve", lambda: nc.vector.scalar_tensor_tensor(
                    out=G2s[:], in0=G2s[:], scalar=1.0, in1=n2b[:NS, :], op0=ALU.add, op1=ALU.mult), [G2s, n2b], [G2s])

                A0 = sb3("A0", [128, 8, 65]); A1 = sb3("A1", [128, 8, 65]); A2 = sb3("A2", [128, 8, 65])
                rl = sb3("rl", [128, 8])
                oaf = sb3("oaf", [128, 512])
                oab = sb3("oab", [128, 512], BF16)
                oaT = sb3("oaT", [128, 4, 128], BF16)
                ybt = sb3("ybt", [128, 4, 128], BF16)
                gt = sb3("gt", [128, 2048], BF16)
                xt3 = sb3("xt3", [128, D])
                t1_ = sb3("t1_", [128, D]); t2_ = sb3("t2_", [128, D])
                mgb = sb3("mgb", [128, D], BF16)
                mT = sb3("mT", [128, 8, 128], BF16)
                x1 = sb3("x1", [128, D])
                h2 = sb3("h2", [128, D])
                sq3 = sb3("sq3", [128, D])
                ssq3 = sb3("ssq3", [128, 1]); rstd3 = sb3("rstd3", [128, 1])
                h2T32 = sb3("h2T32", [128, 8, 128])
                h2Tb = sb3("h2Tb", [128, 8, 128], BF16)
                sc = sb3("sc", [128, NE]); sel = sb3("sel", [128, NE]); selm = sb3("selm", [128, NE])
                mx8 = sb3("mx8", [128, 8, 8]); gsc = sb3("gsc", [128, 8]); gtop = sb3("gtop", [128, 8])
                gmask = sb3("gmask", [128, 8]); top8 = sb3("top8", [128, 8])
                Mk = sb3("Mk", [128, NE]); Wd = sb3("Wd", [128, NE]); den = sb3("den", [128, 1])
                UT = sb3("UT", [128, 128])
                g.dma("sp", UT[:], UT_d, writes=[UT])
                ones_r = sb3("ones_r", [1, 128]); ones_c = sb3("ones_c", [128, 1]); carry = sb3("carry", [1, NE])
                g.op("dve", lambda: nc.vector.memset(ones_r[:], 1.0), [], [ones_r])
                g.op("dve", lambda: nc.vector.memset(ones_c[:], 1.0), [], [ones_c])
                g.op("dve", lambda: nc.vector.memset(carry[:], 0.0), [], [carry])
                pos1t = sb3("pos1t", [128, NE]); key_ = sb3("key_", [128, NE]); junk = sb3("junk", [128, NE])
                h2b = sb3("h2b", [128, D], BF16)
                ptr3 = ps3("ptr3", [128, 8, 128], BF16)
                pbr = [ps3("pbr%d" % j, [128, 512]) for j in range(2)]
                pym = [ps3("pym%d" % j, [128, 512]) for j in range(2)]
                pt32 = [ps3("pt32_%d" % j, [128, 4, 128]) for j in range(2)]
                prt = ps3("prt", [128, 512])

                for i in range(NT + 1):
                    n = 128 if i < NT else NS
                    samp = (i == NT)
                    rows = slice(i * 128, i * 128 + n)
                    gt1, g2m, sh2m = (GT1s, G2s, SH2s) if samp else (GT1, G2, SH2)
                    g.dma("sp", xt3[:n, :], xs if samp else x[rows, :], writes=[xt3])
                    g.dma("act", gt[:n, :], gates_d[rows, :], writes=[gt])
                    g.dma("act", ybt[:].rearrange("p a b -> p (a b)"), ybT_d[i], writes=[ybt])
                    if not samp:
                        g.dma("sp", A0[:].rearrange("p a b -> p (a b)"), A_d[0][rows, :], writes=[A0])
                        g.dma("sp", A1[:].rearrange("p a b -> p (a b)"), A_d[1][rows, :], writes=[A1])
                        g.dma("sp", A2[:].rearrange("p a b -> p (a b)"), A_d[2][rows, :], writes=[A2])
                        g.op("dve", lambda: nc.vector.tensor_add(out=A0[:], in0=A0[:], in1=A1[:]), [A0, A1], [A0])
                        g.op("dve", lambda: nc.vector.tensor_add(out=A0[:], in0=A0[:], in1=A2[:]), [A0, A2], [A0])
                        g.op("dve", lambda: nc.vector.reciprocal(out=rl[:], in_=A0[:, :, 64]), [A0], [rl])
                        g.op("dve", lambda: nc.vector.tensor_tensor(
                            out=oab[:].rearrange("p (h e) -> p h e", e=64), in0=A0[:, :, 0:64],
                            in1=rl[:].unsqueeze(2).to_broadcast([128, 8, 64]), op=ALU.mult), [A0, rl], [oab])
                    else:
                        g.dma("sp", oaf[:NS, :], oas_d, writes=[oaf])
                        g.op("dve", lambda: nc.vector.tensor_copy(out=oab[:NS, :], in_=oaf[:NS, :]), [oaf], [oab])
                    g.tr([(ptr3[:, k, :n], oab[:n, k * 128:(k + 1) * 128], ident_b[:n, :n]) for k in range(4)], [oab, ident_b], ptr3)
                    g.op("act", lambda: nc.scalar.copy(out=oaT[:, :, :n], in_=ptr3[:, 0:4, :n]), [ptr3], [oaT])
                    for half in range(2):
                        cs = slice(half * 512, (half + 1) * 512)
                        g.mm([(pbr[0][:n, :], oaT[:, k, :n], wa_sb[:, k, cs]) for k in range(4)], [oaT, wa_sb], pbr[0])
                        g.mm([(pbr[1][:n, :], ybt[:, k, :n], wb_sb[:, k, cs]) for k in range(4)], [ybt, wb_sb], pbr[1])
                        g.op("dve", lambda: nc.vector.tensor_tensor(out=t1_[:n, cs], in0=pbr[0][:n, :], in1=gt[:n, cs], op=ALU.mult),
                             [pbr[0], gt], [t1_])
                        g.op("dve", lambda: nc.vector.tensor_tensor(out=t2_[:n, cs], in0=pbr[1][:n, :],
                                                                    in1=gt[:n, 1024 + half * 512:1024 + (half + 1) * 512], op=ALU.mult),
                             [pbr[1], gt], [t2_])
                    g.op("dve", lambda: nc.vector.tensor_add(out=mgb[:n, :], in0=t1_[:n, :], in1=t2_[:n, :]), [t1_, t2_], [mgb])
                    g.tr([(ptr3[:, k, :n], mgb[:n, k * 128:(k + 1) * 128], ident_b[:n, :n]) for k in range(8)], [mgb, ident_b], ptr3)
                    g.op("act", lambda: nc.scalar.copy(out=mT[:, :, :n], in_=ptr3[:, :, :n]), [ptr3], [mT])
                    for half in range(2):
                        cs = slice(half * 512, (half + 1) * 512)
                        g.mm([(pym[half][:n, :], mT[:, k, :n], wo_sb[:, k, cs]) for k in range(8)], [mT, wo_sb], pym[half])
                        g.op("dve", lambda: nc.vector.tensor_tensor(out=t1_[:n, cs], in0=pym[half][:n, :], in1=gt1[:n, cs], op=ALU.mult),
                             [pym[half], gt1], [t1_])
                    g.op("dve", lambda: nc.vector.tensor_add(out=x1[:n, :], in0=t1_[:n, :], in1=xt3[:n, :]), [t1_, xt3], [x1])
                    g.dma("sp", x1_d[rows, :], x1[:n, :], reads=[x1])
                    g.op("act", lambda: nc.scalar.activation(out=sq3[:n, :], in_=x1[:n, :], func=AF.Square, accum_out=ssq3[:n, :]),
                         [x1], [sq3, ssq3])
                    g.op("dve", lambda: nc.vector.tensor_scalar(out=rstd3[:n, :], in0=ssq3[:n, :], scalar1=1.0 / D, scalar2=EPS,
                                                                op0=ALU.mult, op1=ALU.add), [ssq3], [rstd3])
                    g.op("pool", lambda: nc.gpsimd.tensor_tensor(out=rstd3[:n, :], in0=rstd3[:n, :], in1=nhalf[:n, 0:1], op=ALU.pow),
                         [rstd3, nhalf], [rstd3])
                    g.op("dve", lambda: nc.vector.scalar_tensor_tensor(out=sq3[:n, :], in0=x1[:n, :], scalar=rstd3[:n, :],
                                                                       in1=g2m[:n, :], op0=ALU.mult, op1=ALU.mult),
                         [x1, rstd3, g2m], [sq3])
                    g.op("dve", lambda: nc.vector.tensor_add(out=h2[:n, :], in0=sq3[:n, :], in1=sh2m[:n, :]), [sq3, sh2m], [h2])
                    for hf in range(2):
                        g.tr([(pt32[hf][:, k, :n], h2[:n, (hf * 4 + k) * 128:(hf * 4 + k + 1) * 128], ident_f[:n, :n]) for k in range(4)],
                             [h2, ident_f], pt32[hf])
                        g.op("act", lambda: nc.scalar.copy(out=h2T32[:, hf * 4:(hf + 1) * 4, :n], in_=pt32[hf][:, :, :n]),
                             [pt32[hf]], [h2T32])
                    if samp:
                        g.op("dve", lambda: nc.vector.memset(h2Tb[:], 0.0), [], [h2Tb])
                    g.op("dve", lambda: nc.vector.tensor_copy(out=h2Tb[:, :, :n], in_=h2T32[:, :, :n]), [h2T32], [h2Tb])
                    g.dma("sp", h2T_d[i], h2Tb[:].rearrange("p a b -> p (a b)"), reads=[h2Tb])
                    g.mm([(prt[:n, :NE], h2T32[:, k, :n], rw_sb[:, k, :]) for k in range(8)], [h2T32, rw_sb], prt)
                    g.op("act", lambda: nc.scalar.activation(out=sc[:n, :], in_=prt[:n, :NE], func=AF.Sigmoid), [prt], [sc])
                    g.op("dve", lambda: nc.vector.tensor_add(out=sel[:n, :], in0=sc[:n, :], in1=rbias[:n, :]), [sc, rbias], [sel])
                    for gq in range(8):
                        g.op("dve", lambda: nc.vector.max(out=mx8[:n, gq, :], in_=sel[:n, gq * 32:(gq + 1) * 32]), [sel], [mx8])
                    g.op("dve", lambda: nc.vector.tensor_add(out=gsc[:n, :], in0=mx8[:n, :, 0], in1=mx8[:n, :, 1]), [mx8], [gsc])
                    g.op("dve", lambda: nc.vector.max(out=gtop[:n, :], in_=gsc[:n, :]), [gsc], [gtop])
                    g.op("dve", lambda: nc.vector.tensor_scalar(out=gmask[:n, :], in0=gsc[:n, :], scalar1=gtop[:n, 3:4], scalar2=None,
                                                                op0=ALU.is_ge), [gsc, gtop], [gmask])
                    g.op("dve", lambda: nc.vector.tensor_scalar(out=gmask[:n, :], in0=gmask[:n, :], scalar1=-1.0, scalar2=1e9,
                                                                op0=ALU.add, op1=ALU.mult), [gmask], [gmask])
                    g.op("dve", lambda: nc.vector.tensor_tensor(
                        out=selm[:n, :].rearrange("p (a b) -> p a b", b=32), in0=sel[:n, :].rearrange("p (a b) -> p a b", b=32),
                        in1=gmask[:n, :].unsqueeze(2).to_broadcast([n, 8, 32]), op=ALU.add), [sel, gmask], [selm])
                    g.op("dve", lambda: nc.vector.max(out=top8[:n, :], in_=selm[:n, :]), [selm], [top8])
                    g.op("dve", lambda: nc.vector.tensor_scalar(out=Mk[:n, :], in0=selm[:n, :], scalar1=top8[:n, 7:8], scalar2=None,
                                                                op0=ALU.is_ge), [selm, top8], [Mk])
                    g.op("dve", lambda: nc.vector.tensor_tensor(out=Wd[:n, :], in0=Mk[:n, :], in1=sc[:n, :], op=ALU.mult), [Mk, sc], [Wd])
                    g.op("dve", lambda: nc.vector.tensor_reduce(out=den[:n, :], in_=Wd[:n, :], axis=AX.X, op=ALU.add), [Wd], [den])
                    g.op("dve", lambda: nc.vector.reciprocal(out=den[:n, :], in_=den[:n, :]), [den], [den])
                    g.op("dve", lambda: nc.vector.tensor_scalar(out=Wd[:n, :], in0=Wd[:n, :], scalar1=den[:n, :], scalar2=2.5,
                                                                op0=ALU.mult, op1=ALU.mult), [Wd, den], [Wd])
                    g.dma("sp", Wd_d[i, 0:n, :], Wd[:n, :], reads=[Wd])
                    if SPARSE:
                        pps_ = pbr[0]; pcs_ = pbr[1]
                        g.mm([(pps_[:n, :NE], UT[:n, :n], Mk[:n, :]), (pps_[:n, :NE], ones_r[0:1, :n], carry[0:1, :])],
                             [UT, Mk, ones_r, carry], pps_)
                        g.op("dve", lambda: nc.vector.tensor_scalar(out=pos1t[:n, :], in0=pps_[:n, :NE], scalar1=1.0, scalar2=None,
                                                                    op0=ALU.add), [pps_], [pos1t])
                        g.dma("sp", pos_d[i, 0:n, :], pos1t[:n, :], reads=[pos1t])
                        g.mm([(pcs_[0:1, :NE], ones_c[:n, 0:1], Mk[:n, :])], [ones_c, Mk], pcs_)
                        g.op("dve", lambda: nc.vector.tensor_add(out=carry[0:1, :], in0=carry[0:1, :], in1=pcs_[0:1, :NE]),
                             [carry, pcs_], [carry])
                        g.op("act", lambda: nc.scalar.copy(out=h2b[:n, :], in_=h2[:n, :]), [h2], [h2b])
                        g.dma("sp", h2b_d[i, 0:n, :], h2b[:n, :], reads=[h2b])
                if SPARSE:
                    ci_ = sb3("ci_", [1, NE], I32)
                    pc = sb3("pc", [1, NE]); pcT = sb3("pcT", [128, 2]); pend = sb3("pend", [1, NE]); ps1r = sb3("ps1r", [1, NE])
                    tri = sb3("tri", [128, 2, NE]); bstart = sb3("bstart", [128, 5])
                    g.dma("sp", tri[:], tri_d.rearrange("c p e -> p c e"), writes=[tri])
                    g.dma("sp", bstart[:], bstart_d, writes=[bstart])
                    g.op("dve", lambda: nc.vector.tensor_scalar(out=pc[:], in0=carry[:], scalar1=127.0, scalar2=None, op0=ALU.add),
                         [carry], [pc])
                    g.op("dve", lambda: nc.vector.tensor_copy(out=ci_[:], in_=pc[:]), [pc], [ci_])
                    g.op("dve", lambda: nc.vector.tensor_single_scalar(out=ci_[:], in_=ci_[:], scalar=7, op=ALU.arith_shift_right),
                         [ci_], [ci_])
                    g.op("dve", lambda: nc.vector.tensor_single_scalar(out=ci_[:], in_=ci_[:], scalar=7, op=ALU.logical_shift_left),
                         [ci_], [ci_])
                    g.op("dve", lambda: nc.vector.tensor_copy(out=pc[:], in_=ci_[:]), [ci_], [pc])
                    g.tr([(pt32[0][:, 0, 0:1], pc[0:1, 0:128], ident_f[0:1, 0:1]), (pt32[0][:, 1, 0:1], pc[0:1, 128:256], ident_f[0:1, 0:1])],
                         [pc, ident_f], pt32[0])
                    g.op("dve", lambda: nc.vector.tensor_copy(out=pcT[:], in_=pt32[0][:, 0:2, 0]), [pt32[0]], [pcT])
                    g.mm([(prt[0:1, :NE], pcT[:, c2:c2 + 1], tri[:, c2, :]) for c2 in range(2)], [pcT, tri], prt)
                    g.op("dve", lambda: nc.vector.tensor_copy(out=pend[:], in_=prt[0:1, :NE]), [prt], [pend])
                    g.op("dve", lambda: nc.vector.tensor_sub(out=ps1r[:], in0=pend[:], in1=pc[:]), [pend, pc], [ps1r])
                    PSb = sb3("PSb", [128, NE]); PEb = sb3("PEb", [128, NE])
                    g.mm([(pbr[0][:, :NE], ones_r[0:1, :], ps1r[0:1, :])], [ones_r, ps1r], pbr[0])
                    g.op("dve", lambda: nc.vector.tensor_copy(out=PSb[:], in_=pbr[0][:, :NE]), [pbr[0]], [PSb])
                    g.mm([(pbr[1][:, :NE], ones_r[0:1, :], pend[0:1, :])], [ones_r, pend], pbr[1])
                    g.op("dve", lambda: nc.vector.tensor_copy(out=PEb[:], in_=pbr[1][:, :NE]), [pbr[1]], [PEb])
                    be = sb3("be", [128, 8])
                    g.op("dve", lambda: nc.vector.memset(be[:], 0.0), [], [be])
                    for j5 in range(5):
                        g.op("dve", lambda j5=j5: nc.vector.tensor_scalar(
                            out=key_[:, :], in0=PEb[:, :], scalar1=bstart[:, j5:j5 + 1], scalar2=None, op0=ALU.is_le, op1=ALU.add,
                            accum_out=be[:, j5:j5 + 1]), [PEb, bstart], [key_, be])
                    g.op("dve", lambda: nc.vector.tensor_scalar_min(out=be[:], in0=be[:], scalar1=float(NE - 1)), [be], [be])
                    g.tr([(pt32[1][0:8, 0, :], be[:, 0:8], ident_f[:, :])], [be, ident_f], pt32[1])
                    beT = sb3("beT", [8, 128])
                    g.op("dve", lambda: nc.vector.tensor_copy(out=beT[:], in_=pt32[1][0:8, 0, :]), [pt32[1]], [beT])
                    g.dma("sp", be_d.rearrange("o (j b) -> (o j) b", j=5), beT[0:5, :], reads=[beT])
                    g.barrier()
                    trashf = sb3("trashf", [128, 8])
                    g.dma("sp", trashf[:], trash_d, writes=[trashf])
                    s8f = sb3("s8f", [128, 8]); w8 = sb3("w8", [128, 8]); s8i = sb3("s8i", [128, 8], I32)
                    g.op("dve", lambda: nc.vector.memset(h2b[:], 0.0), [], [h2b])
                    for i in range(NT + 1):
                        n = 128 if i < NT else NS
                        samp = (i == NT)
                        g.dma("sp", pos1t[:n, :], pos_d[i, 0:n, :], writes=[pos1t])
                        g.dma("act", Wd[:n, :], Wd_d[i, 0:n, :], writes=[Wd])
                        g.dma("act", h2b[:n, :], h2b_d[i, 0:n, :], writes=[h2b])
                        g.op("dve", lambda: nc.vector.tensor_single_scalar(out=Mk[:n, :], in_=Wd[:n, :], scalar=0.0, op=ALU.is_gt),
                             [Wd], [Mk])
                        g.op("dve", lambda: nc.vector.tensor_add(out=key_[:n, :], in0=pos1t[:n, :], in1=PSb[:n, :]), [pos1t, PSb], [key_])
                        g.op("dve", lambda: nc.vector.tensor_mul(out=key_[:n, :], in0=key_[:n, :], in1=Mk[:n, :]), [key_, Mk], [key_])
                        if samp:
                            g.op("dve", lambda: nc.vector.tensor_copy(out=s8f[:], in_=trashf[:]), [trashf], [s8f])
                        g.op("dve", lambda: nc.vector.max(out=s8f[:n, :], in_=key_[:n, :]), [key_], [s8f])
                        for k8 in range(8):
                            g.op("dve", lambda k8=k8: nc.vector.scalar_tensor_tensor(
                                out=junk[:n, :], in0=key_[:n, :], scalar=s8f[:n, k8:k8 + 1], in1=Wd[:n, :],
                                op0=ALU.is_equal, op1=ALU.mult, accum_out=w8[:n, k8:k8 + 1]), [key_, s8f, Wd], [junk, w8])
                        g.op("dve", lambda: nc.vector.tensor_scalar(out=s8f[:, :], in0=s8f[:, :], scalar1=-1.0, scalar2=None,
                                                                    op0=ALU.add), [s8f], [s8f])
                        g.op("dve", lambda: nc.vector.tensor_copy(out=s8i[:, :], in_=s8f[:, :]), [s8f], [s8i])
                        g.dma("sp", slot_d[i], s8i[:, :], reads=[s8i])
                        g.dma("sp", w8_d[i, 0:n, :], w8[:n, :], reads=[w8])
                        for k8 in range(8):
                            g.idma(Xs_d[:, :], bass.IndirectOffsetOnAxis(ap=s8i[:, k8:k8 + 1], axis=0), h2b[:, :], None,
                                   reads=[s8i, h2b])
                g.barrier()
        if ("p4" in stages or "all" in stages) and not SPARSE:
            NG = 3
            tiles_all = list(range(NT + 1))
            per = (len(tiles_all) + NG - 1) // NG
            groups4 = [tiles_all[a:a + per] for a in range(0, len(tiles_all), per)]
            n_exp = int(_os.environ.get("P4_NEXP", str(NE)))
            for tg in groups4:
                p4 = contextlib.ExitStack()
                with p4:
                    def sb4(name, shape, dt=F32):
                        return Buf(p4.enter_context(nc.sbuf_tensor("s4_%d_" % tg[0] + name, list(shape), dt)), name)

                    def ps4(name, shape, dt=F32):
                        return Buf(p4.enter_context(nc.psum_tensor("p4_%d_" % tg[0] + name, list(shape), dt)), "P:" + name)

                    ntl = len(tg)
                    hT4 = sb4("hT4", [128, 8, ntl * 128], BF16)
                    for ti, i in enumerate(tg):
                        g.dma("sp", hT4[:, :, ti * 128:(ti + 1) * 128], h2T_d[i].rearrange("p (k t) -> p k t", k=8),
                              writes=[hT4] if ti == 0 else [])
                    Wg = sb4("Wg", [128, ntl, NE])
                    for ti, i in enumerate(tg):
                        nn = 128 if i < NT else NS
                        g.dma("sp", Wg[:nn, ti, :], Wd_d[i, 0:nn, :], writes=[])
                    acc = sb4("acc", [128, ntl, D])
                    g.op("pool", lambda: nc.gpsimd.memset(acc[:], 0.0), [], [acc])
                    allw = {(("d", i_)): 16 * n_ for i_, (s_, n_) in enumerate(g.dsem) if n_ > 0}
                    hT4.w = dict(allw)
                    Wg.w = dict(allw)
                    wgs = [sb4("wg%d" % j, [128, 8, EH], BF16) for j in range(2)]
                    wus = [sb4("wu%d" % j, [128, 8, EH], BF16) for j in range(2)]
                    wds = [sb4("wd%d" % j, [128, 2, D], BF16) for j in range(2)]
                    sgt = [sb4("sgt%d" % j, [128, 512]) for j in range(2)]
                    act = [sb4("act%d" % j, [128, 512], BF16) for j in range(2)]
                    ph = [[ps4("ph%d_%d" % (a, b), [128, 512]) for b in range(2)] for a in range(2)]
                    py = [[ps4("py%d_%d" % (a, b), [128, 512]) for b in range(2)] for a in range(2)]
                    blocks = [list(range(a, min(a + 4, ntl))) for a in range(0, ntl, 4)]
                    yi = 0
                    elist = list(range(n_exp)) + [NE]
                    for ei, e in enumerate(elist):
                        j2 = ei % 2
                        if e < NE:
                            srcs = (eg[e], eu[e], ed[e])
                        else:
                            srcs = (shg, shu, shd)
                        g.dma("pool", wgs[j2][:], srcs[0].rearrange("(k p) n -> p k n", p=128), writes=[wgs[j2]])
                        g.dma("pool", wus[j2][:], srcs[1].rearrange("(k p) n -> p k n", p=128), writes=[wus[j2]])
                        g.dma("pool", wds[j2][:], srcs[2].rearrange("(k p) n -> p k n", p=128), writes=[wds[j2]])
                        for blk in blocks:
                            t0 = blk[0] * 128
                            ntb = sum(128 if tg[ti] < NT else NS for ti in blk)
                            for hh in range(2):
                                hs = slice(hh * 128, (hh + 1) * 128)
                                g.mm([(ph[0][hh][:, :ntb], wgs[j2][:, k, hs], hT4[:, k, t0:t0 + ntb]) for k in range(8)],
                                     [wgs[j2], hT4], ph[0][hh])
                                g.mm([(ph[1][hh][:, :ntb], wus[j2][:, k, hs], hT4[:, k, t0:t0 + ntb]) for k in range(8)],
                                     [wus[j2], hT4], ph[1][hh])
                                g.op("act", lambda: nc.scalar.activation(out=sgt[hh][:, :ntb], in_=ph[0][hh][:, :ntb], func=AF.Silu),
                                     [ph[0][hh]], [sgt[hh]])
                                g.op("dve", lambda: nc.vector.tensor_tensor(out=act[hh][:, :ntb], in0=ph[1][hh][:, :ntb],
                                                                            in1=sgt[hh][:, :ntb], op=ALU.mult),
                                     [ph[1][hh], sgt[hh]], [act[hh]])
                            for ti in blk:
                                nn = 128 if tg[ti] < NT else NS
                                c0 = (ti - blk[0]) * 128
                                pyy = py[yi % 2]
                                yi += 1
                                for half in range(2):
                                    cs = slice(half * 512, (half + 1) * 512)
                                    g.mm([(pyy[half][:nn, :], act[hh2][:, c0:c0 + nn], wds[j2][:, hh2, cs]) for hh2 in range(2)],
                                         [act[0], act[1], wds[j2]], pyy[half])
                                    if e < NE:
                                        g.op("dve", lambda: nc.vector.scalar_tensor_tensor(
                                            out=acc[:nn, ti, cs], in0=pyy[half][:nn, :], scalar=Wg[:nn, ti, e:e + 1],
                                            in1=acc[:nn, ti, cs], op0=ALU.mult, op1=ALU.add), [pyy[half], Wg, acc], [acc])
                                    else:
                                        g.op("dve", lambda: nc.vector.tensor_tensor(
                                            out=acc[:nn, ti, cs], in0=pyy[half][:nn, :], in1=acc[:nn, ti, cs], op=ALU.add),
                                            [pyy[half], acc], [acc])
                    GT2 = sb4("GT2", [128, D]); GT2s = sb4("GT2s", [NS, D])
                    g.dma("sp", GT2[:], mod_d[0:1, 5 * D:6 * D].to_broadcast([128, D]), writes=[GT2])
                    g.dma("sp", GT2s[:], mod_d[1:5, 5 * D:6 * D], writes=[GT2s])
                    xo = [sb4("xo%d" % j, [128, D]) for j in range(2)]
                    for ti, i in enumerate(tg):
                        nn = 128 if i < NT else NS
                        samp = (i == NT)
                        rows = slice(i * 128, i * 128 + nn)
                        xb_ = xo[ti % 2]
                        gt2 = GT2s if samp else GT2
                        g.dma("sp", xb_[:nn, :], x1_d[rows, :], writes=[xb_])
                        g.op("dve", lambda: nc.vector.tensor_tensor(out=acc[:nn, ti, :], in0=acc[:nn, ti, :], in1=gt2[:nn, :], op=ALU.mult),
                             [acc, gt2], [acc])
                        g.op("dve", lambda: nc.vector.tensor_add(out=xb_[:nn, :], in0=xb_[:nn, :], in1=acc[:nn, ti, :]), [xb_, acc], [xb_])
                        g.dma("sp", ys if samp else y[rows, :], xb_[:nn, :], reads=[xb_], is_output=True)
                    g.barrier()
        if ("p4" in stages or "all" in stages) and SPARSE:
            p4 = contextlib.ExitStack()
            with p4:
                def sb4(name, shape, dt=F32):
                    return Buf(p4.enter_context(nc.sbuf_tensor("s4_" + name, list(shape), dt)), name)

                def ps4(name, shape, dt=F32):
                    return Buf(p4.enter_context(nc.psum_tensor("p4_" + name, list(shape), dt)), "P:" + name)

                NW = 3
                wgs = [sb4("wg%d" % j, [128, 8, EH], BF16) for j in range(NW)]
                wus = [sb4("wu%d" % j, [128, 8, EH], BF16) for j in range(NW)]
                wds = [sb4("wd%d" % j, [128, 2, D], BF16) for j in range(NW)]
                Xsb = [sb4("Xs%d" % j, [128, D], BF16) for j in range(3)]
                XTb = [sb4("XT%d" % j, [128, 8, 512], BF16) for j in range(2)]
                sgt = [sb4("sgt%d" % j, [128, 512]) for j in range(2)]
                act = [sb4("act%d" % j, [128, 512], BF16) for j in range(2)]
                Ysb = [sb4("Ys%d" % j, [128, D], BF16) for j in range(2)]
                Yshb = [sb4("Ysh%d" % j, [128, D]) for j in range(2)]
                ptx = [ps4("ptx%d" % j, [128, 8, 128], BF16) for j in range(2)]
                ph = [[ps4("ph%d_%d" % (a_, b_), [128, 512]) for b_ in range(2)] for a_ in range(2)]
                py = [ps4("py%d" % a_, [128, 512]) for a_ in range(2)]
                cvt_emit(1000)
                for e_ in g.eng:
                    g.eng[e_].wait_ge(cvt_sem, 16 * cvt_total)
                BEb = sb4("BEb", [128, 640]); piota = sb4("piota", [128, 1]); widx = sb4("widx", [128, 640], I32)
                g.dma("sp", BEb[:], be_d.to_broadcast([128, 640]), writes=[BEb])
                g.dma("sp", piota[:], piota_d, writes=[piota])
                g.op("dve", lambda: nc.vector.tensor_scalar(out=BEb[:], in0=BEb[:], scalar1=128.0, scalar2=piota[:, 0:1],
                                                            op0=ALU.mult, op1=ALU.add), [BEb, piota], [BEb])
                g.op("dve", lambda: nc.vector.tensor_copy(out=widx[:], in_=BEb[:]), [BEb], [widx])
                nblk = int(_os.environ.get("P4_NBLK", str(NBLK)))
                yi = 0
                ci = 0

                def expert_block(j2, XT, ntb):
                    for hh in range(2):
                        hs = slice(hh * 128, (hh + 1) * 128)
                        g.mm([(ph[0][hh][:, :ntb], wgs[j2][:, k, hs], XT[:, k, :ntb]) for k in range(8)], [wgs[j2], XT], ph[0][hh])
                        g.mm([(ph[1][hh][:, :ntb], wus[j2][:, k, hs], XT[:, k, :ntb]) for k in range(8)], [wus[j2], XT], ph[1][hh])
                        g.op("act", lambda: nc.scalar.activation(out=sgt[hh][:, :ntb], in_=ph[0][hh][:, :ntb], func=AF.Silu),
                             [ph[0][hh]], [sgt[hh]])
                        g.op("dve", lambda: nc.vector.tensor_tensor(out=act[hh][:, :ntb], in0=ph[1][hh][:, :ntb],
                                                                    in1=sgt[hh][:, :ntb], op=ALU.mult),
                             [ph[1][hh], sgt[hh]], [act[hh]])

                def down(j2, c0, nn):
                    for half in range(2):
                        cs = slice(half * 512, (half + 1) * 512)
                        g.mm([(py[half][:nn, :], act[hh2][:, c0:c0 + nn], wds[j2][:, hh2, cs]) for hh2 in range(2)],
                             [act[0], act[1], wds[j2]], py[half])

                for b4 in range(nblk):
                    j2 = b4 % NW
                    off = bass.IndirectOffsetOnAxis(ap=widx[:, b4:b4 + 1], axis=0)
                    g.idma(wgs[j2][:].rearrange("p k n -> p (k n)"), None, egb[:, :], off, reads=[widx], writes=[wgs[j2]])
                    g.idma(wus[j2][:].rearrange("p k n -> p (k n)"), None, eub[:, :], off, reads=[widx], writes=[wus[j2]])
                    g.idma(wds[j2][:].rearrange("p k n -> p (k n)"), None, edb[:, :], off, reads=[widx], writes=[wds[j2]])
                    Xs = Xsb[b4 % 3]
                    pt_ = ptx[b4 % 2]
                    XT = XTb[b4 % 2]
                    g.dma("sp", Xs[:], Xs_d[b4 * 128:(b4 + 1) * 128, :], writes=[Xs])
                    g.tr([(pt_[:, k, :], Xs[:, k * 128:(k + 1) * 128], ident_b[:, :]) for k in range(8)], [Xs, ident_b], pt_)
                    g.op("act", lambda: nc.scalar.copy(out=XT[:, 0:4, 0:128], in_=pt_[:, 0:4, :]), [pt_], [XT])
                    g.op("dve", lambda: nc.vector.tensor_copy(out=XT[:, 4:8, 0:128], in_=pt_[:, 4:8, :]), [pt_], [XT])
                    expert_block(j2, XT, 128)
                    Ys = Ysb[b4 % 2]
                    down(j2, 0, 128)
                    g.op("act", lambda: nc.scalar.copy(out=Ys[:, 0:512], in_=py[0][:, :]), [py[0]], [Ys])
                    g.op("dve", lambda: nc.vector.tensor_copy(out=Ys[:, 512:1024], in_=py[1][:, :]), [py[1]], [Ys])
                    g.dma("act", Ys_d[b4 * 128:(b4 + 1) * 128, :], Ys[:], reads=[Ys])
                j2 = 0
                g.dma("pool", wgs[j2][:], shg.rearrange("(k p) n -> p k n", p=128), writes=[wgs[j2]])
                g.dma("pool", wus[j2][:], shu.rearrange("(k p) n -> p k n", p=128), writes=[wus[j2]])
                g.dma("pool", wds[j2][:], shd.rearrange("(k p) n -> p k n", p=128), writes=[wds[j2]])
                for t0 in range(0, NT + 1, 4):
                    tl = list(range(t0, min(t0 + 4, NT + 1)))
                    XT = XTb[(t0 // 4) % 2]
                    ntb = sum(128 if i < NT else NS for i in tl)
                    for ti, i in enumerate(tl):
                        g.dma("sp", XT[:, :, ti * 128:(ti + 1) * 128], h2T_d[i].rearrange("p (k t) -> p k t", k=8),
                              writes=[XT] if ti == 0 else [])
                    XT.w = {(("d", i_)): 16 * n_ for i_, (s_, n_) in enumerate(g.dsem) if n_ > 0}
                    expert_block(j2, XT, ntb)
                    for ti, i in enumerate(tl):
                        nn = 128 if i < NT else NS
                        Ysh = Yshb[yi % 2]
                        yi += 1
                        down(j2, ti * 128, nn)
                        g.op("act", lambda: nc.scalar.copy(out=Ysh[:nn, 0:512], in_=py[0][:nn, :]), [py[0]], [Ysh])
                        g.op("dve", lambda: nc.vector.tensor_copy(out=Ysh[:nn, 512:1024], in_=py[1][:nn, :]), [py[1]], [Ysh])
                        g.dma("act", Ysh_d[i * 128:i * 128 + nn, :], Ysh[:nn, :], reads=[Ysh])
                g.barrier()
            p5 = contextlib.ExitStack()
            with p5:
                def sb5(name, shape, dt=F32):
                    return Buf(p5.enter_context(nc.sbuf_tensor("s5_" + name, list(shape), dt)), name)
                GT2 = sb5("GT2", [128, D]); GT2s = sb5("GT2s", [NS, D])
                g.dma("sp", GT2[:], mod_d[0:1, 5 * D:6 * D].to_broadcast([128, D]), writes=[GT2])
                g.dma("sp", GT2s[:], mod_d[1:5, 5 * D:6 * D], writes=[GT2s])
                s8t = [sb5("s8t%d" % j, [128, 8], I32) for j in range(2)]
                w8t = [sb5("w8t%d" % j, [128, 8]) for j in range(2)]
                accf = [sb5("accf%d" % j, [128, D]) for j in range(2)]
                xo = [sb5("xo%d" % j, [128, D]) for j in range(2)]
                Yg = [sb5("Yg%d" % j, [128, D], BF16) for j in range(8)]
                gi_ = 0
                for i in range(NT + 1):
                    nn = 128 if i < NT else NS
                    samp = (i == NT)
                    rows = slice(i * 128, i * 128 + nn)
                    s8 = s8t[i % 2]; w8_ = w8t[i % 2]; af = accf[i % 2]; xb_ = xo[i % 2]
                    gt2 = GT2s if samp else GT2
                    g.dma("sp", s8[:], slot_d[i], writes=[s8])
                    g.dma("sp", w8_[:nn, :], w8_d[i, 0:nn, :], writes=[w8_])
                    g.dma("sp", af[:nn, :], Ysh_d[rows, :], writes=[af])
                    g.dma("sp", xb_[:nn, :], x1_d[rows, :], writes=[xb_])
                    for k8 in range(8):
                        yg = Yg[gi_ % 8]
                        gi_ += 1
                        g.idma(yg[:, :], None, Ys_d[:, :], bass.IndirectOffsetOnAxis(ap=s8[:, k8:k8 + 1], axis=0),
                               reads=[s8], writes=[yg])
                        g.op("dve", lambda: nc.vector.scalar_tensor_tensor(
                            out=af[:nn, :], in0=yg[:nn, :], scalar=w8_[:nn, k8:k8 + 1], in1=af[:nn, :],
                            op0=ALU.mult, op1=ALU.add), [yg, w8_, af], [af])
                    g.op("dve", lambda: nc.vector.tensor_tensor(out=af[:nn, :], in0=af[:nn, :], in1=gt2[:nn, :], op=ALU.mult),
                         [af, gt2], [af])
                    g.op("dve", lambda: nc.vector.tensor_add(out=xb_[:nn, :], in0=xb_[:nn, :], in1=af[:nn, :]), [xb_, af], [xb_])
                    g.dma("sp", ys if samp else y[rows, :], xb_[:nn, :], reads=[xb_], is_output=True)
                g.barrier()
        g.finish()
    return nc, dr


def core_inputs(inp, c):
    m = {}
    m["x"] = np.ascontiguousarray(inp["x_prompt"][c])
    m["xs"] = np.ascontiguousarray(inp["x_sample"][NS * c:NS * c + NS, 0])
    call = np.concatenate([inp["c_prompt"][c:c + 1], inp["c_sample"][NS * c:NS * c + NS]], axis=0)
    m["cin"] = np.ascontiguousarray(call.T.reshape(8, 128, 5).transpose(1, 0, 2))
    m["ada_w"] = inp["ada_w"][0]
    m["ada_b"] = inp["ada_b"]
    m["norm1"] = inp["norm1"]
    m["norm2"] = inp["norm2"]
    m["w_in"] = inp["w_in"][0]
    m["qg"] = np.ascontiguousarray(np.broadcast_to(inp["q_gain"][0][:, None, :], (3, 8, 64)).reshape(1, QW))
    m["kg"] = np.ascontiguousarray(np.broadcast_to(inp["k_gain"][0][:, None, :], (3, 8, 64)).reshape(1, QW))
    m["ident"] = TB["ident"]
    m["bands"] = TB["bands"]
    m["pool_w"] = inp["pool_w"][0]
    m["pool_sc"] = np.ascontiguousarray(inp["pool_scale"][0].reshape(4, 128).T)
    rb = inp["rel_bias"]
    bT = np.zeros((24, 128, 256), np.float32)
    for gi in range(3):
        idx = TB["bkt"][gi][TB["jT"]]
        for h in range(8):
            bT[gi * 8 + h] = rb[idx, gi * 8 + h]
    m["biasT"] = bT
    m["maskT"] = TB["maskT"]
    caches = {"ck128": inp["cache_k_w128"], "cv128": inp["cache_v_w128"], "ck512": inp["cache_k_w512"],
              "cv512": inp["cache_v_w512"], "ck2048": inp["cache_k_w2048"], "cv2048": inp["cache_v_w2048"]}
    for nm, arr in caches.items():
        m[nm] = np.ascontiguousarray(arr[0, NS * c:NS * c + NS].reshape(NS, arr.shape[2], 512))
    m["stp"] = np.ascontiguousarray(inp["state_pool"][0, NS * c:NS * c + NS])
    m["bias0"] = np.ascontiguousarray(rb[0:1, :])
    bs = np.zeros((3, 128, 8), np.float32)
    for gi in range(3):
        bs[gi] = rb[TB["bkt"][gi][128 - np.arange(128)], gi * 8:(gi + 1) * 8]
    m["bias_s"] = bs
    m["bd"] = TB["bd"]
    m["bandS"] = TB["bandS"]
    m["diagS"] = TB["diagS"]
    m["UT"] = TB["UT"]
    m["trash"] = TB["trash"]
    m["tri"] = TB["tri"]
    m["bstart"] = TB["bstart"]
    m["piota"] = TB["piota"]
    m["w_br_a"] = inp["w_br_a"][0]
    m["w_br_b"] = inp["w_br_b"][0]
    m["w_out"] = inp["w_out"][0]
    m["router_w"] = inp["router_w"][0]
    m["router_b"] = inp["router_bias"]
    if SPARSE:
        m["egl"] = inp["_egl"]
        m["eul"] = inp["_eul"]
        m["edl"] = inp["_edl"]
    else:
        m["eg"] = inp["exp_w_gate"][0]
        m["eu"] = inp["exp_w_up"][0]
        m["ed"] = inp["exp_w_down"][0]
    m["shg"] = inp["sh_w_gate"][0]
    m["shu"] = inp["sh_w_up"][0]
    m["shd"] = inp["sh_w_down"][0]
    return m


_NC_CACHE = {}


def prep_experts(inp):
    if SPARSE and "_egl" not in inp:
        inp["_egl"] = np.ascontiguousarray(inp["exp_w_gate"][0].reshape(NE, 8, 128, EH).transpose(0, 2, 1, 3)).reshape(NE * 128, 8 * EH)
        inp["_eul"] = np.ascontiguousarray(inp["exp_w_up"][0].reshape(NE, 8, 128, EH).transpose(0, 2, 1, 3)).reshape(NE * 128, 8 * EH)
        inp["_edl"] = np.ascontiguousarray(inp["exp_w_down"][0].reshape(NE, 2, 128, D).transpose(0, 2, 1, 3)).reshape(NE * 128, 2 * D)


def kernel(**inputs):
    inp = {k: np.asarray(v) for k, v in inputs.items()}
    prep_experts(inp)
    if "nc" not in _NC_CACHE:
        _NC_CACHE["nc"] = build_nc(stages=("all",), debug=False)
    nc, dr = _NC_CACHE["nc"]
    in_maps = []
    for c in range(NCORES):
        m = core_inputs(inp, c)
        in_maps.append({k: np.ascontiguousarray(v, dtype=np.float32) for k, v in m.items() if k in dr})
    res = run_bass_kernel_spmd(nc, in_maps, core_ids=list(range(NCORES)))
    R = res.results
    f = np.float32
    y_prompt = np.stack([R[c]["y"] for c in range(NCORES)], 0).astype(f)
    y_sample = np.concatenate([R[c]["ys"] for c in range(NCORES)], 0).reshape(NCORES * NS, 1, D).astype(f)
    outs = [y_prompt, y_sample]
    for (w, _) in GROUPS:
        for nm in ("pk", "pv"):
            outs.append(np.stack([R[c]["%s%d" % (nm, w)] for c in range(NCORES)], 0).reshape(1, NCORES, w, 8, 64).astype(f))
    outs.append(np.stack([R[c]["ppool"] for c in range(NCORES)], 0).reshape(1, NCORES, 15, 512).astype(f))
    for (w, _) in GROUPS:
        for nm in ("sk", "sv"):
            outs.append(np.concatenate([R[c]["%s%d" % (nm, w)] for c in range(NCORES)], 0).reshape(1, NCORES * NS, w, 8, 64).astype(f))
    outs.append(np.concatenate([R[c]["spool"] for c in range(NCORES)], 0).reshape(1, NCORES * NS, 15, 512).astype(f))
    return tuple(outs)
```

```python
import contextlib
import os as _os
import numpy as np
import concourse.bass as bass
import concourse.mybir as mybir
from concourse.bass_utils import run_bass_kernel_spmd

F32 = mybir.dt.float32
BF16 = mybir.dt.bfloat16
AF = mybir.ActivationFunctionType
ALU = mybir.AluOpType
AX = mybir.AxisListType

NCORES = 8
D = 1024
S = 4096
NT = S // 128
NS = 4
NTOK = S + NS
INC = 7168
QW = 1536
EPS = 1e-6
GROUPS = ((128, 1), (512, 4), (2048, 16))
NE = 256
EH = 256
PAST = 16384
NBLK = 513
NSLOT = NBLK * 128
SPARSE = _os.environ.get('MOE_DENSE') is None
I32 = mybir.dt.int32
VS = 68


class Buf:
    def __init__(self, t, name=""):
        self.t = t
        self.name = name
        self.w = {}
        self.r = {}
        self.excl = name.startswith("P:")

    def __getitem__(self, idx):
        return self.t[idx]


class G:
    def __init__(self, nc, es, n_dma_sems=40):
        self.nc = nc
        self.es = es
        self.eng = {"pe": nc.tensor, "act": nc.scalar, "dve": nc.vector, "pool": nc.gpsimd, "sp": nc.sync}
        self.sem = {}
        self.cnt = {}
        self.seen = {e: {} for e in self.eng}
        for e in self.eng:
            self.sem[e] = es.enter_context(nc.semaphore("sem_" + e))
            self.cnt[e] = 0
        self.dsem = []
        for i in range(n_dma_sems):
            self.dsem.append([es.enter_context(nc.semaphore("dsem%d" % i)), 0])
        self.dnext = 0
        self.out_tickets = []

    def _semof(self, key):
        if isinstance(key, tuple):
            return self.dsem[key[1]][0]
        return self.sem[key]

    def wait(self, e, ticket):
        key, val = ticket
        if key == e and e == "pe":
            return
        if self.seen[e].get(key, 0) >= val:
            return
        self.eng[e].wait_ge(self._semof(key), val)
        self.seen[e][key] = val

    def _deps(self, reads, writes):
        deps = {}
        for b in reads:
            for k, v in b.w.items():
                deps[k] = max(deps.get(k, 0), v)
            if b.excl:
                for k, v in b.r.items():
                    deps[k] = max(deps.get(k, 0), v)
        for b in writes:
            for k, v in b.w.items():
                deps[k] = max(deps.get(k, 0), v)
            for k, v in b.r.items():
                deps[k] = max(deps.get(k, 0), v)
        return deps

    def _mark(self, t, reads, writes):
        for b in reads:
            b.r[t[0]] = max(b.r.get(t[0], 0), t[1])
        for b in writes:
            b.w = {t[0]: t[1]}
            b.r = {}

    def barrier(self):
        for e in self.eng:
            for i, (sem, n) in enumerate(self.dsem):
                if n > 0:
                    self.wait(e, (("d", i), 16 * n))
            for e2 in self.eng:
                if e2 != e and self.cnt[e2] > 0:
                    self.wait(e, (e2, self.cnt[e2]))

    def op(self, e, fn, reads=(), writes=()):
        for k, v in self._deps(reads, writes).items():
            self.wait(e, (k, v))
        inst = fn()
        self.cnt[e] += 1
        inst.then_inc(self.sem[e], 1)
        t = (e, self.cnt[e])
        self._mark(t, reads, writes)
        return t

    def mm(self, mms, reads, out):
        for k, v in self._deps(reads, [out]).items():
            self.wait("pe", (k, v))
        n = len(mms)
        inst = None
        for i, (o, l, r) in enumerate(mms):
            inst = self.nc.tensor.matmul(o, lhsT=l, rhs=r, start=(i == 0), stop=(i == n - 1))
        self.cnt["pe"] += 1
        inst.then_inc(self.sem["pe"], 1)
        t = ("pe", self.cnt["pe"])
        self._mark(t, reads, [out])
        return t

    def tr(self, trs, reads, out):
        for k, v in self._deps(reads, [out]).items():
            self.wait("pe", (k, v))
        inst = None
        for (o, i_, idn) in trs:
            inst = self.nc.tensor.transpose(o, i_, idn)
        self.cnt["pe"] += 1
        inst.then_inc(self.sem["pe"], 1)
        t = ("pe", self.cnt["pe"])
        self._mark(t, reads, [out])
        return t

    def dma(self, q, out, in_, reads=(), writes=(), is_output=False, **kw):
        for k, v in self._deps(reads, writes).items():
            self.wait(q, (k, v))
        i = self.dnext
        self.dnext = (self.dnext + 1) % len(self.dsem)
        sem, n = self.dsem[i]
        if n > 0:
            self.wait(q, (("d", i), 16 * n))
        self.eng[q].dma_start(out=out, in_=in_, **kw).then_inc(sem, 16)
        self.dsem[i][1] = n + 1
        t = (("d", i), 16 * (n + 1))
        self._mark(t, reads, writes)
        if is_output:
            self.out_tickets.append(t)
        return t

    def idma(self, out, out_off, in_, in_off, reads=(), writes=()):
        q = "pool"
        for k, v in self._deps(reads, writes).items():
            self.wait(q, (k, v))
        i = self.dnext
        self.dnext = (self.dnext + 1) % len(self.dsem)
        sem, n = self.dsem[i]
        if n > 0:
            self.wait(q, (("d", i), 16 * n))
        self.nc.gpsimd.indirect_dma_start(out=out, out_offset=out_off, in_=in_, in_offset=in_off).then_inc(sem, 16)
        self.dsem[i][1] = n + 1
        t = (("d", i), 16 * (n + 1))
        self._mark(t, reads, writes)
        return t

    def finish(self):
        for i, (sem, n) in enumerate(self.dsem):
            if n > 0:
                self.wait("sp", (("d", i), 16 * n))
        for e in ("pe", "act", "dve", "pool"):
            if self.cnt[e] > 0:
                self.wait("sp", (e, self.cnt[e]))


def t5_bucket(dist):
    exact = 16
    d = np.asarray(dist)
    large = exact + (np.log(np.maximum(d, 1) / exact) / np.log(2048 / exact) * (32 - exact)).astype(np.int32)
    large = np.minimum(large, 31)
    return np.where(d < exact, d, large).astype(np.int32)


def static_tables():
    tb = {}
    k = np.arange(128)[:, None]
    qq = np.arange(256)[None, :]
    j = qq - k
    valid = (j >= 0) & (j <= 128)
    tb["maskT"] = valid.astype(np.float32)
    tb["jT"] = np.clip(j, 0, 128)
    tb["bkt"] = [t5_bucket(np.arange(129) * dil) for (_, dil) in GROUPS]
    tb["ident"] = np.eye(128, dtype=np.float32)
    bands = np.zeros((3, 4, 128, 128), np.float32)
    for gi, w in enumerate((2, 4, 8, 16)):
        for t in range(128):
            for tp in range(t - w + 1, t + 1):
                if tp >= 0:
                    bands[0, gi, tp, t] += 1.0 / w
                    bands[2, gi, tp, t] += 1.0 / min(t + 1, w)
                else:
                    bands[1, gi, 128 + tp, t] += 1.0 / w
            bands[0, gi, t, t] -= 1.0
            bands[2, gi, t, t] -= 1.0
    tb["bands"] = bands
    tb["UT"] = np.triu(np.ones((128, 128), np.float32), 1)
    tb["trash"] = np.repeat((NSLOT + 1.0 + np.arange(128, dtype=np.float32))[:, None], 8, 1)
    ee = np.arange(NE)[None, None, :]
    tb["tri"] = ((np.arange(2)[:, None, None] * 128 + np.arange(128)[None, :, None]) <= ee).astype(np.float32)
    tb["bstart"] = ((np.arange(5)[None, :] * 128 + np.arange(128)[:, None]) * 128).astype(np.float32)
    tb["piota"] = np.arange(128, dtype=np.float32).reshape(128, 1)
    bd = np.zeros((8, 8, 65), np.float32)
    for h in range(8):
        bd[h, h, :] = 1.0
    tb["bd"] = bd
    bandS = np.zeros((4, 60, NS), np.float32)
    diagS = np.zeros((4, NS, NS), np.float32)
    for gi, w in enumerate((2, 4, 8, 16)):
        for b in range(NS):
            for i in range(15 - (w - 1), 15):
                bandS[gi, b * 15 + i, b] = 1.0 / w
            diagS[gi, b, b] = 1.0 / w - 1.0
    tb["bandS"] = bandS
    tb["diagS"] = diagS
    return tb


TB = static_tables()


def build_nc(stages=("all",), debug=False):
    nc = bass.Bass("TRN2", target_bir_lowering=False)
    es = contextlib.ExitStack()
    dr = {}

    def din(name, shape, dt=F32):
        dr[name] = nc.dram_tensor(name, list(shape), dt, kind="ExternalInput").ap()
        return dr[name]

    def dout(name, shape, dt=F32):
        dr[name] = nc.dram_tensor(name, list(shape), dt, kind="ExternalOutput").ap()
        return dr[name]

    def dscr(name, shape, dt=F32):
        kind = "ExternalOutput" if debug else "Internal"
        dr[name] = nc.dram_tensor(name, list(shape), dt, kind=kind).ap()
        return dr[name]

    x = din("x", [S, D])
    xs = din("xs", [NS, D])
    cin = din("cin", [128, 8, 5])
    ada_w = din("ada_w", [D, 6 * D])
    ada_b = din("ada_b", [1, 6 * D])
    norm1 = din("norm1", [1, D])
    norm2 = din("norm2", [1, D])
    w_in = din("w_in", [D, INC])
    qg = din("qg", [1, QW])
    kg = din("kg", [1, QW])
    ident_d = din("ident", [128, 128])
    bands_d = din("bands", [3, 4, 128, 128])
    pool_w = din("pool_w", [4, 128, 128])
    pool_sc = din("pool_sc", [128, 4])
    y = dout("y", [S, D])
    ys = dout("ys", [NS, D])
    pk = [dout("pk%d" % w, [w, 512]) for (w, _) in GROUPS]
    pv = [dout("pv%d" % w, [w, 512]) for (w, _) in GROUPS]
    ppool = dout("ppool", [15, 512])
    mod_d = dscr("mod_d", [5, 6 * D])
    QT_d = dscr("QT_d", [QW, S], BF16)
    KT_d = dscr("KT_d", [QW, S], BF16)
    V1_d = dscr("V1_d", [S, 24 * VS], BF16)
    qkvs_d = dscr("qkvs_d", [NS, 3 * QW])
    gates_d = dscr("gates_d", [NTOK, 2048], BF16)
    ybT_d = dscr("ybT_d", [NT + 1, 128, 512], BF16)
    us_d = dscr("us_d", [NS, 512])
    biasT_d = din("biasT", [24, 128, 256])
    maskT_d = din("maskT", [128, 256])
    A_d = dscr("A_d", [3, S, 520])
    w_br_a = din("w_br_a", [512, D])
    w_br_b = din("w_br_b", [512, D])
    w_out = din("w_out", [D, D])
    router_w = din("router_w", [D, NE])
    router_b = din("router_b", [1, NE])
    if not SPARSE:
        eg = din("eg", [NE, D, EH])
        eu = din("eu", [NE, D, EH])
        ed = din("ed", [NE, EH, D])
    shg = din("shg", [D, EH])
    shu = din("shu", [D, EH])
    shd = din("shd", [EH, D])
    x1_d = dscr("x1_d", [NTOK, D])
    h2T_d = dscr("h2T_d", [NT + 1, 128, 8 * 128], BF16)
    Wd_d = dscr("Wd_d", [NT + 1, 128, NE])
    oas_d = dscr("oas_d", [NS, 512])
    Xs_d = dscr("Xs_d", [NSLOT + 128, D], BF16)
    Ys_d = dscr("Ys_d", [NSLOT + 128, D], BF16)
    Ysh_d = dscr("Ysh_d", [NTOK, D])
    slot_d = dscr("slot_d", [NT + 1, 128, 8], I32)
    w8_d = dscr("w8_d", [NT + 1, 128, 8])
    pos_d = dscr("pos_d", [NT + 1, 128, NE])
    h2b_d = dscr("h2b_d", [NT + 1, 128, D], BF16)
    be_d = dscr("be_d", [1, 640])
    egb = dscr("egb", [NE * 128, 8 * EH], BF16)
    eub = dscr("eub", [NE * 128, 8 * EH], BF16)
    edb = dscr("edb", [NE * 128, 2 * D], BF16)
    tri_d = din("tri", [2, 128, NE])
    bstart_d = din("bstart", [128, 5])
    piota_d = din("piota", [128, 1])
    egl = din("egl", [NE * 128, 8 * EH])
    eul = din("eul", [NE * 128, 8 * EH])
    edl = din("edl", [NE * 128, 2 * D])
    UT_d = din("UT", [128, 128])
    trash_d = din("trash", [128, 8])
    As_d = dscr("As_d", [NS, 3, 520])
    ck = [din("ck%d" % w, [NS, w, 512]) for (w, _) in GROUPS]
    cv = [din("cv%d" % w, [NS, w, 512]) for (w, _) in GROUPS]
    stp = din("stp", [NS, 15, 512])
    bias0_d = din("bias0", [1, 24])
    bias_s_d = din("bias_s", [3, 128, 8])
    bd_d = din("bd", [8, 8, 65])
    bandS_d = din("bandS", [4, 60, NS])
    diagS_d = din("diagS", [4, NS, NS])
    sk = [dout("sk%d" % w, [NS, w, 512]) for (w, _) in GROUPS]
    sv = [dout("sv%d" % w, [NS, w, 512]) for (w, _) in GROUPS]
    spool = dout("spool", [NS, 15, 512])

    with es:
        g = G(nc, es)
        cvt_sem = es.enter_context(nc.semaphore("cvt_sem"))
        cvt_list = []
        if SPARSE:
            for (src_, dst_) in ((egl, egb), (eul, eub), (edl, edb)):
                for r0_ in range(0, NE * 128, 1024):
                    cvt_list.append((src_[r0_:r0_ + 1024, :], dst_[r0_:r0_ + 1024, :]))
        cvt_total = len(cvt_list)

        def cvt_emit(nmax):
            for _ in range(nmax):
                if cvt_list:
                    s_, d_ = cvt_list.pop(0)
                    nc.gpsimd.dma_start(out=d_, in_=s_).then_inc(cvt_sem, 16)

        def sb(name, shape, dt=F32):
            return Buf(es.enter_context(nc.sbuf_tensor("s_" + name, list(shape), dt)), name)

        def ps(name, shape, dt=F32):
            return Buf(es.enter_context(nc.psum_tensor("p_" + name, list(shape), dt)), name)

        ident_f = sb("ident_f", [128, 128])
        g.dma("sp", ident_f[:], ident_d, writes=[ident_f])
        nhalf = sb("nhalf", [128, 8])
        g.op("dve", lambda: nc.vector.memset(nhalf[:], -0.5), [], [nhalf])
        ident_b = sb("ident_b", [128, 128], BF16)
        g.dma("pool", ident_b[:], ident_d, writes=[ident_b])

        if True:
            p0 = contextlib.ExitStack()
            with p0:
                def sb0(name, shape, dt=F32):
                    return Buf(p0.enter_context(nc.sbuf_tensor("s_" + name, list(shape), dt)), name)
                cT = sb0("cT", [128, 8, 5])
                sg = sb0("sg", [128, 8, 5])
                g.dma("sp", cT[:], cin, writes=[cT])
                g.op("act", lambda: nc.scalar.activation(out=sg[:], in_=cT[:], func=AF.Sigmoid), [cT], [sg])
                g.op("dve", lambda: nc.vector.tensor_mul(out=sg[:], in0=cT[:], in1=sg[:]), [cT, sg], [sg])
                adab = sb0("adab", [5, 6 * D])
                g.dma("act", adab[:], ada_b.to_broadcast([5, 6 * D]), writes=[adab])
                modsb = sb0("modsb", [5, 6 * D])
                wbuf = [sb0("adaw%d" % i, [128, 8, 512]) for i in range(2)]
                pmod = [Buf(p0.enter_context(nc.psum_tensor("pmod%d" % i, [5, 512], F32)), "P:pmod") for i in range(2)]
                for cg in range(12):
                    wb = wbuf[cg % 2]
                    g.dma("sp" if cg % 2 == 0 else "act", wb[:],
                          ada_w[:, cg * 512:(cg + 1) * 512].rearrange("(k p) n -> p k n", p=128), writes=[wb])
                    pm = pmod[cg % 2]
                    g.mm([(pm[:], sg[:, k, :], wb[:, k, :]) for k in range(8)], [sg, wb], pm)
                    g.op("dve", lambda pm=pm, cg=cg: nc.vector.tensor_add(
                        out=modsb[:, cg * 512:(cg + 1) * 512], in0=pm[:], in1=adab[:, cg * 512:(cg + 1) * 512]),
                        [pm, adab], [modsb])
                g.dma("sp", mod_d, modsb[:], reads=[modsb])
                g.barrier()

        if "p1" in stages or "all" in stages:
            p1 = contextlib.ExitStack()
            with p1:
                def sb1(name, shape, dt=F32):
                    return Buf(p1.enter_context(nc.sbuf_tensor("s_" + name, list(shape), dt)), name)

                def ps1(name, shape, dt=F32):
                    return Buf(p1.enter_context(nc.psum_tensor("p_" + name, list(shape), dt)), "P:" + name)

                w_in_sb = sb1("w_in_sb", [128, 8, INC], BF16)
                wchunks = [Buf(None, "wc%d" % i) for i in range(14)]
                _skip = _os.environ.get('SKIP', '').split(',')
                for cg in range(14 if 'win' not in _skip else 0):
                    g.dma("pool", w_in_sb[:, :, cg * 512:(cg + 1) * 512],
                          w_in[:, cg * 512:(cg + 1) * 512].rearrange("(k p) n -> p k n", p=128),
                          writes=[wchunks[cg]])
                G1 = sb1("G1", [128, D]); SH1 = sb1("SH1", [128, D])
                G1s = sb1("G1s", [NS, D]); SH1s = sb1("SH1s", [NS, D])
                n1b = sb1("n1b", [128, D])
                g.dma("sp", n1b[:], norm1.to_broadcast([128, D]), writes=[n1b])
                g.dma("sp", G1[:], mod_d[0:1, D:2 * D].to_broadcast([128, D]), writes=[G1])
                g.dma("sp", SH1[:], mod_d[0:1, 0:D].to_broadcast([128, D]), writes=[SH1])
                g.dma("sp", G1s[:], mod_d[1:5, D:2 * D], writes=[G1s])
                g.dma("sp", SH1s[:], mod_d[1:5, 0:D], writes=[SH1s])
                g.op("dve", lambda: nc.vector.scalar_tensor_tensor(
                    out=G1[:], in0=G1[:], scalar=1.0, in1=n1b[:], op0=ALU.add, op1=ALU.mult), [G1, n1b], [G1])
                g.op("dve", lambda: nc.vector.scalar_tensor_tensor(
                    out=G1s[:], in0=G1s[:], scalar=1.0, in1=n1b[:NS, :], op0=ALU.add, op1=ALU.mult), [G1s, n1b], [G1s])
                qgb = sb1("qgb", [128, QW]); kgb = sb1("kgb", [128, QW])
                g.dma("sp", qgb[:], qg.to_broadcast([128, QW]), writes=[qgb])
                g.dma("sp", kgb[:], kg.to_broadcast([128, QW]), writes=[kgb])
                g.op("dve", lambda: nc.vector.tensor_scalar_mul(out=qgb[:], in0=qgb[:], scalar1=0.125), [qgb], [qgb])
                bands = sb1("bands", [128, 12, 128])
                if 'bands' not in _skip:
                    g.dma("sp", bands[:], bands_d.rearrange("a g p t -> p (a g) t"), writes=[bands])
                pw_sb = sb1("pw_sb", [128, 4, 128], BF16)
                if 'poolw' not in _skip:
                    g.dma("pool", pw_sb[:], pool_w.rearrange("g c d -> c g d"), writes=[pw_sb])
                psc = sb1("psc", [128, 4])
                g.dma("sp", psc[:], pool_sc, writes=[psc])

                xt = [sb1("xt%d" % i, [128, D]) for i in range(2)]
                sq = sb1("sq", [128, D])
                hb = sb1("hb", [128, D], BF16)
                hT = sb1("hT", [128, 8, 128], BF16)
                ssq = sb1("ssq", [128, 1]); rstd = sb1("rstd", [128, 1])
                ss8 = sb1("ss8", [128, 8]); rs8 = sb1("rs8", [128, 8])
                qf = [sb1("qf%d" % i, [128, 512]) for i in range(2)]
                kf = [sb1("kf%d" % i, [128, 512]) for i in range(2)]
                vf = [sb1("vf%d" % i, [128, 512]) for i in range(2)]
                V1 = sb1("V1", [128, 24, VS], BF16)
                if 'memv1' not in _skip:
                    g.op("dve", lambda: nc.vector.memset(V1[:], 1.0), [], [V1])
                QTst = sb1("QTst", [128, 12, 128], BF16)
                KTst = sb1("KTst", [128, 12, 128], BF16)
                ut = [sb1("ut%d" % i, [128, 512]) for i in range(2)]
                gts = sb1("gts", [128, 2048], BF16)
                pooledT = sb1("pooledT", [128, 4, 128], BF16)
                ybT = sb1("ybT", [128, 4, 128], BF16)
                pz = [ps1("pz%d" % i, [128, 512]) for i in range(3)]
                ptr = ps1("ptr", [128, 8, 128], BF16)
                ptq = ps1("ptq", [128, 4, 128])
                ppl = ps1("ppl", [128, 4, 128])
                pmx = ps1("pmx", [128, 4, 128])

                def load_x(i):
                    if i < NT:
                        g.dma("sp", xt[i % 2][:], x[i * 128:(i + 1) * 128, :], writes=[xt[i % 2]])
                    else:
                        g.dma("sp", xt[i % 2][:NS, :], xs, writes=[xt[i % 2]])

                load_x(0)
                pzi = 0
                _tl = _os.environ.get('P1_TILES')
                _tiles = list(range(NT + 1)) if _tl is None else [int(v) for v in _tl.split(',') if v != '']
                for i in _tiles:
                    n = 128 if i < NT else NS
                    samp = (i == NT)
                    if i + 1 <= NT and _tl is None:
                        load_x(i + 1)
                    if _tl is not None and i != 0:
                        load_x(i)
                    xb = xt[i % 2]
                    Gm, Sm = (G1s, SH1s) if samp else (G1, SH1)
                    g.op("act", lambda: nc.scalar.activation(out=sq[:n, :], in_=xb[:n, :], func=AF.Square,
                                                             accum_out=ssq[:n, :]), [xb], [sq, ssq])
                    g.op("dve", lambda: nc.vector.tensor_scalar(out=rstd[:n, :], in0=ssq[:n, :], scalar1=1.0 / D,
                                                                scalar2=EPS, op0=ALU.mult, op1=ALU.add), [ssq], [rstd])
                    g.op("pool", lambda: nc.gpsimd.tensor_tensor(out=rstd[:n, :], in0=rstd[:n, :], in1=nhalf[:n, 0:1],
                                                                 op=ALU.pow), [rstd, nhalf], [rstd])
                    g.op("dve", lambda: nc.vector.scalar_tensor_tensor(
                        out=sq[:n, :], in0=xb[:n, :], scalar=rstd[:n, :], in1=Gm[:n, :], op0=ALU.mult, op1=ALU.mult),
                        [xb, rstd, Gm], [sq])
                    g.op("dve", lambda: nc.vector.tensor_add(out=hb[:n, :], in0=sq[:n, :], in1=Sm[:n, :]), [sq, Sm], [hb])
                    LVL = int(_os.environ.get('P1_LVL', '99'))
                    if LVL < 2:
                        continue
                    g.tr([(ptr[:, k, :n], hb[:n, k * 128:(k + 1) * 128], ident_b[:n, :n]) for k in range(8)],
                         [hb, ident_b], ptr)
                    g.op("act", lambda: nc.scalar.copy(out=hT[:, :, :n], in_=ptr[:, :, :n]), [ptr], [hT])
                    if LVL < 3:
                        continue
                    ucur = ut[i % 2]
                    pend = [None]
                    for cg in range(14):
                        pzb = pz[pzi % 3]
                        pzi += 1
                        g.mm([(pzb[:n, :], hT[:, k, :n], w_in_sb[:, k, cg * 512:(cg + 1) * 512]) for k in range(8)],
                             [hT, wchunks[cg]], pzb)
                        if LVL < 4:
                            continue
                        if cg < 6:
                            gi = cg % 3
                            isq = cg < 3
                            dst = (qf if isq else kf)[gi % 2]
                            gain = qgb if isq else kgb
                            g.op("act", lambda: nc.scalar.copy(out=dst[:n, :], in_=pzb[:n, :]), [pzb], [dst])
                            g.op("act", lambda: nc.scalar.activation(out=sq[:n, :512], in_=dst[:n, :], func=AF.Square),
                                 [dst], [sq])
                            g.op("dve", lambda: nc.vector.tensor_reduce(
                                out=ss8[:n, :], in_=sq[:n, :512].rearrange("p (h e) -> p h e", e=64),
                                axis=AX.X, op=ALU.add), [sq], [ss8])
                            g.op("dve", lambda: nc.vector.tensor_scalar(out=rs8[:n, :], in0=ss8[:n, :], scalar1=1.0 / 64,
                                                                        scalar2=EPS, op0=ALU.mult, op1=ALU.add), [ss8], [rs8])
                            g.op("pool", lambda: nc.gpsimd.tensor_tensor(out=rs8[:n, :], in0=rs8[:n, :], in1=nhalf[:n, :],
                                                                         op=ALU.pow), [rs8, nhalf], [rs8])
                            g.op("dve", lambda: nc.vector.tensor_tensor(
                                out=dst[:n, :].rearrange("p (h e) -> p h e", e=64),
                                in0=dst[:n, :].rearrange("p (h e) -> p h e", e=64),
                                in1=rs8[:n, :].unsqueeze(2).to_broadcast([n, 8, 64]), op=ALU.mult), [dst, rs8], [dst])
                            g.op("dve", lambda: nc.vector.tensor_mul(out=dst[:n, :], in0=dst[:n, :],
                                                                     in1=gain[:n, gi * 512:(gi + 1) * 512]), [dst, gain], [dst])

                            def finish_qk(dst=dst, isq=isq, gi=gi, n=n, samp=samp, i=i):
                                if not samp:
                                    g.tr([(ptq[:, j, :n], dst[:n, j * 128:(j + 1) * 128], ident_f[:n, :n]) for j in range(4)],
                                         [dst, ident_f], ptq)
                                    st = QTst if isq else KTst
                                    g.op("act", lambda: nc.scalar.copy(out=st[:, gi * 4:(gi + 1) * 4, :], in_=ptq[:]), [ptq], [st])
                                    if not isq:
                                        W = GROUPS[gi][0]
                                        r0 = i * 128 - (S - W)
                                        if r0 >= 0:
                                            g.dma("sp", pk[gi][r0:r0 + 128, :], dst[:], reads=[dst], is_output=True)
                                else:
                                    off = (0 if isq else QW) + gi * 512
                                    g.dma("sp", qkvs_d[:, off:off + 512], dst[:NS, :], reads=[dst])
                            if pend[0] is not None:
                                pend[0]()
                            pend[0] = finish_qk
                            continue
                        if pend[0] is not None:
                            pend[0]()
                            pend[0] = None
                            if not samp:
                                cvt_emit(1)
                        if LVL < 6:
                            continue
                        if cg < 9:
                            gi = cg - 6
                            dst = vf[gi % 2]
                            if 'vact' not in _skip:
                                g.op("dve", lambda: nc.vector.tensor_copy(out=dst[:n, :], in_=pzb[:n, :]), [pzb], [dst])
                            if not samp and 'vcopy' not in _skip:
                                g.op("act", lambda: nc.scalar.copy(
                                    out=V1[:, gi * 8:(gi + 1) * 8, 0:64], in_=dst[:, :].rearrange("p (h e) -> p h e", e=64)),
                                    [dst], [V1])
                                W = GROUPS[gi][0]
                                r0 = i * 128 - (S - W)
                                if r0 >= 0:
                                    g.dma("sp", pv[gi][r0:r0 + 128, :], dst[:], reads=[dst], is_output=True)
                            else:
                                off = 2 * QW + gi * 512
                                g.dma("sp", qkvs_d[:, off:off + 512], dst[:NS, :], reads=[dst])
                        elif LVL < 7:
                            continue
                        elif cg == 9:
                            g.op("act", lambda: nc.scalar.copy(out=ucur[:n, :], in_=pzb[:n, :]), [pzb], [ucur])
                        else:
                            c0 = (cg - 10) * 512
                            g.op("act", lambda: nc.scalar.activation(out=gts[:n, c0:c0 + 512], in_=pzb[:n, :],
                                                                     func=AF.Sigmoid), [pzb], [gts])
                    if LVL < 8:
                        continue
                    if not samp:
                        g.dma("sp", QT_d[:, i * 128:(i + 1) * 128].rearrange("(j p) t -> p j t", p=128), QTst[:], reads=[QTst])
                        g.dma("sp", KT_d[:, i * 128:(i + 1) * 128].rearrange("(j p) t -> p j t", p=128), KTst[:], reads=[KTst])
                        g.dma("sp", V1_d[i * 128:(i + 1) * 128, :], V1[:].rearrange("p a b -> p (a b)"), reads=[V1])
                        g.dma("sp", gates_d[i * 128:(i + 1) * 128, :], gts[:], reads=[gts])
                        if i == NT - 1:
                            g.dma("sp", ppool, ucur[113:128, :], reads=[ucur], is_output=True)
                        if LVL < 9:
                            continue
                        uprev = ut[(i + 1) % 2]
                        for gi in range(4):
                            cs = slice(gi * 128, (gi + 1) * 128)
                            if i == 0:
                                mms = [(ppl[:, gi, :], ucur[:, cs], bands[:, 8 + gi, :])]
                            else:
                                mms = [(ppl[:, gi, :], ucur[:, cs], bands[:, gi, :]),
                                       (ppl[:, gi, :], uprev[:, cs], bands[:, 4 + gi, :])]
                            g.mm(mms, [ucur, uprev, bands], ppl)
                        g.op("dve", lambda: nc.vector.tensor_copy(out=pooledT[:], in_=ppl[:]), [ppl], [pooledT])
                        for gi in range(4):
                            g.mm([(pmx[:, gi, :], pw_sb[:, gi, :], pooledT[:, gi, :])], [pw_sb, pooledT], pmx)
                        for gi in range(4):
                            g.op("act", lambda gi=gi: nc.scalar.activation(out=ybT[:, gi, :], in_=pmx[:, gi, :], func=AF.Copy,
                                                                          scale=psc[:, gi:gi + 1]), [pmx, psc], [ybT])
                        g.dma("sp", ybT_d[i], ybT[:].rearrange("p a b -> p (a b)"), reads=[ybT])
                    else:
                        g.dma("sp", gates_d[S:S + NS, :], gts[:NS, :], reads=[gts])
                        g.dma("sp", us_d, ucur[:NS, :], reads=[ucur])
                g.barrier()
        if "p2" in stages or "all" in stages:
            p2 = contextlib.ExitStack()
            with p2:
                def sb2(name, shape, dt=F32):
                    return Buf(p2.enter_context(nc.sbuf_tensor("s2_" + name, list(shape), dt)), name)

                def ps2(name, shape, dt=F32):
                    return Buf(p2.enter_context(nc.psum_tensor("p2_" + name, list(shape), dt)), "P:" + name)

                Eb = sb2("Eb", [128, 24, 256], BF16)
                mk = sb2("mk", [128, 256])
                g.dma("sp", mk[:], maskT_d, writes=[mk])
                for c4 in range(6):
                    bt = sb2("bt%d" % c4, [128, 4, 256])
                    g.dma("sp", bt[:], biasT_d[c4 * 4:(c4 + 1) * 4].rearrange("a k q -> k a q"), writes=[bt])
                    g.op("act", lambda: nc.scalar.activation(out=bt[:], in_=bt[:], func=AF.Exp), [bt], [bt])
                    g.op("dve", lambda: nc.vector.tensor_tensor(
                        out=Eb[:, c4 * 4:(c4 + 1) * 4, :], in0=bt[:], in1=mk[:].unsqueeze(1).to_broadcast([128, 4, 256]),
                        op=ALU.mult), [bt, mk], [Eb])
                QTg = sb2("QTg", [128, 4, S], BF16)
                KTg = sb2("KTg", [128, 4, S], BF16)
                V1g = sb2("V1g", [128, 32, 8 * VS], BF16)
                PT = [[sb2("PT%d_%d" % (h, j), [128, 256], BF16) for j in range(2)] for h in range(8)]
                pe32 = [sb2("pe32_%d" % j, [128, 256]) for j in range(2)]
                oacc = [sb2("oacc%d" % j, [128, 8, 65]) for j in range(2)]
                pS = [ps2("pS%d" % j, [128, 512]) for j in range(4)]
                pO = [[ps2("pO%d_%d" % (j, hh), [128, 512]) for hh in range(2)] for j in range(2)]

                def sl(s0, c, st):
                    return slice(s0, s0 + st * (c - 1) + 1, st)

                si = 0
                oi = 0
                for gi, (W, dil) in enumerate(GROUPS):
                    L = S // dil
                    nb = L // 128
                    g.dma("sp", QTg[:], QT_d[gi * 512:(gi + 1) * 512, :].rearrange("(j p) t -> p j t", p=128), writes=[QTg])
                    g.dma("act", KTg[:], KT_d[gi * 512:(gi + 1) * 512, :].rearrange("(j p) t -> p j t", p=128), writes=[KTg])
                    vsrc = V1_d[:, gi * 8 * VS:(gi + 1) * 8 * VS].rearrange("(cb a r) c -> a r cb c", a=128, r=dil)
                    vdst = V1g[:].rearrange("p (r cb) c -> p r cb c", r=dil)
                    nsplit = max(1, 4 // dil)
                    cbs = nb // nsplit
                    wt = []
                    for r in range(dil):
                        for sp_ in range(nsplit):
                            g.dma("sp", vdst[:, r, sp_ * cbs:(sp_ + 1) * cbs, :], vsrc[:, r, sp_ * cbs:(sp_ + 1) * cbs, :],
                                  writes=[V1g] if (r == 0 and sp_ == 0) else [])
                    V1g.w = {(("d", i)): 16 * n for i, (s_, n) in enumerate(g.dsem) if n > 0}
                    Adst = A_d[gi].rearrange("(cb a r) c -> r cb a c", a=128, r=dil)
                    for r in range(dil):
                        cvt_emit(1)
                        for kb in range(nb):
                            nq = 256 if kb < nb - 1 else 128
                            for h in range(8):
                                j = h // 2
                                rows = slice((h % 2) * 64, (h % 2) * 64 + 64)
                                psb = pS[si % 4]
                                si += 1
                                g.mm([(psb[:, :nq], KTg[rows, j, sl(r + dil * kb * 128, 128, dil)],
                                       QTg[rows, j, sl(r + dil * kb * 128, nq, dil)])], [KTg, QTg], psb)
                                e32 = pe32[si % 2]
                                g.op("act", lambda: nc.scalar.activation(out=e32[:, :nq], in_=psb[:, :nq], func=AF.Exp),
                                     [psb], [e32])
                                ptb = PT[h][kb % 2]
                                g.op("dve", lambda: nc.vector.tensor_tensor(out=ptb[:, :nq], in0=e32[:, :nq],
                                                                            in1=Eb[:, gi * 8 + h, :nq], op=ALU.mult),
                                     [e32, Eb], [ptb])
                            po = pO[oi % 2]
                            ob = oacc[oi % 2]
                            oi += 1
                            bi = r * nb + kb
                            for h in range(8):
                                pob = po[h // 4]
                                mms = []
                                if kb > 0:
                                    mms.append((pob[:, (h % 4) * 65:(h % 4) * 65 + 65], PT[h][(kb - 1) % 2][:, 128:256], V1g[:, bi - 1, h * VS:h * VS + 65]))
                                mms.append((pob[:, (h % 4) * 65:(h % 4) * 65 + 65], PT[h][kb % 2][:, 0:128], V1g[:, bi, h * VS:h * VS + 65]))
                                g.mm(mms, [PT[h][0], PT[h][1], V1g], pob)
                            g.op("act", lambda: nc.scalar.copy(out=ob[:, 0:4, :].rearrange("p a b -> p (a b)"), in_=po[0][:, 0:260]), [po[0]], [ob])
                            g.op("dve", lambda: nc.vector.tensor_copy(out=ob[:, 4:8, :].rearrange("p a b -> p (a b)"), in_=po[1][:, 0:260]), [po[1]], [ob])
                            g.dma("sp", Adst[r, kb], ob[:].rearrange("p a b -> p (a b)"), reads=[ob])
                g.barrier()
        if "p2b" in stages or "all" in stages:
            for gi, (W, dil) in enumerate(GROUPS):
                for b in range(NS):
                    for (src, dst, off) in ((ck[gi], sk[gi], QW), (cv[gi], sv[gi], 2 * QW)):
                        for r0 in range(1, W, 512):
                            r1 = min(W, r0 + 512)
                            g.dma("act", dst[b, r0 - 1:r1 - 1, :], src[b, r0:r1, :], is_output=True)
                        g.dma("act", dst[b, W - 1:W, :], qkvs_d[b:b + 1, off + gi * 512:off + (gi + 1) * 512], is_output=True)
            for b in range(NS):
                g.dma("act", spool[b, 0:14, :], stp[b, 1:15, :], is_output=True)
                g.dma("act", spool[b, 14:15, :], us_d[b:b + 1, :], is_output=True)
            pb = contextlib.ExitStack()
            with pb:
                def sbb(name, shape, dt=F32):
                    return Buf(pb.enter_context(nc.sbuf_tensor("sb_" + name, list(shape), dt)), name)

                def psb_(name, shape, dt=F32):
                    return Buf(pb.enter_context(nc.psum_tensor("pb_" + name, list(shape), dt)), "P:" + name)

                qs = sbb("qs", [NS, QW]); ks = sbb("ks", [NS, QW]); vs_ = sbb("vs", [NS, QW])
                g.dma("sp", qs[:], qkvs_d[:, 0:QW], writes=[qs])
                g.dma("sp", ks[:], qkvs_d[:, QW:2 * QW], writes=[ks])
                g.dma("sp", vs_[:], qkvs_d[:, 2 * QW:3 * QW], writes=[vs_])
                prod = sbb("prod", [NS, QW])
                s0 = sbb("s0", [NS, 24]); b0 = sbb("b0", [NS, 24]); p0 = sbb("p0", [NS, 24])
                num0 = sbb("num0", [NS, 24, 64])
                g.dma("sp", b0[:], bias0_d.to_broadcast([NS, 24]), writes=[b0])
                g.op("dve", lambda: nc.vector.tensor_mul(out=prod[:], in0=qs[:], in1=ks[:]), [qs, ks], [prod])
                g.op("dve", lambda: nc.vector.tensor_reduce(out=s0[:], in_=prod[:].rearrange("p (h e) -> p h e", e=64),
                                                            axis=AX.X, op=ALU.add), [prod], [s0])
                g.op("dve", lambda: nc.vector.tensor_add(out=s0[:], in0=s0[:], in1=b0[:]), [s0, b0], [s0])
                g.op("act", lambda: nc.scalar.activation(out=p0[:], in_=s0[:], func=AF.Exp), [s0], [p0])
                g.op("dve", lambda: nc.vector.tensor_tensor(
                    out=num0[:], in0=vs_[:].rearrange("p (h e) -> p h e", e=64),
                    in1=p0[:].unsqueeze(2).to_broadcast([NS, 24, 64]), op=ALU.mult), [vs_, p0], [num0])
                BD = sbb("BD", [8, 8, 65])
                g.dma("sp", BD[:], bd_d, writes=[BD])
                ones8 = sbb("ones8", [8, 1])
                g.op("dve", lambda: nc.vector.memset(ones8[:], 1.0), [], [ones8])
                bsm = sbb("bsm", [128, 3, 8])
                g.dma("sp", bsm[:], bias_s_d.rearrange("g k h -> k g h"), writes=[bsm])
                Ksel = [sbb("Ksel%d" % j, [128, 512]) for j in range(2)]
                V1s = [sbb("V1s%d" % j, [128, 8, 65]) for j in range(2)]
                for j in range(2):
                    g.op("dve", lambda j=j: nc.vector.memset(V1s[j][:], 1.0), [], [V1s[j]])
                qbc = [sbb("qbc%d" % j, [128, 512]) for j in range(2)]
                pr2 = sbb("pr2", [128, 512])
                sc_ = sbb("sc_", [128, 8]); pp = sbb("pp", [128, 8])
                m1 = sbb("m1", [8, 8, 65])
                arow = sbb("arow", [1, 520])
                po1 = [psb_("po1_%d" % j, [128, 512]) for j in range(2)]
                po2 = [psb_("po2_%d" % j, [128, 512]) for j in range(2)]
                it = 0
                for b in range(NS):
                    for gi, (W, dil) in enumerate(GROUPS):
                        kb_ = Ksel[it % 2]; vb_ = V1s[it % 2]; qb_ = qbc[it % 2]
                        it += 1
                        g.dma("sp", kb_[:], ck[gi][b, 0:W:dil, :], writes=[kb_])
                        g.dma("act", vb_[:, :, 0:64], cv[gi][b, 0:W:dil, :].rearrange("r (h e) -> r h e", e=64), writes=[vb_])
                        g.dma("sp", qb_[:], qkvs_d[b:b + 1, gi * 512:(gi + 1) * 512].to_broadcast([128, 512]), writes=[qb_])
                        g.op("dve", lambda: nc.vector.tensor_mul(out=pr2[:], in0=kb_[:], in1=qb_[:]), [kb_, qb_], [pr2])
                        g.op("dve", lambda: nc.vector.tensor_reduce(out=sc_[:], in_=pr2[:].rearrange("p (h e) -> p h e", e=64),
                                                                    axis=AX.X, op=ALU.add), [pr2], [sc_])
                        g.op("dve", lambda: nc.vector.tensor_add(out=sc_[:], in0=sc_[:], in1=bsm[:, gi, :]), [sc_, bsm], [sc_])
                        g.op("act", lambda: nc.scalar.activation(out=pp[:], in_=sc_[:], func=AF.Exp), [sc_], [pp])
                        for hf in range(2):
                            g.mm([(po1[hf][0:8, 0:260], pp[:, :], vb_[:, hf * 4:(hf + 1) * 4, :].rearrange("p a b -> p (a b)"))],
                                 [pp, vb_], po1[hf])
                            g.op("dve", lambda: nc.vector.tensor_tensor(
                                out=m1[:, hf * 4:(hf + 1) * 4, :].rearrange("p a b -> p (a b)"), in0=po1[hf][0:8, 0:260],
                                in1=BD[:, hf * 4:(hf + 1) * 4, :].rearrange("p a b -> p (a b)"), op=ALU.mult), [po1[hf], BD], [m1])
                        for hf in range(2):
                            g.mm([(po2[hf][0:1, 0:260], ones8[:, :], m1[:, hf * 4:(hf + 1) * 4, :].rearrange("p a b -> p (a b)"))],
                                 [ones8, m1], po2[hf])
                            g.op("act", lambda: nc.scalar.copy(out=arow[:, hf * 260:(hf + 1) * 260], in_=po2[hf][0:1, 0:260]),
                                 [po2[hf]], [arow])
                        g.dma("sp", As_d[b, gi:gi + 1, :], arow[:], reads=[arow])
                st = sbb("st", [60, 512]); us = sbb("us", [NS, 512])
                g.dma("sp", st[:], stp.rearrange("b r c -> (b r) c"), writes=[st])
                g.dma("sp", us[:], us_d, writes=[us])
                bS = sbb("bS", [60, 4, NS]); dS = sbb("dS", [NS, 4, NS])
                g.dma("sp", bS[:], bandS_d.rearrange("g k b -> k g b"), writes=[bS])
                g.dma("sp", dS[:], diagS_d.rearrange("g k b -> k g b"), writes=[dS])
                pw2 = sbb("pw2", [128, 4, 128], BF16)
                pw2f = sbb("pw2f", [128, 4, 128])
                g.dma("sp", pw2f[:], pool_w.rearrange("g c d -> c g d"), writes=[pw2f])
                g.op("dve", lambda: nc.vector.tensor_copy(out=pw2[:], in_=pw2f[:]), [pw2f], [pw2])
                psc2 = sbb("psc2", [128, 4])
                g.dma("sp", psc2[:], pool_sc, writes=[psc2])
                pps = psb_("pps", [128, 512]); pmxs = psb_("pmxs", [128, 512])
                for gq in range(4):
                    cs = slice(gq * 128, (gq + 1) * 128)
                    g.mm([(pps[:, gq * NS:(gq + 1) * NS], st[:, cs], bS[:, gq, :]),
                          (pps[:, gq * NS:(gq + 1) * NS], us[:, cs], dS[:, gq, :])], [st, us, bS, dS], pps)
                pTs = sbb("pTs", [128, 4 * NS], BF16)
                g.op("dve", lambda: nc.vector.tensor_copy(out=pTs[:], in_=pps[:, 0:4 * NS]), [pps], [pTs])
                for gq in range(4):
                    g.mm([(pmxs[:, gq * NS:(gq + 1) * NS], pw2[:, gq, :], pTs[:, gq * NS:(gq + 1) * NS])], [pw2, pTs], pmxs)
                ybs = sbb("ybs", [128, 4, 128], BF16)
                g.op("dve", lambda: nc.vector.memset(ybs[:], 0.0), [], [ybs])
                for gq in range(4):
                    g.op("act", lambda gq=gq: nc.scalar.activation(out=ybs[:, gq, 0:NS], in_=pmxs[:, gq * NS:(gq + 1) * NS], func=AF.Copy,
                                                                  scale=psc2[:, gq:gq + 1]), [pmxs, psc2], [ybs])
                g.dma("sp", ybT_d[NT], ybs[:].rearrange("p a b -> p (a b)"), reads=[ybs])
                g.barrier()
                As = sbb("As", [NS, 3, 8, 65])
                g.dma("sp", As[:].rearrange("p a b c -> p (a b c)"), As_d.rearrange("b g c -> b (g c)"), writes=[As])
                numt = sbb("numt", [NS, 8, 64]); lt = sbb("lt", [NS, 8])
                g.op("dve", lambda: nc.vector.tensor_add(out=numt[:], in0=As[:, 0, :, 0:64], in1=As[:, 1, :, 0:64]), [As], [numt])
                g.op("dve", lambda: nc.vector.tensor_add(out=numt[:], in0=numt[:], in1=As[:, 2, :, 0:64]), [As, numt], [numt])
                g.op("dve", lambda: nc.vector.tensor_add(out=lt[:], in0=As[:, 0, :, 64], in1=As[:, 1, :, 64]), [As], [lt])
                g.op("dve", lambda: nc.vector.tensor_add(out=lt[:], in0=lt[:], in1=As[:, 2, :, 64]), [As, lt], [lt])
                for gi in range(3):
                    g.op("dve", lambda gi=gi: nc.vector.tensor_add(out=numt[:], in0=numt[:], in1=num0[:, gi * 8:(gi + 1) * 8, :]),
                         [numt, num0], [numt])
                    g.op("dve", lambda gi=gi: nc.vector.tensor_add(out=lt[:], in0=lt[:], in1=p0[:, gi * 8:(gi + 1) * 8]), [lt, p0], [lt])
                g.op("dve", lambda: nc.vector.reciprocal(out=lt[:], in_=lt[:]), [lt], [lt])
                oas = sbb("oas", [NS, 512])
                g.op("dve", lambda: nc.vector.tensor_tensor(out=oas[:].rearrange("p (h e) -> p h e", e=64), in0=numt[:],
                                                            in1=lt[:].unsqueeze(2).to_broadcast([NS, 8, 64]), op=ALU.mult),
                     [numt, lt], [oas])
                g.dma("sp", oas_d, oas[:], reads=[oas])
                g.barrier()
        if "p3" in stages or "all" in stages:
            p3 = contextlib.ExitStack()
            with p3:
                def sb3(name, shape, dt=F32):
                    return Buf(p3.enter_context(nc.sbuf_tensor("s3_" + name, list(shape), dt)), name)

                def ps3(name, shape, dt=F32):
                    return Buf(p3.enter_context(nc.psum_tensor("p3_" + name, list(shape), dt)), "P:" + name)

                wa_sb = sb3("wa", [128, 4, D], BF16)
                wb_sb = sb3("wb", [128, 4, D], BF16)
                wo_sb = sb3("wo", [128, 8, D], BF16)
                stg = sb3("stg", [128, 8, D])
                g.dma("sp", stg[:, 0:4, :], w_br_a.rearrange("(k p) n -> p k n", p=128), writes=[stg])
                g.op("act", lambda: nc.scalar.copy(out=wa_sb[:], in_=stg[:, 0:4, :]), [stg], [wa_sb])
                stg2 = sb3("stg2", [128, 4, D])
                g.dma("act", stg2[:], w_br_b.rearrange("(k p) n -> p k n", p=128), writes=[stg2])
                g.op("dve", lambda: nc.vector.tensor_copy(out=wb_sb[:], in_=stg2[:]), [stg2], [wb_sb])
                g.dma("sp", stg[:], w_out.rearrange("(k p) n -> p k n", p=128), writes=[stg])
                g.op("act", lambda: nc.scalar.copy(out=wo_sb[:, 0:4, :], in_=stg[:, 0:4, :]), [stg], [wo_sb])
                g.op("dve", lambda: nc.vector.tensor_copy(out=wo_sb[:, 4:8, :], in_=stg[:, 4:8, :]), [stg], [wo_sb])
                rw_sb = sb3("rw", [128, 8, NE])
                g.dma("sp", rw_sb[:], router_w.rearrange("(k p) n -> p k n", p=128), writes=[rw_sb])
                rbias = sb3("rbias", [128, NE])
                g.dma("sp", rbias[:], router_b.to_broadcast([128, NE]), writes=[rbias])
                GT1 = sb3("GT1", [128, D]); G2 = sb3("G2", [128, D]); SH2 = sb3("SH2", [128, D]); n2b = sb3("n2b", [128, D])
                GT1s = sb3("GT1s", [NS, D]); G2s = sb3("G2s", [NS, D]); SH2s = sb3("SH2s", [NS, D])
                g.dma("sp", n2b[:], norm2.to_broadcast([128, D]), writes=[n2b])
                g.dma("sp", GT1[:], mod_d[0:1, 2 * D:3 * D].to_broadcast([128, D]), writes=[GT1])
                g.dma("sp", SH2[:], mod_d[0:1, 3 * D:4 * D].to_broadcast([128, D]), writes=[SH2])
                g.dma("sp", G2[:], mod_d[0:1, 4 * D:5 * D].to_broadcast([128, D]), writes=[G2])
                g.dma("sp", GT1s[:], mod_d[1:5, 2 * D:3 * D], writes=[GT1s])
                g.dma("sp", SH2s[:], mod_d[1:5, 3 * D:4 * D], writes=[SH2s])
                g.dma("sp", G2s[:], mod_d[1:5, 4 * D:5 * D], writes=[G2s])
                g.op("dve", lambda: nc.vector.scalar_tensor_tensor(
                    out=G2[:], in0=G2[:], scalar=1.0, in1=n2b[:], op0=ALU.add, op1=ALU.mult), [G2, n2b], [G2])
                g.op("dve", lambda: nc.vector.scalar_tensor_tensor(
                    out=G2s[:], in0=G2s[:], scalar=1.0, in1=n2b[:NS, :], op0=ALU.add, op1=ALU.mult), [G2s, n2b], [G2s])

                A0 = sb3("A0", [128, 8, 65]); A1 = sb3("A1", [128, 8, 65]); A2 = sb3("A2", [128, 8, 65])
                rl = sb3("rl", [128, 8])
                oaf = sb3("oaf", [128, 512])
                oab = sb3("oab", [128, 512], BF16)
                oaT = sb3("oaT", [128, 4, 128], BF16)
                ybt = sb3("ybt", [128, 4, 128], BF16)
                gt = sb3("gt", [128, 2048], BF16)
                xt3 = sb3("xt3", [128, D])
                t1_ = sb3("t1_", [128, D]); t2_ = sb3("t2_", [128, D])
                mgb = sb3("mgb", [128, D], BF16)
                mT = sb3("mT", [128, 8, 128], BF16)
                x1 = sb3("x1", [128, D])
                h2 = sb3("h2", [128, D])
                sq3 = sb3("sq3", [128, D])
                ssq3 = sb3("ssq3", [128, 1]); rstd3 = sb3("rstd3", [128, 1])
                h2T32 = sb3("h2T32", [128, 8, 128])
                h2Tb = sb3("h2Tb", [128, 8, 128], BF16)
                sc = sb3("sc", [128, NE]); sel = sb3("sel", [128, NE]); selm = sb3("selm", [128, NE])
                mx8 = sb3("mx8", [128, 8, 8]); gsc = sb3("gsc", [128, 8]); gtop = sb3("gtop", [128, 8])
                gmask = sb3("gmask", [128, 8]); top8 = sb3("top8", [128, 8])
                Mk = sb3("Mk", [128, NE]); Wd = sb3("Wd", [128, NE]); den = sb3("den", [128, 1])
                UT = sb3("UT", [128, 128])
                g.dma("sp", UT[:], UT_d, writes=[UT])
                ones_r = sb3("ones_r", [1, 128]); ones_c = sb3("ones_c", [128, 1]); carry = sb3("carry", [1, NE])
                g.op("dve", lambda: nc.vector.memset(ones_r[:], 1.0), [], [ones_r])
                g.op("dve", lambda: nc.vector.memset(ones_c[:], 1.0), [], [ones_c])
                g.op("dve", lambda: nc.vector.memset(carry[:], 0.0), [], [carry])
                pos1t = sb3("pos1t", [128, NE]); key_ = sb3("key_", [128, NE]); junk = sb3("junk", [128, NE])
                h2b = sb3("h2b", [128, D], BF16)
                ptr3 = ps3("ptr3", [128, 8, 128], BF16)
                pbr = [ps3("pbr%d" % j, [128, 512]) for j in range(2)]
                pym = [ps3("pym%d" % j, [128, 512]) for j in range(2)]
                pt32 = [ps3("pt32_%d" % j, [128, 4, 128]) for j in range(2)]
                prt = ps3("prt", [128, 512])

                for i in range(NT + 1):
                    n = 128 if i < NT else NS
                    samp = (i == NT)
                    rows = slice(i * 128, i * 128 + n)
                    gt1, g2m, sh2m = (GT1s, G2s, SH2s) if samp else (GT1, G2, SH2)
                    g.dma("sp", xt3[:n, :], xs if samp else x[rows, :], writes=[xt3])
                    g.dma("act", gt[:n, :], gates_d[rows, :], writes=[gt])
                    g.dma("act", ybt[:].rearrange("p a b -> p (a b)"), ybT_d[i], writes=[ybt])
                    if not samp:
                        g.dma("sp", A0[:].rearrange("p a b -> p (a b)"), A_d[0][rows, :], writes=[A0])
                        g.dma("sp", A1[:].rearrange("p a b -> p (a b)"), A_d[1][rows, :], writes=[A1])
                        g.dma("sp", A2[:].rearrange("p a b -> p (a b)"), A_d[2][rows, :], writes=[A2])
                        g.op("dve", lambda: nc.vector.tensor_add(out=A0[:], in0=A0[:], in1=A1[:]), [A0, A1], [A0])
                        g.op("dve", lambda: nc.vector.tensor_add(out=A0[:], in0=A0[:], in1=A2[:]), [A0, A2], [A0])
                        g.op("dve", lambda: nc.vector.reciprocal(out=rl[:], in_=A0[:, :, 64]), [A0], [rl])
                        g.op("dve", lambda: nc.vector.tensor_tensor(
                            out=oab[:].rearrange("p (h e) -> p h e", e=64), in0=A0[:, :, 0:64],
                            in1=rl[:].unsqueeze(2).to_broadcast([128, 8, 64]), op=ALU.mult), [A0, rl], [oab])
                    else:
                        g.dma("sp", oaf[:NS, :], oas_d, writes=[oaf])
                        g.op("dve", lambda: nc.vector.tensor_copy(out=oab[:NS, :], in_=oaf[:NS, :]), [oaf], [oab])
                    g.tr([(ptr3[:, k, :n], oab[:n, k * 128:(k + 1) * 128], ident_b[:n, :n]) for k in range(4)], [oab, ident_b], ptr3)
                    g.op("act", lambda: nc.scalar.copy(out=oaT[:, :, :n], in_=ptr3[:, 0:4, :n]), [ptr3], [oaT])
                    for half in range(2):
                        cs = slice(half * 512, (half + 1) * 512)
                        g.mm([(pbr[0][:n, :], oaT[:, k, :n], wa_sb[:, k, cs]) for k in range(4)], [oaT, wa_sb], pbr[0])
                        g.mm([(pbr[1][:n, :], ybt[:, k, :n], wb_sb[:, k, cs]) for k in range(4)], [ybt, wb_sb], pbr[1])
                        g.op("dve", lambda: nc.vector.tensor_tensor(out=t1_[:n, cs], in0=pbr[0][:n, :], in1=gt[:n, cs], op=ALU.mult),
                             [pbr[0], gt], [t1_])
                        g.op("dve", lambda: nc.vector.tensor_tensor(out=t2_[:n, cs], in0=pbr[1][:n, :],
                                                                    in1=gt[:n, 1024 + half * 512:1024 + (half + 1) * 512], op=ALU.mult),
                             [pbr[1], gt], [t2_])
                    g.op("dve", lambda: nc.vector.tensor_add(out=mgb[:n, :], in0=t1_[:n, :], in1=t2_[:n, :]), [t1_, t2_], [mgb])
                    g.tr([(ptr3[:, k, :n], mgb[:n, k * 128:(k + 1) * 128], ident_b[:n, :n]) for k in range(8)], [mgb, ident_b], ptr3)
                    g.op("act", lambda: nc.scalar.copy(out=mT[:, :, :n], in_=ptr3[:, :, :n]), [ptr3], [mT])
                    for half in range(2):
                        cs = slice(half * 512, (half + 1) * 512)
                        g.mm([(pym[half][:n, :], mT[:, k, :n], wo_sb[:, k, cs]) for k in range(8)], [mT, wo_sb], pym[half])
                        g.op("dve", lambda: nc.vector.tensor_tensor(out=t1_[:n, cs], in0=pym[half][:n, :], in1=gt1[:n, cs], op=ALU.mult),
                             [pym[half], gt1], [t1_])
                    g.op("dve", lambda: nc.vector.tensor_add(out=x1[:n, :], in0=t1_[:n, :], in1=xt3[:n, :]), [t1_, xt3], [x1])
                    g.dma("sp", x1_d[rows, :], x1[:n, :], reads=[x1])
                    g.op("act", lambda: nc.scalar.activation(out=sq3[:n, :], in_=x1[:n, :], func=AF.Square, accum_out=ssq3[:n, :]),
                         [x1], [sq3, ssq3])
                    g.op("dve", lambda: nc.vector.tensor_scalar(out=rstd3[:n, :], in0=ssq3[:n, :], scalar1=1.0 / D, scalar2=EPS,
                                                                op0=ALU.mult, op1=ALU.add), [ssq3], [rstd3])
                    g.op("pool", lambda: nc.gpsimd.tensor_tensor(out=rstd3[:n, :], in0=rstd3[:n, :], in1=nhalf[:n, 0:1], op=ALU.pow),
                         [rstd3, nhalf], [rstd3])
                    cvt_emit(1)
                    g.op("dve", lambda: nc.vector.scalar_tensor_tensor(out=sq3[:n, :], in0=x1[:n, :], scalar=rstd3[:n, :],
                                                                       in1=g2m[:n, :], op0=ALU.mult, op1=ALU.mult),
                         [x1, rstd3, g2m], [sq3])
                    g.op("dve", lambda: nc.vector.tensor_add(out=h2[:n, :], in0=sq3[:n, :], in1=sh2m[:n, :]), [sq3, sh2m], [h2])
                    for hf in range(2):
                        g.tr([(pt32[hf][:, k, :n], h2[:n, (hf * 4 + k) * 128:(hf * 4 + k + 1) * 128], ident_f[:n, :n]) for k in range(4)],
                             [h2, ident_f], pt32[hf])
                        g.op("act", lambda: nc.scalar.copy(out=h2T32[:, hf * 4:(hf + 1) * 4, :n], in_=pt32[hf][:, :, :n]),
                             [pt32[hf]], [h2T32])
                    if samp:
                        g.op("dve", lambda: nc.vector.memset(h2Tb[:], 0.0), [], [h2Tb])
                    g.op("dve", lambda: nc.vector.tensor_copy(out=h2Tb[:, :, :n], in_=h2T32[:, :, :n]), [h2T32], [h2Tb])
                    g.dma("sp", h2T_d[i], h2Tb[:].rearrange("p a b -> p (a b)"), reads=[h2Tb])
                    g.mm([(prt[:n, :NE], h2T32[:, k, :n], rw_sb[:, k, :]) for k in range(8)], [h2T32, rw_sb], prt)
                    g.op("act", lambda: nc.scalar.activation(out=sc[:n, :], in_=prt[:n, :NE], func=AF.Sigmoid), [prt], [sc])
                    g.op("dve", lambda: nc.vector.tensor_add(out=sel[:n, :], in0=sc[:n, :], in1=rbias[:n, :]), [sc, rbias], [sel])
                    for gq in range(8):
                        g.op("dve", lambda: nc.vector.max(out=mx8[:n, gq, :], in_=sel[:n, gq * 32:(gq + 1) * 32]), [sel], [mx8])
                    g.op("dve", lambda: nc.vector.tensor_add(out=gsc[:n, :], in0=mx8[:n, :, 0], in1=mx8[:n, :, 1]), [mx8], [gsc])
                    g.op("dve", lambda: nc.vector.max(out=gtop[:n, :], in_=gsc[:n, :]), [gsc], [gtop])
                    g.op("dve", lambda: nc.vector.tensor_scalar(out=gmask[:n, :], in0=gsc[:n, :], scalar1=gtop[:n, 3:4], scalar2=None,
                                                                op0=ALU.is_ge), [gsc, gtop], [gmask])
                    g.op("dve", lambda: nc.vector.tensor_scalar(out=gmask[:n, :], in0=gmask[:n, :], scalar1=-1.0, scalar2=1e9,
                                                                op0=ALU.add, op1=ALU.mult), [gmask], [gmask])
                    g.op("dve", lambda: nc.vector.tensor_tensor(
                        out=selm[:n, :].rearrange("p (a b) -> p a b", b=32), in0=sel[:n, :].rearrange("p (a b) -> p a b", b=32),
                        in1=gmask[:n, :].unsqueeze(2).to_broadcast([n, 8, 32]), op=ALU.add), [sel, gmask], [selm])
                    g.op("dve", lambda: nc.vector.max(out=top8[:n, :], in_=selm[:n, :]), [selm], [top8])
                    g.op("dve", lambda: nc.vector.tensor_scalar(out=Mk[:n, :], in0=selm[:n, :], scalar1=top8[:n, 7:8], scalar2=None,
                                                                op0=ALU.is_ge), [selm, top8], [Mk])
                    g.op("dve", lambda: nc.vector.tensor_tensor(out=Wd[:n, :], in0=Mk[:n, :], in1=sc[:n, :], op=ALU.mult), [Mk, sc], [Wd])
                    g.op("dve", lambda: nc.vector.tensor_reduce(out=den[:n, :], in_=Wd[:n, :], axis=AX.X, op=ALU.add), [Wd], [den])
                    g.op("dve", lambda: nc.vector.reciprocal(out=den[:n, :], in_=den[:n, :]), [den], [den])
                    g.op("dve", lambda: nc.vector.tensor_scalar(out=Wd[:n, :], in0=Wd[:n, :], scalar1=den[:n, :], scalar2=2.5,
                                                                op0=ALU.mult, op1=ALU.mult), [Wd, den], [Wd])
                    g.dma("sp", Wd_d[i, 0:n, :], Wd[:n, :], reads=[Wd])
                    if SPARSE:
                        pps_ = pbr[0]; pcs_ = pbr[1]
                        g.mm([(pps_[:n, :NE], UT[:n, :n], Mk[:n, :]), (pps_[:n, :NE], ones_r[0:1, :n], carry[0:1, :])],
                             [UT, Mk, ones_r, carry], pps_)
                        g.op("dve", lambda: nc.vector.tensor_scalar(out=pos1t[:n, :], in0=pps_[:n, :NE], scalar1=1.0, scalar2=None,
                                                                    op0=ALU.add), [pps_], [pos1t])
                        g.dma("sp", pos_d[i, 0:n, :], pos1t[:n, :], reads=[pos1t])
                        g.mm([(pcs_[0:1, :NE], ones_c[:n, 0:1], Mk[:n, :])], [ones_c, Mk], pcs_)
                        g.op("dve", lambda: nc.vector.tensor_add(out=carry[0:1, :], in0=carry[0:1, :], in1=pcs_[0:1, :NE]),
                             [carry, pcs_], [carry])
                        g.op("act", lambda: nc.scalar.copy(out=h2b[:n, :], in_=h2[:n, :]), [h2], [h2b])
                        g.dma("sp", h2b_d[i, 0:n, :], h2b[:n, :], reads=[h2b])
                if SPARSE:
                    ci_ = sb3("ci_", [1, NE], I32)
                    pc = sb3("pc", [1, NE]); pcT = sb3("pcT", [128, 2]); pend = sb3("pend", [1, NE]); ps1r = sb3("ps1r", [1, NE])
                    tri = sb3("tri", [128, 2, NE]); bstart = sb3("bstart", [128, 5])
                    g.dma("sp", tri[:], tri_d.rearrange("c p e -> p c e"), writes=[tri])
                    g.dma("sp", bstart[:], bstart_d, writes=[bstart])
                    g.op("dve", lambda: nc.vector.tensor_scalar(out=pc[:], in0=carry[:], scalar1=127.0, scalar2=None, op0=ALU.add),
                         [carry], [pc])
                    g.op("dve", lambda: nc.vector.tensor_copy(out=ci_[:], in_=pc[:]), [pc], [ci_])
                    g.op("dve", lambda: nc.vector.tensor_single_scalar(out=ci_[:], in_=ci_[:], scalar=7, op=ALU.arith_shift_right),
                         [ci_], [ci_])
                    g.op("dve", lambda: nc.vector.tensor_single_scalar(out=ci_[:], in_=ci_[:], scalar=7, op=ALU.logical_shift_left),
                         [ci_], [ci_])
                    g.op("dve", lambda: nc.vector.tensor_copy(out=pc[:], in_=ci_[:]), [ci_], [pc])
                    g.tr([(pt32[0][:, 0, 0:1], pc[0:1, 0:128], ident_f[0:1, 0:1]), (pt32[0][:, 1, 0:1], pc[0:1, 128:256], ident_f[0:1, 0:1])],
                         [pc, ident_f], pt32[0])
                    g.op("dve", lambda: nc.vector.tensor_copy(out=pcT[:], in_=pt32[0][:, 0:2, 0]), [pt32[0]], [pcT])
                    g.mm([(prt[0:1, :NE], pcT[:, c2:c2 + 1], tri[:, c2, :]) for c2 in range(2)], [pcT, tri], prt)
                    g.op("dve", lambda: nc.vector.tensor_copy(out=pend[:], in_=prt[0:1, :NE]), [prt], [pend])
                    g.op("dve", lambda: nc.vector.tensor_sub(out=ps1r[:], in0=pend[:], in1=pc[:]), [pend, pc], [ps1r])
                    PSb = sb3("PSb", [128, NE]); PEb = sb3("PEb", [128, NE])
                    g.mm([(pbr[0][:, :NE], ones_r[0:1, :], ps1r[0:1, :])], [ones_r, ps1r], pbr[0])
                    g.op("dve", lambda: nc.vector.tensor_copy(out=PSb[:], in_=pbr[0][:, :NE]), [pbr[0]], [PSb])
                    g.mm([(pbr[1][:, :NE], ones_r[0:1, :], pend[0:1, :])], [ones_r, pend], pbr[1])
                    g.op("dve", lambda: nc.vector.tensor_copy(out=PEb[:], in_=pbr[1][:, :NE]), [pbr[1]], [PEb])
                    be = sb3("be", [128, 8])
                    g.op("dve", lambda: nc.vector.memset(be[:], 0.0), [], [be])
                    for j5 in range(5):
                        g.op("dve", lambda j5=j5: nc.vector.tensor_scalar(
                            out=key_[:, :], in0=PEb[:, :], scalar1=bstart[:, j5:j5 + 1], scalar2=None, op0=ALU.is_le, op1=ALU.add,
                            accum_out=be[:, j5:j5 + 1]), [PEb, bstart], [key_, be])
                    g.op("dve", lambda: nc.vector.tensor_scalar_min(out=be[:], in0=be[:], scalar1=float(NE - 1)), [be], [be])
                    g.tr([(pt32[1][0:8, 0, :], be[:, 0:8], ident_f[:, :])], [be, ident_f], pt32[1])
                    beT = sb3("beT", [8, 128])
                    g.op("dve", lambda: nc.vector.tensor_copy(out=beT[:], in_=pt32[1][0:8, 0, :]), [pt32[1]], [beT])
                    g.dma("sp", be_d.rearrange("o (j b) -> (o j) b", j=5), beT[0:5, :], reads=[beT])
                    cvt_emit(1000)
                    g.barrier()
                    trashf = sb3("trashf", [128, 8])
                    g.dma("sp", trashf[:], trash_d, writes=[trashf])
                    s8f = sb3("s8f", [128, 8]); w8 = sb3("w8", [128, 8]); s8i = sb3("s8i", [128, 8], I32)
                    g.op("dve", lambda: nc.vector.memset(h2b[:], 0.0), [], [h2b])
                    for i in range(NT + 1):
                        n = 128 if i < NT else NS
                        samp = (i == NT)
                        g.dma("sp", pos1t[:n, :], pos_d[i, 0:n, :], writes=[pos1t])
                        g.dma("act", Wd[:n, :], Wd_d[i, 0:n, :], writes=[Wd])
                        g.dma("act", h2b[:n, :], h2b_d[i, 0:n, :], writes=[h2b])
                        g.op("dve", lambda: nc.vector.tensor_single_scalar(out=Mk[:n, :], in_=Wd[:n, :], scalar=0.0, op=ALU.is_gt),
                             [Wd], [Mk])
                        g.op("dve", lambda: nc.vector.tensor_add(out=key_[:n, :], in0=pos1t[:n, :], in1=PSb[:n, :]), [pos1t, PSb], [key_])
                        g.op("dve", lambda: nc.vector.tensor_mul(out=key_[:n, :], in0=key_[:n, :], in1=Mk[:n, :]), [key_, Mk], [key_])
                        if samp:
                            g.op("dve", lambda: nc.vector.tensor_copy(out=s8f[:], in_=trashf[:]), [trashf], [s8f])
                        g.op("dve", lambda: nc.vector.max(out=s8f[:n, :], in_=key_[:n, :]), [key_], [s8f])
                        for k8 in range(8):
                            g.op("dve", lambda k8=k8: nc.vector.scalar_tensor_tensor(
                                out=junk[:n, :], in0=key_[:n, :], scalar=s8f[:n, k8:k8 + 1], in1=Wd[:n, :],
                                op0=ALU.is_equal, op1=ALU.mult, accum_out=w8[:n, k8:k8 + 1]), [key_, s8f, Wd], [junk, w8])
                        g.op("dve", lambda: nc.vector.tensor_scalar(out=s8f[:, :], in0=s8f[:, :], scalar1=-1.0, scalar2=None,
                                                                    op0=ALU.add), [s8f], [s8f])
                        g.op("dve", lambda: nc.vector.tensor_copy(out=s8i[:, :], in_=s8f[:, :]), [s8f], [s8i])
                        g.dma("sp", slot_d[i], s8i[:, :], reads=[s8i])
                        g.dma("sp", w8_d[i, 0:n, :], w8[:n, :], reads=[w8])
                        for k8 in range(8):
                            g.idma(Xs_d[:, :], bass.IndirectOffsetOnAxis(ap=s8i[:, k8:k8 + 1], axis=0), h2b[:, :], None,
                                   reads=[s8i, h2b])
                g.barrier()
        if ("p4" in stages or "all" in stages) and not SPARSE:
            NG = 3
            tiles_all = list(range(NT + 1))
            per = (len(tiles_all) + NG - 1) // NG
            groups4 = [tiles_all[a:a + per] for a in range(0, len(tiles_all), per)]
            n_exp = int(_os.environ.get("P4_NEXP", str(NE)))
            for tg in groups4:
                p4 = contextlib.ExitStack()
                with p4:
                    def sb4(name, shape, dt=F32):
                        return Buf(p4.enter_context(nc.sbuf_tensor("s4_%d_" % tg[0] + name, list(shape), dt)), name)

                    def ps4(name, shape, dt=F32):
                        return Buf(p4.enter_context(nc.psum_tensor("p4_%d_" % tg[0] + name, list(shape), dt)), "P:" + name)

                    ntl = len(tg)
                    hT4 = sb4("hT4", [128, 8, ntl * 128], BF16)
                    for ti, i in enumerate(tg):
                        g.dma("sp", hT4[:, :, ti * 128:(ti + 1) * 128], h2T_d[i].rearrange("p (k t) -> p k t", k=8),
                              writes=[hT4] if ti == 0 else [])
                    Wg = sb4("Wg", [128, ntl, NE])
                    for ti, i in enumerate(tg):
                        nn = 128 if i < NT else NS
                        g.dma("sp", Wg[:nn, ti, :], Wd_d[i, 0:nn, :], writes=[])
                    acc = sb4("acc", [128, ntl, D])
                    g.op("pool", lambda: nc.gpsimd.memset(acc[:], 0.0), [], [acc])
                    allw = {(("d", i_)): 16 * n_ for i_, (s_, n_) in enumerate(g.dsem) if n_ > 0}
                    hT4.w = dict(allw)
                    Wg.w = dict(allw)
                    wgs = [sb4("wg%d" % j, [128, 8, EH], BF16) for j in range(2)]
                    wus = [sb4("wu%d" % j, [128, 8, EH], BF16) for j in range(2)]
                    wds = [sb4("wd%d" % j, [128, 2, D], BF16) for j in range(2)]
                    sgt = [sb4("sgt%d" % j, [128, 512]) for j in range(2)]
                    act = [sb4("act%d" % j, [128, 512], BF16) for j in range(2)]
                    ph = [[ps4("ph%d_%d" % (a, b), [128, 512]) for b in range(2)] for a in range(2)]
                    py = [[ps4("py%d_%d" % (a, b), [128, 512]) for b in range(2)] for a in range(2)]
                    blocks = [list(range(a, min(a + 4, ntl))) for a in range(0, ntl, 4)]
                    yi = 0
                    elist = list(range(n_exp)) + [NE]
                    for ei, e in enumerate(elist):
                        j2 = ei % 2
                        if e < NE:
                            srcs = (eg[e], eu[e], ed[e])
                        else:
                            srcs = (shg, shu, shd)
                        g.dma("pool", wgs[j2][:], srcs[0].rearrange("(k p) n -> p k n", p=128), writes=[wgs[j2]])
                        g.dma("pool", wus[j2][:], srcs[1].rearrange("(k p) n -> p k n", p=128), writes=[wus[j2]])
                        g.dma("pool", wds[j2][:], srcs[2].rearrange("(k p) n -> p k n", p=128), writes=[wds[j2]])
                        for blk in blocks:
                            t0 = blk[0] * 128
                            ntb = sum(128 if tg[ti] < NT else NS for ti in blk)
                            for hh in range(2):
                                hs = slice(hh * 128, (hh + 1) * 128)
                                g.mm([(ph[0][hh][:, :ntb], wgs[j2][:, k, hs], hT4[:, k, t0:t0 + ntb]) for k in range(8)],
                                     [wgs[j2], hT4], ph[0][hh])
                                g.mm([(ph[1][hh][:, :ntb], wus[j2][:, k, hs], hT4[:, k, t0:t0 + ntb]) for k in range(8)],
                                     [wus[j2], hT4], ph[1][hh])
                                g.op("act", lambda: nc.scalar.activation(out=sgt[hh][:, :ntb], in_=ph[0][hh][:, :ntb], func=AF.Silu),
                                     [ph[0][hh]], [sgt[hh]])
                                g.op("dve", lambda: nc.vector.tensor_tensor(out=act[hh][:, :ntb], in0=ph[1][hh][:, :ntb],
                                                                            in1=sgt[hh][:, :ntb], op=ALU.mult),
                                     [ph[1][hh], sgt[hh]], [act[hh]])
                            for ti in blk:
                                nn = 128 if tg[ti] < NT else NS
                                c0 = (ti - blk[0]) * 128
                                pyy = py[yi % 2]
                                yi += 1
                                for half in range(2):
                                    cs = slice(half * 512, (half + 1) * 512)
                                    g.mm([(pyy[half][:nn, :], act[hh2][:, c0:c0 + nn], wds[j2][:, hh2, cs]) for hh2 in range(2)],
                                         [act[0], act[1], wds[j2]], pyy[half])
                                    if e < NE:
                                        g.op("dve", lambda: nc.vector.scalar_tensor_tensor(
                                            out=acc[:nn, ti, cs], in0=pyy[half][:nn, :], scalar=Wg[:nn, ti, e:e + 1],
                                            in1=acc[:nn, ti, cs], op0=ALU.mult, op1=ALU.add), [pyy[half], Wg, acc], [acc])
                                    else:
                                        g.op("dve", lambda: nc.vector.tensor_tensor(
                                            out=acc[:nn, ti, cs], in0=pyy[half][:nn, :], in1=acc[:nn, ti, cs], op=ALU.add),
                                            [pyy[half], acc], [acc])
                    GT2 = sb4("GT2", [128, D]); GT2s = sb4("GT2s", [NS, D])
                    g.dma("sp", GT2[:], mod_d[0:1, 5 * D:6 * D].to_broadcast([128, D]), writes=[GT2])
                    g.dma("sp", GT2s[:], mod_d[1:5, 5 * D:6 * D], writes=[GT2s])
                    xo = [sb4("xo%d" % j, [128, D]) for j in range(2)]
                    for ti, i in enumerate(tg):
                        nn = 128 if i < NT else NS
                        samp = (i == NT)
                        rows = slice(i * 128, i * 128 + nn)
                        xb_ = xo[ti % 2]
                        gt2 = GT2s if samp else GT2
                        g.dma("sp", xb_[:nn, :], x1_d[rows, :], writes=[xb_])
                        g.op("dve", lambda: nc.vector.tensor_tensor(out=acc[:nn, ti, :], in0=acc[:nn, ti, :], in1=gt2[:nn, :], op=ALU.mult),
                             [acc, gt2], [acc])
                        g.op("dve", lambda: nc.vector.tensor_add(out=xb_[:nn, :], in0=xb_[:nn, :], in1=acc[:nn, ti, :]), [xb_, acc], [xb_])
                        g.dma("sp", ys if samp else y[rows, :], xb_[:nn, :], reads=[xb_], is_output=True)
                    g.barrier()
        if ("p4" in stages or "all" in stages) and SPARSE:
            p4 = contextlib.ExitStack()
            with p4:
                def sb4(name, shape, dt=F32):
                    return Buf(p4.enter_context(nc.sbuf_tensor("s4_" + name, list(shape), dt)), name)

                def ps4(name, shape, dt=F32):
                    return Buf(p4.enter_context(nc.psum_tensor("p4_" + name, list(shape), dt)), "P:" + name)

                NW = 5
                wgs = [sb4("wg%d" % j, [128, 8, EH], BF16) for j in range(NW)]
                wus = [sb4("wu%d" % j, [128, 8, EH], BF16) for j in range(NW)]
                wds = [sb4("wd%d" % j, [128, 2, D], BF16) for j in range(NW)]
                Xsb = [sb4("Xs%d" % j, [128, D], BF16) for j in range(3)]
                XTb = [sb4("XT%d" % j, [128, 8, 512], BF16) for j in range(2)]
                sgt = [sb4("sgt%d" % j, [128, 512]) for j in range(2)]
                act = [sb4("act%d" % j, [128, 512], BF16) for j in range(2)]
                Ysb = [sb4("Ys%d" % j, [128, D], BF16) for j in range(2)]
                Yshb = [sb4("Ysh%d" % j, [128, D]) for j in range(2)]
                ptx = [ps4("ptx%d" % j, [128, 8, 128], BF16) for j in range(2)]
                ph = [[ps4("ph%d_%d" % (a_, b_), [128, 512]) for b_ in range(2)] for a_ in range(2)]
                py = [ps4("py%d" % a_, [128, 512]) for a_ in range(2)]
                cvt_emit(1000)
                for e_ in g.eng:
                    g.eng[e_].wait_ge(cvt_sem, 16 * cvt_total)
                BEb = sb4("BEb", [128, 640]); piota = sb4("piota", [128, 1]); widx = sb4("widx", [128, 640], I32)
                g.dma("sp", BEb[:], be_d.to_broadcast([128, 640]), writes=[BEb])
                g.dma("sp", piota[:], piota_d, writes=[piota])
                g.op("dve", lambda: nc.vector.tensor_scalar(out=BEb[:], in0=BEb[:], scalar1=128.0, scalar2=piota[:, 0:1],
                                                            op0=ALU.mult, op1=ALU.add), [BEb, piota], [BEb])
                g.op("dve", lambda: nc.vector.tensor_copy(out=widx[:], in_=BEb[:]), [BEb], [widx])
                nblk = int(_os.environ.get("P4_NBLK", str(NBLK)))
                yi = 0
                ci = 0

                def expert_block(j2, XT, ntb):
                    for hh in range(2):
                        hs = slice(hh * 128, (hh + 1) * 128)
                        g.mm([(ph[0][hh][:, :ntb], wgs[j2][:, k, hs], XT[:, k, :ntb]) for k in range(8)], [wgs[j2], XT], ph[0][hh])
                        g.mm([(ph[1][hh][:, :ntb], wus[j2][:, k, hs], XT[:, k, :ntb]) for k in range(8)], [wus[j2], XT], ph[1][hh])
                        g.op("act", lambda: nc.scalar.activation(out=sgt[hh][:, :ntb], in_=ph[0][hh][:, :ntb], func=AF.Silu),
                             [ph[0][hh]], [sgt[hh]])
                        g.op("dve", lambda: nc.vector.tensor_tensor(out=act[hh][:, :ntb], in0=ph[1][hh][:, :ntb],
                                                                    in1=sgt[hh][:, :ntb], op=ALU.mult),
                             [ph[1][hh], sgt[hh]], [act[hh]])

                def down(j2, c0, nn):
                    for half in range(2):
                        cs = slice(half * 512, (half + 1) * 512)
                        g.mm([(py[half][:nn, :], act[hh2][:, c0:c0 + nn], wds[j2][:, hh2, cs]) for hh2 in range(2)],
                             [act[0], act[1], wds[j2]], py[half])

                for b4 in range(nblk):
                    j2 = b4 % NW
                    off = bass.IndirectOffsetOnAxis(ap=widx[:, b4:b4 + 1], axis=0)
                    g.idma(wgs[j2][:].rearrange("p k n -> p (k n)"), None, egb[:, :], off, reads=[widx], writes=[wgs[j2]])
                    g.idma(wus[j2][:].rearrange("p k n -> p (k n)"), None, eub[:, :], off, reads=[widx], writes=[wus[j2]])
                    g.idma(wds[j2][:].rearrange("p k n -> p (k n)"), None, edb[:, :], off, reads=[widx], writes=[wds[j2]])
                    Xs = Xsb[b4 % 3]
                    pt_ = ptx[b4 % 2]
                    XT = XTb[b4 % 2]
                    g.dma("sp", Xs[:], Xs_d[b4 * 128:(b4 + 1) * 128, :], writes=[Xs])
                    g.tr([(pt_[:, k, :], Xs[:, k * 128:(k + 1) * 128], ident_b[:, :]) for k in range(8)], [Xs, ident_b], pt_)
                    g.op("act", lambda: nc.scalar.copy(out=XT[:, 0:4, 0:128], in_=pt_[:, 0:4, :]), [pt_], [XT])
                    g.op("dve", lambda: nc.vector.tensor_copy(out=XT[:, 4:8, 0:128], in_=pt_[:, 4:8, :]), [pt_], [XT])
                    expert_block(j2, XT, 128)
                    Ys = Ysb[b4 % 2]
                    down(j2, 0, 128)
                    g.op("act", lambda: nc.scalar.copy(out=Ys[:, 0:512], in_=py[0][:, :]), [py[0]], [Ys])
                    g.op("dve", lambda: nc.vector.tensor_copy(out=Ys[:, 512:1024], in_=py[1][:, :]), [py[1]], [Ys])
                    g.dma("act", Ys_d[b4 * 128:(b4 + 1) * 128, :], Ys[:], reads=[Ys])
                j2 = 0
                g.dma("pool", wgs[j2][:], shg.rearrange("(k p) n -> p k n", p=128), writes=[wgs[j2]])
                g.dma("pool", wus[j2][:], shu.rearrange("(k p) n -> p k n", p=128), writes=[wus[j2]])
                g.dma("pool", wds[j2][:], shd.rearrange("(k p) n -> p k n", p=128), writes=[wds[j2]])
                for t0 in range(0, NT + 1, 4):
                    tl = list(range(t0, min(t0 + 4, NT + 1)))
                    XT = XTb[(t0 // 4) % 2]
                    ntb = sum(128 if i < NT else NS for i in tl)
                    for ti, i in enumerate(tl):
                        g.dma("sp", XT[:, :, ti * 128:(ti + 1) * 128], h2T_d[i].rearrange("p (k t) -> p k t", k=8),
                              writes=[XT] if ti == 0 else [])
                    XT.w = {(("d", i_)): 16 * n_ for i_, (s_, n_) in enumerate(g.dsem) if n_ > 0}
                    expert_block(j2, XT, ntb)
                    for ti, i in enumerate(tl):
                        nn = 128 if i < NT else NS
                        Ysh = Yshb[yi % 2]
                        yi += 1
                        down(j2, ti * 128, nn)
                        g.op("act", lambda: nc.scalar.copy(out=Ysh[:nn, 0:512], in_=py[0][:nn, :]), [py[0]], [Ysh])
                        g.op("dve", lambda: nc.vector.tensor_copy(out=Ysh[:nn, 512:1024], in_=py[1][:nn, :]), [py[1]], [Ysh])
                        g.dma("act", Ysh_d[i * 128:i * 128 + nn, :], Ysh[:nn, :], reads=[Ysh])
                g.barrier()
            p5 = contextlib.ExitStack()
            with p5:
                def sb5(name, shape, dt=F32):
                    return Buf(p5.enter_context(nc.sbuf_tensor("s5_" + name, list(shape), dt)), name)
                GT2 = sb5("GT2", [128, D]); GT2s = sb5("GT2s", [NS, D])
                g.dma("sp", GT2[:], mod_d[0:1, 5 * D:6 * D].to_broadcast([128, D]), writes=[GT2])
                g.dma("sp", GT2s[:], mod_d[1:5, 5 * D:6 * D], writes=[GT2s])
                s8t = [sb5("s8t%d" % j, [128, 8], I32) for j in range(2)]
                w8t = [sb5("w8t%d" % j, [128, 8]) for j in range(2)]
                accf = [sb5("accf%d" % j, [128, D]) for j in range(2)]
                xo = [sb5("xo%d" % j, [128, D]) for j in range(2)]
                Yg = [sb5("Yg%d" % j, [128, D], BF16) for j in range(8)]
                gi_ = 0
                for i in range(NT + 1):
                    nn = 128 if i < NT else NS
                    samp = (i == NT)
                    rows = slice(i * 128, i * 128 + nn)
                    s8 = s8t[i % 2]; w8_ = w8t[i % 2]; af = accf[i % 2]; xb_ = xo[i % 2]
                    gt2 = GT2s if samp else GT2
                    g.dma("sp", s8[:], slot_d[i], writes=[s8])
                    g.dma("sp", w8_[:nn, :], w8_d[i, 0:nn, :], writes=[w8_])
                    g.dma("sp", af[:nn, :], Ysh_d[rows, :], writes=[af])
                    g.dma("sp", xb_[:nn, :], x1_d[rows, :], writes=[xb_])
                    for k8 in range(8):
                        yg = Yg[gi_ % 8]
                        gi_ += 1
                        g.idma(yg[:, :], None, Ys_d[:, :], bass.IndirectOffsetOnAxis(ap=s8[:, k8:k8 + 1], axis=0),
                               reads=[s8], writes=[yg])
                        g.op("dve", lambda: nc.vector.scalar_tensor_tensor(
                            out=af[:nn, :], in0=yg[:nn, :], scalar=w8_[:nn, k8:k8 + 1], in1=af[:nn, :],
                            op0=ALU.mult, op1=ALU.add), [yg, w8_, af], [af])
                    g.op("dve", lambda: nc.vector.tensor_tensor(out=af[:nn, :], in0=af[:nn, :], in1=gt2[:nn, :], op=ALU.mult),
                         [af, gt2], [af])
                    g.op("dve", lambda: nc.vector.tensor_add(out=xb_[:nn, :], in0=xb_[:nn, :], in1=af[:nn, :]), [xb_, af], [xb_])
                    g.dma("sp", ys if samp else y[rows, :], xb_[:nn, :], reads=[xb_], is_output=True)
                g.barrier()
        g.finish()
    return nc, dr


def core_inputs(inp, c):
    m = {}
    m["x"] = np.ascontiguousarray(inp["x_prompt"][c])
    m["xs"] = np.ascontiguousarray(inp["x_sample"][NS * c:NS * c + NS, 0])
    call = np.concatenate([inp["c_prompt"][c:c + 1], inp["c_sample"][NS * c:NS * c + NS]], axis=0)
    m["cin"] = np.ascontiguousarray(call.T.reshape(8, 128, 5).transpose(1, 0, 2))
    m["ada_w"] = inp["ada_w"][0]
    m["ada_b"] = inp["ada_b"]
    m["norm1"] = inp["norm1"]
    m["norm2"] = inp["norm2"]
    m["w_in"] = inp["w_in"][0]
    m["qg"] = np.ascontiguousarray(np.broadcast_to(inp["q_gain"][0][:, None, :], (3, 8, 64)).reshape(1, QW))
    m["kg"] = np.ascontiguousarray(np.broadcast_to(inp["k_gain"][0][:, None, :], (3, 8, 64)).reshape(1, QW))
    m["ident"] = TB["ident"]
    m["bands"] = TB["bands"]
    m["pool_w"] = inp["pool_w"][0]
    m["pool_sc"] = np.ascontiguousarray(inp["pool_scale"][0].reshape(4, 128).T)
    rb = inp["rel_bias"]
    bT = np.zeros((24, 128, 256), np.float32)
    for gi in range(3):
        idx = TB["bkt"][gi][TB["jT"]]
        for h in range(8):
            bT[gi * 8 + h] = rb[idx, gi * 8 + h]
    m["biasT"] = bT
    m["maskT"] = TB["maskT"]
    caches = {"ck128": inp["cache_k_w128"], "cv128": inp["cache_v_w128"], "ck512": inp["cache_k_w512"],
              "cv512": inp["cache_v_w512"], "ck2048": inp["cache_k_w2048"], "cv2048": inp["cache_v_w2048"]}
    for nm, arr in caches.items():
        m[nm] = np.ascontiguousarray(arr[0, NS * c:NS * c + NS].reshape(NS, arr.shape[2], 512))
    m["stp"] = np.ascontiguousarray(inp["state_pool"][0, NS * c:NS * c + NS])
    m["bias0"] = np.ascontiguousarray(rb[0:1, :])
    bs = np.zeros((3, 128, 8), np.float32)
    for gi in range(3):
        bs[gi] = rb[TB["bkt"][gi][128 - np.arange(128)], gi * 8:(gi + 1) * 8]
    m["bias_s"] = bs
    m["bd"] = TB["bd"]
    m["bandS"] = TB["bandS"]
    m["diagS"] = TB["diagS"]
    m["UT"] = TB["UT"]
    m["trash"] = TB["trash"]
    m["tri"] = TB["tri"]
    m["bstart"] = TB["bstart"]
    m["piota"] = TB["piota"]
    m["w_br_a"] = inp["w_br_a"][0]
    m["w_br_b"] = inp["w_br_b"][0]
    m["w_out"] = inp["w_out"][0]
    m["router_w"] = inp["router_w"][0]
    m["router_b"] = inp["router_bias"]
    if SPARSE:
        m["egl"] = inp["_egl"]
        m["eul"] = inp["_eul"]
        m["edl"] = inp["_edl"]
    else:
        m["eg"] = inp["exp_w_gate"][0]
        m["eu"] = inp["exp_w_up"][0]
        m["ed"] = inp["exp_w_down"][0]
    m["shg"] = inp["sh_w_gate"][0]
    m["shu"] = inp["sh_w_up"][0]
    m["shd"] = inp["sh_w_down"][0]
    return m


_NC_CACHE = {}


def prep_experts(inp):
    if SPARSE and "_egl" not in inp:
        inp["_egl"] = np.ascontiguousarray(inp["exp_w_gate"][0].reshape(NE, 8, 128, EH).transpose(0, 2, 1, 3)).reshape(NE * 128, 8 * EH)
        inp["_eul"] = np.ascontiguousarray(inp["exp_w_up"][0].reshape(NE, 8, 128, EH).transpose(0, 2, 1, 3)).reshape(NE * 128, 8 * EH)
        inp["_edl"] = np.ascontiguousarray(inp["exp_w_down"][0].reshape(NE, 2, 128, D).transpose(0, 2, 1, 3)).reshape(NE * 128, 2 * D)


def kernel(**inputs):
    inp = {k: np.asarray(v) for k, v in inputs.items()}
    prep_experts(inp)
    if "nc" not in _NC_CACHE:
        _NC_CACHE["nc"] = build_nc(stages=("all",), debug=False)
    nc, dr = _NC_CACHE["nc"]
    in_maps = []
    for c in range(NCORES):
        m = core_inputs(inp, c)
        in_maps.append({k: np.ascontiguousarray(v, dtype=np.float32) for k, v in m.items() if k in dr})
    res = run_bass_kernel_spmd(nc, in_maps, core_ids=list(range(NCORES)))
    R = res.results
    f = np.float32
    y_prompt = np.stack([R[c]["y"] for c in range(NCORES)], 0).astype(f)
    y_sample = np.concatenate([R[c]["ys"] for c in range(NCORES)], 0).reshape(NCORES * NS, 1, D).astype(f)
    outs = [y_prompt, y_sample]
    for (w, _) in GROUPS:
        for nm in ("pk", "pv"):
            outs.append(np.stack([R[c]["%s%d" % (nm, w)] for c in range(NCORES)], 0).reshape(1, NCORES, w, 8, 64).astype(f))
    outs.append(np.stack([R[c]["ppool"] for c in range(NCORES)], 0).reshape(1, NCORES, 15, 512).astype(f))
    for (w, _) in GROUPS:
        for nm in ("sk", "sv"):
            outs.append(np.concatenate([R[c]["%s%d" % (nm, w)] for c in range(NCORES)], 0).reshape(1, NCORES * NS, w, 8, 64).astype(f))
    outs.append(np.concatenate([R[c]["spool"] for c in range(NCORES)], 0).reshape(1, NCORES * NS, 15, 512).astype(f))
    return tuple(outs)
```
